# Optimizing a Trainium2 kernel written in Bass

```python
import math
import jax, jax.numpy as jnp
from jax import lax
import numpy as np

D_MODEL = 1024
BATCH = 16
SEQ = 256
DEPTH = 4
DEC_BATCH = 8
DEC_SEQ = 4096
PAST_LEN = 512

GRID_W = 64
N_MIXERS = 4
NORM_EPS = 1e-6

RWKV_HEAD = 64
RWKV_H = D_MODEL // RWKV_HEAD
RWKV_DECAY_LORA = 64
RWKV_AAA_LORA = 64
RWKV_GATE_LORA = 128
RWKV_GN_EPS = 64e-5

ATT_HEAD_DIM = 64
ATT_Q_HEADS = D_MODEL // ATT_HEAD_DIM
ATT_KV_HEADS = 4
ATT_REP = ATT_Q_HEADS // ATT_KV_HEADS
WINDOW = 128
ATT_BLOCK = 128
ROPE_THETA = 10000.0

HY_ORDER = 2
HY_EMB = 33
HY_FILTER_HIDDEN = 64
HY_SHORT = 3
HY_FAST_DECAY = 0.3
HY_SLOW_DECAY = 1.5
HY_DECAY_TARGET = 1e-2

RET_H = 4
RET_DK = D_MODEL // RET_H
RET_DV = 2 * D_MODEL // RET_H
RET_CHUNK = 128
RET_THETA = 10000.0
RET_DECAY_OFFSET_F = 5.0
RET_DECAY_OFFSET_B = 5.5

FFN_DENSE = 2816
N_EXPERTS = 8
TOP_K = 2
FFN_EXPERT = 3584

kernel_name = 'hybrid_diffusion_prefix_step'


def _rmsnorm(x, eps=NORM_EPS):
    xf = x.astype(jnp.float32)
    return (xf * lax.rsqrt(jnp.mean(xf * xf, axis=-1, keepdims=True) + eps)).astype(x.dtype)


def _ada_norm(x, shift, scale):
    return _rmsnorm(x) * (1.0 + scale) + shift


def _heads(t, n):
    return t.reshape(t.shape[:-1] + (t.shape[-1] // n, n))


def _flip(t):
    return jnp.flip(t, axis=1)


def _centred_shift(x):
    xp = jnp.pad(x, ((0, 0), (1, 1), (0, 0)))
    return 0.5 * (xp[:, :-2] + xp[:, 2:]) - x


def _rwkv7_scan(s0, r, w, k, v, kk, a):
    def step(S, inp):
        r_t, w_t, k_t, v_t, kk_t, a_t = inp
        sa = jnp.einsum('bhvk,bhk->bhv', S, -kk_t)
        S = (S * w_t[:, :, None, :] + sa[..., None] * (kk_t * a_t)[:, :, None, :]
             + v_t[..., None] * k_t[:, :, None, :])
        return S, jnp.einsum('bhvk,bhk->bhv', S, r_t)
    xs = tuple(jnp.moveaxis(t, 1, 0) for t in (r, w, k, v, kk, a))
    S, y = lax.scan(step, s0.astype(jnp.float32), xs)
    return jnp.moveaxis(y, 0, 1), S


def _rwkv7_mixer(h, s0_f, s0_b, mix, w_rkv, w0, w1, w2, a0, a1, a2, g1, g2, k_k, k_a, r_k,
                 ln_w, ln_b, w_o):
    f32 = jnp.float32
    B, L, D = h.shape
    xx = _centred_shift(h)
    xr, xw, xk, xv, xa, xg = (h + xx * mix[i] for i in range(6))
    rkv = jnp.einsum('nbld,nde->nble', jnp.stack([xr, xk, xv]), w_rkv)
    r, k, v = rkv[0], rkv[1], rkv[2]
    w_raw = w0[:, None, None, :] + jnp.einsum(
        'nblr,nrd->nbld', jnp.tanh(jnp.einsum('bld,ndr->nblr', xw, w1)), w2)
    decay = jnp.exp(-jnp.exp(-jax.nn.softplus(-w_raw.astype(f32)) - 0.5))
    a = jax.nn.sigmoid((a0[:, None, None, :] + jnp.einsum(
        'nblr,nrd->nbld', jnp.einsum('bld,ndr->nblr', xa, a1), a2)).astype(f32))
    g = jax.nn.sigmoid(xg @ g1) @ g2
    kk = _heads((k * k_k).astype(f32), RWKV_HEAD)
    kk = kk / jnp.maximum(jnp.linalg.norm(kk, axis=-1, keepdims=True), 1e-12)
    k_dir = k.astype(f32)[None] * (1.0 + (a - 1.0) * k_a.astype(f32))
    rh = _heads(r.astype(f32), RWKV_HEAD)
    vh = _heads(v.astype(f32), RWKV_HEAD)
    dh, ah, kh = _heads(decay, RWKV_HEAD), _heads(a, RWKV_HEAD), _heads(k_dir, RWKV_HEAD)
    y_f, s_f = _rwkv7_scan(s0_f, rh, dh[0], kh[0], vh, kk, ah[0])
    y_b, s_b = _rwkv7_scan(s0_b, _flip(rh), _flip(dh[1]), _flip(kh[1]), _flip(vh), _flip(kk),
                           _flip(ah[1]))
    y = y_f + _flip(y_b)
    mu = jnp.mean(y, axis=-1, keepdims=True)
    var = jnp.mean(jnp.square(y - mu), axis=-1, keepdims=True)
    y = ((y - mu) * lax.rsqrt(var + RWKV_GN_EPS)).reshape(B, L, D) * ln_w + ln_b
    bonus = jnp.sum(rh * (kh[0] + kh[1]) * r_k, axis=-1, keepdims=True) * vh
    y = y + bonus.reshape(B, L, D)
    out = (y.astype(h.dtype) * g) @ w_o
    return out, s_f.astype(h.dtype), s_b.astype(h.dtype)


def _axial_rope(L):
    t = jnp.arange(L)
    row = (t // GRID_W).astype(jnp.float32)
    col = (t % GRID_W).astype(jnp.float32)
    n_freq = ATT_HEAD_DIM // 4
    inv = ROPE_THETA ** (-jnp.arange(n_freq, dtype=jnp.float32) / n_freq)
    ang = jnp.concatenate([row[:, None] * inv[None, :], col[:, None] * inv[None, :]], axis=-1)
    ang = jnp.concatenate([ang, ang], axis=-1)
    return jnp.cos(ang), jnp.sin(ang)


def _rope(x, cos, sin):
    shape = (1, x.shape[1]) + (1,) * (x.ndim - 3) + (x.shape[-1],)
    c = cos.reshape(shape).astype(x.dtype)
    s = sin.reshape(shape).astype(x.dtype)
    half = x.shape[-1] // 2
    rot = jnp.concatenate([-x[..., half:], x[..., :half]], axis=-1)
    return x * c + rot * s


def _att_project(h, w_qkv, q_norm, k_norm):
    B, L, _ = h.shape
    qd = ATT_Q_HEADS * ATT_HEAD_DIM
    kd = ATT_KV_HEADS * ATT_HEAD_DIM
    q, k, v = jnp.split(h @ w_qkv, [qd, qd + kd], axis=-1)
    q = _rmsnorm(q.reshape(B, L, ATT_KV_HEADS, ATT_REP, ATT_HEAD_DIM)) * q_norm
    k = _rmsnorm(k.reshape(B, L, ATT_KV_HEADS, ATT_HEAD_DIM)) * k_norm
    v = v.reshape(B, L, ATT_KV_HEADS, ATT_HEAD_DIM)
    return q, k, v


def _attend(q, k, v, mask, sink):
    s = jnp.einsum('bqgrd,bkgd->bgrqk', q, k).astype(jnp.float32) * (q.shape[-1] ** -0.5)
    if mask is not None:
        s = jnp.where(mask, s, -1e30)
    sk = jnp.broadcast_to(sink.astype(jnp.float32)[None, :, :, None, None], s.shape[:-1] + (1,))
    p = jax.nn.softmax(jnp.concatenate([s, sk], axis=-1), axis=-1)[..., :-1]
    return jnp.einsum('bgrqk,bkgd->bqgrd', p.astype(v.dtype), v)


def _att_context(h, w_qkv, q_norm, k_norm, sink, w_o):
    B, L, _ = h.shape
    q, k, v = _att_project(h, w_qkv, q_norm, k_norm)
    sk = sink.reshape(ATT_KV_HEADS, ATT_REP)
    qb = jnp.moveaxis(q.reshape(B, L // ATT_BLOCK, ATT_BLOCK, ATT_KV_HEADS, ATT_REP, ATT_HEAD_DIM), 1, 0)
    o = lax.map(lambda qq: _attend(qq, k, v, None, sk), qb)
    o = jnp.moveaxis(o, 0, 1).reshape(B, L, ATT_Q_HEADS * ATT_HEAD_DIM)
    return o @ w_o, k, v


def _att_latent(h, ctx_k, ctx_v, w_qkv, q_norm, k_norm, sink, w_o):
    B, L, _ = h.shape
    nb = L // ATT_BLOCK
    q, k, v = _att_project(h, w_qkv, q_norm, k_norm)
    cos, sin = _axial_rope(L)
    q, k = _rope(q, cos, sin), _rope(k, cos, sin)

    def band(t):
        tp = jnp.pad(t, ((0, 0), (ATT_BLOCK, ATT_BLOCK), (0, 0), (0, 0)))
        tb = tp.reshape(B, nb + 2, ATT_BLOCK, ATT_KV_HEADS, ATT_HEAD_DIM)
        return jnp.concatenate([tb[:, :-2], tb[:, 1:-1], tb[:, 2:]], axis=2)

    kb, vb = band(k), band(v)
    qb = q.reshape(B, nb, ATT_BLOCK, ATT_KV_HEADS, ATT_REP, ATT_HEAD_DIM)
    lc = ctx_k.shape[1]
    sk = sink.reshape(ATT_KV_HEADS, ATT_REP)

    def one(args):
        i, qq, kk, vv = args
        qpos = i * ATT_BLOCK + jnp.arange(ATT_BLOCK)
        kpos = (i - 1) * ATT_BLOCK + jnp.arange(3 * ATT_BLOCK)
        near = ((jnp.abs(qpos[:, None] - kpos[None, :]) <= WINDOW)
                & (kpos >= 0)[None, :] & (kpos < L)[None, :])
        mask = jnp.concatenate([jnp.ones((ATT_BLOCK, lc), dtype=bool), near], axis=1)
        return _attend(qq, jnp.concatenate([ctx_k, kk], axis=1),
                       jnp.concatenate([ctx_v, vv], axis=1), mask, sk)

    o = lax.map(one, (jnp.arange(nb), jnp.moveaxis(qb, 1, 0), jnp.moveaxis(kb, 1, 0),
                      jnp.moveaxis(vb, 1, 0)))
    o = jnp.moveaxis(o, 0, 1).reshape(B, L, ATT_Q_HEADS * ATT_HEAD_DIM)
    return o @ w_o


def _dwconv(u, w, b):
    C = u.shape[-1]
    y = lax.conv_general_dilated(u, w[:, None, :].astype(u.dtype), window_strides=(1,),
                                 padding=[(HY_SHORT // 2, HY_SHORT // 2)],
                                 dimension_numbers=('NWC', 'WIO', 'NWC'), feature_group_count=C)
    return y + b


def _hyena_filters(L, f_w1, f_b1, f_freq1, f_w2, f_b2, f_freq2, f_w3):
    f32 = jnp.float32
    t = jnp.arange(L, dtype=f32)
    t_norm = t / max(L - 1, 1)
    bands = (HY_EMB - 1) // 2
    fr = jnp.linspace(1e-4, bands - 1, bands, dtype=f32)
    ph = 2.0 * math.pi * t[:, None] * fr[None, :] / L
    z = jnp.concatenate([t_norm[:, None], jnp.cos(ph), -jnp.sin(ph)], axis=-1)
    a = jnp.sin(f_freq1 * (z @ f_w1 + f_b1))
    a = jnp.sin(f_freq2 * (a @ f_w2 + f_b2))
    filt = (a @ f_w3).astype(f32).reshape(L, 2, D_MODEL)
    deltas = jnp.abs(jnp.linspace(math.log(HY_DECAY_TARGET) / HY_SLOW_DECAY,
                                  math.log(HY_DECAY_TARGET) / HY_FAST_DECAY, D_MODEL, dtype=f32))
    window = jnp.exp(-t_norm[:, None] * deltas[None, :])
    filt = filt * window[:, None, :]
    return filt[:, 0], filt[:, 1]


def _bidir_long_conv(z, h_f, h_b, skip):
    L = z.shape[1]
    circ = jnp.concatenate([h_f, jnp.zeros((1, h_f.shape[1]), h_f.dtype), jnp.flip(h_b[1:], axis=0)], axis=0)
    zf = jnp.fft.rfft(z.astype(jnp.float32), n=2 * L, axis=1)
    cf = jnp.fft.rfft(circ, n=2 * L, axis=0)
    y = jnp.fft.irfft(zf * cf[None], n=2 * L, axis=1)[:, :L]
    return (y + z.astype(jnp.float32) * skip).astype(z.dtype)


def _hyena_mixer(h, w_in, b_in, conv_w, conv_b, f_w1, f_b1, f_freq1, f_w2, f_b2, f_freq2, f_w3,
                 skip, w_out, b_out):
    L = h.shape[1]
    u = _dwconv(h @ w_in + b_in, conv_w, conv_b)
    x0, x1, v = jnp.split(u, HY_ORDER + 1, axis=-1)
    h_f, h_b = _hyena_filters(L, f_w1, f_b1, f_freq1, f_w2, f_b2, f_freq2, f_w3)
    z = _bidir_long_conv(x1 * v, h_f, h_b, skip)
    return (x0 * z) @ w_out + b_out


def _ret_log_gammas():
    hh = jnp.arange(RET_H, dtype=jnp.float32)
    return (jnp.log1p(-jnp.exp2(-RET_DECAY_OFFSET_F - hh)),
            jnp.log1p(-jnp.exp2(-RET_DECAY_OFFSET_B - hh)))


def _xpos_rotate(x):
    f32 = jnp.float32
    L, d = x.shape[1], x.shape[-1]
    ang = jnp.repeat(1.0 / (RET_THETA ** jnp.linspace(0.0, 1.0, d // 2, dtype=f32)), 2)
    ph = jnp.arange(L, dtype=f32)[:, None] * ang[None, :]
    cos = jnp.cos(ph)[None, :, None, :].astype(x.dtype)
    sin = jnp.sin(ph)[None, :, None, :].astype(x.dtype)
    rot = jnp.stack([-x[..., 1::2], x[..., ::2]], axis=-1).reshape(x.shape)
    return x * cos + rot * sin


def _retention_chunked(q, k, v, log_gamma, s0):
    f32 = jnp.float32
    B, L, H, dk = q.shape
    dv = v.shape[-1]
    C = RET_CHUNK
    n = L // C
    qc = q.astype(f32).reshape(B, n, C, H, dk)
    kc = k.astype(f32).reshape(B, n, C, H, dk)
    vc = v.astype(f32).reshape(B, n, C, H, dv)
    idx = jnp.arange(C, dtype=f32)
    diff = idx[:, None] - idx[None, :]
    dmask = jnp.where(diff >= 0, jnp.exp(log_gamma[:, None, None] * jnp.maximum(diff, 0.0)), 0.0)
    inner = jnp.einsum('bnhij,bnjhe->bnihe',
                       jnp.einsum('bnihd,bnjhd->bnhij', qc, kc) * dmask, vc)
    q_dec = jnp.exp(log_gamma[None, :] * (idx[:, None] + 1.0))
    k_dec = jnp.exp(log_gamma[None, :] * (C - 1.0 - idx[:, None]))
    c_dec = jnp.exp(log_gamma * C)

    def step(S, inp):
        q_, k_, v_ = inp
        cross = jnp.einsum('bihd,bhde->bihe', q_ * q_dec[None, :, :, None], S)
        S = S * c_dec[None, :, None, None] + jnp.einsum('bjhd,bjhe->bhde', k_ * k_dec[None, :, :, None], v_)
        return S, cross

    S, cross = lax.scan(step, s0.astype(f32),
                        (jnp.moveaxis(qc, 1, 0), jnp.moveaxis(kc, 1, 0), jnp.moveaxis(vc, 1, 0)))
    out = inner + jnp.moveaxis(cross, 0, 1)
    return out.reshape(B, L, H, dv), S


def _ret_mixer(h, s0_f, s0_b, w_in, w_out, rotate):
    B, L, _ = h.shape
    dk_t, dv_t = RET_H * RET_DK, RET_H * RET_DV
    q, k, v, g_f, g_b = jnp.split(h @ w_in, [dk_t, 2 * dk_t, 2 * dk_t + dv_t, 2 * dk_t + 2 * dv_t], axis=-1)
    q = q.reshape(B, L, RET_H, RET_DK)
    k = k.reshape(B, L, RET_H, RET_DK) * (RET_DK ** -0.5)
    v = v.reshape(B, L, RET_H, RET_DV)
    if rotate:
        q, k = _xpos_rotate(q), _xpos_rotate(k)
    lg_f, lg_b = _ret_log_gammas()
    y_f, s_f = _retention_chunked(q, k, v, lg_f, s0_f)
    y_b, s_b = _retention_chunked(_flip(q), _flip(k), _flip(v), lg_b, s0_b)
    y_b = _flip(y_b)
    y = (jax.nn.silu(g_f) * _rmsnorm(y_f).reshape(B, L, dv_t).astype(h.dtype)
         + jax.nn.silu(g_b) * _rmsnorm(y_b).reshape(B, L, dv_t).astype(h.dtype))
    return y @ w_out, s_f.astype(h.dtype), s_b.astype(h.dtype)


def _swiglu(x, w_in, w_out):
    g, u = jnp.split(x @ w_in, 2, axis=-1)
    return (jax.nn.silu(g) * u) @ w_out


def _moe(x, router, w_in, w_out):
    logits = (x @ router).astype(jnp.float32)
    top_v, top_i = lax.top_k(logits, TOP_K)
    top_w = jax.nn.softmax(top_v, axis=-1)
    gates = jnp.sum(jax.nn.one_hot(top_i, N_EXPERTS, dtype=jnp.float32) * top_w[..., None], axis=-2)
    y = jnp.zeros_like(x)
    for e in range(N_EXPERTS):
        y = y + gates[..., e:e + 1].astype(x.dtype) * _swiglu(x, w_in[e], w_out[e])
    return y


def setup_inputs(seed: int = 0) -> dict:
    key = jax.random.key(seed)
    ks = iter(jax.random.split(key, 80))
    f32 = jnp.float32
    D = D_MODEL

    def nrm(shape, scale):
        return jax.random.normal(next(ks), shape, f32) * scale

    n_dense = (DEPTH + 1) // 2
    n_moe = DEPTH // 2
    inp = {}
    inp['x_prompt'] = nrm((BATCH, SEQ, D), 1.0)
    inp['x_sample'] = nrm((DEC_BATCH, DEC_SEQ, D), 1.0)
    inp['state_rwkv'] = nrm((DEC_BATCH, 2, RWKV_H, RWKV_HEAD, RWKV_HEAD), 0.1)
    inp['cache_att_k'] = nrm((DEC_BATCH, PAST_LEN, ATT_KV_HEADS, ATT_HEAD_DIM), 1.0)
    inp['cache_att_v'] = nrm((DEC_BATCH, PAST_LEN, ATT_KV_HEADS, ATT_HEAD_DIM), 1.0)
    inp['state_ret'] = nrm((DEC_BATCH, 2, RET_H, RET_DK, RET_DV), 0.1)
    inp['c'] = nrm((DEC_BATCH, D), 1.0)
    inp['c_ctx'] = nrm((D,), 1.0)
    inp['ada_w'] = nrm((DEPTH, D, 6 * D), 0.5 * D ** -0.5)
    inp['ada_b'] = nrm((DEPTH, 6 * D), 0.02)
    inp['rwkv_mix'] = jax.random.uniform(next(ks), (6, D), f32)
    inp['rwkv_w_rkv'] = nrm((3, D, D), D ** -0.5)
    inp['rwkv_w0'] = jnp.linspace(-6.0, -1.0, D, dtype=f32)[None, :] + nrm((2, D), 0.1)
    inp['rwkv_w1'] = nrm((2, D, RWKV_DECAY_LORA), D ** -0.5)
    inp['rwkv_w2'] = nrm((2, RWKV_DECAY_LORA, D), 0.1 * RWKV_DECAY_LORA ** -0.5)
    inp['rwkv_a0'] = nrm((2, D), 0.5)
    inp['rwkv_a1'] = nrm((2, D, RWKV_AAA_LORA), D ** -0.5)
    inp['rwkv_a2'] = nrm((2, RWKV_AAA_LORA, D), RWKV_AAA_LORA ** -0.5)
    inp['rwkv_g1'] = nrm((D, RWKV_GATE_LORA), D ** -0.5)
    inp['rwkv_g2'] = nrm((RWKV_GATE_LORA, D), RWKV_GATE_LORA ** -0.5)
    inp['rwkv_k_k'] = 0.85 + nrm((D,), 0.05)
    inp['rwkv_k_a'] = 1.0 + nrm((D,), 0.05)
    inp['rwkv_r_k'] = nrm((RWKV_H, RWKV_HEAD), 0.1)
    inp['rwkv_ln_w'] = 1.0 + nrm((D,), 0.05)
    inp['rwkv_ln_b'] = nrm((D,), 0.02)
    inp['rwkv_w_o'] = nrm((D, D), D ** -0.5)
    inp['att_w_qkv'] = nrm((D, (ATT_Q_HEADS + 2 * ATT_KV_HEADS) * ATT_HEAD_DIM), D ** -0.5)
    inp['att_q_norm'] = 1.0 + nrm((ATT_HEAD_DIM,), 0.05)
    inp['att_k_norm'] = 1.0 + nrm((ATT_HEAD_DIM,), 0.05)
    inp['att_sink'] = nrm((ATT_Q_HEADS,), 0.5)
    inp['att_w_o'] = nrm((ATT_Q_HEADS * ATT_HEAD_DIM, D), (ATT_Q_HEADS * ATT_HEAD_DIM) ** -0.5)
    inp['hy_w_in'] = nrm((D, (HY_ORDER + 1) * D), D ** -0.5)
    inp['hy_b_in'] = nrm(((HY_ORDER + 1) * D,), 0.02)
    inp['hy_conv_w'] = nrm((HY_SHORT, (HY_ORDER + 1) * D), HY_SHORT ** -0.5)
    inp['hy_conv_b'] = nrm(((HY_ORDER + 1) * D,), 0.02)
    inp['hy_f_w1'] = nrm((HY_EMB, HY_FILTER_HIDDEN), HY_EMB ** -0.5)
    inp['hy_f_b1'] = nrm((HY_FILTER_HIDDEN,), 0.1)
    inp['hy_f_freq1'] = 1.0 + nrm((HY_FILTER_HIDDEN,), 0.05)
    inp['hy_f_w2'] = nrm((HY_FILTER_HIDDEN, HY_FILTER_HIDDEN), HY_FILTER_HIDDEN ** -0.5)
    inp['hy_f_b2'] = nrm((HY_FILTER_HIDDEN,), 0.1)
    inp['hy_f_freq2'] = 1.0 + nrm((HY_FILTER_HIDDEN,), 0.05)
    inp['hy_f_w3'] = nrm((HY_FILTER_HIDDEN, 2 * D), 0.005)
    inp['hy_skip'] = nrm((D,), 0.1)
    inp['hy_w_out'] = nrm((D, D), D ** -0.5)
    inp['hy_b_out'] = nrm((D,), 0.02)
    inp['ret_w_in'] = nrm((D, 2 * RET_H * RET_DK + 3 * RET_H * RET_DV), D ** -0.5)
    inp['ret_w_out'] = nrm((RET_H * RET_DV, D), (RET_H * RET_DV) ** -0.5)
    inp['ffn_w_in'] = nrm((n_dense, D, 2 * FFN_DENSE), D ** -0.5)
    inp['ffn_w_out'] = nrm((n_dense, FFN_DENSE, D), FFN_DENSE ** -0.5)
    inp['moe_router'] = nrm((n_moe, D, N_EXPERTS), D ** -0.5)
    inp['moe_w_in'] = nrm((n_moe, N_EXPERTS, D, 2 * FFN_EXPERT), D ** -0.5)
    inp['moe_w_out'] = nrm((n_moe, N_EXPERTS, FFN_EXPERT, D), FFN_EXPERT ** -0.5)
    return inp


def reference(x_prompt, x_sample, state_rwkv, cache_att_k, cache_att_v, state_ret, c, c_ctx,
              ada_w, ada_b,
              rwkv_mix, rwkv_w_rkv, rwkv_w0, rwkv_w1, rwkv_w2, rwkv_a0, rwkv_a1, rwkv_a2,
              rwkv_g1, rwkv_g2, rwkv_k_k, rwkv_k_a, rwkv_r_k, rwkv_ln_w, rwkv_ln_b, rwkv_w_o,
              att_w_qkv, att_q_norm, att_k_norm, att_sink, att_w_o,
              hy_w_in, hy_b_in, hy_conv_w, hy_conv_b, hy_f_w1, hy_f_b1, hy_f_freq1, hy_f_w2,
              hy_f_b2, hy_f_freq2, hy_f_w3, hy_skip, hy_w_out, hy_b_out,
              ret_w_in, ret_w_out,
              ffn_w_in, ffn_w_out, moe_router, moe_w_in, moe_w_out):
    xp, xs = x_prompt, x_sample
    bp = xp.shape[0]
    rwkv_p = (rwkv_mix, rwkv_w_rkv, rwkv_w0, rwkv_w1, rwkv_w2, rwkv_a0, rwkv_a1, rwkv_a2,
              rwkv_g1, rwkv_g2, rwkv_k_k, rwkv_k_a, rwkv_r_k, rwkv_ln_w, rwkv_ln_b, rwkv_w_o)
    hy_p = (hy_w_in, hy_b_in, hy_conv_w, hy_conv_b, hy_f_w1, hy_f_b1, hy_f_freq1, hy_f_w2,
            hy_f_b2, hy_f_freq2, hy_f_w3, hy_skip, hy_w_out, hy_b_out)
    for l in range(DEPTH):
        kind = l % N_MIXERS
        mc = (jax.nn.silu(c_ctx) @ ada_w[l] + ada_b[l]).reshape(6, D_MODEL)
        ms = (jax.nn.silu(c) @ ada_w[l] + ada_b[l]).reshape(-1, 1, 6, D_MODEL)
        hp = _ada_norm(xp, mc[0], mc[1])
        hs = _ada_norm(xs, ms[:, :, 0], ms[:, :, 1])
        if kind == 0:
            zero = jnp.zeros((bp, RWKV_H, RWKV_HEAD, RWKV_HEAD), jnp.float32)
            op, s_f, s_b = _rwkv7_mixer(hp, zero, zero, *rwkv_p)
            new_state_rwkv = jnp.stack([s_f, s_b], axis=1)
            os_, _, _ = _rwkv7_mixer(hs, state_rwkv[:, 0], state_rwkv[:, 1], *rwkv_p)
        elif kind == 1:
            op, new_cache_att_k, new_cache_att_v = _att_context(hp, att_w_qkv, att_q_norm, att_k_norm,
                                                                att_sink, att_w_o)
            os_ = _att_latent(hs, cache_att_k, cache_att_v, att_w_qkv, att_q_norm, att_k_norm,
                              att_sink, att_w_o)
        elif kind == 2:
            op = _hyena_mixer(hp, *hy_p)
            os_ = _hyena_mixer(hs, *hy_p)
        else:
            zero = jnp.zeros((bp, RET_H, RET_DK, RET_DV), jnp.float32)
            op, s_f, s_b = _ret_mixer(hp, zero, zero, ret_w_in, ret_w_out, False)
            new_state_ret = jnp.stack([s_f, s_b], axis=1)
            os_, _, _ = _ret_mixer(hs, state_ret[:, 0], state_ret[:, 1], ret_w_in, ret_w_out, True)
        xp = xp + mc[2] * op
        xs = xs + ms[:, :, 2] * os_
        hp = _ada_norm(xp, mc[3], mc[4])
        hs = _ada_norm(xs, ms[:, :, 3], ms[:, :, 4])
        if l % 2 == 0:
            fp = _swiglu(hp, ffn_w_in[l // 2], ffn_w_out[l // 2])
            fs = _swiglu(hs, ffn_w_in[l // 2], ffn_w_out[l // 2])
        else:
            fp = _moe(hp, moe_router[l // 2], moe_w_in[l // 2], moe_w_out[l // 2])
            fs = _moe(hs, moe_router[l // 2], moe_w_in[l // 2], moe_w_out[l // 2])
        xp = xp + mc[5] * fp
        xs = xs + ms[:, :, 5] * fs
    return (xp, xs, new_state_rwkv, new_cache_att_k, new_cache_att_v, new_state_ret)
```

```python
import numpy as np
from contextlib import ExitStack
import concourse.bass as bass
import concourse.mybir as mybir
from concourse.bass_utils import run_bass_kernel_spmd

F32 = mybir.dt.float32
BF16 = mybir.dt.bfloat16
AF = mybir.ActivationFunctionType
ALU = mybir.AluOpType
AX = mybir.AxisListType

D = 1024
NCORES = 8
NDSEM = 8
EPS = 1e-6


class Reg:
    __slots__ = ('lastw', 'reads')

    def __init__(self):
        self.lastw = None
        self.reads = {}


class Prog:
    def __init__(self, nc):
        self.nc = nc
        self.es = ExitStack()
        self.h = {'pe': nc.tensor, 'dve': nc.vector, 'act': nc.scalar, 'pool': nc.gpsimd, 'sp': nc.sync}
        self.cnt = {e: 0 for e in self.h}
        self.known = {e: {} for e in self.h}
        self.sem = {}
        for e in ['pe', 'dve', 'act', 'pool']:
            self.sem[e] = self.es.enter_context(nc.semaphore('s_' + e))
        self.dtot = {}
        self.drr = {}
        for q in ['sp', 'pool', 'act']:
            for i in range(NDSEM):
                k = 'd_%s_%d' % (q, i)
                self.sem[k] = self.es.enter_context(nc.semaphore(k))
                self.dtot[k] = 0
            self.drr[q] = 0
        self.ninst = 0

    def sbuf(self, es, name, shape, dtype):
        self.ninst += 0
        self.nname = getattr(self, 'nname', 0) + 1
        return es.enter_context(self.nc.sbuf_tensor('%s_%d' % (name, self.nname), list(shape), dtype))

    def psum(self, es, name, shape, dtype):
        return es.enter_context(self.nc.psum_tensor(name, list(shape), dtype))

    def _collect(self, eng, reads, writes):
        waits = {}
        kn = self.known[eng]

        def need(ev):
            if ev is None:
                return
            k, v = ev
            if k == 'pe' and eng == 'pe':
                return
            if kn.get(k, 0) >= v:
                return
            if waits.get(k, 0) < v:
                waits[k] = v

        for r in reads:
            need(r.lastw)
        for w in writes:
            need(w.lastw)
            for ev in w.reads.items():
                need(ev)
        for k, v in waits.items():
            kn[k] = v
        return waits

    def op(self, eng, fn, r=(), w=()):
        waits = self._collect(eng, r, w)
        h = self.h[eng]
        for k, v in waits.items():
            h.wait_ge(self.sem[k], v)
        self.cnt[eng] += 1
        fn(h).then_inc(self.sem[eng], 1)
        self.ninst += 1
        ev = (eng, self.cnt[eng])
        for x in r:
            x.reads[ev[0]] = ev[1]
        for x in w:
            x.lastw = ev
            x.reads = {}
        return ev

    def dma(self, q, pairs, r=(), w=(), **kw):
        waits = self._collect(q, r, w)
        i = self.drr[q]
        self.drr[q] = (i + 1) % NDSEM
        k = 'd_%s_%d' % (q, i)
        prev = self.dtot[k]
        if prev > 0 and self.known[q].get(k, 0) < prev:
            waits[k] = max(waits.get(k, 0), prev)
            self.known[q][k] = prev
        h = self.h[q]
        for k2, v2 in waits.items():
            h.wait_ge(self.sem[k2], v2)
        for (o, a) in pairs:
            h.dma_start(out=o, in_=a, **kw).then_inc(self.sem[k], 16)
            self.ninst += 1
        self.dtot[k] = prev + 16 * len(pairs)
        ev = (k, self.dtot[k])
        for x in r:
            x.reads[ev[0]] = ev[1]
        for x in w:
            x.lastw = ev
            x.reads = {}
        return ev

    def barrier(self):
        tot = {}
        for k, v in self.dtot.items():
            if v > 0:
                tot[k] = v
        for e in ['pe', 'dve', 'act', 'pool']:
            if self.cnt[e] > 0:
                tot[e] = self.cnt[e]
        for eng, h in self.h.items():
            for k, v in tot.items():
                if k == eng:
                    continue
                if self.known[eng].get(k, 0) < v:
                    h.wait_ge(self.sem[k], v)
                    self.known[eng][k] = v

    def finish(self):
        self.barrier()
        self.es.close()


class Cfg:
    def __init__(self, LS=4096, LP=256, NP=2, PAST=512):
        self.LS, self.LP, self.NP, self.PAST = LS, LP, NP, PAST


FFN_DENSE = 2816
FFN_EXPERT = 3584
N_EXPERTS = 8

WEIGHT_SHAPES = {
    'ada_w': (4, D, 6 * D),
    'rwkv_w_rkv': (3, D, D), 'rwkv_w1': (2, D, 64), 'rwkv_w2': (2, 64, D),
    'rwkv_a1': (2, D, 64), 'rwkv_a2': (2, 64, D), 'rwkv_g1': (D, 128), 'rwkv_g2': (128, D),
    'rwkv_w_o': (D, D),
    'att_w_qkv': (D, 1536), 'att_w_o': (D, D),
    'hy_w_in': (D, 3 * D), 'hy_w_out': (D, D), 'hy_f_w1': (33, 64), 'hy_f_w2': (64, 64), 'hy_f_w3': (64, 2 * D),
    'ret_w_in': (D, 8192), 'ret_w_out': (2048, D),
    'ffn_w_in': (2, D, 2 * FFN_DENSE), 'ffn_w_out': (2, FFN_DENSE, D),
    'moe_router': (2, D, 8), 'moe_w_in': (2, 8, D, 2 * FFN_EXPERT), 'moe_w_out': (2, 8, FFN_EXPERT, D),
}


class LazyW(dict):
    def __init__(self, k):
        super().__init__()
        self.k = k

    def __missing__(self, name):
        ap = self.k._din(name, WEIGHT_SHAPES[name])
        self[name] = ap
        return ap


class K:
    def __init__(self, cfg, enable=('ffn',)):
        self.cfg = cfg
        self.enable = enable
        nc = self.nc = bass.Bass("TRN2", target_bir_lowering=False)
        self.p = Prog(nc)
        c = cfg
        self.TS = c.LS
        self.TP = c.NP * c.LP

        def din(name, shape, dt=F32):
            return nc.dram_tensor(name, list(shape), dt, kind="ExternalInput").ap()

        def dout(name, shape, dt=F32):
            return nc.dram_tensor(name, list(shape), dt, kind="ExternalOutput").ap()

        self.xs = din('xs', [c.LS, D])
        self.xp = din('xp', [self.TP, D])
        self.cfm = din('cfm', [128, 8, 2])
        self.adab = din('adab', [128, 4, 48])
        self.ident_d = din('ident', [128, 128])
        self._din = din
        self.W = LazyW(self)
        self.ys = dout('ys', [c.LS, D])
        self.yp = dout('yp', [self.TP, D])
        self._dout = dout

        def dint(name, shape, dt=F32):
            return nc.dram_tensor(name, list(shape), dt, kind="Internal").ap()
        self._dint = dint
        self.units = []
        for i in range(c.LS // 128):
            self.units.append(dict(src=self.xs[i * 128:(i + 1) * 128, :], dst=self.ys[i * 128:(i + 1) * 128, :], g=0, reg=Reg()))
        for i in range(self.TP // 128):
            self.units.append(dict(src=self.xp[i * 128:(i + 1) * 128, :], dst=self.yp[i * 128:(i + 1) * 128, :], g=1, reg=Reg()))
        self.first_touch = True

    def setup(self):
        p, nc = self.p, self.nc
        es = p.es
        self.identF = p.sbuf(es, 'identF', [128, 128], F32)
        self.identB = p.sbuf(es, 'identB', [128, 128], BF16)
        self.onesF = p.sbuf(es, 'onesF', [128, 128], F32)
        self.mod = p.sbuf(es, 'mod', [128, 4, 48, 2], F32)
        self.r_const = Reg()
        self.r_mod = Reg()
        self.ps = [p.psum(es, 'ps%d' % i, [128, 512], F32) for i in range(8)]
        self.r_ps = [Reg() for _ in range(8)]
        self.ps_rr = 0
        self.ps_lim = 8
        p.dma('sp', [(self.identF[:, :], self.ident_d)], w=[self.r_const])
        p.op('dve', lambda e: e.tensor_copy(out=self.identB[:, :], in_=self.identF[:, :]), r=[self.r_const], w=[self.r_const])
        p.op('dve', lambda e: e.memset(self.onesF[:, :], 1.0), w=[self.r_const])
        with ExitStack() as s:
            sc = p.sbuf(s, 'sc', [128, 8, 2], F32)
            ab = p.sbuf(s, 'ab', [128, 4, 48], F32)
            wb = [p.sbuf(s, 'adaw%d' % i, [128, 8, 512], F32) for i in range(2)]
            r_sc, r_ab = Reg(), Reg()
            r_wb = [Reg(), Reg()]
            p.dma('sp', [(sc[:, :, :], self.cfm)], w=[r_sc])
            p.dma('sp', [(ab[:, :, :], self.adab)], w=[r_ab])
            p.op('act', lambda e: e.activation(out=sc[:, :, :], in_=sc[:, :, :], func=AF.Silu), r=[r_sc], w=[r_sc])
            it = 0
            for l in range(4):
                for blk in range(12):
                    b = it % 2
                    it += 1
                    src = self.W['ada_w'][l, :, blk * 512:(blk + 1) * 512].rearrange("(c p) n -> p c n", p=128)
                    p.dma('sp', [(wb[b][:, :, :], src)], w=[r_wb[b]])
                    pb, rpb = self.next_ps()
                    for sub in range(4):
                        for c in range(8):
                            p.op('pe', lambda e, c=c, sub=sub, b=b, pb=pb: e.matmul(
                                pb[:, sub * 2:sub * 2 + 2], lhsT=wb[b][:, c, sub * 128:(sub + 1) * 128], rhs=sc[:, c, :],
                                start=(c == 0), stop=(c == 7)), r=[r_wb[b], r_sc], w=[rpb])
                    for sub in range(4):
                        jc = blk * 4 + sub
                        p.op('dve', lambda e, sub=sub, jc=jc, l=l, pb=pb: e.tensor_scalar(
                            out=self.mod[:, l, jc, :], in0=pb[:, sub * 2:sub * 2 + 2], scalar1=ab[:, l, jc:jc + 1],
                            scalar2=None, op0=ALU.add), r=[rpb, r_ab], w=[self.r_mod])
            for j in (1, 4):
                p.op('dve', lambda e, j=j: e.tensor_scalar(
                    out=self.mod[:, :, j * 8:(j + 1) * 8, :], in0=self.mod[:, :, j * 8:(j + 1) * 8, :],
                    scalar1=1.0, scalar2=None, op0=ALU.add), r=[self.r_mod], w=[self.r_mod])
            p.barrier()

    def next_ps(self):
        i = self.ps_rr % self.ps_lim
        self.ps_rr = (i + 1) % self.ps_lim
        return self.ps[i], self.r_ps[i]

    def modap(self, l, j, c, g):
        return self.mod[:, l, j * 8 + c, g:g + 1]

    def gate_bc(self, out_tile, r_out, l, j, g, diag, r_diag):
        p = self.p
        for half in range(2):
            pb, rpb = self.next_ps()
            for cc in range(4):
                c = half * 4 + cc
                p.op('dve', lambda e, c=c: e.tensor_scalar(out=diag[:, :], in0=self.identF[:, :], scalar1=self.modap(l, j, c, g),
                                                            scalar2=None, op0=ALU.mult), r=[self.r_const, self.r_mod], w=[r_diag])
                p.op('pe', lambda e, cc=cc, pb=pb: e.matmul(pb[:, cc * 128:(cc + 1) * 128], lhsT=self.onesF[:, :], rhs=diag[:, :],
                                                            start=True, stop=True), r=[r_diag, self.r_const], w=[rpb])
            p.op('act', lambda e, half=half, pb=pb: e.activation(out=out_tile[:, half * 512:(half + 1) * 512], in_=pb[:, :], func=AF.Copy),
                 r=[rpb], w=[r_out])

    def norm_hT(self, xt, r_xt, l, jsh, g, hT_dst, r_hT, scr, hTf=None, r_hTf=None):
        p = self.p
        ss, xn, junk, r_ss, r_xn, r_junk = scr
        p.op('act', lambda e: e.activation(out=junk[:, :], in_=xt, func=AF.Square, accum_out=ss[:, 0:1]), r=[r_xt], w=[r_junk, r_ss])
        p.op('act', lambda e: e.activation(out=ss[:, 0:1], in_=ss[:, 0:1], func=AF.Sqrt, scale=1.0 / D, bias=EPS), r=[r_ss], w=[r_ss])
        p.op('dve', lambda e: e.reciprocal(out=ss[:, 0:1], in_=ss[:, 0:1]), r=[r_ss], w=[r_ss])
        p.op('act', lambda e: e.activation(out=xn[:, :], in_=xt, func=AF.Copy, scale=ss[:, 0:1]), r=[r_xt, r_ss], w=[r_xn])
        for half in range(2):
            pb, rpb = self.next_ps()
            for cc in range(4):
                c = half * 4 + cc
                p.op('pe', lambda e, c=c, cc=cc, pb=pb: e.transpose(out=pb[:, cc * 128:(cc + 1) * 128], in_=xn[:, c * 128:(c + 1) * 128],
                                                                    identity=self.identF[:, :]), r=[r_xn, self.r_const], w=[rpb])
            for cc in range(4):
                c = half * 4 + cc
                p.op('dve', lambda e, c=c, cc=cc, pb=pb: e.tensor_scalar(
                    out=hT_dst(c), in0=pb[:, cc * 128:(cc + 1) * 128], scalar1=self.modap(l, jsh + 1, c, g),
                    scalar2=self.modap(l, jsh, c, g), op0=ALU.mult, op1=ALU.add), r=[rpb, self.r_mod], w=[r_hT])
                if hTf is not None:
                    p.op('dve', lambda e, c=c, cc=cc, pb=pb: e.tensor_scalar(
                        out=hTf[:, c, :], in0=pb[:, cc * 128:(cc + 1) * 128], scalar1=self.modap(l, jsh + 1, c, g),
                        scalar2=self.modap(l, jsh, c, g), op0=ALU.mult, op1=ALU.add), r=[rpb, self.r_mod], w=[r_hTf])

    def ffn_phase(self, l):
        p, nc = self.p, self.nc
        moe = (l % 2 == 1)
        li = l // 2
        if moe:
            nexp, H = N_EXPERTS, FFN_EXPERT
        else:
            nexp, H = 1, FFN_DENSE
        HB = 256
        npiece = H // HB
        nu = len(self.units)
        halves = [list(range(0, nu // 2)), list(range(nu // 2, nu))]
        for hu in halves:
            nh = len(hu)
            T = nh * 128
            with ExitStack() as s:
                hT = p.sbuf(s, 'f_hT', [128, 8, T], BF16)
                acc = p.sbuf(s, 'f_acc', [128, nh, D], F32)
                xt = [p.sbuf(s, 'f_xt%d' % i, [128, D], F32) for i in range(2)]
                r_xt = [Reg(), Reg()]
                ss = p.sbuf(s, 'f_ss', [128, 2], F32)
                xn = p.sbuf(s, 'f_xn', [128, D], F32)
                junk = p.sbuf(s, 'f_junk', [128, D], BF16)
                scr = (ss, xn, junk, Reg(), Reg(), Reg())
                gbc = [p.sbuf(s, 'f_gbc%d' % g, [128, D], F32) for g in range(2)]
                r_gbc = [Reg(), Reg()]
                diag = p.sbuf(s, 'f_diag', [128, 128], F32)
                r_diag = Reg()
                win = [p.sbuf(s, 'f_win%d' % i, [128, 8, 2 * HB], BF16) for i in range(2)]
                wout = [p.sbuf(s, 'f_wout%d' % i, [128, HB // 128, D], BF16) for i in range(2)]
                r_w = [Reg(), Reg()]
                sg = [p.sbuf(s, 'f_sg%d' % i, [128, 512], BF16) for i in range(2)]
                hid = [p.sbuf(s, 'f_hid%d' % i, [128, HB // 128, 512], BF16) for i in range(2)]
                r_sg = [Reg(), Reg()]
                r_hid = [Reg(), Reg()]
                r_hT = [Reg() for _ in range(nh)]
                r_acc = [Reg() for _ in range(nh)]
                if moe:
                    hTf = p.sbuf(s, 'f_hTf', [128, 8, 128], F32)
                    r_hTf = Reg()
                    rt = p.sbuf(s, 'f_rt', [128, 8, 8], F32)
                    r_rt = Reg()
                    gates = p.sbuf(s, 'f_gates', [128, nh, 8], F32)
                    r_gates = [Reg() for _ in range(nh)]
                    tk = p.sbuf(s, 'f_tk', [128, 48], F32)
                    r_tk = Reg()
                    p.dma('sp', [(rt[:, :, :], self.W['moe_router'][li].rearrange("(c p) e -> p c e", p=128))], w=[r_rt])
                for g in range(2):
                    self.gate_bc(gbc[g], r_gbc[g], l, 5, g, diag, r_diag)
                for ii, ui in enumerate(hu):
                    u = self.units[ui]
                    b = ii % 2
                    src = u['src'] if self.first_touch else u['dst']
                    p.dma('sp', [(xt[b][:, :], src)], r=[u['reg']], w=[r_xt[b]])
                    dbg = ''
                    norouter = 'norouter' in dbg
                    self.norm_hT(xt[b][:, :], r_xt[b], l, 3, u['g'], lambda c, ii=ii: hT[:, c, ii * 128:(ii + 1) * 128], r_hT[ii], scr,
                                 hTf if (moe and not norouter) else None, r_hTf if moe else None)
                    if moe and norouter:
                        p.op('dve', lambda e, ii=ii: e.memset(gates[:, ii, :], 0.125), w=[r_gates[ii]])
                    elif moe:
                        pb, rpb = self.next_ps()
                        for c in range(8):
                            p.op('pe', lambda e, c=c, pb=pb: e.matmul(pb[:, 0:8], lhsT=hTf[:, c, :], rhs=rt[:, c, :], start=(c == 0), stop=(c == 7)),
                                 r=[r_hTf, r_rt], w=[rpb])
                        if 'nogate' in dbg:
                            p.op('dve', lambda e, ii=ii: e.memset(gates[:, ii, :], 0.125), r=[rpb], w=[r_gates[ii]])
                            continue
                        lg, m1, eq, lg2, m2, sel, ex, sm = (tk[:, 0:8], tk[:, 8:9], tk[:, 9:17], tk[:, 17:25], tk[:, 25:26], tk[:, 26:34],
                                                            tk[:, 34:42], tk[:, 42:43])
                        rr = [r_tk]
                        p.op('dve', lambda e, pb=pb: e.tensor_copy(out=lg, in_=pb[:, 0:8]), r=[rpb], w=rr)
                        p.op('dve', lambda e: e.tensor_reduce(out=m1, in_=lg, axis=AX.X, op=ALU.max), r=rr, w=rr)
                        p.op('dve', lambda e: e.tensor_scalar(out=eq, in0=lg, scalar1=m1, scalar2=-1e30, op0=ALU.is_equal, op1=ALU.mult), r=rr, w=rr)
                        p.op('dve', lambda e: e.tensor_tensor(out=lg2, in0=eq, in1=lg, op=ALU.add), r=rr, w=rr)
                        p.op('dve', lambda e: e.tensor_reduce(out=m2, in_=lg2, axis=AX.X, op=ALU.max), r=rr, w=rr)
                        p.op('dve', lambda e: e.tensor_scalar(out=sel, in0=lg, scalar1=m2, scalar2=None, op0=ALU.is_ge), r=rr, w=rr)
                        p.op('dve', lambda e: e.tensor_scalar(out=m1, in0=m1, scalar1=-1.0, scalar2=None, op0=ALU.mult), r=rr, w=rr)
                        p.op('act', lambda e: e.activation(out=ex, in_=lg, func=AF.Exp, bias=m1, scale=1.0), r=rr, w=rr)
                        p.op('dve', lambda e: e.tensor_tensor(out=ex, in0=ex, in1=sel, op=ALU.mult), r=rr, w=rr)
                        p.op('dve', lambda e: e.tensor_reduce(out=sm, in_=ex, axis=AX.X, op=ALU.add), r=rr, w=rr)
                        p.op('dve', lambda e: e.reciprocal(out=sm, in_=sm), r=rr, w=rr)
                        p.op('dve', lambda e, ii=ii: e.tensor_scalar(out=gates[:, ii, :], in0=ex, scalar1=sm, scalar2=None, op0=ALU.mult),
                             r=rr, w=[r_gates[ii]])
                pc = 0
                chunks = [(t0, min(512, T - t0)) for t0 in range(0, T, 512)]
                for ex_i in range(nexp):
                    if moe:
                        w_in_d = self.W['moe_w_in'][li, ex_i]
                        w_out_d = self.W['moe_w_out'][li, ex_i]
                    else:
                        w_in_d = self.W['ffn_w_in'][li]
                        w_out_d = self.W['ffn_w_out'][li]
                    for pj in range(npiece):
                        b = pc % 2
                        first = (pc == 0)
                        pc += 1
                        h0 = pj * HB
                        p.dma('pool', [(win[b][:, :, 0:HB], w_in_d[:, h0:h0 + HB].rearrange("(c p) n -> p c n", p=128)),
                                       (win[b][:, :, HB:2 * HB], w_in_d[:, H + h0:H + h0 + HB].rearrange("(c p) n -> p c n", p=128)),
                                       (wout[b][:, :, :], w_out_d[h0:h0 + HB, :].rearrange("(c p) n -> p c n", p=128))], w=[r_w[b]])
                        for ci, (t0, tn) in enumerate(chunks):
                            rh = [r_hT[i] for i in range(t0 // 128, (t0 + tn) // 128)]
                            hb = (pc + ci) % 2
                            for hc in range(HB // 128):
                                pg, rpg = self.next_ps()
                                pu, rpu = self.next_ps()
                                for c in range(8):
                                    p.op('pe', lambda e, c=c, hc=hc, pg=pg: e.matmul(pg[:, 0:tn], lhsT=win[b][:, c, hc * 128:(hc + 1) * 128],
                                                                                      rhs=hT[:, c, t0:t0 + tn], start=(c == 0), stop=(c == 7)),
                                         r=rh + [r_w[b]], w=[rpg])
                                for c in range(8):
                                    p.op('pe', lambda e, c=c, hc=hc, pu=pu: e.matmul(pu[:, 0:tn], lhsT=win[b][:, c, HB + hc * 128:HB + (hc + 1) * 128],
                                                                                      rhs=hT[:, c, t0:t0 + tn], start=(c == 0), stop=(c == 7)),
                                         r=rh + [r_w[b]], w=[rpu])
                                sb = hc % 2
                                p.op('act', lambda e, pg=pg, sb=sb: e.activation(out=sg[sb][:, 0:tn], in_=pg[:, 0:tn], func=AF.Silu),
                                     r=[rpg], w=[r_sg[sb]])
                                p.op('dve', lambda e, pu=pu, sb=sb, hc=hc, hb=hb: e.tensor_tensor(out=hid[hb][:, hc, 0:tn], in0=sg[sb][:, 0:tn],
                                                                                                   in1=pu[:, 0:tn], op=ALU.mult),
                                     r=[r_sg[sb], rpu], w=[r_hid[hb]])
                            for st in range(tn // 128):
                                ui_loc = t0 // 128 + st
                                for ch in range(2):
                                    po, rpo = self.next_ps()
                                    for hc in range(HB // 128):
                                        p.op('pe', lambda e, hc=hc, st=st, ch=ch, po=po, hb=hb: e.matmul(
                                            po[:, :], lhsT=hid[hb][:, hc, st * 128:(st + 1) * 128], rhs=wout[b][:, hc, ch * 512:(ch + 1) * 512],
                                            start=(hc == 0), stop=(hc == HB // 128 - 1)), r=[r_hid[hb], r_w[b]], w=[rpo])
                                    accv = acc[:, ui_loc, ch * 512:(ch + 1) * 512]
                                    if moe:
                                        gsc = gates[:, ui_loc, ex_i:ex_i + 1]
                                        rg = [r_gates[ui_loc]]
                                    else:
                                        gsc = 1.0
                                        rg = []
                                    if first:
                                        p.op('dve', lambda e, po=po, accv=accv, gsc=gsc: e.tensor_scalar(out=accv, in0=po[:, :], scalar1=gsc, scalar2=None,
                                                                                                     op0=ALU.mult), r=[rpo] + rg, w=[r_acc[ui_loc]])
                                    else:
                                        p.op('dve', lambda e, po=po, accv=accv, gsc=gsc: e.scalar_tensor_tensor(out=accv, in0=po[:, :], scalar=gsc, in1=accv,
                                                                                                            op0=ALU.mult, op1=ALU.add),
                                             r=[rpo] + rg, w=[r_acc[ui_loc]])
                for ii, ui in enumerate(hu):
                    u = self.units[ui]
                    b = ii % 2
                    src = u['src'] if self.first_touch else u['dst']
                    p.dma('sp', [(xt[b][:, :], src)], r=[u['reg']], w=[r_xt[b]])
                    g = u['g']
                    p.op('dve', lambda e, ii=ii, g=g: e.tensor_tensor(out=acc[:, ii, :], in0=acc[:, ii, :], in1=gbc[g][:, :], op=ALU.mult),
                         r=[r_gbc[g]], w=[r_acc[ii]])
                    p.op('dve', lambda e, ii=ii, b=b: e.tensor_tensor(out=acc[:, ii, :], in0=acc[:, ii, :], in1=xt[b][:, :], op=ALU.add),
                         r=[r_xt[b]], w=[r_acc[ii]])
                    p.dma('sp', [(u['dst'], acc[:, ii, :])], r=[r_acc[ii]], w=[u['reg']])
                p.barrier()
        self.first_touch = False

    def rwkv_phase(self, l, seqs):
        p, nc, W = self.p, self.nc, self.W
        NT = 256
        self.ps_lim = 7
        with ExitStack() as s:
            def sb(name, shape, dt):
                return p.sbuf(s, 'rw_' + name, shape, dt)
            r_wt = Reg()
            Wr, Wk, Wv, Wo = (sb(n, [128, 8, 1024], BF16) for n in ('Wr', 'Wk', 'Wv', 'Wo'))
            W1 = sb('W1', [128, 8, 2, 64], BF16)
            A1 = sb('A1', [128, 8, 2, 64], BF16)
            W2 = sb('W2', [64, 2, 1024], BF16)
            A2 = sb('A2', [64, 2, 1024], BF16)
            G1 = sb('G1', [128, 8, 128], BF16)
            G2 = sb('G2', [128, 1024], BF16)
            wrkv = W['rwkv_w_rkv']
            p.dma('pool', [(Wr[:, :, :], wrkv[0].rearrange("(c p) n -> p c n", p=128)),
                           (Wk[:, :, :], wrkv[1].rearrange("(c p) n -> p c n", p=128)),
                           (Wv[:, :, :], wrkv[2].rearrange("(c p) n -> p c n", p=128)),
                           (Wo[:, :, :], W['rwkv_w_o'].rearrange("(c p) n -> p c n", p=128))], w=[r_wt])
            prs = []
            for d in range(2):
                prs += [(W1[:, :, d, :], W['rwkv_w1'][d].rearrange("(c p) r -> p c r", p=128)),
                        (A1[:, :, d, :], W['rwkv_a1'][d].rearrange("(c p) r -> p c r", p=128)),
                        (W2[:, d, :], W['rwkv_w2'][d]), (A2[:, d, :], W['rwkv_a2'][d])]
            prs += [(G1[:, :, :], W['rwkv_g1'].rearrange("(c p) r -> p c r", p=128)), (G2[:, :], W['rwkv_g2'])]
            p.dma('pool', prs, w=[r_wt])
            mixS = sb('mixS', [128, 6, 8], F32)
            hmv = sb('hmv', [64, 8, 16], F32)
            rows = sb('rows', [128, 2, 1024], F32)
            mask4 = sb('mask4', [128, 2, 512], F32)
            maskN = sb('maskN', [128, 4, 128], F32)
            r_c = Reg()
            p.dma('sp', [(mixS[:, :, :], self.rw_mix), (hmv[:, :, :], self.rw_hm), (rows[:, :, :], self.rw_rows),
                         (mask4[:, :, :], self.rw_mask4), (maskN[:, :, :], self.rw_maskN)], w=[r_c])
            omk = sb('omk', [64, 16], F32)
            p.op('dve', lambda e: e.tensor_scalar(out=omk[:, :], in0=hmv[:, 5, :], scalar1=-1.0, scalar2=1.0, op0=ALU.mult, op1=ALU.add), r=[r_c], w=[r_c])
            ones64 = sb('ones64', [64, 64], BF16)
            onesF = sb('onesF', [64, NT], F32)
            p.op('dve', lambda e: e.memset(ones64[:, :], 1.0), w=[r_c])
            p.op('dve', lambda e: e.memset(onesF[:, :], 1.0), w=[r_c])
            gbc = sb('gbc', [128, 1024], F32)
            r_gbc = Reg()
            diag = sb('diag', [128, 128], F32)
            r_diag = Reg()
            S32 = sb('S32', [64, 16, 2, 64], F32)
            r_S = [[Reg() for _ in range(2)] for _ in range(16)]
            hTt = sb('hTt', [128, 8, NT + 2], BF16); r_hTt = Reg()
            xx = sb('xx', [128, 8, NT], BF16); r_xx = Reg()
            xi = [sb('xi%d' % i, [128, 8, NT], BF16) for i in range(2)]; r_xi = [Reg(), Reg()]
            tmpx = xi[1]; r_tmpx = r_xi[1]
            rH = sb('rH', [64, 16, NT], BF16); r_rH = Reg()
            kH = sb('kH', [64, 16, NT], BF16); r_kH = Reg()
            Vtok = sb('Vtok', [128, NT // 128, 1024], BF16); r_V = Reg()
            hw = sb('hw', [64, NT], BF16); ha = sb('ha', [64, NT], BF16); hg = sb('hg', [128, NT], BF16)
            r_hw, r_ha, r_hg = Reg(), Reg(), Reg()
            ytok = sb('ytok', [128, NT // 128, 1024], F32); r_y = Reg()
            bon = sb('bon', [128, NT // 128, 16], F32); r_bon = Reg()
            TF = ['lw', 'aa', 'kkr', 'nr', 'kk', 't1', 'kd', 'bb', 'G', 'Dd', 'Dl', 'E1', 'E2']
            TB = ['sq', 'pr', 'AT', 'RT', 'BT', 'KT']
            tset = []
            for i in range(1):
                dd = {n: sb('%s%d' % (n, i), [64, NT], F32) for n in TF}
                dd.update({n: sb('%s%d' % (n, i), [128 if n in ('AT', 'RT', 'BT', 'KT') else 64, NT], BF16) for n in TB})
                for n in ('AT', 'RT', 'BT', 'KT'):
                    p.op('dve', lambda e, t=dd[n]: e.memset(t[64:128, :], 0.0), w=[r_c])
                dd['sc'] = sb('sc%d' % i, [64, NT // 128, 3], F32)
                dd['r'] = {n: Reg() for n in TF + TB + ['sc']}
                tset.append(dd)
            uset = []
            for i in range(2):
                dd = dict(BK=sb('BK%d' % i, [128, 128], BF16), Am=sb('Am%d' % i, [128, 512], BF16), Nm=sb('Nm%d' % i, [128, 128], BF16),
                          TT=[sb('TT%d_%d' % (i, k), [128, 128], BF16) for k in range(2)],
                          PP=[sb('PP%d_%d' % (i, k), [128, 256], BF16) for k in range(2)],
                          Sbf=sb('Sbf%d' % i, [128, 64], BF16), Xsb=sb('Xsb%d' % i, [128, 64], BF16), Usb=sb('Usb%d' % i, [128, 64], BF16),
                          tmpS=sb('tmpS%d' % i, [64, 64], F32), NdT=sb('NdT%d' % i, [128, 128], BF16), NTo=sb('NTo%d' % i, [128, 128], BF16), acc=sb('acc%d' % i, [128, 64], BF16))
                dd['r'] = {n: Reg() for n in ['BK', 'Am', 'Nm', 'TT0', 'TT1', 'PP0', 'PP1', 'Sbf', 'Xsb', 'Usb', 'tmpS', 'NdT', 'NTo', 'acc']}
                p.op('dve', lambda e, t=dd['Sbf']: e.memset(t[64:128, :], 0.0), w=[r_c])
                uset.append(dd)
            xt = sb('xt', [128, 1024], F32); r_xt = Reg()
            yf = sb('yf', [128, 1024], F32); r_yf = Reg()
            bf_ = sb('bf', [128, 16], F32); r_bf = Reg()
            yj = sb('yj', [128, 1024], F32); r_yj = Reg()
            ysq = sb('ysq', [128, 1024], F32); r_ysq = Reg()
            st = sb('st', [128, 4, 16], F32); r_st = Reg()
            gTs = sb('gTs', [128, 8, 128], BF16); r_gTs = Reg()
            ygT = sb('ygT', [128, 8, 128], BF16); r_ygT = Reg()
            osb = yf; r_osb = r_yf
            ss = sb('ss', [128, 2], F32)
            scr = (ss, yj, ysq, Reg(), r_yj, r_ysq)
            hts = sb('hts', [128, 8, 128], BF16); r_hts = Reg()
            zer = sb('zer', [128, 8, 1], BF16); r_zer = Reg()
            p.op('dve', lambda e: e.memset(zer[:, :, :], 0.0), w=[r_zer])
            stio = sb('stio', [64, 64], F32); r_stio = Reg()

            for si, sq_ in enumerate(seqs):
                L, g, units = sq_['L'], sq_['g'], sq_['units']
                hTd, yfd, bfd = sq_['hTd'], sq_['yfd'], sq_['bfd']
                r_hTd, r_yfd, r_bfd = Reg(), Reg(), Reg()
                self.gate_bc(gbc, r_gbc, l, 2, g, diag, r_diag)
                p.dma('sp', [(hTd[:, :, 0:1], zer[:, :, :]), (hTd[:, :, L + 1:L + 2], zer[:, :, :])], r=[r_zer], w=[r_hTd], allow_slow_non_contiguous=True)
                for i, u in enumerate(units):
                    src = u['src'] if self.first_touch else u['dst']
                    p.dma('sp', [(xt[:, :], src)], r=[u['reg']], w=[r_xt])
                    self.norm_hT(xt[:, :], r_xt, l, 0, g, lambda c: hts[:, c, :], r_hts, scr)
                    p.dma('sp', [(hTd[:, :, 1 + i * 128:1 + (i + 1) * 128], hts[:, :, :])], r=[r_hts], w=[r_hTd])
                for h in range(16):
                    for d in range(2):
                        if sq_['s0'] is None:
                            p.op('dve', lambda e, h=h, d=d: e.memset(S32[:, h, d, :], 0.0), w=[r_S[h][d]])
                        else:
                            p.dma('sp', [(stio[:, :], sq_['s0'][d, h])], w=[r_stio])
                            pb, rpb = self.next_ps()
                            p.op('pe', lambda e, pb=pb: e.transpose(out=pb[0:64, 0:64], in_=stio[:, :], identity=self.identF[0:64, 0:64]),
                                 r=[r_stio, self.r_const], w=[rpb])
                            p.op('act', lambda e, pb=pb, h=h, d=d: e.activation(out=S32[:, h, d, :], in_=pb[0:64, 0:64], func=AF.Copy),
                                 r=[rpb], w=[r_S[h][d]])
                ntile = L // NT
                for d in range(2):
                    tiles = list(range(ntile)) if d == 0 else list(range(ntile - 1, -1, -1))
                    for ti in tiles:
                        t0 = ti * NT
                        n = NT
                        nj = n // 128
                        p.dma('sp', [(hTt[:, :, :], hTd[:, :, t0:t0 + n + 2])], r=[r_hTd], w=[r_hTt])
                        p.op('dve', lambda e: e.tensor_tensor(out=tmpx[:, :, :], in0=hTt[:, :, 0:n], in1=hTt[:, :, 2:n + 2], op=ALU.add),
                             r=[r_hTt], w=[r_tmpx])
                        p.op('dve', lambda e: e.scalar_tensor_tensor(out=xx[:, :, :], in0=tmpx[:, :, :], scalar=0.5, in1=hTt[:, :, 1:n + 1],
                                                                      op0=ALU.mult, op1=ALU.subtract), r=[r_tmpx, r_hTt], w=[r_xx])
                        vi = [0]

                        def variant(i):
                            b = vi[0] % 2
                            vi[0] += 1
                            for c in range(8):
                                eng = 'dve' if c % 2 == 0 else 'dve'
                                p.op(eng, lambda e, c=c, b=b, i=i: e.scalar_tensor_tensor(
                                    out=xi[b][:, c, :], in0=xx[:, c, :], scalar=mixS[:, i, c:c + 1], in1=hTt[:, c, 1:n + 1],
                                    op0=ALU.mult, op1=ALU.add), r=[r_xx, r_hTt, r_c], w=[r_xi[b]])
                            return xi[b], r_xi[b]

                        for (i, Wm, dst, rdst) in ((0, Wr, rH, r_rH), (2, Wk, kH, r_kH)):
                            xb, rxb = variant(i)
                            for h in range(16):
                                pb, rpb = self.next_ps()
                                for c in range(8):
                                    p.op('pe', lambda e, c=c, h=h, pb=pb, xb=xb, Wm=Wm: e.matmul(
                                        pb[0:64, 0:n], lhsT=Wm[:, c, h * 64:(h + 1) * 64], rhs=xb[:, c, :], start=(c == 0), stop=(c == 7)),
                                        r=[rxb, r_wt], w=[rpb])
                                p.op('act', lambda e, h=h, pb=pb, dst=dst: e.activation(out=dst[:, h, :], in_=pb[0:64, 0:n], func=AF.Copy),
                                     r=[rpb], w=[rdst])
                        xb, rxb = variant(3)
                        for j in range(nj):
                            for hf in range(2):
                                pb, rpb = self.next_ps()
                                for c in range(8):
                                    p.op('pe', lambda e, c=c, j=j, hf=hf, pb=pb, xb=xb: e.matmul(
                                        pb[:, :], lhsT=xb[:, c, j * 128:(j + 1) * 128], rhs=Wv[:, c, hf * 512:(hf + 1) * 512],
                                        start=(c == 0), stop=(c == 7)), r=[rxb, r_wt], w=[rpb])
                                p.op('act', lambda e, j=j, hf=hf, pb=pb: e.activation(out=Vtok[:, j, hf * 512:(hf + 1) * 512], in_=pb[:, :], func=AF.Copy),
                                     r=[rpb], w=[r_V])
                        for (i, Wm, dst, rdst, fn) in ((1, W1, hw, r_hw, AF.Tanh), (4, A1, ha, r_ha, AF.Copy)):
                            xb, rxb = variant(i)
                            pb, rpb = self.next_ps()
                            for c in range(8):
                                p.op('pe', lambda e, c=c, pb=pb, xb=xb, Wm=Wm: e.matmul(pb[0:64, 0:n], lhsT=Wm[:, c, d, :], rhs=xb[:, c, :],
                                                                                         start=(c == 0), stop=(c == 7)), r=[rxb, r_wt], w=[rpb])
                            p.op('act', lambda e, pb=pb, dst=dst, fn=fn: e.activation(out=dst[:, :], in_=pb[0:64, 0:n], func=fn), r=[rpb], w=[rdst])
                        if d == 1:
                            xb, rxb = variant(5)
                            pb, rpb = self.next_ps()
                            for c in range(8):
                                p.op('pe', lambda e, c=c, pb=pb, xb=xb: e.matmul(pb[:, 0:n], lhsT=G1[:, c, :], rhs=xb[:, c, :],
                                                                                 start=(c == 0), stop=(c == 7)), r=[rxb, r_wt], w=[rpb])
                            p.op('act', lambda e, pb=pb: e.activation(out=hg[:, :], in_=pb[:, 0:n], func=AF.Sigmoid), r=[rpb], w=[r_hg])
                        pbon, rpbon = self.ps[7], self.r_ps[7]
                        for h in range(16):
                            T = tset[0]
                            R = T['r']
                            hs = slice(h * 64, (h + 1) * 64)
                            pb, rpb = self.next_ps()
                            p.op('pe', lambda e, pb=pb, hs=hs: e.matmul(pb[0:64, 0:n], lhsT=W2[:, d, hs], rhs=hw[:, :], start=True, stop=True),
                                 r=[r_hw, r_wt], w=[rpb])
                            p.op('act', lambda e, pb=pb, T=T, h=h: e.activation(out=T['lw'][:, :], in_=pb[0:64, 0:n], func=AF.Sigmoid,
                                                                                 bias=hmv[:, 0 + d, h:h + 1], scale=1.0), r=[rpb, r_c], w=[R['lw']])
                            p.op('pool', lambda e, T=T: e.tensor_scalar(out=T['lw'][:, :], in0=T['lw'][:, :], scalar1=-0.6065306597126334, scalar2=None,
                                                                         op0=ALU.mult), r=[R['lw']], w=[R['lw']])
                            pb, rpb = self.next_ps()
                            p.op('pe', lambda e, pb=pb, hs=hs: e.matmul(pb[0:64, 0:n], lhsT=A2[:, d, hs], rhs=ha[:, :], start=True, stop=True),
                                 r=[r_ha, r_wt], w=[rpb])
                            p.op('act', lambda e, pb=pb, T=T, h=h: e.activation(out=T['aa'][:, :], in_=pb[0:64, 0:n], func=AF.Sigmoid,
                                                                                 bias=hmv[:, 2 + d, h:h + 1], scale=1.0), r=[rpb, r_c], w=[R['aa']])
                            p.op('dve', lambda e, T=T, h=h: e.tensor_scalar(out=T['kkr'][:, :], in0=kH[:, h, :], scalar1=hmv[:, 4, h:h + 1], scalar2=None,
                                                                             op0=ALU.mult), r=[r_kH, r_c], w=[R['kkr']])
                            p.op('pool', lambda e, T=T: e.tensor_tensor(out=T['sq'][:, :], in0=T['kkr'][:, :], in1=T['kkr'][:, :], op=ALU.mult),
                                 r=[R['kkr']], w=[R['sq']])
                            pb, rpb = self.next_ps()
                            p.op('pe', lambda e, pb=pb, T=T: e.matmul(pb[0:64, 0:n], lhsT=ones64[:, :], rhs=T['sq'][:, :], start=True, stop=True),
                                 r=[R['sq'], r_c], w=[rpb])
                            p.op('act', lambda e, pb=pb, T=T: e.activation(out=T['nr'][:, :], in_=pb[0:64, 0:n], func=AF.Sqrt), r=[rpb], w=[R['nr']])
                            p.op('dve', lambda e, T=T: e.tensor_scalar(out=T['nr'][:, :], in0=T['nr'][:, :], scalar1=1e-12, scalar2=None, op0=ALU.max),
                                 r=[R['nr']], w=[R['nr']])
                            p.op('dve', lambda e, T=T: e.reciprocal(out=T['nr'][:, :], in_=T['nr'][:, :]), r=[R['nr']], w=[R['nr']])
                            p.op('dve', lambda e, T=T: e.tensor_tensor(out=T['kk'][:, :], in0=T['kkr'][:, :], in1=T['nr'][:, :], op=ALU.mult),
                                 r=[R['kkr'], R['nr']], w=[R['kk']])
                            p.op('dve', lambda e, T=T, h=h: e.tensor_scalar(out=T['t1'][:, :], in0=T['aa'][:, :], scalar1=hmv[:, 5, h:h + 1],
                                                                             scalar2=omk[:, h:h + 1], op0=ALU.mult, op1=ALU.add),
                                 r=[R['aa'], r_c], w=[R['t1']])
                            p.op('dve', lambda e, T=T, h=h: e.tensor_tensor(out=T['kd'][:, :], in0=T['t1'][:, :], in1=kH[:, h, :], op=ALU.mult),
                                 r=[R['t1'], r_kH], w=[R['kd']])
                            p.op('pool', lambda e, T=T: e.tensor_tensor(out=T['bb'][:, :], in0=T['kk'][:, :], in1=T['aa'][:, :], op=ALU.mult),
                                 r=[R['kk'], R['aa']], w=[R['bb']])
                            p.op('dve', lambda e, T=T, h=h: e.scalar_tensor_tensor(out=T['pr'][:, :], in0=T['kd'][:, :], scalar=hmv[:, 6, h:h + 1],
                                                                                    in1=rH[:, h, :], op0=ALU.mult, op1=ALU.mult),
                                 r=[R['kd'], r_rH, r_c], w=[R['pr']])
                            for j in range(nj):
                                p.op('pe', lambda e, T=T, j=j, h=h: e.matmul(pbon[:, j * 16 + h:j * 16 + h + 1], lhsT=T['pr'][:, j * 128:(j + 1) * 128],
                                                                             rhs=ones64[:, 0:1], start=True, stop=True), r=[R['pr'], r_c], w=[rpbon])
                            p.op('dve', lambda e, T=T: e.tensor_tensor_scan(out=T['G'][:, :], data0=onesF[:, 0:n], data1=T['lw'][:, :], initial=0.0,
                                                                             op0=ALU.mult, op1=ALU.add), r=[R['lw'], r_c], w=[R['G']])
                            for j in range(nj):
                                js = slice(j * 128, (j + 1) * 128)
                                p.op('dve', lambda e, T=T, j=j, js=js: e.tensor_scalar(out=T['Dd'][:, js], in0=T['G'][:, js],
                                                                                       scalar1=T['G'][:, j * 128 + 63:j * 128 + 64], scalar2=None,
                                                                                       op0=ALU.subtract), r=[R['G']], w=[R['Dd']])
                                if j == 0:
                                    p.op('pool', lambda e, T=T, j=j: e.tensor_copy(out=T['sc'][:, j, 0:1], in_=T['G'][:, 63:64]), r=[R['G']], w=[R['sc']])
                                else:
                                    p.op('pool', lambda e, T=T, j=j: e.tensor_tensor(out=T['sc'][:, j, 0:1], in0=T['G'][:, j * 128 + 63:j * 128 + 64],
                                                                                     in1=T['G'][:, j * 128 - 1:j * 128], op=ALU.subtract),
                                         r=[R['G']], w=[R['sc']])
                                p.op('pool', lambda e, T=T, j=j: e.tensor_tensor(out=T['sc'][:, j, 1:2], in0=T['G'][:, j * 128 + 127:j * 128 + 128],
                                                                                 in1=T['G'][:, j * 128 + 63:j * 128 + 64], op=ALU.subtract),
                                     r=[R['G']], w=[R['sc']])
                                p.op('pool', lambda e, T=T, j=j: e.tensor_tensor(out=T['sc'][:, j, 2:3], in0=T['sc'][:, j, 0:1], in1=T['sc'][:, j, 1:2],
                                                                                 op=ALU.add), r=[R['sc']], w=[R['sc']])
                            p.op('act', lambda e, T=T: e.activation(out=T['sc'][:, :, :], in_=T['sc'][:, :, :], func=AF.Exp), r=[R['sc']], w=[R['sc']])
                            p.op('pool', lambda e, T=T: e.tensor_tensor(out=T['Dl'][:, :], in0=T['Dd'][:, :], in1=T['lw'][:, :], op=ALU.subtract),
                                 r=[R['Dd'], R['lw']], w=[R['Dl']])
                            if d == 0:
                                p.op('act', lambda e, T=T: e.activation(out=T['E1'][:, :], in_=T['Dl'][:, :], func=AF.Exp), r=[R['Dl']], w=[R['E1']])
                                p.op('dve', lambda e, T=T: e.scalar_tensor_tensor(out=T['AT'][0:64, :], in0=T['kk'][:, :], scalar=-1.0, in1=T['E1'][:, :],
                                                                                   op0=ALU.mult, op1=ALU.mult), r=[R['kk'], R['E1']], w=[R['AT']])
                                p.op('act', lambda e, T=T: e.activation(out=T['E2'][:, :], in_=T['Dd'][:, :], func=AF.Exp), r=[R['Dd']], w=[R['E2']])
                                p.op('dve', lambda e, T=T, h=h: e.tensor_tensor(out=T['RT'][0:64, :], in0=rH[:, h, :], in1=T['E2'][:, :], op=ALU.mult),
                                     r=[r_rH, R['E2']], w=[R['RT']])
                                p.op('act', lambda e, T=T: e.activation(out=T['E1'][:, :], in_=T['Dd'][:, :], func=AF.Exp, scale=-1.0), r=[R['Dd'], R['AT']], w=[R['E1']])
                                p.op('dve', lambda e, T=T: e.tensor_tensor(out=T['BT'][0:64, :], in0=T['bb'][:, :], in1=T['E1'][:, :], op=ALU.mult),
                                     r=[R['bb'], R['E1']], w=[R['BT']])
                                p.op('pool', lambda e, T=T: e.tensor_tensor(out=T['KT'][0:64, :], in0=T['kd'][:, :], in1=T['E1'][:, :], op=ALU.mult),
                                     r=[R['kd'], R['E1']], w=[R['KT']])
                            else:
                                p.op('act', lambda e, T=T: e.activation(out=T['E1'][:, :], in_=T['Dd'][:, :], func=AF.Exp, scale=-1.0), r=[R['Dd']], w=[R['E1']])
                                p.op('dve', lambda e, T=T: e.scalar_tensor_tensor(out=T['AT'][0:64, :], in0=T['kk'][:, :], scalar=-1.0, in1=T['E1'][:, :],
                                                                                   op0=ALU.mult, op1=ALU.mult), r=[R['kk'], R['E1']], w=[R['AT']])
                                p.op('act', lambda e, T=T: e.activation(out=T['E2'][:, :], in_=T['Dl'][:, :], func=AF.Exp, scale=-1.0), r=[R['Dl']], w=[R['E2']])
                                p.op('dve', lambda e, T=T, h=h: e.tensor_tensor(out=T['RT'][0:64, :], in0=rH[:, h, :], in1=T['E2'][:, :], op=ALU.mult),
                                     r=[r_rH, R['E2']], w=[R['RT']])
                                p.op('act', lambda e, T=T: e.activation(out=T['E1'][:, :], in_=T['Dl'][:, :], func=AF.Exp), r=[R['Dl'], R['AT']], w=[R['E1']])
                                p.op('dve', lambda e, T=T: e.tensor_tensor(out=T['BT'][0:64, :], in0=T['bb'][:, :], in1=T['E1'][:, :], op=ALU.mult),
                                     r=[R['bb'], R['E1']], w=[R['BT']])
                                p.op('pool', lambda e, T=T: e.tensor_tensor(out=T['KT'][0:64, :], in0=T['kd'][:, :], in1=T['E1'][:, :], op=ALU.mult),
                                     r=[R['kd'], R['E1']], w=[R['KT']])
                            rATs = [R['AT'], R['RT'], R['BT'], R['KT']]
                            for j in (range(nj) if d == 0 else range(nj - 1, -1, -1)):
                                U = uset[j % 2]
                                UR = U['r']
                                js = slice(j * 128, (j + 1) * 128)
                                em = T['sc'][:, j, 0:1] if d == 0 else T['sc'][:, j, 1:2]
                                e2 = T['sc'][:, j, 1:2] if d == 0 else T['sc'][:, j, 0:1]
                                e1 = T['sc'][:, j, 2:3]
                                pb, rpb = self.next_ps()
                                pv = pb[:, :].bitcast(BF16)
                                p.op('pe', lambda e, pv=pv, T=T, js=js: e.transpose(out=pv[:, 0:64], in_=T['BT'][0:64, js], identity=self.identB[0:64, 0:64]),
                                     r=[R['BT'], self.r_const], w=[rpb])
                                p.op('pe', lambda e, pv=pv, T=T, js=js: e.transpose(out=pv[:, 64:128], in_=T['KT'][0:64, js], identity=self.identB[0:64, 0:64]),
                                     r=[R['KT'], self.r_const], w=[rpb])
                                p.op('act', lambda e, pv=pv, U=U: e.activation(out=U['BK'][:, :], in_=pv[:, 0:128], func=AF.Copy), r=[rpb], w=[UR['BK']])
                                pb, rpb = self.next_ps()
                                for q, (la, ra) in enumerate((('BT', 'AT'), ('BT', 'RT'), ('KT', 'AT'), ('KT', 'RT'))):
                                    p.op('pe', lambda e, pb=pb, q=q, la=la, ra=ra, T=T, js=js: e.matmul(
                                        pb[:, q * 128:(q + 1) * 128], lhsT=T[la][:, js], rhs=T[ra][:, js], start=True, stop=True), r=rATs, w=[rpb])
                                p.op('dve', lambda e, pb=pb, U=U: e.tensor_tensor(out=U['Am'][:, :], in0=pb[:, :], in1=mask4[:, d, :], op=ALU.mult),
                                     r=[rpb, r_c], w=[UR['Am']])
                                pb, rpb = self.next_ps()
                                p.op('pe', lambda e, pb=pb, T=T, js=js: e.matmul(pb[:, 0:128], lhsT=T['AT'][:, js], rhs=T['BT'][:, js], start=True, stop=True),
                                     r=rATs, w=[rpb])
                                p.op('dve', lambda e, pb=pb, U=U: e.tensor_tensor(out=U['Nm'][:, :], in0=pb[:, 0:128], in1=maskN[:, d, :], op=ALU.mult),
                                     r=[rpb, r_c], w=[UR['Nm']])
                                p.op('pool', lambda e, U=U: e.tensor_tensor(out=U['NdT'][:, :], in0=U['Am'][:, 0:128], in1=maskN[:, 2, :], op=ALU.mult),
                                     r=[UR['Am'], r_c], w=[UR['NdT']])
                                p.op('pool', lambda e, U=U: e.tensor_tensor(out=U['NTo'][:, :], in0=U['Am'][:, 0:128], in1=maskN[:, 3, :], op=ALU.mult),
                                     r=[UR['Am'], r_c], w=[UR['NTo']])
                                p.op('pool', lambda e, U=U: e.tensor_tensor(out=U['TT'][0][:, :], in0=U['NdT'][:, :], in1=self.identB[:, :], op=ALU.add),
                                     r=[UR['NdT'], self.r_const], w=[UR['TT0']])
                                Pm, rP = U['Nm'][:, :], UR['Nm']
                                PT, rPT = U['NdT'][:, :], UR['NdT']
                                tcur = 0
                                NIT = 4
                                for it in range(NIT):
                                    pb, rpb = self.next_ps()
                                    PPt, rPP = U['PP'][it % 2], UR['PP%d' % (it % 2)]
                                    p.op('pe', lambda e, pb=pb, Pm=Pm, PT=PT: e.matmul(pb[:, 0:128], lhsT=PT, rhs=Pm, start=True, stop=True), r=[rP, rPT], w=[rpb])
                                    if it < NIT - 1:
                                        p.op('pe', lambda e, pb=pb, Pm=Pm, PT=PT: e.matmul(pb[:, 128:256], lhsT=Pm, rhs=PT, start=True, stop=True), r=[rP, rPT], w=[rpb])
                                    p.op('act', lambda e, pb=pb, PPt=PPt: e.activation(out=PPt[:, :], in_=pb[:, 0:256], func=AF.Copy), r=[rpb], w=[rPP])
                                    pb2, rpb2 = self.next_ps()
                                    TTc, rTTc = U['TT'][tcur], UR['TT%d' % tcur]
                                    TTn, rTTn = U['TT'][1 - tcur], UR['TT%d' % (1 - tcur)]
                                    p.op('pe', lambda e, pb2=pb2, PPt=PPt, TTc=TTc: e.matmul(pb2[:, 0:128], lhsT=PPt[:, 0:128], rhs=TTc[:, :], start=True, stop=True),
                                         r=[rPP, rTTc], w=[rpb2])
                                    p.op('dve', lambda e, pb2=pb2, TTc=TTc, TTn=TTn: e.tensor_tensor(out=TTn[:, :], in0=pb2[:, 0:128], in1=TTc[:, :], op=ALU.add),
                                         r=[rpb2, rTTc], w=[rTTn])
                                    tcur = 1 - tcur
                                    Pm, rP = PPt[:, 0:128], rPP
                                    PT, rPT = PPt[:, 128:256], rPP
                                TTf, rTTf = U['TT'][tcur], UR['TT%d' % tcur]
                                rS = r_S[h][d]
                                p.op('dve', lambda e, U=U, em=em: e.tensor_scalar(out=U['Sbf'][0:64, :], in0=S32[:, h, d, :], scalar1=em, scalar2=None, op0=ALU.mult),
                                     r=[rS, R['sc']], w=[UR['Sbf']])
                                pb, rpb = self.next_ps()
                                p.op('pe', lambda e, pb=pb, T=T, U=U, js=js: e.matmul(pb[:, 0:64], lhsT=T['AT'][:, js], rhs=U['Sbf'][:, :], start=True, stop=False),
                                     r=[R['AT'], UR['Sbf']], w=[rpb])
                                p.op('pe', lambda e, pb=pb, U=U, j=j, hs=hs: e.matmul(pb[:, 0:64], lhsT=U['Am'][:, 256:384], rhs=Vtok[:, j, hs], start=False, stop=True),
                                     r=[UR['Am'], r_V], w=[rpb])
                                p.op('act', lambda e, pb=pb, U=U: e.activation(out=U['Xsb'][:, :], in_=pb[:, 0:64], func=AF.Copy), r=[rpb], w=[UR['Xsb']])
                                pb, rpb = self.next_ps()
                                p.op('pe', lambda e, pb=pb, TTf=TTf, U=U: e.matmul(pb[:, 0:64], lhsT=TTf[:, :], rhs=U['Xsb'][:, :], start=True, stop=True),
                                     r=[rTTf, UR['Xsb']], w=[rpb])
                                p.op('act', lambda e, pb=pb, U=U: e.activation(out=U['Usb'][:, :], in_=pb[:, 0:64], func=AF.Copy), r=[rpb], w=[UR['Usb']])
                                for sweep in range(3):
                                    pb, rpb = self.next_ps()
                                    p.op('pe', lambda e, pb=pb, U=U: e.matmul(pb[:, 0:64], lhsT=U['NTo'][:, :], rhs=U['Usb'][:, :], start=True, stop=True),
                                         r=[UR['NTo'], UR['Usb']], w=[rpb])
                                    p.op('dve', lambda e, pb=pb, U=U: e.tensor_tensor(out=U['acc'][:, :], in0=pb[:, 0:64], in1=U['Xsb'][:, :], op=ALU.add),
                                         r=[rpb, UR['Xsb']], w=[UR['acc']])
                                    pb, rpb = self.next_ps()
                                    p.op('pe', lambda e, pb=pb, TTf=TTf, U=U: e.matmul(pb[:, 0:64], lhsT=TTf[:, :], rhs=U['acc'][:, :], start=True, stop=True),
                                         r=[rTTf, UR['acc']], w=[rpb])
                                    p.op('act', lambda e, pb=pb, U=U: e.activation(out=U['Usb'][:, :], in_=pb[:, 0:64], func=AF.Copy), r=[rpb], w=[UR['Usb']])
                                pb, rpb = self.next_ps()
                                p.op('pe', lambda e, pb=pb, T=T, U=U, js=js: e.matmul(pb[:, 0:64], lhsT=T['RT'][:, js], rhs=U['Sbf'][:, :], start=True, stop=False),
                                     r=[R['RT'], UR['Sbf']], w=[rpb])
                                p.op('pe', lambda e, pb=pb, U=U: e.matmul(pb[:, 0:64], lhsT=U['Am'][:, 128:256], rhs=U['Usb'][:, :], start=False, stop=False),
                                     r=[UR['Am'], UR['Usb']], w=[rpb])
                                p.op('pe', lambda e, pb=pb, U=U, j=j, hs=hs: e.matmul(pb[:, 0:64], lhsT=U['Am'][:, 384:512], rhs=Vtok[:, j, hs], start=False, stop=True),
                                     r=[UR['Am'], r_V], w=[rpb])
                                p.op('act', lambda e, pb=pb, j=j, hs=hs: e.activation(out=ytok[:, j, hs], in_=pb[:, 0:64], func=AF.Copy), r=[rpb], w=[r_y])
                                pb, rpb = self.next_ps()
                                p.op('pe', lambda e, pb=pb, U=U: e.matmul(pb[0:64, 0:64], lhsT=U['BK'][:, 0:64], rhs=U['Usb'][:, :], start=True, stop=False),
                                     r=[UR['BK'], UR['Usb']], w=[rpb])
                                p.op('pe', lambda e, pb=pb, U=U, j=j, hs=hs: e.matmul(pb[0:64, 0:64], lhsT=U['BK'][:, 64:128], rhs=Vtok[:, j, hs], start=False, stop=True),
                                     r=[UR['BK'], r_V], w=[rpb])
                                p.op('dve', lambda e, U=U, e1=e1: e.tensor_scalar(out=U['tmpS'][:, :], in0=S32[:, h, d, :], scalar1=e1, scalar2=None, op0=ALU.mult),
                                     r=[rS, R['sc']], w=[UR['tmpS']])
                                p.op('dve', lambda e, pb=pb, U=U, e2=e2: e.scalar_tensor_tensor(out=S32[:, h, d, :], in0=pb[0:64, 0:64], scalar=e2, in1=U['tmpS'][:, :],
                                                                                               op0=ALU.mult, op1=ALU.add), r=[rpb, UR['tmpS'], R['sc']], w=[rS])
                        p.op('act', lambda e, pbon=pbon: e.activation(out=bon[:, :, :], in_=pbon[:, 0:nj * 16], func=AF.Copy), r=[rpbon], w=[r_bon])
                        if d == 0:
                            p.dma('sp', [(yfd[t0:t0 + n, :].rearrange("(j p) f -> p j f", p=128), ytok[:, :, :]),
                                         (bfd[t0:t0 + n, :].rearrange("(j p) f -> p j f", p=128), bon[:, :, :])], r=[r_y, r_bon], w=[r_yfd, r_bfd])
                        else:
                            for j in range(nj):
                                u = units[(t0 // 128) + j]
                                js = slice(j * 128, (j + 1) * 128)
                                tt = t0 + j * 128
                                p.dma('sp', [(yf[:, :], yfd[tt:tt + 128, :]), (bf_[:, :], bfd[tt:tt + 128, :])], r=[r_yfd, r_bfd], w=[r_yf, r_bf])
                                src = u['src'] if self.first_touch else u['dst']
                                p.dma('sp', [(xt[:, :], src)], r=[u['reg']], w=[r_xt])
                                p.op('dve', lambda e, j=j: e.tensor_tensor(out=yj[:, :], in0=ytok[:, j, :], in1=yf[:, :], op=ALU.add), r=[r_y, r_yf], w=[r_yj])
                                p.op('pool', lambda e, j=j: e.tensor_tensor(out=bf_[:, :], in0=bf_[:, :], in1=bon[:, j, :], op=ALU.add), r=[r_bon], w=[r_bf])
                                yv = yj[:, :].rearrange("p (h k) -> p h k", k=64)
                                sqv = ysq[:, :].rearrange("p (h k) -> p h k", k=64)
                                p.op('act', lambda e: e.activation(out=ysq[:, :], in_=yj[:, :], func=AF.Square), r=[r_yj], w=[r_ysq])
                                p.op('dve', lambda e, yv=yv: e.tensor_reduce(out=st[:, 0, :], in_=yv, axis=AX.X, op=ALU.add), r=[r_yj], w=[r_st])
                                p.op('dve', lambda e, sqv=sqv: e.tensor_reduce(out=st[:, 1, :], in_=sqv, axis=AX.X, op=ALU.add), r=[r_ysq], w=[r_st])
                                p.op('dve', lambda e: e.tensor_scalar(out=st[:, 0, :], in0=st[:, 0, :], scalar1=1.0 / 64, scalar2=None, op0=ALU.mult), r=[r_st], w=[r_st])
                                p.op('dve', lambda e: e.tensor_tensor(out=st[:, 2, :], in0=st[:, 0, :], in1=st[:, 0, :], op=ALU.mult), r=[r_st], w=[r_st])
                                p.op('dve', lambda e: e.scalar_tensor_tensor(out=st[:, 1, :], in0=st[:, 1, :], scalar=1.0 / 64, in1=st[:, 2, :],
                                                                              op0=ALU.mult, op1=ALU.subtract), r=[r_st], w=[r_st])
                                p.op('act', lambda e: e.activation(out=st[:, 1, :], in_=st[:, 1, :], func=AF.Sqrt, bias=64e-5, scale=1.0), r=[r_st], w=[r_st])
                                p.op('dve', lambda e: e.reciprocal(out=st[:, 1, :], in_=st[:, 1, :]), r=[r_st], w=[r_st])
                                mub = st[:, 0, :].unsqueeze(2).to_broadcast([128, 16, 64])
                                rsb = st[:, 1, :].unsqueeze(2).to_broadcast([128, 16, 64])
                                bfb = bf_[:, :].unsqueeze(2).to_broadcast([128, 16, 64])
                                p.op('dve', lambda e, yv=yv, mub=mub: e.tensor_tensor(out=yv, in0=yv, in1=mub, op=ALU.subtract), r=[r_st], w=[r_yj])
                                p.op('dve', lambda e, yv=yv, rsb=rsb: e.tensor_tensor(out=yv, in0=yv, in1=rsb, op=ALU.mult), r=[r_st], w=[r_yj])
                                p.op('dve', lambda e: e.tensor_tensor(out=yj[:, :], in0=yj[:, :], in1=rows[:, 0, :], op=ALU.mult), r=[r_c], w=[r_yj])
                                p.op('dve', lambda e: e.tensor_tensor(out=yj[:, :], in0=yj[:, :], in1=rows[:, 1, :], op=ALU.add), r=[r_c], w=[r_yj])
                                vv = Vtok[:, j, :].rearrange("p (h k) -> p h k", k=64)
                                p.op('dve', lambda e, sqv=sqv, vv=vv, bfb=bfb: e.tensor_tensor(out=sqv, in0=vv, in1=bfb, op=ALU.mult), r=[r_V, r_bf], w=[r_ysq])
                                p.op('dve', lambda e: e.tensor_tensor(out=yj[:, :], in0=yj[:, :], in1=ysq[:, :], op=ALU.add), r=[r_ysq], w=[r_yj])
                                for half in range(2):
                                    pbg, rpbg = self.next_ps()
                                    for cc in range(4):
                                        c = half * 4 + cc
                                        p.op('pe', lambda e, c=c, cc=cc, pbg=pbg, js=js: e.matmul(pbg[:, cc * 128:(cc + 1) * 128], lhsT=G2[:, c * 128:(c + 1) * 128],
                                                                                                   rhs=hg[:, js], start=True, stop=True), r=[r_hg, r_wt], w=[rpbg])
                                    p.op('act', lambda e, half=half, pbg=pbg: e.activation(out=gTs[:, half * 4:(half + 1) * 4, :], in_=pbg[:, :], func=AF.Copy),
                                         r=[rpbg], w=[r_gTs])
                                    pbt, rpbt = self.next_ps()
                                    for cc in range(4):
                                        c = half * 4 + cc
                                        p.op('pe', lambda e, c=c, cc=cc, pbt=pbt: e.transpose(out=pbt[:, cc * 128:(cc + 1) * 128], in_=yj[:, c * 128:(c + 1) * 128],
                                                                                              identity=self.identF[:, :]), r=[r_yj, self.r_const], w=[rpbt])
                                    p.op('dve', lambda e, half=half, pbt=pbt: e.tensor_tensor(out=ygT[:, half * 4:(half + 1) * 4, :], in0=pbt[:, :],
                                                                                              in1=gTs[:, half * 4:(half + 1) * 4, :], op=ALU.mult),
                                         r=[rpbt, r_gTs], w=[r_ygT])
                                for hf in range(2):
                                    pbo, rpbo = self.next_ps()
                                    for c in range(8):
                                        p.op('pe', lambda e, c=c, hf=hf, pbo=pbo: e.matmul(pbo[:, :], lhsT=ygT[:, c, :], rhs=Wo[:, c, hf * 512:(hf + 1) * 512],
                                                                                           start=(c == 0), stop=(c == 7)), r=[r_ygT, r_wt], w=[rpbo])
                                    p.op('dve', lambda e, hf=hf, pbo=pbo: e.tensor_tensor(out=osb[:, hf * 512:(hf + 1) * 512], in0=pbo[:, :],
                                                                                          in1=gbc[:, hf * 512:(hf + 1) * 512], op=ALU.mult), r=[rpbo, r_gbc], w=[r_osb])
                                p.op('dve', lambda e: e.tensor_tensor(out=osb[:, :], in0=osb[:, :], in1=xt[:, :], op=ALU.add), r=[r_xt], w=[r_osb])
                                p.dma('sp', [(u['dst'], osb[:, :])], r=[r_osb], w=[u['reg']])
                if sq_['sout'] is not None:
                    for h in range(16):
                        for d in range(2):
                            pb, rpb = self.next_ps()
                            p.op('pe', lambda e, pb=pb, h=h, d=d: e.transpose(out=pb[0:64, 0:64], in_=S32[:, h, d, :], identity=self.identF[0:64, 0:64]),
                                 r=[r_S[h][d], self.r_const], w=[rpb])
                            p.op('act', lambda e, pb=pb: e.activation(out=stio[:, :], in_=pb[0:64, 0:64], func=AF.Copy), r=[rpb], w=[r_stio])
                            p.dma('sp', [(sq_['sout'][d, h], stio[:, :])], r=[r_stio])
            p.barrier()
        self.ps_lim = 8

    def att_setup(self):
        c = self.cfg
        din, dout, dint = self._din, self._dout, self._dint
        self.at_rows = din('at_rows', [128, 20, 64])
        self.at_sink = din('at_sink', [64, 16])
        self.at_cos = din('at_cos', [c.LS, 64])
        self.at_sin = din('at_sin', [c.LS, 64])
        self.at_mask = din('at_mask', [128, 2, 128])
        self.ck = din('ck', [c.PAST, 256])
        self.cv = din('cv', [c.PAST, 256])
        self.o_k = dout('o_k', [self.TP, 256])
        self.o_v = dout('o_v', [self.TP, 256])
        seqs = self.make_seqs()
        for sq in seqs:
            L = sq['L']
            sq['qTd'] = dint('at_qTd%d' % sq['idx'], [64, 16, L], BF16)
            sq['kTd'] = dint('at_kTd%d' % sq['idx'], [64, 4, L], BF16)
            sq['vd'] = dint('at_vd%d' % sq['idx'], [L, 256], BF16)
        return seqs

    def att_phase(self, l, seqs):
        p, nc, W = self.p, self.nc, self.W
        c = self.cfg
        NCB = c.PAST // 128
        self.ps_lim = 6
        with ExitStack() as s:
            def sb(name, shape, dt):
                return p.sbuf(s, 'at_' + name, shape, dt)
            r_wt = Reg()
            Wqkv = sb('Wqkv', [128, 8, 1536], BF16)
            WoH = sb('WoH', [64, 16, 1024], BF16)
            p.dma('pool', [(Wqkv[:, :, :], W['att_w_qkv'].rearrange("(c p) n -> p c n", p=128)),
                           (WoH[:, :, :], W['att_w_o'].rearrange("(h p) n -> p h n", p=64))], w=[r_wt])
            rowsN = sb('rowsN', [128, 20, 64], F32)
            sinkE = sb('sinkE', [64, 16], F32)
            bmask = sb('bmask', [128, 2, 128], F32)
            r_c = Reg()
            p.dma('sp', [(rowsN[:, :, :], self.at_rows), (sinkE[:, :], self.at_sink), (bmask[:, :, :], self.at_mask)], w=[r_c])
            p.op('act', lambda e: e.activation(out=sinkE[:, :], in_=sinkE[:, :], func=AF.Exp), r=[r_c], w=[r_c])
            ones128 = sb('ones128', [128, 64], BF16)
            p.op('dve', lambda e: e.memset(ones128[:, :], 1.0), w=[r_c])
            ckT = sb('ckT', [64, 4, c.PAST], BF16); r_ckT = Reg()
            cvt = sb('cvt', [128, NCB, 256], BF16); r_cvt = Reg()
            ckt = sb('ckt', [128, NCB, 256], F32); r_ckt = Reg()
            p.dma('sp', [(ckt[:, :, :], self.ck.rearrange("(b p) f -> p b f", p=128))], w=[r_ckt])
            p.dma('pool', [(cvt[:, :, :], self.cv.rearrange("(b p) f -> p b f", p=128))], w=[r_cvt])
            for b in range(NCB):
                pb, rpb = self.next_ps()
                for g in range(4):
                    p.op('pe', lambda e, pb=pb, b=b, g=g: e.transpose(out=pb[0:64, g * 128:(g + 1) * 128], in_=ckt[:, b, g * 64:(g + 1) * 64],
                                                                      identity=self.identF[:, :]), r=[r_ckt, self.r_const], w=[rpb])
                p.op('act', lambda e, pb=pb, b=b: e.activation(out=ckT[:, :, b * 128:(b + 1) * 128],
                                                               in_=pb[0:64, :].rearrange("p (g t) -> p g t", g=4), func=AF.Copy), r=[rpb], w=[r_ckT])
            gbc = sb('gbc', [128, 1024], F32); r_gbc = Reg()
            diag = sb('diag', [128, 128], F32); r_diag = Reg()
            xt = sb('xt', [128, 1024], F32); r_xt = Reg()
            ss = sb('ss', [128, 2], F32); xn = sb('xn', [128, 1024], F32); junk = sb('junk', [128, 1024], BF16)
            scr = (ss, xn, junk, Reg(), Reg(), Reg())
            hts = sb('hts', [128, 8, 128], BF16); r_hts = Reg()
            qkv = sb('qkv', [128, 1536], F32); r_qkv = Reg()
            sq2 = sb('sq2', [128, 1280], F32); r_sq2 = Reg()
            rs = sb('rs', [128, 20], F32); r_rs = Reg()
            rot = sb('rot', [128, 1280], F32); r_rot = Reg()
            cs = sb('cs', [128, 2, 64], F32); r_cs = Reg()
            qTs = sb('qTs', [64, 20, 128], BF16); r_qTs = Reg()
            vbf = sb('vbf', [128, 256], BF16); r_vbf = Reg()
            qTb = sb('qTb', [64, 16, 128], BF16); r_qTb = Reg()
            kTb = sb('kTb', [64, 4, 384], BF16); r_kTb = Reg()
            vb = sb('vb', [128, 3, 256], BF16); r_vb = Reg()
            Et = [sb('E%d' % i, [128, 512], BF16) for i in range(3)]; r_E = [Reg() for _ in range(3)]
            OT = sb('OT', [64, 16, 128], BF16); r_OT = Reg()
            den = sb('den', [64, 512], F32); r_den = Reg()
            osb = sb('osb', [128, 1024], F32); r_osb = Reg()

            for sq_ in seqs:
                L, g_, units = sq_['L'], sq_['g'], sq_['units']
                qTd, kTd, vd = sq_['qTd'], sq_['kTd'], sq_['vd']
                r_sd = Reg()
                latent = (g_ == 0)
                self.gate_bc(gbc, r_gbc, l, 2, g_, diag, r_diag)
                nb = L // 128
                for i, u in enumerate(units):
                    p.dma('sp', [(xt[:, :], u['src'] if self.first_touch else u['dst'])], r=[u['reg']], w=[r_xt])
                    self.norm_hT(xt[:, :], r_xt, l, 0, g_, lambda cc: hts[:, cc, :], r_hts, scr)
                    for part in range(3):
                        pb, rpb = self.next_ps()
                        for cc in range(8):
                            p.op('pe', lambda e, cc=cc, pb=pb, part=part: e.matmul(pb[:, :], lhsT=hts[:, cc, :], rhs=Wqkv[:, cc, part * 512:(part + 1) * 512],
                                                                                   start=(cc == 0), stop=(cc == 7)), r=[r_hts, r_wt], w=[rpb])
                        p.op('act', lambda e, pb=pb, part=part: e.activation(out=qkv[:, part * 512:(part + 1) * 512], in_=pb[:, :], func=AF.Copy), r=[rpb], w=[r_qkv])
                    qk3 = qkv[:, 0:1280].rearrange("p (h k) -> p h k", k=64)
                    sq3 = sq2[:, :].rearrange("p (h k) -> p h k", k=64)
                    p.op('act', lambda e: e.activation(out=sq2[:, :], in_=qkv[:, 0:1280], func=AF.Square), r=[r_qkv], w=[r_sq2])
                    p.op('dve', lambda e, sq3=sq3: e.tensor_reduce(out=rs[:, :], in_=sq3, axis=AX.X, op=ALU.add), r=[r_sq2], w=[r_rs])
                    p.op('act', lambda e: e.activation(out=rs[:, :], in_=rs[:, :], func=AF.Sqrt, scale=1.0 / 64, bias=EPS), r=[r_rs], w=[r_rs])
                    p.op('dve', lambda e: e.reciprocal(out=rs[:, :], in_=rs[:, :]), r=[r_rs], w=[r_rs])
                    rsb = rs[:, :].unsqueeze(2).to_broadcast([128, 20, 64])
                    p.op('dve', lambda e, qk3=qk3, rsb=rsb: e.tensor_tensor(out=qk3, in0=qk3, in1=rsb, op=ALU.mult), r=[r_rs], w=[r_qkv])
                    p.op('dve', lambda e, qk3=qk3: e.tensor_tensor(out=qk3, in0=qk3, in1=rowsN[:, :, :], op=ALU.mult), r=[r_c], w=[r_qkv])
                    if latent:
                        t0 = i * 128
                        p.dma('sp', [(cs[:, 0, :], self.at_cos[t0:t0 + 128, :]), (cs[:, 1, :], self.at_sin[t0:t0 + 128, :])], w=[r_cs])
                        rot3 = rot[:, :].rearrange("p (h k) -> p h k", k=64)
                        cosb = cs[:, 0, :].unsqueeze(1).to_broadcast([128, 20, 64])
                        for (lo, hi, sgn) in ((0, 32, -1.0), (32, 64, 1.0)):
                            olo, ohi = (32, 64) if lo == 0 else (0, 32)
                            sinb = cs[:, 1, lo:hi].unsqueeze(1).to_broadcast([128, 20, 32])
                            p.op('dve', lambda e, rot3=rot3, qk3=qk3, sinb=sinb, lo=lo, hi=hi, olo=olo, ohi=ohi, sgn=sgn: e.scalar_tensor_tensor(
                                out=rot3[:, :, lo:hi], in0=qk3[:, :, olo:ohi], scalar=sgn, in1=sinb, op0=ALU.mult, op1=ALU.mult), r=[r_qkv, r_cs], w=[r_rot])
                        p.op('dve', lambda e, qk3=qk3, cosb=cosb: e.tensor_tensor(out=qk3, in0=qk3, in1=cosb, op=ALU.mult), r=[r_cs, r_rot], w=[r_qkv])
                        p.op('dve', lambda e: e.tensor_tensor(out=qkv[:, 0:1280], in0=qkv[:, 0:1280], in1=rot[:, :], op=ALU.add), r=[r_rot], w=[r_qkv])
                    else:
                        pi = sq_['idx'] - 1
                        r0 = pi * L + i * 128
                        p.dma('sp', [(self.o_k[r0:r0 + 128, :], qkv[:, 1024:1280]), (self.o_v[r0:r0 + 128, :], qkv[:, 1280:1536])], r=[r_qkv])
                    for hh in range(5):
                        pb, rpb = self.next_ps()
                        for q4 in range(4):
                            hd = hh * 4 + q4
                            p.op('pe', lambda e, pb=pb, q4=q4, hd=hd: e.transpose(out=pb[0:64, q4 * 128:(q4 + 1) * 128], in_=qkv[:, hd * 64:(hd + 1) * 64],
                                                                                  identity=self.identF[:, :]), r=[r_qkv, self.r_const], w=[rpb])
                        p.op('act', lambda e, pb=pb, hh=hh: e.activation(out=qTs[:, hh * 4:(hh + 1) * 4, :], in_=pb[0:64, :].rearrange("p (g t) -> p g t", g=4),
                                                                         func=AF.Copy), r=[rpb], w=[r_qTs])
                    p.op('act', lambda e: e.activation(out=vbf[:, :], in_=qkv[:, 1280:1536], func=AF.Copy), r=[r_qkv], w=[r_vbf])
                    ts = slice(i * 128, (i + 1) * 128)
                    p.dma('sp', [(qTd[:, :, ts], qTs[:, 0:16, :]), (kTd[:, :, ts], qTs[:, 16:20, :]), (vd[ts, :], vbf[:, :])], r=[r_qTs, r_vbf], w=[r_sd])
                for i, u in enumerate(units):
                    ts = slice(i * 128, (i + 1) * 128)
                    if latent:
                        kblocks = [bb for bb in (i - 1, i, i + 1) if 0 <= bb < nb]
                    else:
                        kblocks = list(range(nb))
                    k0 = kblocks[0]
                    nkb = len(kblocks)
                    pr = [(qTb[:, :, :], qTd[:, :, ts]), (kTb[:, :, 0:nkb * 128], kTd[:, :, k0 * 128:(k0 + nkb) * 128]),
                          (vb[:, 0:nkb, :], vd[k0 * 128:(k0 + nkb) * 128, :].rearrange("(b p) f -> p b f", p=128))]
                    p.dma('sp', pr, r=[r_sd], w=[r_qTb, r_kTb, r_vb])
                    p.dma('sp', [(xt[:, :], u['src'] if self.first_touch else u['dst'])], r=[u['reg']], w=[r_xt])
                    ei = 0
                    for g in range(4):
                        po, rpo = self.ps[6], self.r_ps[6]
                        pd, rpd = self.ps[7], self.r_ps[7]
                        klist = []
                        if latent:
                            for b in range(NCB):
                                klist.append((ckT[:, g, b * 128:(b + 1) * 128], cvt[:, b, g * 64:(g + 1) * 64], None, [r_ckT, r_cvt]))
                        for bi, bb in enumerate(kblocks):
                            mk = None
                            if latent and bb == i - 1:
                                mk = 0
                            if latent and bb == i + 1:
                                mk = 1
                            klist.append((kTb[:, g, bi * 128:(bi + 1) * 128], vb[:, bi, g * 64:(g + 1) * 64], mk, [r_kTb, r_vb]))
                        qrhs = qTb[:, g * 4:(g + 1) * 4, :]
                        for ki, (kap, vap, mk, rk) in enumerate(klist):
                            psc, rpsc = self.next_ps()
                            p.op('pe', lambda e, psc=psc, kap=kap, qrhs=qrhs: e.matmul(psc[:, :], lhsT=kap, rhs=qrhs, start=True, stop=True), r=rk + [r_qTb], w=[rpsc])
                            E, rE = Et[ei % 3], r_E[ei % 3]
                            ei += 1
                            p.op('act', lambda e, psc=psc, E=E: e.activation(out=E[:, :], in_=psc[:, :], func=AF.Exp, scale=0.125), r=[rpsc], w=[rE])
                            if mk is not None:
                                E3 = E[:, :].rearrange("p (r t) -> p r t", r=4)
                                mb = bmask[:, mk, :].unsqueeze(1).to_broadcast([128, 4, 128])
                                p.op('dve', lambda e, E3=E3, mb=mb: e.tensor_tensor(out=E3, in0=E3, in1=mb, op=ALU.mult), r=[r_c], w=[rE])
                            p.op('pe', lambda e, po=po, vap=vap, E=E, ki=ki: e.matmul(po[0:64, :], lhsT=vap, rhs=E[:, :], start=(ki == 0), stop=(ki == len(klist) - 1)),
                                 r=rk + [rE], w=[rpo])
                            p.op('pe', lambda e, pd=pd, E=E, ki=ki: e.matmul(pd[0:64, :], lhsT=ones128[:, :], rhs=E[:, :], start=(ki == 0), stop=(ki == len(klist) - 1)),
                                 r=[rE, r_c], w=[rpd])
                        for rr in range(4):
                            hd = g * 4 + rr
                            p.op('dve', lambda e, pd=pd, rr=rr, hd=hd: e.tensor_scalar(out=den[:, rr * 128:(rr + 1) * 128], in0=pd[0:64, rr * 128:(rr + 1) * 128],
                                                                                        scalar1=sinkE[:, hd:hd + 1], scalar2=None, op0=ALU.add), r=[rpd, r_c], w=[r_den])
                        p.op('dve', lambda e: e.reciprocal(out=den[:, :], in_=den[:, :]), r=[r_den], w=[r_den])
                        p.op('dve', lambda e, po=po, g=g: e.tensor_tensor(out=OT[:, g * 4:(g + 1) * 4, :], in0=po[0:64, :].rearrange("p (r t) -> p r t", r=4),
                                                                          in1=den[:, :].rearrange("p (r t) -> p r t", r=4), op=ALU.mult), r=[rpo, r_den], w=[r_OT])
                    for hf in range(2):
                        pbo, rpbo = self.next_ps()
                        for hd in range(16):
                            p.op('pe', lambda e, hd=hd, hf=hf, pbo=pbo: e.matmul(pbo[:, :], lhsT=OT[:, hd, :], rhs=WoH[:, hd, hf * 512:(hf + 1) * 512],
                                                                                 start=(hd == 0), stop=(hd == 15)), r=[r_OT, r_wt], w=[rpbo])
                        p.op('dve', lambda e, hf=hf, pbo=pbo: e.tensor_tensor(out=osb[:, hf * 512:(hf + 1) * 512], in0=pbo[:, :],
                                                                              in1=gbc[:, hf * 512:(hf + 1) * 512], op=ALU.mult), r=[rpbo, r_gbc], w=[r_osb])
                    p.op('dve', lambda e: e.tensor_tensor(out=osb[:, :], in0=osb[:, :], in1=xt[:, :], op=ALU.add), r=[r_xt], w=[r_osb])
                    p.dma('sp', [(u['dst'], osb[:, :])], r=[r_osb], w=[u['reg']])
            p.barrier()
        self.ps_lim = 8

    def ret_setup(self):
        c = self.cfg
        din, dout, dint = self._din, self._dout, self._dint
        self.rt_dmask = din('rt_dmask', [128, 4, 2, 128])
        self.rt_qdec = din('rt_qdec', [128, 4, 2, 128])
        self.rt_kdec = din('rt_kdec', [128, 4, 2])
        self.rt_cos = din('rt_cos', [c.LS, 256])
        self.rt_sin = din('rt_sin', [c.LS, 256])
        self.st_ret = din('st_ret', [2, 4, 256, 512])
        self.o_ret = dout('o_ret', [c.NP, 2, 4, 256, 512])
        seqs = self.make_seqs()
        ntok = len(self.units) * 128
        self.rt_proj = dint('rt_proj', [ntok, 8192])
        self.rt_yf = dint('rt_yf', [ntok, 2048])
        for sq in seqs:
            if sq['g'] == 0:
                sq['s0'], sq['sout'] = self.st_ret, None
            else:
                sq['s0'], sq['sout'] = None, self.o_ret[sq['idx'] - 1]
        return seqs

    def ret_phase(self, l, seqs):
        p, nc, W = self.p, self.nc, self.W
        c = self.cfg
        nu = len(self.units)
        proj, yfd = self.rt_proj, self.rt_yf
        r_proj = [Reg() for _ in range(nu)]
        with ExitStack() as s:
            def sb(name, shape, dt):
                return p.sbuf(s, 'ra_' + name, shape, dt)
            hT = sb('hT', [128, 8, nu * 128], BF16)
            r_hT = [Reg() for _ in range(nu)]
            xt = [sb('xt%d' % i, [128, 1024], F32) for i in range(2)]; r_xt = [Reg(), Reg()]
            ss = sb('ss', [128, 2], F32); xn = sb('xn', [128, 1024], F32); junk = sb('junk', [128, 1024], BF16)
            scr = (ss, xn, junk, Reg(), Reg(), Reg())
            wp = [sb('wp%d' % i, [128, 8, 512], BF16) for i in range(2)]; r_wp = [Reg(), Reg()]
            stg = [sb('stg%d' % i, [128, 512], F32) for i in range(3)]; r_stg = [Reg() for _ in range(3)]
            for ui, u in enumerate(self.units):
                b = ui % 2
                p.dma('sp', [(xt[b][:, :], u['src'] if self.first_touch else u['dst'])], r=[u['reg']], w=[r_xt[b]])
                self.norm_hT(xt[b][:, :], r_xt[b], l, 0, u['g'], lambda cc, ui=ui: hT[:, cc, ui * 128:(ui + 1) * 128], r_hT[ui], scr)
            k = 0
            for cg in range(16):
                b = cg % 2
                p.dma('pool', [(wp[b][:, :, :], W['ret_w_in'][:, cg * 512:(cg + 1) * 512].rearrange("(c p) n -> p c n", p=128))], w=[r_wp[b]])
                for ui in range(nu):
                    pb, rpb = self.next_ps()
                    for cc in range(8):
                        p.op('pe', lambda e, cc=cc, pb=pb, ui=ui, b=b: e.matmul(pb[:, :], lhsT=hT[:, cc, ui * 128:(ui + 1) * 128], rhs=wp[b][:, cc, :],
                                                                                start=(cc == 0), stop=(cc == 7)), r=[r_hT[ui], r_wp[b]], w=[rpb])
                    sbi = k % 3
                    k += 1
                    p.op('act' if k % 2 else 'dve', (lambda e, pb=pb, sbi=sbi: e.activation(out=stg[sbi][:, :], in_=pb[:, :], func=AF.Copy)) if k % 2 else
                         (lambda e, pb=pb, sbi=sbi: e.tensor_copy(out=stg[sbi][:, :], in_=pb[:, :])), r=[rpb], w=[r_stg[sbi]])
                    p.dma('sp', [(proj[ui * 128:(ui + 1) * 128, cg * 512:(cg + 1) * 512], stg[sbi][:, :])], r=[r_stg[sbi]], w=[r_proj[ui]])
            p.barrier()
        lgf = [float(np.log1p(-2.0 ** (-5.0 - h))) for h in range(4)]
        lgb = [float(np.log1p(-2.0 ** (-5.5 - h))) for h in range(4)]
        cdec = [[float(np.exp(lgf[h] * 128)), float(np.exp(lgb[h] * 128))] for h in range(4)]
        self.ps_lim = 6
        with ExitStack() as s:
            def sb(name, shape, dt):
                return p.sbuf(s, 'rb_' + name, shape, dt)
            r_wt = Reg()
            Wout = sb('Wout', [128, 16, 1024], BF16)
            p.dma('pool', [(Wout[:, :, :], W['ret_w_out'].rearrange("(c p) n -> p c n", p=128))], w=[r_wt])
            dmask = sb('dmask', [128, 4, 2, 128], F32)
            qdec = sb('qdec', [128, 4, 2, 128], F32)
            kdec = sb('kdec', [128, 4, 2], F32)
            r_c = Reg()
            p.dma('sp', [(dmask[:, :, :, :], self.rt_dmask), (qdec[:, :, :, :], self.rt_qdec), (kdec[:, :, :], self.rt_kdec)], w=[r_c])
            S32 = sb('S32', [128, 4, 2, 2, 512], F32)
            Sbf = sb('Sbf', [128, 4, 2, 2, 512], BF16)
            r_S = [[Reg() for _ in range(2)] for _ in range(4)]
            r_Sb = [[Reg() for _ in range(2)] for _ in range(4)]
            gbc = sb('gbc', [128, 1024], F32); r_gbc = Reg()
            diag = sb('diag', [128, 128], F32); r_diag = Reg()
            qk = sb('qk', [128, 2048], F32); r_qk = Reg()
            rot = sb('rot', [128, 2048], F32); r_rot = Reg()
            cs = sb('cs', [128, 2, 256], F32); r_cs = Reg()
            qT = sb('qT', [128, 8, 128], BF16); kT = sb('kT', [128, 8, 128], BF16); qdT = sb('qdT', [128, 2, 128], BF16)
            r_qT, r_kT, r_qdT = Reg(), Reg(), Reg()
            Kd = sb('Kd', [128, 1024], BF16); r_Kd = Reg()
            Vb = sb('Vb', [128, 2048], BF16); r_Vb = Reg()
            gg = sb('gg', [128, 2048], F32); r_gg = Reg()
            term = sb('term', [128, 2048], F32); r_term = Reg()
            yfw = sb('yfw', [128, 2048], F32); r_yfw = Reg()
            scT = sb('scT', [128, 128], BF16); r_scT = Reg()
            st = sb('st', [128, 4], F32); r_st = Reg()
            junk2 = sb('junk2', [128, 512], BF16); r_junk2 = Reg()
            yT = sb('yT', [128, 16, 128], BF16); r_yT = Reg()
            xt2 = sb('xt2', [128, 1024], F32); r_xt2 = Reg()
            osb = sb('osb', [128, 1024], F32); r_osb = Reg()
            ubase = 0
            for sq_ in seqs:
                L, g_, units = sq_['L'], sq_['g'], sq_['units']
                latent = (g_ == 0)
                nchunk = L // 128
                self.gate_bc(gbc, r_gbc, l, 2, g_, diag, r_diag)
                allS = [r_S[h][d] for h in range(4) for d in range(2)]
                if sq_['s0'] is None:
                    p.op('dve', lambda e: e.memset(S32[:, :, :, :, :], 0.0), w=allS)
                else:
                    for rdir in range(2):
                        for h in range(4):
                            p.dma('sp', [(S32[:, h, rdir, :, :], sq_['s0'][rdir, h].rearrange("(c p) e -> p c e", p=128))], w=[r_S[h][rdir]])
                for h in range(4):
                    for d in range(2):
                        p.op('act', lambda e, h=h, d=d: e.activation(out=Sbf[:, h, d, :, :], in_=S32[:, h, d, :, :], func=AF.Copy), r=[r_S[h][d]], w=[r_Sb[h][d]])
                for d in range(2):
                    chunks = list(range(nchunk)) if d == 0 else list(range(nchunk - 1, -1, -1))
                    for ci in chunks:
                        ui = ubase + ci
                        u = units[ci]
                        rows = slice(ui * 128, (ui + 1) * 128)
                        p.dma('sp', [(qk[:, :], proj[rows, 0:2048])], r=[r_proj[ui]], w=[r_qk])
                        p.dma('pool', [(Vb[:, :], proj[rows, 2048:4096])], r=[r_proj[ui]], w=[r_Vb])
                        gc0 = 4096 + d * 2048
                        p.dma('sp', [(gg[:, :], proj[rows, gc0:gc0 + 2048])], r=[r_proj[ui]], w=[r_gg])
                        p.op('act', lambda e: e.activation(out=gg[:, :], in_=gg[:, :], func=AF.Silu), r=[r_gg], w=[r_gg])
                        p.op('dve', lambda e: e.tensor_scalar(out=qk[:, 1024:2048], in0=qk[:, 1024:2048], scalar1=1.0 / 16.0, scalar2=None, op0=ALU.mult), r=[r_qk], w=[r_qk])
                        if latent:
                            t0 = ci * 128
                            p.dma('sp', [(cs[:, 0, :], self.rt_cos[t0:t0 + 128, :]), (cs[:, 1, :], self.rt_sin[t0:t0 + 128, :])], w=[r_cs])
                            x4 = qk[:, :].rearrange("p (h i two) -> p h i two", h=8, two=2)
                            r4 = rot[:, :].rearrange("p (h i two) -> p h i two", h=8, two=2)
                            cos4 = cs[:, 0, :].rearrange("p (i two) -> p i two", two=2)
                            sin4 = cs[:, 1, :].rearrange("p (i two) -> p i two", two=2)
                            for (o_, i_, sgn) in ((0, 1, -1.0), (1, 0, 1.0)):
                                sinb = sin4[:, :, o_].unsqueeze(1).to_broadcast([128, 8, 128])
                                p.op('dve', lambda e, r4=r4, x4=x4, sinb=sinb, o_=o_, i_=i_, sgn=sgn: e.scalar_tensor_tensor(
                                    out=r4[:, :, :, o_], in0=x4[:, :, :, i_], scalar=sgn, in1=sinb, op0=ALU.mult, op1=ALU.mult), r=[r_qk, r_cs], w=[r_rot])
                            cosb = cs[:, 0, :].unsqueeze(1).to_broadcast([128, 8, 256])
                            x3 = qk[:, :].rearrange("p (h k) -> p h k", h=8)
                            p.op('dve', lambda e, x3=x3, cosb=cosb: e.tensor_tensor(out=x3, in0=x3, in1=cosb, op=ALU.mult), r=[r_cs, r_rot], w=[r_qk])
                            p.op('dve', lambda e: e.tensor_tensor(out=qk[:, :], in0=qk[:, :], in1=rot[:, :], op=ALU.add), r=[r_rot], w=[r_qk])
                        for which, dstT, rdst in ((0, qT, r_qT), (1, kT, r_kT)):
                            for half in range(2):
                                pb, rpb = self.next_ps()
                                for q4 in range(4):
                                    cc = half * 4 + q4
                                    col = which * 1024 + cc * 128
                                    p.op('pe', lambda e, pb=pb, q4=q4, col=col: e.transpose(out=pb[:, q4 * 128:(q4 + 1) * 128], in_=qk[:, col:col + 128],
                                                                                           identity=self.identF[:, :]), r=[r_qk, self.r_const], w=[rpb])
                                p.op('act', lambda e, pb=pb, half=half, dstT=dstT: e.activation(out=dstT[:, half * 4:(half + 1) * 4, :],
                                                                                              in_=pb[:, :].rearrange("p (g t) -> p g t", g=4), func=AF.Copy), r=[rpb], w=[rdst])
                        for h in range(4):
                            p.op('dve', lambda e, h=h: e.tensor_scalar(out=Kd[:, h * 256:(h + 1) * 256], in0=qk[:, 1024 + h * 256:1024 + (h + 1) * 256],
                                                                        scalar1=kdec[:, h, d:d + 1], scalar2=None, op0=ALU.mult), r=[r_qk, r_c], w=[r_Kd])
                        for h in range(4):
                            hs = slice(h * 512, (h + 1) * 512)
                            psc, rpsc = self.next_ps()
                            for dc in range(2):
                                p.op('pe', lambda e, psc=psc, h=h, dc=dc: e.matmul(psc[:, 0:128], lhsT=kT[:, h * 2 + dc, :], rhs=qT[:, h * 2 + dc, :],
                                                                                  start=(dc == 0), stop=(dc == 1)), r=[r_kT, r_qT], w=[rpsc])
                            p.op('dve', lambda e, psc=psc, h=h: e.tensor_tensor(out=scT[:, :], in0=psc[:, 0:128], in1=dmask[:, h, d, :], op=ALU.mult),
                                 r=[rpsc, r_c], w=[r_scT])
                            for dc in range(2):
                                p.op('pool', lambda e, h=h, dc=dc: e.tensor_tensor(out=qdT[:, dc, :], in0=qT[:, h * 2 + dc, :], in1=qdec[:, h, d, :], op=ALU.mult),
                                     r=[r_qT, r_c], w=[r_qdT])
                            po, rpo = self.ps[6], self.r_ps[6]
                            p.op('pe', lambda e, po=po, hs=hs: e.matmul(po[:, :], lhsT=scT[:, :], rhs=Vb[:, hs], start=True, stop=False), r=[r_scT, r_Vb], w=[rpo])
                            for dc in range(2):
                                p.op('pe', lambda e, po=po, h=h, dc=dc: e.matmul(po[:, :], lhsT=qdT[:, dc, :], rhs=Sbf[:, h, d, dc, :], start=False, stop=(dc == 1)),
                                     r=[r_qdT, r_Sb[h][d]], w=[rpo])
                            p.op('act', lambda e, po=po, h=h: e.activation(out=junk2[:, :], in_=po[:, :], func=AF.Square, accum_out=st[:, h:h + 1]),
                                 r=[rpo], w=[r_junk2, r_st])
                            p.op('act', lambda e, h=h: e.activation(out=st[:, h:h + 1], in_=st[:, h:h + 1], func=AF.Sqrt, scale=1.0 / 512, bias=EPS), r=[r_st], w=[r_st])
                            p.op('dve', lambda e, h=h: e.reciprocal(out=st[:, h:h + 1], in_=st[:, h:h + 1]), r=[r_st], w=[r_st])
                            p.op('dve', lambda e, po=po, h=h, hs=hs: e.scalar_tensor_tensor(out=term[:, hs], in0=po[:, :], scalar=st[:, h:h + 1], in1=gg[:, hs],
                                                                                           op0=ALU.mult, op1=ALU.mult), r=[rpo, r_st, r_gg], w=[r_term])
                            for dc in range(2):
                                pss, rpss = self.ps[7], self.r_ps[7]
                                p.op('pe', lambda e, pss=pss, h=h, dc=dc, hs=hs: e.matmul(pss[:, :], lhsT=Kd[:, h * 256 + dc * 128:h * 256 + (dc + 1) * 128], rhs=Vb[:, hs],
                                                                                          start=True, stop=True), r=[r_Kd, r_Vb], w=[rpss])
                                p.op('dve', lambda e, pss=pss, h=h, dc=dc: e.scalar_tensor_tensor(out=S32[:, h, d, dc, :], in0=S32[:, h, d, dc, :], scalar=cdec[h][d],
                                                                                                 in1=pss[:, :], op0=ALU.mult, op1=ALU.add), r=[rpss, r_Sb[h][d]], w=[r_S[h][d]])
                                p.op('act', lambda e, h=h, dc=dc: e.activation(out=Sbf[:, h, d, dc, :], in_=S32[:, h, d, dc, :], func=AF.Copy), r=[r_S[h][d]], w=[r_Sb[h][d]])
                        if d == 0:
                            p.dma('sp', [(yfd[rows, :], term[:, :])], r=[r_term], w=[r_proj[ui]])
                        else:
                            p.dma('sp', [(yfw[:, :], yfd[rows, :])], r=[r_proj[ui]], w=[r_yfw])
                            p.dma('sp', [(xt2[:, :], u['src'] if self.first_touch else u['dst'])], r=[u['reg']], w=[r_xt2])
                            p.op('dve', lambda e: e.tensor_tensor(out=term[:, :], in0=term[:, :], in1=yfw[:, :], op=ALU.add), r=[r_yfw], w=[r_term])
                            for qd in range(4):
                                pb, rpb = self.next_ps()
                                for q4 in range(4):
                                    cc = qd * 4 + q4
                                    p.op('pe', lambda e, pb=pb, q4=q4, cc=cc: e.transpose(out=pb[:, q4 * 128:(q4 + 1) * 128], in_=term[:, cc * 128:(cc + 1) * 128],
                                                                                          identity=self.identF[:, :]), r=[r_term, self.r_const], w=[rpb])
                                p.op('act', lambda e, pb=pb, qd=qd: e.activation(out=yT[:, qd * 4:(qd + 1) * 4, :], in_=pb[:, :].rearrange("p (g t) -> p g t", g=4),
                                                                               func=AF.Copy), r=[rpb], w=[r_yT])
                            for hf in range(2):
                                pbo, rpbo = self.next_ps()
                                for cc in range(16):
                                    p.op('pe', lambda e, cc=cc, hf=hf, pbo=pbo: e.matmul(pbo[:, :], lhsT=yT[:, cc, :], rhs=Wout[:, cc, hf * 512:(hf + 1) * 512],
                                                                                         start=(cc == 0), stop=(cc == 15)), r=[r_yT, r_wt], w=[rpbo])
                                p.op('dve', lambda e, hf=hf, pbo=pbo: e.tensor_tensor(out=osb[:, hf * 512:(hf + 1) * 512], in0=pbo[:, :],
                                                                                      in1=gbc[:, hf * 512:(hf + 1) * 512], op=ALU.mult), r=[rpbo, r_gbc], w=[r_osb])
                            p.op('dve', lambda e: e.tensor_tensor(out=osb[:, :], in0=osb[:, :], in1=xt2[:, :], op=ALU.add), r=[r_xt2], w=[r_osb])
                            p.dma('sp', [(u['dst'], osb[:, :])], r=[r_osb], w=[u['reg']])
                if sq_['sout'] is not None:
                    for rdir in range(2):
                        for h in range(4):
                            p.dma('sp', [(sq_['sout'][rdir, h].rearrange("(c p) e -> p c e", p=128), S32[:, h, rdir, :, :])], r=[r_S[h][rdir]])
                ubase += nchunk
            p.barrier()
        self.ps_lim = 8

    def hy_setup(self):
        c = self.cfg
        din, dout, dint = self._din, self._dout, self._dint
        self.hy_fm = din('hy_fm', [128, 6, 24])
        self.hy_rows = din('hy_rows', [128, 2, 1024])
        self.hy_fsm = din('hy_fsm', [64, 4])
        seqs = self.make_seqs()
        self.hy_L = {}
        for sq in seqs:
            L = sq['L']
            sq['hTd'] = dint('hy_hTd%d' % sq['idx'], [128, 8, L + 2], BF16)
            sq['zd'] = dint('hy_zd%d' % sq['idx'], [L, 1024], BF16)
            sq['x0Td'] = dint('hy_x0Td%d' % sq['idx'], [128, 8, L], BF16)
            sq['zTd'] = dint('hy_zTd%d' % sq['idx'], [128, 8, L], BF16)
            sq['gTd'] = dint('hy_gTd%d' % sq['idx'], [128, 8, L], BF16)
            if L not in self.hy_L:
                TC = L // 128
                NFc = TC + 1
                self.hy_L[L] = dict(TC=TC, NFc=NFc,
                                    zpos=din('hy_zpos%d' % L, [33, L]), tn=din('hy_tn%d' % L, [L, 1]),
                                    Fc=din('hy_Fc%d' % L, [NFc, 128, TC * 128], BF16), Fs=din('hy_Fs%d' % L, [NFc, 128, TC * 128], BF16),
                                    Gc=din('hy_Gc%d' % L, [NFc * 128, L], BF16), Gs=din('hy_Gs%d' % L, [NFc * 128, L], BF16),
                                    hsd=dint('hy_hsd%d' % L, [L, 1024], BF16), hdd=dint('hy_hdd%d' % L, [L, 1024], BF16), r=Reg())
        return seqs

    def hy_phase(self, l, seqs):
        p, nc, W = self.p, self.nc, self.W
        c = self.cfg
        with ExitStack() as s:
            def sb(name, shape, dt):
                return p.sbuf(s, 'ha_' + name, shape, dt)
            NT = 256
            r_wt = Reg()
            Win = sb('Win', [128, 8, 3072], BF16)
            p.dma('pool', [(Win[:, :, 0:1536], W['hy_w_in'][:, 0:1536].rearrange("(c p) n -> p c n", p=128)),
                           (Win[:, :, 1536:3072], W['hy_w_in'][:, 1536:3072].rearrange("(c p) n -> p c n", p=128))], w=[r_wt])
            fmv = sb('fmv', [128, 6, 24], F32); r_c = Reg()
            p.dma('sp', [(fmv[:, :, :], self.hy_fm)], w=[r_c])
            xt = sb('xt', [128, 1024], F32); r_xt = Reg()
            ss = sb('ss', [128, 2], F32); xn = sb('xn', [128, 1024], F32); junk = sb('junk', [128, 1024], BF16)
            scr = (ss, xn, junk, Reg(), Reg(), Reg())
            hts = sb('hts', [128, 8, 128], BF16); r_hts = Reg()
            zer = sb('zer', [128, 8, 1], BF16); r_zer = Reg()
            p.op('dve', lambda e: e.memset(zer[:, :, :], 0.0), w=[r_zer])
            hTt = sb('hTt', [128, 8, NT + 2], BF16); r_hTt = Reg()
            pT = [sb('pT%d' % i, [128, NT + 2], F32) for i in range(2)]; r_pT = [Reg(), Reg()]
            uu = [sb('uu%d' % i, [128, NT], F32) for i in range(2)]; r_uu = [Reg(), Reg()]
            x0T = sb('x0T', [128, 8, NT], BF16); r_x0T = Reg()
            x1T = sb('x1T', [128, 8, NT], F32); r_x1T = Reg()
            zT = sb('zT', [128, 8, NT], BF16); r_zT = Reg()
            ztok = sb('ztok', [128, NT // 128, 1024], BF16); r_ztok = Reg()
            for sq_ in seqs:
                L, g_, units = sq_['L'], sq_['g'], sq_['units']
                hTd = sq_['hTd']; r_hTd = Reg()
                sq_['r_sc'] = Reg()
                p.dma('sp', [(hTd[:, :, 0:1], zer[:, :, :]), (hTd[:, :, L + 1:L + 2], zer[:, :, :])], r=[r_zer], w=[r_hTd], allow_slow_non_contiguous=True)
                for i, u in enumerate(units):
                    p.dma('sp', [(xt[:, :], u['src'] if self.first_touch else u['dst'])], r=[u['reg']], w=[r_xt])
                    self.norm_hT(xt[:, :], r_xt, l, 0, g_, lambda cc: hts[:, cc, :], r_hts, scr)
                    p.dma('sp', [(hTd[:, :, 1 + i * 128:1 + (i + 1) * 128], hts[:, :, :])], r=[r_hts], w=[r_hTd])
                for ti in range(L // NT):
                    t0 = ti * NT
                    n = NT
                    p.dma('sp', [(hTt[:, :, :], hTd[:, :, t0:t0 + n + 2])], r=[r_hTd], w=[r_hTt])
                    for oc in range(24):
                        b = oc % 2
                        pb, rpb = self.next_ps()
                        for cc in range(8):
                            p.op('pe', lambda e, cc=cc, pb=pb, oc=oc: e.matmul(pb[:, 0:n + 2], lhsT=Win[:, cc, oc * 128:(oc + 1) * 128], rhs=hTt[:, cc, :],
                                                                               start=(cc == 0), stop=(cc == 7)), r=[r_hTt, r_wt], w=[rpb])
                        p.op('dve', lambda e, pb=pb, b=b, oc=oc: e.tensor_scalar(out=pT[b][:, :], in0=pb[:, 0:n + 2], scalar1=fmv[:, 0, oc:oc + 1], scalar2=None, op0=ALU.add),
                             r=[rpb, r_c], w=[r_pT[b]])
                        if t0 == 0:
                            p.op('dve', lambda e, b=b: e.memset(pT[b][:, 0:1], 0.0), w=[r_pT[b]])
                        if t0 + n == L:
                            p.op('dve', lambda e, b=b: e.memset(pT[b][:, n + 1:n + 2], 0.0), w=[r_pT[b]])
                        p.op('dve', lambda e, b=b, oc=oc: e.tensor_scalar(out=uu[b][:, :], in0=pT[b][:, 0:n], scalar1=fmv[:, 1, oc:oc + 1], scalar2=fmv[:, 4, oc:oc + 1],
                                                                        op0=ALU.mult, op1=ALU.add), r=[r_pT[b], r_c], w=[r_uu[b]])
                        p.op('dve', lambda e, b=b, oc=oc: e.scalar_tensor_tensor(out=uu[b][:, :], in0=pT[b][:, 1:n + 1], scalar=fmv[:, 2, oc:oc + 1], in1=uu[b][:, :],
                                                                               op0=ALU.mult, op1=ALU.add), r=[r_pT[b], r_c], w=[r_uu[b]])
                        which, cc8 = oc // 8, oc % 8
                        if which == 0:
                            p.op('dve', lambda e, b=b, oc=oc, cc8=cc8: e.scalar_tensor_tensor(out=x0T[:, cc8, :], in0=pT[b][:, 2:n + 2], scalar=fmv[:, 3, oc:oc + 1], in1=uu[b][:, :],
                                                                                              op0=ALU.mult, op1=ALU.add), r=[r_pT[b], r_uu[b], r_c], w=[r_x0T])
                        elif which == 1:
                            p.op('dve', lambda e, b=b, oc=oc, cc8=cc8: e.scalar_tensor_tensor(out=x1T[:, cc8, :], in0=pT[b][:, 2:n + 2], scalar=fmv[:, 3, oc:oc + 1], in1=uu[b][:, :],
                                                                                              op0=ALU.mult, op1=ALU.add), r=[r_pT[b], r_uu[b], r_c], w=[r_x1T])
                        else:
                            p.op('dve', lambda e, b=b, oc=oc: e.scalar_tensor_tensor(out=uu[b][:, :], in0=pT[b][:, 2:n + 2], scalar=fmv[:, 3, oc:oc + 1], in1=uu[b][:, :],
                                                                                   op0=ALU.mult, op1=ALU.add), r=[r_pT[b], r_c], w=[r_uu[b]])
                            p.op('dve', lambda e, b=b, cc8=cc8: e.tensor_tensor(out=zT[:, cc8, :], in0=uu[b][:, :], in1=x1T[:, cc8, :], op=ALU.mult),
                                 r=[r_uu[b], r_x1T], w=[r_zT])
                    for j in range(n // 128):
                        for half in range(2):
                            pb, rpb = self.next_ps()
                            pv = pb[:, :].bitcast(BF16)
                            for q4 in range(4):
                                cc = half * 4 + q4
                                p.op('pe', lambda e, pv=pv, q4=q4, cc=cc, j=j: e.transpose(out=pv[:, q4 * 128:(q4 + 1) * 128], in_=zT[:, cc, j * 128:(j + 1) * 128],
                                                                                           identity=self.identB[:, :]), r=[r_zT, self.r_const], w=[rpb])
                            p.op('act', lambda e, pv=pv, j=j, half=half: e.activation(out=ztok[:, j, half * 512:(half + 1) * 512], in_=pv[:, 0:512], func=AF.Copy),
                                 r=[rpb], w=[r_ztok])
                    p.dma('sp', [(sq_['zd'][t0:t0 + n, :].rearrange("(j p) f -> p j f", p=128), ztok[:, :, :]),
                                 (sq_['x0Td'][:, :, t0:t0 + n], x0T[:, :, :]), (sq_['zTd'][:, :, t0:t0 + n], zT[:, :, :])],
                          r=[r_ztok, r_x0T, r_zT], w=[sq_['r_sc']])
            p.barrier()
        with ExitStack() as s:
            def sb(name, shape, dt):
                return p.sbuf(s, 'hb_' + name, shape, dt)
            w1 = sb('w1', [33, 64], F32); w2 = sb('w2', [64, 64], F32); w3 = sb('w3', [64, 2048], F32)
            fsm = sb('fsm', [64, 4], F32); rows = sb('rows', [128, 2, 1024], F32)
            r_c = Reg()
            p.dma('sp', [(w1[:, :], W['hy_f_w1']), (w2[:, :], W['hy_f_w2']), (w3[:, :], W['hy_f_w3']), (fsm[:, :], self.hy_fsm), (rows[:, :, :], self.hy_rows)], w=[r_c])
            zp = sb('zp', [33, 128], F32); r_zp = Reg()
            tn = sb('tn', [128, 1], F32); r_tn = Reg()
            ar = sb('ar', [64, 128], F32); s2 = sb('s2', [64, 128], F32); s4 = sb('s4', [64, 128], F32); a1 = sb('a1', [64, 128], F32); a2 = sb('a2', [64, 128], F32)
            r_a = Reg()
            wnd = sb('wnd', [128, 1024], F32); r_wnd = Reg()
            hf = sb('hf', [128, 1024], F32); hb = sb('hb', [128, 1024], F32); r_h = Reg()
            hs = sb('hs', [128, 1024], BF16); hd = sb('hd', [128, 1024], BF16); r_hsd = Reg()

            def sin_layer(pb, rpb, bi, fi, dst):
                p.op('dve', lambda e: e.tensor_scalar(out=ar[:, :], in0=pb[0:64, 0:128], scalar1=fsm[:, bi:bi + 1], scalar2=fsm[:, fi:fi + 1], op0=ALU.add, op1=ALU.mult),
                     r=[rpb, r_c], w=[r_a])
                p.op('act', lambda e: e.activation(out=s2[:, :], in_=ar[:, :], func=AF.Sin, scale=0.5), r=[r_a], w=[r_a])
                p.op('act', lambda e: e.activation(out=s4[:, :], in_=ar[:, :], func=AF.Sin, scale=0.25), r=[r_a], w=[r_a])
                p.op('dve', lambda e: e.tensor_tensor(out=s4[:, :], in0=s4[:, :], in1=s4[:, :], op=ALU.mult), r=[r_a], w=[r_a])
                p.op('dve', lambda e: e.tensor_scalar(out=s4[:, :], in0=s4[:, :], scalar1=-2.0, scalar2=1.0, op0=ALU.mult, op1=ALU.add), r=[r_a], w=[r_a])
                p.op('dve', lambda e: e.scalar_tensor_tensor(out=dst[:, :], in0=s2[:, :], scalar=2.0, in1=s4[:, :], op0=ALU.mult, op1=ALU.mult), r=[r_a], w=[r_a])

            for L, info in self.hy_L.items():
                for ti in range(L // 128):
                    rows_t = slice(ti * 128, (ti + 1) * 128)
                    p.dma('sp', [(zp[:, :], info['zpos'][:, rows_t]), (tn[:, :], info['tn'][rows_t, :])], w=[r_zp, r_tn])
                    pb, rpb = self.next_ps()
                    p.op('pe', lambda e, pb=pb: e.matmul(pb[0:64, 0:128], lhsT=w1[:, :], rhs=zp[:, :], start=True, stop=True), r=[r_zp, r_c], w=[rpb])
                    sin_layer(pb, rpb, 0, 1, a1)
                    pb, rpb = self.next_ps()
                    p.op('pe', lambda e, pb=pb: e.matmul(pb[0:64, 0:128], lhsT=w2[:, :], rhs=a1[:, :], start=True, stop=True), r=[r_a, r_c], w=[rpb])
                    sin_layer(pb, rpb, 2, 3, a2)
                    p.op('dve', lambda e: e.tensor_scalar(out=tn[:, :], in0=tn[:, :], scalar1=-1.0, scalar2=None, op0=ALU.mult), r=[r_tn], w=[r_tn])
                    p.op('act', lambda e: e.activation(out=wnd[:, :], in_=rows[:, 1, :], func=AF.Exp, scale=tn[:, 0:1]), r=[r_tn, r_c], w=[r_wnd])
                    for q in range(4):
                        pb, rpb = self.next_ps()
                        p.op('pe', lambda e, pb=pb, q=q: e.matmul(pb[:, :], lhsT=a2[:, :], rhs=w3[:, q * 512:(q + 1) * 512], start=True, stop=True), r=[r_a, r_c], w=[rpb])
                        dst = hf if q < 2 else hb
                        qq = q % 2
                        p.op('dve', lambda e, pb=pb, dst=dst, qq=qq: e.tensor_tensor(out=dst[:, qq * 512:(qq + 1) * 512], in0=pb[:, :], in1=wnd[:, qq * 512:(qq + 1) * 512], op=ALU.mult),
                             r=[rpb, r_wnd], w=[r_h])
                    if ti == 0:
                        p.op('dve', lambda e: e.memset(hb[0:1, :], 0.0), w=[r_h])
                    p.op('dve', lambda e: e.tensor_tensor(out=hs[:, :], in0=hf[:, :], in1=hb[:, :], op=ALU.add), r=[r_h], w=[r_hsd])
                    p.op('dve', lambda e: e.tensor_tensor(out=hd[:, :], in0=hb[:, :], in1=hf[:, :], op=ALU.subtract), r=[r_h], w=[r_hsd])
                    p.dma('sp', [(info['hsd'][rows_t, :], hs[:, :]), (info['hdd'][rows_t, :], hd[:, :])], r=[r_hsd], w=[info['r']])
            p.barrier()
        self.ps_lim = 6
        with ExitStack() as s:
            def sb(name, shape, dt):
                return p.sbuf(s, 'hc_' + name, shape, dt)
            TCM = max(i_['TC'] for i_ in self.hy_L.values())
            NFM = TCM + 1
            Rc = sb('Rc', [128, TCM, 512], BF16); Rs = sb('Rs', [128, TCM, 512], BF16); r_R = Reg()
            Fcb = [sb('Fcb%d' % i, [128, TCM * 128], BF16) for i in range(2)]; Fsb = [sb('Fsb%d' % i, [128, TCM * 128], BF16) for i in range(2)]
            r_F = [Reg(), Reg()]
            Yre = sb('Yre', [128, NFM, 256], BF16); Yim = sb('Yim', [128, NFM, 256], BF16); r_Y = Reg()
            ec = sb('ec', [128, 512], F32); es_ = sb('es', [128, 512], F32); r_ec, r_es = Reg(), Reg()
            t1 = sb('t1', [128, 256], F32); t2 = sb('t2', [128, 256], F32); r_t1, r_t2 = Reg(), Reg()
            GB = 4
            Gcb = [sb('Gcb%d' % i, [128, GB, 512], BF16) for i in range(2)]; Gsb = [sb('Gsb%d' % i, [128, GB, 512], BF16) for i in range(2)]
            r_G = [Reg(), Reg()]
            x0t = sb('x0t', [128, 2, 512], BF16); zt = sb('zt', [128, 2, 512], BF16); r_xz = Reg()
            go = sb('go', [128, 2, 512], BF16); r_go = Reg()
            tmp = sb('tmp', [128, 512], F32); r_tmp = Reg()
            fmv = sb('fmv', [128, 6, 24], F32); r_c = Reg()
            p.dma('sp', [(fmv[:, :, :], self.hy_fm)], w=[r_c])
            fi = 0
            gi = 0
            for sq_ in seqs:
                L = sq_['L']
                info = self.hy_L[L]
                TC, NFc = info['TC'], info['NFc']
                TW = min(512, L)
                sq_['r_g'] = Reg()
                for gq in range(4):
                    gcols = slice(gq * 256, (gq + 1) * 256)
                    p.dma('sp', [(Rc[:, 0:TC, 0:256], sq_['zd'][:, gcols].rearrange("(c p) f -> p c f", p=128)),
                                 (Rc[:, 0:TC, 256:512], info['hsd'][:, gcols].rearrange("(c p) f -> p c f", p=128)),
                                 (Rs[:, 0:TC, 0:256], sq_['zd'][:, gcols].rearrange("(c p) f -> p c f", p=128)),
                                 (Rs[:, 0:TC, 256:512], info['hdd'][:, gcols].rearrange("(c p) f -> p c f", p=128))],
                          r=[sq_['r_sc'], info['r']], w=[r_R])
                    for fc in range(NFc):
                        b = fi % 2
                        fi += 1
                        p.dma('sp', [(Fcb[b][:, 0:TC * 128], info['Fc'][fc]), (Fsb[b][:, 0:TC * 128], info['Fs'][fc])], w=[r_F[b]])
                        pc, rpc = self.next_ps()
                        for tc in range(TC):
                            p.op('pe', lambda e, pc=pc, tc=tc, b=b: e.matmul(pc[:, :], lhsT=Fcb[b][:, tc * 128:(tc + 1) * 128], rhs=Rc[:, tc, :], start=(tc == 0), stop=(tc == TC - 1)),
                                 r=[r_F[b], r_R], w=[rpc])
                        pS, rpS = self.next_ps()
                        for tc in range(TC):
                            p.op('pe', lambda e, pS=pS, tc=tc, b=b: e.matmul(pS[:, :], lhsT=Fsb[b][:, tc * 128:(tc + 1) * 128], rhs=Rs[:, tc, :], start=(tc == 0), stop=(tc == TC - 1)),
                                 r=[r_F[b], r_R], w=[rpS])
                        p.op('act', lambda e, pc=pc: e.activation(out=ec[:, :], in_=pc[:, :], func=AF.Copy), r=[rpc], w=[r_ec])
                        p.op('act', lambda e, pS=pS: e.activation(out=es_[:, :], in_=pS[:, :], func=AF.Copy), r=[rpS], w=[r_es])
                        p.op('dve', lambda e: e.tensor_tensor(out=t1[:, :], in0=ec[:, 0:256], in1=ec[:, 256:512], op=ALU.mult), r=[r_ec], w=[r_t1])
                        p.op('pool', lambda e: e.tensor_tensor(out=t2[:, :], in0=es_[:, 0:256], in1=es_[:, 256:512], op=ALU.mult), r=[r_es], w=[r_t2])
                        p.op('dve', lambda e, fc=fc: e.tensor_tensor(out=Yre[:, fc, :], in0=t1[:, :], in1=t2[:, :], op=ALU.add), r=[r_t1, r_t2], w=[r_Y])
                        p.op('dve', lambda e: e.tensor_tensor(out=t1[:, :], in0=ec[:, 0:256], in1=es_[:, 256:512], op=ALU.mult), r=[r_ec, r_es], w=[r_t1])
                        p.op('pool', lambda e: e.tensor_tensor(out=t2[:, :], in0=es_[:, 0:256], in1=ec[:, 256:512], op=ALU.mult), r=[r_ec, r_es], w=[r_t2])
                        p.op('dve', lambda e, fc=fc: e.tensor_tensor(out=Yim[:, fc, :], in0=t1[:, :], in1=t2[:, :], op=ALU.subtract), r=[r_t1, r_t2], w=[r_Y])
                    for tt in range(L // TW):
                        tsl = slice(tt * TW, (tt + 1) * TW)
                        pa = [self.ps[6], self.ps[7]]
                        rpa = [self.r_ps[6], self.r_ps[7]]
                        p.dma('sp', [(x0t[:, :, 0:TW], sq_['x0Td'][:, gq * 2:gq * 2 + 2, tsl]), (zt[:, :, 0:TW], sq_['zTd'][:, gq * 2:gq * 2 + 2, tsl])],
                              r=[sq_['r_sc']], w=[r_xz])
                        nbat = (NFc + GB - 1) // GB
                        for bt in range(nbat):
                            f0 = bt * GB
                            nf = min(GB, NFc - f0)
                            b = gi % 2
                            gi += 1
                            p.dma('sp', [(Gcb[b][:, 0:nf, 0:TW], info['Gc'][f0 * 128:(f0 + nf) * 128, tsl].rearrange("(c p) t -> p c t", p=128)),
                                         (Gsb[b][:, 0:nf, 0:TW], info['Gs'][f0 * 128:(f0 + nf) * 128, tsl].rearrange("(c p) t -> p c t", p=128))], w=[r_G[b]])
                            for k in range(nf):
                                fc = f0 + k
                                for dq in range(2):
                                    p.op('pe', lambda e, dq=dq, fc=fc, k=k, b=b: e.matmul(pa[dq][:, 0:TW], lhsT=Yre[:, fc, dq * 128:(dq + 1) * 128], rhs=Gcb[b][:, k, 0:TW],
                                                                                        start=(fc == 0), stop=False), r=[r_Y, r_G[b]], w=[rpa[dq]])
                                    p.op('pe', lambda e, dq=dq, fc=fc, k=k, b=b: e.matmul(pa[dq][:, 0:TW], lhsT=Yim[:, fc, dq * 128:(dq + 1) * 128], rhs=Gsb[b][:, k, 0:TW],
                                                                                        start=False, stop=(fc == NFc - 1)), r=[r_Y, r_G[b]], w=[rpa[dq]])
                        for dq in range(2):
                            gch = gq * 2 + dq
                            p.op('dve', lambda e, dq=dq, gch=gch: e.scalar_tensor_tensor(out=tmp[:, 0:TW], in0=zt[:, dq, 0:TW], scalar=fmv[:, 5, gch:gch + 1], in1=pa[dq][:, 0:TW],
                                                                                       op0=ALU.mult, op1=ALU.add), r=[r_xz, rpa[dq], r_c], w=[r_tmp])
                            p.op('dve', lambda e, dq=dq: e.tensor_tensor(out=go[:, dq, 0:TW], in0=tmp[:, 0:TW], in1=x0t[:, dq, 0:TW], op=ALU.mult), r=[r_tmp, r_xz], w=[r_go])
                        p.dma('sp', [(sq_['gTd'][:, gq * 2:gq * 2 + 2, tsl], go[:, :, 0:TW])], r=[r_go], w=[sq_['r_g']])
            p.barrier()
        self.ps_lim = 8
        with ExitStack() as s:
            def sb(name, shape, dt):
                return p.sbuf(s, 'hd_' + name, shape, dt)
            r_wt = Reg()
            Wo = sb('Wo', [128, 8, 1024], BF16)
            p.dma('pool', [(Wo[:, :, :], W['hy_w_out'].rearrange("(c p) n -> p c n", p=128))], w=[r_wt])
            rows = sb('rows', [128, 2, 1024], F32); r_c = Reg()
            p.dma('sp', [(rows[:, :, :], self.hy_rows)], w=[r_c])
            gbc = sb('gbc', [128, 1024], F32); r_gbc = Reg()
            diag = sb('diag', [128, 128], F32); r_diag = Reg()
            gT = [sb('gT%d' % i, [128, 8, 128], BF16) for i in range(2)]; r_gT = [Reg(), Reg()]
            xt = [sb('xt%d' % i, [128, 1024], F32) for i in range(2)]; r_xt = [Reg(), Reg()]
            osb = [sb('osb%d' % i, [128, 1024], F32) for i in range(2)]; r_osb = [Reg(), Reg()]
            k = 0
            for sq_ in seqs:
                self.gate_bc(gbc, r_gbc, l, 2, sq_['g'], diag, r_diag)
                for i, u in enumerate(sq_['units']):
                    b = k % 2
                    k += 1
                    p.dma('sp', [(gT[b][:, :, :], sq_['gTd'][:, :, i * 128:(i + 1) * 128])], r=[sq_['r_g']], w=[r_gT[b]])
                    p.dma('sp', [(xt[b][:, :], u['src'] if self.first_touch else u['dst'])], r=[u['reg']], w=[r_xt[b]])
                    for hf_ in range(2):
                        cs_ = slice(hf_ * 512, (hf_ + 1) * 512)
                        pbo, rpbo = self.next_ps()
                        for cc in range(8):
                            p.op('pe', lambda e, cc=cc, pbo=pbo, b=b, cs_=cs_: e.matmul(pbo[:, :], lhsT=gT[b][:, cc, :], rhs=Wo[:, cc, cs_], start=(cc == 0), stop=(cc == 7)),
                                 r=[r_gT[b], r_wt], w=[rpbo])
                        p.op('dve', lambda e, pbo=pbo, b=b, cs_=cs_: e.tensor_tensor(out=osb[b][:, cs_], in0=pbo[:, :], in1=rows[:, 0, cs_], op=ALU.add), r=[rpbo, r_c], w=[r_osb[b]])
                    p.op('dve', lambda e, b=b: e.tensor_tensor(out=osb[b][:, :], in0=osb[b][:, :], in1=gbc[:, :], op=ALU.mult), r=[r_gbc], w=[r_osb[b]])
                    p.op('dve', lambda e, b=b: e.tensor_tensor(out=osb[b][:, :], in0=osb[b][:, :], in1=xt[b][:, :], op=ALU.add), r=[r_xt[b]], w=[r_osb[b]])
                    p.dma('sp', [(u['dst'], osb[b][:, :])], r=[r_osb[b]], w=[u['reg']])
            p.barrier()

    def make_seqs(self):
        c = self.cfg
        ns = c.LS // 128
        seqs = [dict(L=c.LS, g=0, units=self.units[0:ns], idx=0)]
        npu = c.LP // 128
        for i in range(c.NP):
            seqs.append(dict(L=c.LP, g=1, units=self.units[ns + i * npu: ns + (i + 1) * npu], idx=1 + i))
        return seqs

    def rwkv_setup(self):
        c = self.cfg
        din, dout, dint = self._din, self._dout, self._dint
        self.rw_mix = din('rw_mix', [128, 6, 8])
        self.rw_hm = din('rw_hm', [64, 8, 16])
        self.rw_rows = din('rw_rows', [128, 2, 1024])
        self.rw_mask4 = din('rw_mask4', [128, 2, 512])
        self.rw_maskN = din('rw_maskN', [128, 4, 128])
        self.st_rwkv = din('st_rwkv', [2, 16, 64, 64])
        self.o_rwkv = dout('o_rwkv', [c.NP, 2, 16, 64, 64])
        seqs = self.make_seqs()
        for sq in seqs:
            L = sq['L']
            sq['hTd'] = dint('rw_hTd%d' % sq['idx'], [128, 8, L + 2], BF16)
            sq['yfd'] = dint('rw_yfd%d' % sq['idx'], [L, 1024])
            sq['bfd'] = dint('rw_bfd%d' % sq['idx'], [L, 16])
            if sq['g'] == 0:
                sq['s0'], sq['sout'] = self.st_rwkv, None
            else:
                sq['s0'], sq['sout'] = None, self.o_rwkv[sq['idx'] - 1]
        return seqs

    def build(self):
        self.setup()
        for l in range(4):
            if l == 0 and 'rwkv' in self.enable:
                self.rwkv_phase(0, self.rwkv_setup())
                self.first_touch = False
            if l == 1 and 'att' in self.enable:
                self.att_phase(1, self.att_setup())
            if l == 2 and 'hy' in self.enable:
                self.hy_phase(2, self.hy_setup())
            if l == 3 and 'ret' in self.enable:
                self.ret_phase(3, self.ret_setup())
            if 'ffn' in self.enable:
                self.ffn_phase(l)
        self.p.finish()
        return self.nc


def fm(vec):
    v = np.asarray(vec, dtype=np.float32)
    return np.ascontiguousarray(v.reshape(-1, 128).T)


def hm(vec):
    v = np.asarray(vec, dtype=np.float32).reshape(-1)
    return np.ascontiguousarray(v.reshape(16, 64).T)


def rwkv_consts(inp):
    out = {}
    out['rw_mix'] = np.ascontiguousarray(np.stack([fm(inp['rwkv_mix'][i]) for i in range(6)], axis=1))
    z = np.zeros((64, 16), np.float32)
    out['rw_hm'] = np.ascontiguousarray(np.stack([hm(inp['rwkv_w0'][0]), hm(inp['rwkv_w0'][1]), hm(inp['rwkv_a0'][0]), hm(inp['rwkv_a0'][1]),
                                                  hm(inp['rwkv_k_k']), hm(inp['rwkv_k_a']), hm(inp['rwkv_r_k']), z], axis=1))
    rows = np.stack([np.asarray(inp['rwkv_ln_w'], np.float32), np.asarray(inp['rwkv_ln_b'], np.float32)], axis=0)
    out['rw_rows'] = np.ascontiguousarray(np.broadcast_to(rows[None], (128, 2, 1024)))
    s_ = np.arange(128)[:, None]
    t_ = np.arange(128)[None, :]
    m4 = np.zeros((128, 2, 512), np.float32)
    mN = np.zeros((128, 4, 128), np.float32)
    bd32 = np.kron(np.eye(4), np.ones((32, 32))).astype(np.float32)
    mN[:, 2, :] = bd32
    mN[:, 3, :] = 1.0 - bd32
    for d in range(2):
        strict = (s_ < t_) if d == 0 else (s_ > t_)
        incl = (s_ <= t_) if d == 0 else (s_ >= t_)
        m4[:, d, :] = np.concatenate([strict, incl, strict, incl], axis=1).astype(np.float32)
        mN[:, d, :] = strict.T.astype(np.float32) * bd32
    out['rw_mask4'] = m4
    out['rw_maskN'] = mN
    return out


def att_consts(inp, cfg):
    out = {}
    qn = np.asarray(inp['att_q_norm'], np.float32)
    kn = np.asarray(inp['att_k_norm'], np.float32)
    rows = np.concatenate([np.tile(qn[None], (16, 1)), np.tile(kn[None], (4, 1))], axis=0)
    out['at_rows'] = np.ascontiguousarray(np.broadcast_to(rows[None], (128, 20, 64)))
    out['at_sink'] = np.ascontiguousarray(np.broadcast_to(np.asarray(inp['att_sink'], np.float32)[None], (64, 16)))
    L = cfg.LS
    t = np.arange(L)
    row = (t // 64).astype(np.float32)
    col = (t % 64).astype(np.float32)
    nf = 16
    inv = (np.float32(10000.0) ** (-np.arange(nf, dtype=np.float32) / np.float32(nf))).astype(np.float32)
    ang = np.concatenate([row[:, None] * inv[None], col[:, None] * inv[None]], axis=-1)
    ang = np.concatenate([ang, ang], axis=-1).astype(np.float32)
    out['at_cos'] = np.cos(ang).astype(np.float32)
    out['at_sin'] = np.sin(ang).astype(np.float32)
    a = np.arange(128)[:, None]
    b = np.arange(128)[None, :]
    m = np.zeros((128, 2, 128), np.float32)
    m[:, 0, :] = (b <= a)
    m[:, 1, :] = (a <= b)
    out['at_mask'] = m
    return out


def ret_consts(cfg):
    out = {}
    lg = [np.log1p(-np.exp2(-5.0 - np.arange(4, dtype=np.float64))), np.log1p(-np.exp2(-5.5 - np.arange(4, dtype=np.float64)))]
    s_ = np.arange(128)[:, None].astype(np.float64)
    t_ = np.arange(128)[None, :].astype(np.float64)
    dm = np.zeros((128, 4, 2, 128), np.float64)
    qd = np.zeros((128, 4, 2, 128), np.float64)
    kd = np.zeros((128, 4, 2), np.float64)
    i_ = np.arange(128).astype(np.float64)
    for h in range(4):
        dm[:, h, 0, :] = np.where(t_ >= s_, np.exp(lg[0][h] * np.maximum(t_ - s_, 0)), 0.0)
        dm[:, h, 1, :] = np.where(s_ >= t_, np.exp(lg[1][h] * np.maximum(s_ - t_, 0)), 0.0)
        qd[:, h, 0, :] = np.exp(lg[0][h] * (i_ + 1.0))[None, :]
        qd[:, h, 1, :] = np.exp(lg[1][h] * (128.0 - i_))[None, :]
        kd[:, h, 0] = np.exp(lg[0][h] * (127.0 - i_))
        kd[:, h, 1] = np.exp(lg[1][h] * i_)
    out['rt_dmask'] = dm.astype(np.float32)
    out['rt_qdec'] = qd.astype(np.float32)
    out['rt_kdec'] = kd.astype(np.float32)
    L = cfg.LS
    ang = np.repeat((1.0 / (np.float32(10000.0) ** np.linspace(0.0, 1.0, 128, dtype=np.float32))).astype(np.float32), 2)
    ph = (np.arange(L, dtype=np.float32)[:, None] * ang[None, :]).astype(np.float32)
    out['rt_cos'] = np.cos(ph).astype(np.float32)
    out['rt_sin'] = np.sin(ph).astype(np.float32)
    return out


def hy_consts(inp, cfg):
    import ml_dtypes
    out = {}
    z24 = np.zeros((128, 24), np.float32)
    skip = z24.copy()
    skip[:, 0:8] = fm(inp['hy_skip'])
    cw = np.asarray(inp['hy_conv_w'], np.float32)
    out['hy_fm'] = np.ascontiguousarray(np.stack([fm(inp['hy_b_in']), fm(cw[0]), fm(cw[1]), fm(cw[2]), fm(inp['hy_conv_b']), skip], axis=1))
    deltas = np.abs(np.linspace(np.log(1e-2) / 1.5, np.log(1e-2) / 0.3, 1024, dtype=np.float32)).astype(np.float32)
    rows = np.stack([np.asarray(inp['hy_b_out'], np.float32), deltas], axis=0)
    out['hy_rows'] = np.ascontiguousarray(np.broadcast_to(rows[None], (128, 2, 1024)))
    out['hy_fsm'] = np.ascontiguousarray(np.stack([np.asarray(inp[k], np.float32) for k in ('hy_f_b1', 'hy_f_freq1', 'hy_f_b2', 'hy_f_freq2')], axis=1))
    for L in sorted(set([cfg.LS, cfg.LP])):
        t = np.arange(L, dtype=np.float32)
        tn = (t / np.float32(max(L - 1, 1))).astype(np.float32)
        bands = 16
        fr = np.linspace(1e-4, bands - 1, bands, dtype=np.float32)
        ph = (np.float32(2.0 * np.pi) * t[:, None] * fr[None, :] / np.float32(L)).astype(np.float32)
        zpos = np.concatenate([tn[:, None], np.cos(ph), -np.sin(ph)], axis=-1).astype(np.float32)
        out['hy_zpos%d' % L] = np.ascontiguousarray(zpos.T)
        out['hy_tn%d' % L] = np.ascontiguousarray(tn[:, None])
        N = 2 * L
        TC = L // 128
        NFc = TC + 1
        f = np.arange(NFc * 128, dtype=np.int64)
        tt = np.arange(L, dtype=np.int64)
        ang = (2.0 * np.pi / N) * ((f[:, None] * tt[None, :]) % N).astype(np.float64)
        valid = (f <= L).astype(np.float64)[:, None]
        C = np.cos(ang) * valid
        S = np.sin(ang) * valid
        wf = np.where((f == 0) | (f == L), 1.0, 2.0)[:, None] / N
        out['hy_Gc%d' % L] = np.ascontiguousarray((C * wf).astype(np.float32).astype(ml_dtypes.bfloat16))
        out['hy_Gs%d' % L] = np.ascontiguousarray((-S * wf).astype(np.float32).astype(ml_dtypes.bfloat16))
        Ct = C.T.reshape(TC, 128, NFc, 128).transpose(2, 1, 0, 3).reshape(NFc, 128, TC * 128)
        St = S.T.reshape(TC, 128, NFc, 128).transpose(2, 1, 0, 3).reshape(NFc, 128, TC * 128)
        out['hy_Fc%d' % L] = np.ascontiguousarray(Ct.astype(np.float32).astype(ml_dtypes.bfloat16))
        out['hy_Fs%d' % L] = np.ascontiguousarray(St.astype(np.float32).astype(ml_dtypes.bfloat16))
    return out


def make_in_maps(inp, cfg, used):
    maps = []
    adab = np.stack([fm(inp['ada_b'][l]) for l in range(4)], axis=1)
    ident = np.eye(128, dtype=np.float32)
    shared = {k: np.ascontiguousarray(np.asarray(inp[k], dtype=np.float32)) for k in used}
    for i in range(NCORES):
        m = dict(shared)
        m['xs'] = np.ascontiguousarray(inp['x_sample'][i])
        m['xp'] = np.ascontiguousarray(inp['x_prompt'][cfg.NP * i:cfg.NP * (i + 1)].reshape(cfg.NP * cfg.LP, D))
        m['cfm'] = np.ascontiguousarray(np.stack([fm(inp['c'][i]), fm(inp['c_ctx'])], axis=-1))
        m['adab'] = np.ascontiguousarray(adab)
        m['ident'] = ident
        if 'rwkv_w_rkv' in used:
            m.update(rwkv_consts(inp))
            m['st_rwkv'] = np.ascontiguousarray(inp['state_rwkv'][i])
        if 'att_w_qkv' in used:
            if i == 0:
                _att = att_consts(inp, cfg)
            m.update(_att)
            m['ck'] = np.ascontiguousarray(inp['cache_att_k'][i].reshape(cfg.PAST, 256))
            m['cv'] = np.ascontiguousarray(inp['cache_att_v'][i].reshape(cfg.PAST, 256))
        if 'hy_w_in' in used:
            if i == 0:
                _hy = hy_consts(inp, cfg)
            m.update(_hy)
        if 'ret_w_in' in used:
            if i == 0:
                _ret = ret_consts(cfg)
            m.update(_ret)
            m['st_ret'] = np.ascontiguousarray(inp['state_ret'][i])
        maps.append(m)
    return maps


_CACHE = {}


def kernel(**inp):
    cfg = Cfg()
    inp = {k: np.asarray(v) for k, v in inp.items()}
    kb = K(cfg, enable=('rwkv', 'att', 'hy', 'ret', 'ffn'))
    nc = kb.build()
    maps = make_in_maps(inp, cfg, list(kb.W.keys()))
    res = run_bass_kernel_spmd(nc, maps, core_ids=list(range(NCORES)))
    R = res.results
    NP, LP = cfg.NP, cfg.LP
    y_sample = np.stack([R[i]['ys'] for i in range(NCORES)], axis=0)
    y_prompt = np.concatenate([R[i]['yp'].reshape(NP, LP, D) for i in range(NCORES)], axis=0)
    st_rwkv = np.concatenate([R[i]['o_rwkv'] for i in range(NCORES)], axis=0)
    ck = np.concatenate([R[i]['o_k'].reshape(NP, LP, 4, 64) for i in range(NCORES)], axis=0)
    cv = np.concatenate([R[i]['o_v'].reshape(NP, LP, 4, 64) for i in range(NCORES)], axis=0)
    st_ret = np.concatenate([R[i]['o_ret'] for i in range(NCORES)], axis=0)
    return (y_prompt, y_sample, st_rwkv, ck, cv, st_ret)
```

```python
import numpy as np
from contextlib import ExitStack
import concourse.bass as bass
import concourse.mybir as mybir
from concourse.bass_utils import run_bass_kernel_spmd

F32 = mybir.dt.float32
BF16 = mybir.dt.bfloat16
AF = mybir.ActivationFunctionType
ALU = mybir.AluOpType
AX = mybir.AxisListType

D = 1024
NCORES = 8
NDSEM = 8
EPS = 1e-6


class Reg:
    __slots__ = ('lastw', 'reads')

    def __init__(self):
        self.lastw = None
        self.reads = {}


class Prog:
    def __init__(self, nc):
        self.nc = nc
        self.es = ExitStack()
        self.h = {'pe': nc.tensor, 'dve': nc.vector, 'act': nc.scalar, 'pool': nc.gpsimd, 'sp': nc.sync}
        self.cnt = {e: 0 for e in self.h}
        self.known = {e: {} for e in self.h}
        self.sem = {}
        for e in ['pe', 'dve', 'act', 'pool']:
            self.sem[e] = self.es.enter_context(nc.semaphore('s_' + e))
        self.dtot = {}
        self.drr = {}
        for q in ['sp', 'pool', 'act']:
            for i in range(NDSEM):
                k = 'd_%s_%d' % (q, i)
                self.sem[k] = self.es.enter_context(nc.semaphore(k))
                self.dtot[k] = 0
            self.drr[q] = 0
        self.ninst = 0

    def sbuf(self, es, name, shape, dtype):
        self.ninst += 0
        self.nname = getattr(self, 'nname', 0) + 1
        return es.enter_context(self.nc.sbuf_tensor('%s_%d' % (name, self.nname), list(shape), dtype))

    def psum(self, es, name, shape, dtype):
        return es.enter_context(self.nc.psum_tensor(name, list(shape), dtype))

    def _collect(self, eng, reads, writes):
        waits = {}
        kn = self.known[eng]

        def need(ev):
            if ev is None:
                return
            k, v = ev
            if k == 'pe' and eng == 'pe':
                return
            if kn.get(k, 0) >= v:
                return
            if waits.get(k, 0) < v:
                waits[k] = v

        for r in reads:
            need(r.lastw)
        for w in writes:
            need(w.lastw)
            for ev in w.reads.items():
                need(ev)
        for k, v in waits.items():
            kn[k] = v
        return waits

    def op(self, eng, fn, r=(), w=()):
        waits = self._collect(eng, r, w)
        h = self.h[eng]
        for k, v in waits.items():
            h.wait_ge(self.sem[k], v)
        self.cnt[eng] += 1
        fn(h).then_inc(self.sem[eng], 1)
        self.ninst += 1
        ev = (eng, self.cnt[eng])
        for x in r:
            x.reads[ev[0]] = ev[1]
        for x in w:
            x.lastw = ev
            x.reads = {}
        return ev

    def dma(self, q, pairs, r=(), w=(), **kw):
        waits = self._collect(q, r, w)
        i = self.drr[q]
        self.drr[q] = (i + 1) % NDSEM
        k = 'd_%s_%d' % (q, i)
        prev = self.dtot[k]
        if prev > 0 and self.known[q].get(k, 0) < prev:
            waits[k] = max(waits.get(k, 0), prev)
            self.known[q][k] = prev
        h = self.h[q]
        for k2, v2 in waits.items():
            h.wait_ge(self.sem[k2], v2)
        for (o, a) in pairs:
            h.dma_start(out=o, in_=a, **kw).then_inc(self.sem[k], 16)
            self.ninst += 1
        self.dtot[k] = prev + 16 * len(pairs)
        ev = (k, self.dtot[k])
        for x in r:
            x.reads[ev[0]] = ev[1]
        for x in w:
            x.lastw = ev
            x.reads = {}
        return ev

    def barrier(self):
        tot = {}
        for k, v in self.dtot.items():
            if v > 0:
                tot[k] = v
        for e in ['pe', 'dve', 'act', 'pool']:
            if self.cnt[e] > 0:
                tot[e] = self.cnt[e]
        for eng, h in self.h.items():
            for k, v in tot.items():
                if k == eng:
                    continue
                if self.known[eng].get(k, 0) < v:
                    h.wait_ge(self.sem[k], v)
                    self.known[eng][k] = v

    def finish(self):
        self.barrier()
        self.es.close()


class Cfg:
    def __init__(self, LS=4096, LP=256, NP=2, PAST=512):
        self.LS, self.LP, self.NP, self.PAST = LS, LP, NP, PAST


FFN_DENSE = 2816
FFN_EXPERT = 3584
N_EXPERTS = 8

WEIGHT_SHAPES = {
    'ada_w': (4, D, 6 * D),
    'rwkv_w_rkv': (3, D, D), 'rwkv_w1': (2, D, 64), 'rwkv_w2': (2, 64, D),
    'rwkv_a1': (2, D, 64), 'rwkv_a2': (2, 64, D), 'rwkv_g1': (D, 128), 'rwkv_g2': (128, D),
    'rwkv_w_o': (D, D),
    'att_w_qkv': (D, 1536), 'att_w_o': (D, D),
    'hy_w_in': (D, 3 * D), 'hy_w_out': (D, D), 'hy_f_w1': (33, 64), 'hy_f_w2': (64, 64), 'hy_f_w3': (64, 2 * D),
    'ret_w_in': (D, 8192), 'ret_w_out': (2048, D),
    'ffn_w_in': (2, D, 2 * FFN_DENSE), 'ffn_w_out': (2, FFN_DENSE, D),
    'moe_router': (2, D, 8), 'moe_w_in': (2, 8, D, 2 * FFN_EXPERT), 'moe_w_out': (2, 8, FFN_EXPERT, D),
}


class LazyW(dict):
    def __init__(self, k):
        super().__init__()
        self.k = k

    def __missing__(self, name):
        ap = self.k._din(name, WEIGHT_SHAPES[name])
        self[name] = ap
        return ap


class K:
    def __init__(self, cfg, enable=('ffn',)):
        self.cfg = cfg
        self.enable = enable
        nc = self.nc = bass.Bass("TRN2", target_bir_lowering=False)
        self.p = Prog(nc)
        c = cfg
        self.TS = c.LS
        self.TP = c.NP * c.LP

        def din(name, shape, dt=F32):
            return nc.dram_tensor(name, list(shape), dt, kind="ExternalInput").ap()

        def dout(name, shape, dt=F32):
            return nc.dram_tensor(name, list(shape), dt, kind="ExternalOutput").ap()

        self.xs = din('xs', [c.LS, D])
        self.xp = din('xp', [self.TP, D])
        self.cfm = din('cfm', [128, 8, 2])
        self.adab = din('adab', [128, 4, 48])
        self.ident_d = din('ident', [128, 128])
        self._din = din
        self.W = LazyW(self)
        self.ys = dout('ys', [c.LS, D])
        self.yp = dout('yp', [self.TP, D])
        self._dout = dout

        def dint(name, shape, dt=F32):
            return nc.dram_tensor(name, list(shape), dt, kind="Internal").ap()
        self._dint = dint
        self.units = []
        for i in range(c.LS // 128):
            self.units.append(dict(src=self.xs[i * 128:(i + 1) * 128, :], dst=self.ys[i * 128:(i + 1) * 128, :], g=0, reg=Reg()))
        for i in range(self.TP // 128):
            self.units.append(dict(src=self.xp[i * 128:(i + 1) * 128, :], dst=self.yp[i * 128:(i + 1) * 128, :], g=1, reg=Reg()))
        self.first_touch = True

    def setup(self):
        p, nc = self.p, self.nc
        es = p.es
        self.identF = p.sbuf(es, 'identF', [128, 128], F32)
        self.identB = p.sbuf(es, 'identB', [128, 128], BF16)
        self.onesF = p.sbuf(es, 'onesF', [128, 128], F32)
        self.mod = p.sbuf(es, 'mod', [128, 4, 48, 2], F32)
        self.r_const = Reg()
        self.r_mod = Reg()
        self.ps = [p.psum(es, 'ps%d' % i, [128, 512], F32) for i in range(8)]
        self.r_ps = [Reg() for _ in range(8)]
        self.ps_rr = 0
        self.ps_lim = 8
        p.dma('sp', [(self.identF[:, :], self.ident_d)], w=[self.r_const])
        p.op('dve', lambda e: e.tensor_copy(out=self.identB[:, :], in_=self.identF[:, :]), r=[self.r_const], w=[self.r_const])
        p.op('dve', lambda e: e.memset(self.onesF[:, :], 1.0), w=[self.r_const])
        with ExitStack() as s:
            sc = p.sbuf(s, 'sc', [128, 8, 2], F32)
            ab = p.sbuf(s, 'ab', [128, 4, 48], F32)
            wb = [p.sbuf(s, 'adaw%d' % i, [128, 8, 512], F32) for i in range(2)]
            r_sc, r_ab = Reg(), Reg()
            r_wb = [Reg(), Reg()]
            p.dma('sp', [(sc[:, :, :], self.cfm)], w=[r_sc])
            p.dma('sp', [(ab[:, :, :], self.adab)], w=[r_ab])
            p.op('act', lambda e: e.activation(out=sc[:, :, :], in_=sc[:, :, :], func=AF.Silu), r=[r_sc], w=[r_sc])
            it = 0
            for l in range(4):
                for blk in range(12):
                    b = it % 2
                    it += 1
                    src = self.W['ada_w'][l, :, blk * 512:(blk + 1) * 512].rearrange("(c p) n -> p c n", p=128)
                    p.dma('sp', [(wb[b][:, :, :], src)], w=[r_wb[b]])
                    pb, rpb = self.next_ps()
                    for sub in range(4):
                        for c in range(8):
                            p.op('pe', lambda e, c=c, sub=sub, b=b, pb=pb: e.matmul(
                                pb[:, sub * 2:sub * 2 + 2], lhsT=wb[b][:, c, sub * 128:(sub + 1) * 128], rhs=sc[:, c, :],
                                start=(c == 0), stop=(c == 7)), r=[r_wb[b], r_sc], w=[rpb])
                    for sub in range(4):
                        jc = blk * 4 + sub
                        p.op('dve', lambda e, sub=sub, jc=jc, l=l, pb=pb: e.tensor_scalar(
                            out=self.mod[:, l, jc, :], in0=pb[:, sub * 2:sub * 2 + 2], scalar1=ab[:, l, jc:jc + 1],
                            scalar2=None, op0=ALU.add), r=[rpb, r_ab], w=[self.r_mod])
            for j in (1, 4):
                p.op('dve', lambda e, j=j: e.tensor_scalar(
                    out=self.mod[:, :, j * 8:(j + 1) * 8, :], in0=self.mod[:, :, j * 8:(j + 1) * 8, :],
                    scalar1=1.0, scalar2=None, op0=ALU.add), r=[self.r_mod], w=[self.r_mod])
            p.barrier()

    def next_ps(self):
        i = self.ps_rr % self.ps_lim
        self.ps_rr = (i + 1) % self.ps_lim
        return self.ps[i], self.r_ps[i]

    def modap(self, l, j, c, g):
        return self.mod[:, l, j * 8 + c, g:g + 1]

    def gate_bc(self, out_tile, r_out, l, j, g, diag, r_diag):
        p = self.p
        for half in range(2):
            pb, rpb = self.next_ps()
            for cc in range(4):
                c = half * 4 + cc
                p.op('dve', lambda e, c=c: e.tensor_scalar(out=diag[:, :], in0=self.identF[:, :], scalar1=self.modap(l, j, c, g),
                                                            scalar2=None, op0=ALU.mult), r=[self.r_const, self.r_mod], w=[r_diag])
                p.op('pe', lambda e, cc=cc, pb=pb: e.matmul(pb[:, cc * 128:(cc + 1) * 128], lhsT=self.onesF[:, :], rhs=diag[:, :],
                                                            start=True, stop=True), r=[r_diag, self.r_const], w=[rpb])
            p.op('act', lambda e, half=half, pb=pb: e.activation(out=out_tile[:, half * 512:(half + 1) * 512], in_=pb[:, :], func=AF.Copy),
                 r=[rpb], w=[r_out])

    def norm_hT(self, xt, r_xt, l, jsh, g, hT_dst, r_hT, scr, hTf=None, r_hTf=None):
        p = self.p
        ss, xn, junk, r_ss, r_xn, r_junk = scr
        p.op('act', lambda e: e.activation(out=junk[:, :], in_=xt, func=AF.Square, accum_out=ss[:, 0:1]), r=[r_xt], w=[r_junk, r_ss])
        p.op('act', lambda e: e.activation(out=ss[:, 0:1], in_=ss[:, 0:1], func=AF.Sqrt, scale=1.0 / D, bias=EPS), r=[r_ss], w=[r_ss])
        p.op('dve', lambda e: e.reciprocal(out=ss[:, 0:1], in_=ss[:, 0:1]), r=[r_ss], w=[r_ss])
        p.op('act', lambda e: e.activation(out=xn[:, :], in_=xt, func=AF.Copy, scale=ss[:, 0:1]), r=[r_xt, r_ss], w=[r_xn])
        for half in range(2):
            pb, rpb = self.next_ps()
            for cc in range(4):
                c = half * 4 + cc
                p.op('pe', lambda e, c=c, cc=cc, pb=pb: e.transpose(out=pb[:, cc * 128:(cc + 1) * 128], in_=xn[:, c * 128:(c + 1) * 128],
                                                                    identity=self.identF[:, :]), r=[r_xn, self.r_const], w=[rpb])
            for cc in range(4):
                c = half * 4 + cc
                p.op('dve', lambda e, c=c, cc=cc, pb=pb: e.tensor_scalar(
                    out=hT_dst(c), in0=pb[:, cc * 128:(cc + 1) * 128], scalar1=self.modap(l, jsh + 1, c, g),
                    scalar2=self.modap(l, jsh, c, g), op0=ALU.mult, op1=ALU.add), r=[rpb, self.r_mod], w=[r_hT])
                if hTf is not None:
                    p.op('dve', lambda e, c=c, cc=cc, pb=pb: e.tensor_scalar(
                        out=hTf[:, c, :], in0=pb[:, cc * 128:(cc + 1) * 128], scalar1=self.modap(l, jsh + 1, c, g),
                        scalar2=self.modap(l, jsh, c, g), op0=ALU.mult, op1=ALU.add), r=[rpb, self.r_mod], w=[r_hTf])

    def ffn_phase(self, l):
        p, nc = self.p, self.nc
        moe = (l % 2 == 1)
        li = l // 2
        if moe:
            nexp, H = N_EXPERTS, FFN_EXPERT
        else:
            nexp, H = 1, FFN_DENSE
        HB = 512 if moe else 256
        npiece = H // HB
        nu = len(self.units)
        halves = [list(range(0, nu // 2)), list(range(nu // 2, nu))]
        for hu in halves:
            nh = len(hu)
            T = nh * 128
            with ExitStack() as s:
                hT = p.sbuf(s, 'f_hT', [128, 8, T], BF16)
                acc = p.sbuf(s, 'f_acc', [128, nh, D], F32)
                xt = [p.sbuf(s, 'f_xt%d' % i, [128, D], F32) for i in range(2)]
                r_xt = [Reg(), Reg()]
                ss = p.sbuf(s, 'f_ss', [128, 2], F32)
                xn = p.sbuf(s, 'f_xn', [128, D], F32)
                junk = p.sbuf(s, 'f_junk', [128, D], BF16)
                scr = (ss, xn, junk, Reg(), Reg(), Reg())
                gbc = [p.sbuf(s, 'f_gbc%d' % g, [128, D], F32) for g in range(2)]
                r_gbc = [Reg(), Reg()]
                diag = p.sbuf(s, 'f_diag', [128, 128], F32)
                r_diag = Reg()
                win = [p.sbuf(s, 'f_win%d' % i, [128, 8, 2 * HB], BF16) for i in range(2)]
                wout = [p.sbuf(s, 'f_wout%d' % i, [128, HB // 128, D], BF16) for i in range(2)]
                r_w = [Reg(), Reg()]
                sg = [p.sbuf(s, 'f_sg%d' % i, [128, 512], BF16) for i in range(2)]
                hid = [p.sbuf(s, 'f_hid%d' % i, [128, HB // 128, 512], BF16) for i in range(2)]
                r_sg = [Reg(), Reg()]
                r_hid = [Reg(), Reg()]
                r_hT = [Reg() for _ in range(nh)]
                r_acc = [Reg() for _ in range(nh)]
                if moe:
                    hTf = p.sbuf(s, 'f_hTf', [128, 8, 128], F32)
                    r_hTf = Reg()
                    rt = p.sbuf(s, 'f_rt', [128, 8, 8], F32)
                    r_rt = Reg()
                    gates = p.sbuf(s, 'f_gates', [128, nh, 8], F32)
                    r_gates = [Reg() for _ in range(nh)]
                    tk = p.sbuf(s, 'f_tk', [128, 48], F32)
                    r_tk = Reg()
                    p.dma('sp', [(rt[:, :, :], self.W['moe_router'][li].rearrange("(c p) e -> p c e", p=128))], w=[r_rt])
                for g in range(2):
                    self.gate_bc(gbc[g], r_gbc[g], l, 5, g, diag, r_diag)
                for ii, ui in enumerate(hu):
                    u = self.units[ui]
                    b = ii % 2
                    src = u['src'] if self.first_touch else u['dst']
                    p.dma('sp', [(xt[b][:, :], src)], r=[u['reg']], w=[r_xt[b]])
                    dbg = ''
                    norouter = 'norouter' in dbg
                    self.norm_hT(xt[b][:, :], r_xt[b], l, 3, u['g'], lambda c, ii=ii: hT[:, c, ii * 128:(ii + 1) * 128], r_hT[ii], scr,
                                 hTf if (moe and not norouter) else None, r_hTf if moe else None)
                    if moe and norouter:
                        p.op('dve', lambda e, ii=ii: e.memset(gates[:, ii, :], 0.125), w=[r_gates[ii]])
                    elif moe:
                        pb, rpb = self.next_ps()
                        for c in range(8):
                            p.op('pe', lambda e, c=c, pb=pb: e.matmul(pb[:, 0:8], lhsT=hTf[:, c, :], rhs=rt[:, c, :], start=(c == 0), stop=(c == 7)),
                                 r=[r_hTf, r_rt], w=[rpb])
                        if 'nogate' in dbg:
                            p.op('dve', lambda e, ii=ii: e.memset(gates[:, ii, :], 0.125), r=[rpb], w=[r_gates[ii]])
                            continue
                        lg, m1, eq, lg2, m2, sel, ex, sm = (tk[:, 0:8], tk[:, 8:9], tk[:, 9:17], tk[:, 17:25], tk[:, 25:26], tk[:, 26:34],
                                                            tk[:, 34:42], tk[:, 42:43])
                        rr = [r_tk]
                        p.op('dve', lambda e, pb=pb: e.tensor_copy(out=lg, in_=pb[:, 0:8]), r=[rpb], w=rr)
                        p.op('dve', lambda e: e.tensor_reduce(out=m1, in_=lg, axis=AX.X, op=ALU.max), r=rr, w=rr)
                        p.op('dve', lambda e: e.tensor_scalar(out=eq, in0=lg, scalar1=m1, scalar2=-1e30, op0=ALU.is_equal, op1=ALU.mult), r=rr, w=rr)
                        p.op('dve', lambda e: e.tensor_tensor(out=lg2, in0=eq, in1=lg, op=ALU.add), r=rr, w=rr)
                        p.op('dve', lambda e: e.tensor_reduce(out=m2, in_=lg2, axis=AX.X, op=ALU.max), r=rr, w=rr)
                        p.op('dve', lambda e: e.tensor_scalar(out=sel, in0=lg, scalar1=m2, scalar2=None, op0=ALU.is_ge), r=rr, w=rr)
                        p.op('dve', lambda e: e.tensor_scalar(out=m1, in0=m1, scalar1=-1.0, scalar2=None, op0=ALU.mult), r=rr, w=rr)
                        p.op('act', lambda e: e.activation(out=ex, in_=lg, func=AF.Exp, bias=m1, scale=1.0), r=rr, w=rr)
                        p.op('dve', lambda e: e.tensor_tensor(out=ex, in0=ex, in1=sel, op=ALU.mult), r=rr, w=rr)
                        p.op('dve', lambda e: e.tensor_reduce(out=sm, in_=ex, axis=AX.X, op=ALU.add), r=rr, w=rr)
                        p.op('dve', lambda e: e.reciprocal(out=sm, in_=sm), r=rr, w=rr)
                        p.op('dve', lambda e, ii=ii: e.tensor_scalar(out=gates[:, ii, :], in0=ex, scalar1=sm, scalar2=None, op0=ALU.mult),
                             r=rr, w=[r_gates[ii]])
                pc = 0
                chunks = [(t0, min(512, T - t0)) for t0 in range(0, T, 512)]
                for ex_i in range(nexp):
                    if moe:
                        w_in_d = self.W['moe_w_in'][li, ex_i]
                        w_out_d = self.W['moe_w_out'][li, ex_i]
                    else:
                        w_in_d = self.W['ffn_w_in'][li]
                        w_out_d = self.W['ffn_w_out'][li]
                    for pj in range(npiece):
                        b = pc % 2
                        first = (pc == 0)
                        pc += 1
                        h0 = pj * HB
                        p.dma('pool', [(win[b][:, :, 0:HB], w_in_d[:, h0:h0 + HB].rearrange("(c p) n -> p c n", p=128)),
                                       (win[b][:, :, HB:2 * HB], w_in_d[:, H + h0:H + h0 + HB].rearrange("(c p) n -> p c n", p=128)),
                                       (wout[b][:, :, :], w_out_d[h0:h0 + HB, :].rearrange("(c p) n -> p c n", p=128))], w=[r_w[b]])
                        for ci, (t0, tn) in enumerate(chunks):
                            rh = [r_hT[i] for i in range(t0 // 128, (t0 + tn) // 128)]
                            hb = (pc + ci) % 2
                            for hc in range(HB // 128):
                                pg, rpg = self.next_ps()
                                pu, rpu = self.next_ps()
                                for c in range(8):
                                    p.op('pe', lambda e, c=c, hc=hc, pg=pg: e.matmul(pg[:, 0:tn], lhsT=win[b][:, c, hc * 128:(hc + 1) * 128],
                                                                                      rhs=hT[:, c, t0:t0 + tn], start=(c == 0), stop=(c == 7)),
                                         r=rh + [r_w[b]], w=[rpg])
                                for c in range(8):
                                    p.op('pe', lambda e, c=c, hc=hc, pu=pu: e.matmul(pu[:, 0:tn], lhsT=win[b][:, c, HB + hc * 128:HB + (hc + 1) * 128],
                                                                                      rhs=hT[:, c, t0:t0 + tn], start=(c == 0), stop=(c == 7)),
                                         r=rh + [r_w[b]], w=[rpu])
                                sb = hc % 2
                                p.op('act', lambda e, pg=pg, sb=sb: e.activation(out=sg[sb][:, 0:tn], in_=pg[:, 0:tn], func=AF.Silu),
                                     r=[rpg], w=[r_sg[sb]])
                                p.op('dve', lambda e, pu=pu, sb=sb, hc=hc, hb=hb: e.tensor_tensor(out=hid[hb][:, hc, 0:tn], in0=sg[sb][:, 0:tn],
                                                                                                   in1=pu[:, 0:tn], op=ALU.mult),
                                     r=[r_sg[sb], rpu], w=[r_hid[hb]])
                            for st in range(tn // 128):
                                ui_loc = t0 // 128 + st
                                for ch in range(2):
                                    po, rpo = self.next_ps()
                                    for hc in range(HB // 128):
                                        p.op('pe', lambda e, hc=hc, st=st, ch=ch, po=po, hb=hb: e.matmul(
                                            po[:, :], lhsT=hid[hb][:, hc, st * 128:(st + 1) * 128], rhs=wout[b][:, hc, ch * 512:(ch + 1) * 512],
                                            start=(hc == 0), stop=(hc == HB // 128 - 1)), r=[r_hid[hb], r_w[b]], w=[rpo])
                                    accv = acc[:, ui_loc, ch * 512:(ch + 1) * 512]
                                    if moe:
                                        gsc = gates[:, ui_loc, ex_i:ex_i + 1]
                                        rg = [r_gates[ui_loc]]
                                    else:
                                        gsc = 1.0
                                        rg = []
                                    if first:
                                        p.op('dve', lambda e, po=po, accv=accv, gsc=gsc: e.tensor_scalar(out=accv, in0=po[:, :], scalar1=gsc, scalar2=None,
                                                                                                     op0=ALU.mult), r=[rpo] + rg, w=[r_acc[ui_loc]])
                                    else:
                                        p.op('dve', lambda e, po=po, accv=accv, gsc=gsc: e.scalar_tensor_tensor(out=accv, in0=po[:, :], scalar=gsc, in1=accv,
                                                                                                            op0=ALU.mult, op1=ALU.add),
                                             r=[rpo] + rg, w=[r_acc[ui_loc]])
                for ii, ui in enumerate(hu):
                    u = self.units[ui]
                    b = ii % 2
                    src = u['src'] if self.first_touch else u['dst']
                    p.dma('sp', [(xt[b][:, :], src)], r=[u['reg']], w=[r_xt[b]])
                    g = u['g']
                    p.op('dve', lambda e, ii=ii, g=g: e.tensor_tensor(out=acc[:, ii, :], in0=acc[:, ii, :], in1=gbc[g][:, :], op=ALU.mult),
                         r=[r_gbc[g]], w=[r_acc[ii]])
                    p.op('dve', lambda e, ii=ii, b=b: e.tensor_tensor(out=acc[:, ii, :], in0=acc[:, ii, :], in1=xt[b][:, :], op=ALU.add),
                         r=[r_xt[b]], w=[r_acc[ii]])
                    p.dma('sp', [(u['dst'], acc[:, ii, :])], r=[r_acc[ii]], w=[u['reg']])
                p.barrier()
        self.first_touch = False

    def rwkv_phase(self, l, seqs):
        p, nc, W = self.p, self.nc, self.W
        NT = 256
        self.ps_lim = 7
        with ExitStack() as s:
            def sb(name, shape, dt):
                return p.sbuf(s, 'rw_' + name, shape, dt)
            r_wt = Reg()
            Wr, Wk, Wv, Wo = (sb(n, [128, 8, 1024], BF16) for n in ('Wr', 'Wk', 'Wv', 'Wo'))
            W1 = sb('W1', [128, 8, 2, 64], BF16)
            A1 = sb('A1', [128, 8, 2, 64], BF16)
            W2 = sb('W2', [64, 2, 1024], BF16)
            A2 = sb('A2', [64, 2, 1024], BF16)
            G1 = sb('G1', [128, 8, 128], BF16)
            G2 = sb('G2', [128, 1024], BF16)
            wrkv = W['rwkv_w_rkv']
            p.dma('pool', [(Wr[:, :, :], wrkv[0].rearrange("(c p) n -> p c n", p=128)),
                           (Wk[:, :, :], wrkv[1].rearrange("(c p) n -> p c n", p=128)),
                           (Wv[:, :, :], wrkv[2].rearrange("(c p) n -> p c n", p=128)),
                           (Wo[:, :, :], W['rwkv_w_o'].rearrange("(c p) n -> p c n", p=128))], w=[r_wt])
            prs = []
            for d in range(2):
                prs += [(W1[:, :, d, :], W['rwkv_w1'][d].rearrange("(c p) r -> p c r", p=128)),
                        (A1[:, :, d, :], W['rwkv_a1'][d].rearrange("(c p) r -> p c r", p=128)),
                        (W2[:, d, :], W['rwkv_w2'][d]), (A2[:, d, :], W['rwkv_a2'][d])]
            prs += [(G1[:, :, :], W['rwkv_g1'].rearrange("(c p) r -> p c r", p=128)), (G2[:, :], W['rwkv_g2'])]
            p.dma('pool', prs, w=[r_wt])
            mixS = sb('mixS', [128, 6, 8], F32)
            hmv = sb('hmv', [64, 8, 16], F32)
            rows = sb('rows', [128, 2, 1024], F32)
            mask4 = sb('mask4', [128, 2, 512], F32)
            maskN = sb('maskN', [128, 4, 128], F32)
            r_c = Reg()
            p.dma('sp', [(mixS[:, :, :], self.rw_mix), (hmv[:, :, :], self.rw_hm), (rows[:, :, :], self.rw_rows),
                         (mask4[:, :, :], self.rw_mask4), (maskN[:, :, :], self.rw_maskN)], w=[r_c])
            omk = sb('omk', [64, 16], F32)
            p.op('dve', lambda e: e.tensor_scalar(out=omk[:, :], in0=hmv[:, 5, :], scalar1=-1.0, scalar2=1.0, op0=ALU.mult, op1=ALU.add), r=[r_c], w=[r_c])
            ones64 = sb('ones64', [64, 64], BF16)
            onesF = sb('onesF', [64, NT], F32)
            p.op('dve', lambda e: e.memset(ones64[:, :], 1.0), w=[r_c])
            p.op('dve', lambda e: e.memset(onesF[:, :], 1.0), w=[r_c])
            gbc = sb('gbc', [128, 1024], F32)
            r_gbc = Reg()
            diag = sb('diag', [128, 128], F32)
            r_diag = Reg()
            S32 = sb('S32', [64, 16, 2, 64], F32)
            r_S = [[Reg() for _ in range(2)] for _ in range(16)]
            hTt = sb('hTt', [128, 8, NT + 2], BF16); r_hTt = Reg()
            xx = sb('xx', [128, 8, NT], BF16); r_xx = Reg()
            xi = [sb('xi%d' % i, [128, 8, NT], BF16) for i in range(2)]; r_xi = [Reg(), Reg()]
            tmpx = xi[1]; r_tmpx = r_xi[1]
            rH = sb('rH', [64, 16, NT], BF16); r_rH = Reg()
            kH = sb('kH', [64, 16, NT], BF16); r_kH = Reg()
            Vtok = sb('Vtok', [128, NT // 128, 1024], BF16); r_V = Reg()
            hw = sb('hw', [64, NT], BF16); ha = sb('ha', [64, NT], BF16); hg = sb('hg', [128, NT], BF16)
            r_hw, r_ha, r_hg = Reg(), Reg(), Reg()
            ytok = sb('ytok', [128, NT // 128, 1024], F32); r_y = Reg()
            bon = sb('bon', [128, NT // 128, 16], F32); r_bon = Reg()
            TF = ['lw', 'aa', 'kkr', 'nr', 'kk', 't1', 'kd', 'bb', 'G', 'Dd', 'Dl', 'E1', 'E2']
            TB = ['sq', 'pr', 'AT', 'RT', 'BT', 'KT']
            tset = []
            for i in range(1):
                dd = {n: sb('%s%d' % (n, i), [64, NT], F32) for n in TF}
                dd.update({n: sb('%s%d' % (n, i), [128 if n in ('AT', 'RT', 'BT', 'KT') else 64, NT], BF16) for n in TB})
                for n in ('AT', 'RT', 'BT', 'KT'):
                    p.op('dve', lambda e, t=dd[n]: e.memset(t[64:128, :], 0.0), w=[r_c])
                dd['sc'] = sb('sc%d' % i, [64, NT // 128, 3], F32)
                dd['r'] = {n: Reg() for n in TF + TB + ['sc']}
                tset.append(dd)
            uset = []
            for i in range(2):
                dd = dict(BK=sb('BK%d' % i, [128, 128], BF16), Am=sb('Am%d' % i, [128, 512], BF16), Nm=sb('Nm%d' % i, [128, 128], BF16),
                          TT=[sb('TT%d_%d' % (i, k), [128, 128], BF16) for k in range(2)],
                          PP=[sb('PP%d_%d' % (i, k), [128, 256], BF16) for k in range(2)],
                          Sbf=sb('Sbf%d' % i, [128, 64], BF16), Xsb=sb('Xsb%d' % i, [128, 64], BF16), Usb=sb('Usb%d' % i, [128, 64], BF16),
                          tmpS=sb('tmpS%d' % i, [64, 64], F32), NdT=sb('NdT%d' % i, [128, 128], BF16), NTo=sb('NTo%d' % i, [128, 128], BF16), acc=sb('acc%d' % i, [128, 64], BF16))
                dd['r'] = {n: Reg() for n in ['BK', 'Am', 'Nm', 'TT0', 'TT1', 'PP0', 'PP1', 'Sbf', 'Xsb', 'Usb', 'tmpS', 'NdT', 'NTo', 'acc']}
                p.op('dve', lambda e, t=dd['Sbf']: e.memset(t[64:128, :], 0.0), w=[r_c])
                uset.append(dd)
            xt = sb('xt', [128, 1024], F32); r_xt = Reg()
            yf = sb('yf', [128, 1024], F32); r_yf = Reg()
            bf_ = sb('bf', [128, 16], F32); r_bf = Reg()
            yj = sb('yj', [128, 1024], F32); r_yj = Reg()
            ysq = sb('ysq', [128, 1024], F32); r_ysq = Reg()
            st = sb('st', [128, 4, 16], F32); r_st = Reg()
            gTs = sb('gTs', [128, 8, 128], BF16); r_gTs = Reg()
            ygT = sb('ygT', [128, 8, 128], BF16); r_ygT = Reg()
            osb = yf; r_osb = r_yf
            ss = sb('ss', [128, 2], F32)
            scr = (ss, yj, ysq, Reg(), r_yj, r_ysq)
            hts = sb('hts', [128, 8, 128], BF16); r_hts = Reg()
            zer = sb('zer', [128, 8, 1], BF16); r_zer = Reg()
            p.op('dve', lambda e: e.memset(zer[:, :, :], 0.0), w=[r_zer])
            stio = sb('stio', [64, 64], F32); r_stio = Reg()

            for si, sq_ in enumerate(seqs):
                L, g, units = sq_['L'], sq_['g'], sq_['units']
                hTd, yfd, bfd = sq_['hTd'], sq_['yfd'], sq_['bfd']
                r_hTd, r_yfd, r_bfd = Reg(), Reg(), Reg()
                self.gate_bc(gbc, r_gbc, l, 2, g, diag, r_diag)
                p.dma('sp', [(hTd[:, :, 0:1], zer[:, :, :]), (hTd[:, :, L + 1:L + 2], zer[:, :, :])], r=[r_zer], w=[r_hTd], allow_slow_non_contiguous=True)
                for i, u in enumerate(units):
                    src = u['src'] if self.first_touch else u['dst']
                    p.dma('sp', [(xt[:, :], src)], r=[u['reg']], w=[r_xt])
                    self.norm_hT(xt[:, :], r_xt, l, 0, g, lambda c: hts[:, c, :], r_hts, scr)
                    p.dma('sp', [(hTd[:, :, 1 + i * 128:1 + (i + 1) * 128], hts[:, :, :])], r=[r_hts], w=[r_hTd])
                for h in range(16):
                    for d in range(2):
                        if sq_['s0'] is None:
                            p.op('dve', lambda e, h=h, d=d: e.memset(S32[:, h, d, :], 0.0), w=[r_S[h][d]])
                        else:
                            p.dma('sp', [(stio[:, :], sq_['s0'][d, h])], w=[r_stio])
                            pb, rpb = self.next_ps()
                            p.op('pe', lambda e, pb=pb: e.transpose(out=pb[0:64, 0:64], in_=stio[:, :], identity=self.identF[0:64, 0:64]),
                                 r=[r_stio, self.r_const], w=[rpb])
                            p.op('act', lambda e, pb=pb, h=h, d=d: e.activation(out=S32[:, h, d, :], in_=pb[0:64, 0:64], func=AF.Copy),
                                 r=[rpb], w=[r_S[h][d]])
                ntile = L // NT
                for d in range(2):
                    tiles = list(range(ntile)) if d == 0 else list(range(ntile - 1, -1, -1))
                    for ti in tiles:
                        t0 = ti * NT
                        n = NT
                        nj = n // 128
                        p.dma('sp', [(hTt[:, :, :], hTd[:, :, t0:t0 + n + 2])], r=[r_hTd], w=[r_hTt])
                        p.op('dve', lambda e: e.tensor_tensor(out=tmpx[:, :, :], in0=hTt[:, :, 0:n], in1=hTt[:, :, 2:n + 2], op=ALU.add),
                             r=[r_hTt], w=[r_tmpx])
                        p.op('dve', lambda e: e.scalar_tensor_tensor(out=xx[:, :, :], in0=tmpx[:, :, :], scalar=0.5, in1=hTt[:, :, 1:n + 1],
                                                                      op0=ALU.mult, op1=ALU.subtract), r=[r_tmpx, r_hTt], w=[r_xx])
                        vi = [0]

                        def variant(i):
                            b = vi[0] % 2
                            vi[0] += 1
                            for c in range(8):
                                eng = 'dve' if c % 2 == 0 else 'dve'
                                p.op(eng, lambda e, c=c, b=b, i=i: e.scalar_tensor_tensor(
                                    out=xi[b][:, c, :], in0=xx[:, c, :], scalar=mixS[:, i, c:c + 1], in1=hTt[:, c, 1:n + 1],
                                    op0=ALU.mult, op1=ALU.add), r=[r_xx, r_hTt, r_c], w=[r_xi[b]])
                            return xi[b], r_xi[b]

                        for (i, Wm, dst, rdst) in ((0, Wr, rH, r_rH), (2, Wk, kH, r_kH)):
                            xb, rxb = variant(i)
                            for h in range(16):
                                pb, rpb = self.next_ps()
                                for c in range(8):
                                    p.op('pe', lambda e, c=c, h=h, pb=pb, xb=xb, Wm=Wm: e.matmul(
                                        pb[0:64, 0:n], lhsT=Wm[:, c, h * 64:(h + 1) * 64], rhs=xb[:, c, :], start=(c == 0), stop=(c == 7)),
                                        r=[rxb, r_wt], w=[rpb])
                                p.op('act', lambda e, h=h, pb=pb, dst=dst: e.activation(out=dst[:, h, :], in_=pb[0:64, 0:n], func=AF.Copy),
                                     r=[rpb], w=[rdst])
                        xb, rxb = variant(3)
                        for j in range(nj):
                            for hf in range(2):
                                pb, rpb = self.next_ps()
                                for c in range(8):
                                    p.op('pe', lambda e, c=c, j=j, hf=hf, pb=pb, xb=xb: e.matmul(
                                        pb[:, :], lhsT=xb[:, c, j * 128:(j + 1) * 128], rhs=Wv[:, c, hf * 512:(hf + 1) * 512],
                                        start=(c == 0), stop=(c == 7)), r=[rxb, r_wt], w=[rpb])
                                p.op('act', lambda e, j=j, hf=hf, pb=pb: e.activation(out=Vtok[:, j, hf * 512:(hf + 1) * 512], in_=pb[:, :], func=AF.Copy),
                                     r=[rpb], w=[r_V])
                        for (i, Wm, dst, rdst, fn) in ((1, W1, hw, r_hw, AF.Tanh), (4, A1, ha, r_ha, AF.Copy)):
                            xb, rxb = variant(i)
                            pb, rpb = self.next_ps()
                            for c in range(8):
                                p.op('pe', lambda e, c=c, pb=pb, xb=xb, Wm=Wm: e.matmul(pb[0:64, 0:n], lhsT=Wm[:, c, d, :], rhs=xb[:, c, :],
                                                                                         start=(c == 0), stop=(c == 7)), r=[rxb, r_wt], w=[rpb])
                            p.op('act', lambda e, pb=pb, dst=dst, fn=fn: e.activation(out=dst[:, :], in_=pb[0:64, 0:n], func=fn), r=[rpb], w=[rdst])
                        if d == 1:
                            xb, rxb = variant(5)
                            pb, rpb = self.next_ps()
                            for c in range(8):
                                p.op('pe', lambda e, c=c, pb=pb, xb=xb: e.matmul(pb[:, 0:n], lhsT=G1[:, c, :], rhs=xb[:, c, :],
                                                                                 start=(c == 0), stop=(c == 7)), r=[rxb, r_wt], w=[rpb])
                            p.op('act', lambda e, pb=pb: e.activation(out=hg[:, :], in_=pb[:, 0:n], func=AF.Sigmoid), r=[rpb], w=[r_hg])
                        pbon, rpbon = self.ps[7], self.r_ps[7]
                        for h in range(16):
                            T = tset[0]
                            R = T['r']
                            hs = slice(h * 64, (h + 1) * 64)
                            pb, rpb = self.next_ps()
                            p.op('pe', lambda e, pb=pb, hs=hs: e.matmul(pb[0:64, 0:n], lhsT=W2[:, d, hs], rhs=hw[:, :], start=True, stop=True),
                                 r=[r_hw, r_wt], w=[rpb])
                            p.op('act', lambda e, pb=pb, T=T, h=h: e.activation(out=T['lw'][:, :], in_=pb[0:64, 0:n], func=AF.Sigmoid,
                                                                                 bias=hmv[:, 0 + d, h:h + 1], scale=1.0), r=[rpb, r_c], w=[R['lw']])
                            p.op('pool', lambda e, T=T: e.tensor_scalar(out=T['lw'][:, :], in0=T['lw'][:, :], scalar1=-0.6065306597126334, scalar2=None,
                                                                         op0=ALU.mult), r=[R['lw']], w=[R['lw']])
                            pb, rpb = self.next_ps()
                            p.op('pe', lambda e, pb=pb, hs=hs: e.matmul(pb[0:64, 0:n], lhsT=A2[:, d, hs], rhs=ha[:, :], start=True, stop=True),
                                 r=[r_ha, r_wt], w=[rpb])
                            p.op('act', lambda e, pb=pb, T=T, h=h: e.activation(out=T['aa'][:, :], in_=pb[0:64, 0:n], func=AF.Sigmoid,
                                                                                 bias=hmv[:, 2 + d, h:h + 1], scale=1.0), r=[rpb, r_c], w=[R['aa']])
                            p.op('dve', lambda e, T=T, h=h: e.tensor_scalar(out=T['kkr'][:, :], in0=kH[:, h, :], scalar1=hmv[:, 4, h:h + 1], scalar2=None,
                                                                             op0=ALU.mult), r=[r_kH, r_c], w=[R['kkr']])
                            p.op('pool', lambda e, T=T: e.tensor_tensor(out=T['sq'][:, :], in0=T['kkr'][:, :], in1=T['kkr'][:, :], op=ALU.mult),
                                 r=[R['kkr']], w=[R['sq']])
                            pb, rpb = self.next_ps()
                            p.op('pe', lambda e, pb=pb, T=T: e.matmul(pb[0:64, 0:n], lhsT=ones64[:, :], rhs=T['sq'][:, :], start=True, stop=True),
                                 r=[R['sq'], r_c], w=[rpb])
                            p.op('act', lambda e, pb=pb, T=T: e.activation(out=T['nr'][:, :], in_=pb[0:64, 0:n], func=AF.Sqrt), r=[rpb], w=[R['nr']])
                            p.op('dve', lambda e, T=T: e.tensor_scalar(out=T['nr'][:, :], in0=T['nr'][:, :], scalar1=1e-12, scalar2=None, op0=ALU.max),
                                 r=[R['nr']], w=[R['nr']])
                            p.op('dve', lambda e, T=T: e.reciprocal(out=T['nr'][:, :], in_=T['nr'][:, :]), r=[R['nr']], w=[R['nr']])
                            p.op('dve', lambda e, T=T: e.tensor_tensor(out=T['kk'][:, :], in0=T['kkr'][:, :], in1=T['nr'][:, :], op=ALU.mult),
                                 r=[R['kkr'], R['nr']], w=[R['kk']])
                            p.op('dve', lambda e, T=T, h=h: e.tensor_scalar(out=T['t1'][:, :], in0=T['aa'][:, :], scalar1=hmv[:, 5, h:h + 1],
                                                                             scalar2=omk[:, h:h + 1], op0=ALU.mult, op1=ALU.add),
                                 r=[R['aa'], r_c], w=[R['t1']])
                            p.op('dve', lambda e, T=T, h=h: e.tensor_tensor(out=T['kd'][:, :], in0=T['t1'][:, :], in1=kH[:, h, :], op=ALU.mult),
                                 r=[R['t1'], r_kH], w=[R['kd']])
                            p.op('pool', lambda e, T=T: e.tensor_tensor(out=T['bb'][:, :], in0=T['kk'][:, :], in1=T['aa'][:, :], op=ALU.mult),
                                 r=[R['kk'], R['aa']], w=[R['bb']])
                            p.op('dve', lambda e, T=T, h=h: e.scalar_tensor_tensor(out=T['pr'][:, :], in0=T['kd'][:, :], scalar=hmv[:, 6, h:h + 1],
                                                                                    in1=rH[:, h, :], op0=ALU.mult, op1=ALU.mult),
                                 r=[R['kd'], r_rH, r_c], w=[R['pr']])
                            for j in range(nj):
                                p.op('pe', lambda e, T=T, j=j, h=h: e.matmul(pbon[:, j * 16 + h:j * 16 + h + 1], lhsT=T['pr'][:, j * 128:(j + 1) * 128],
                                                                             rhs=ones64[:, 0:1], start=True, stop=True), r=[R['pr'], r_c], w=[rpbon])
                            p.op('dve', lambda e, T=T: e.tensor_tensor_scan(out=T['G'][:, :], data0=onesF[:, 0:n], data1=T['lw'][:, :], initial=0.0,
                                                                             op0=ALU.mult, op1=ALU.add), r=[R['lw'], r_c], w=[R['G']])
                            for j in range(nj):
                                js = slice(j * 128, (j + 1) * 128)
                                p.op('dve', lambda e, T=T, j=j, js=js: e.tensor_scalar(out=T['Dd'][:, js], in0=T['G'][:, js],
                                                                                       scalar1=T['G'][:, j * 128 + 63:j * 128 + 64], scalar2=None,
                                                                                       op0=ALU.subtract), r=[R['G']], w=[R['Dd']])
                                if j == 0:
                                    p.op('pool', lambda e, T=T, j=j: e.tensor_copy(out=T['sc'][:, j, 0:1], in_=T['G'][:, 63:64]), r=[R['G']], w=[R['sc']])
                                else:
                                    p.op('pool', lambda e, T=T, j=j: e.tensor_tensor(out=T['sc'][:, j, 0:1], in0=T['G'][:, j * 128 + 63:j * 128 + 64],
                                                                                     in1=T['G'][:, j * 128 - 1:j * 128], op=ALU.subtract),
                                         r=[R['G']], w=[R['sc']])
                                p.op('pool', lambda e, T=T, j=j: e.tensor_tensor(out=T['sc'][:, j, 1:2], in0=T['G'][:, j * 128 + 127:j * 128 + 128],
                                                                                 in1=T['G'][:, j * 128 + 63:j * 128 + 64], op=ALU.subtract),
                                     r=[R['G']], w=[R['sc']])
                                p.op('pool', lambda e, T=T, j=j: e.tensor_tensor(out=T['sc'][:, j, 2:3], in0=T['sc'][:, j, 0:1], in1=T['sc'][:, j, 1:2],
                                                                                 op=ALU.add), r=[R['sc']], w=[R['sc']])
                            p.op('act', lambda e, T=T: e.activation(out=T['sc'][:, :, :], in_=T['sc'][:, :, :], func=AF.Exp), r=[R['sc']], w=[R['sc']])
                            p.op('pool', lambda e, T=T: e.tensor_tensor(out=T['Dl'][:, :], in0=T['Dd'][:, :], in1=T['lw'][:, :], op=ALU.subtract),
                                 r=[R['Dd'], R['lw']], w=[R['Dl']])
                            if d == 0:
                                p.op('act', lambda e, T=T: e.activation(out=T['E1'][:, :], in_=T['Dl'][:, :], func=AF.Exp), r=[R['Dl']], w=[R['E1']])
                                p.op('dve', lambda e, T=T: e.scalar_tensor_tensor(out=T['AT'][0:64, :], in0=T['kk'][:, :], scalar=-1.0, in1=T['E1'][:, :],
                                                                                   op0=ALU.mult, op1=ALU.mult), r=[R['kk'], R['E1']], w=[R['AT']])
                                p.op('act', lambda e, T=T: e.activation(out=T['E2'][:, :], in_=T['Dd'][:, :], func=AF.Exp), r=[R['Dd']], w=[R['E2']])
                                p.op('dve', lambda e, T=T, h=h: e.tensor_tensor(out=T['RT'][0:64, :], in0=rH[:, h, :], in1=T['E2'][:, :], op=ALU.mult),
                                     r=[r_rH, R['E2']], w=[R['RT']])
                                p.op('act', lambda e, T=T: e.activation(out=T['E1'][:, :], in_=T['Dd'][:, :], func=AF.Exp, scale=-1.0), r=[R['Dd'], R['AT']], w=[R['E1']])
                                p.op('dve', lambda e, T=T: e.tensor_tensor(out=T['BT'][0:64, :], in0=T['bb'][:, :], in1=T['E1'][:, :], op=ALU.mult),
                                     r=[R['bb'], R['E1']], w=[R['BT']])
                                p.op('pool', lambda e, T=T: e.tensor_tensor(out=T['KT'][0:64, :], in0=T['kd'][:, :], in1=T['E1'][:, :], op=ALU.mult),
                                     r=[R['kd'], R['E1']], w=[R['KT']])
                            else:
                                p.op('act', lambda e, T=T: e.activation(out=T['E1'][:, :], in_=T['Dd'][:, :], func=AF.Exp, scale=-1.0), r=[R['Dd']], w=[R['E1']])
                                p.op('dve', lambda e, T=T: e.scalar_tensor_tensor(out=T['AT'][0:64, :], in0=T['kk'][:, :], scalar=-1.0, in1=T['E1'][:, :],
                                                                                   op0=ALU.mult, op1=ALU.mult), r=[R['kk'], R['E1']], w=[R['AT']])
                                p.op('act', lambda e, T=T: e.activation(out=T['E2'][:, :], in_=T['Dl'][:, :], func=AF.Exp, scale=-1.0), r=[R['Dl']], w=[R['E2']])
                                p.op('dve', lambda e, T=T, h=h: e.tensor_tensor(out=T['RT'][0:64, :], in0=rH[:, h, :], in1=T['E2'][:, :], op=ALU.mult),
                                     r=[r_rH, R['E2']], w=[R['RT']])
                                p.op('act', lambda e, T=T: e.activation(out=T['E1'][:, :], in_=T['Dl'][:, :], func=AF.Exp), r=[R['Dl'], R['AT']], w=[R['E1']])
                                p.op('dve', lambda e, T=T: e.tensor_tensor(out=T['BT'][0:64, :], in0=T['bb'][:, :], in1=T['E1'][:, :], op=ALU.mult),
                                     r=[R['bb'], R['E1']], w=[R['BT']])
                                p.op('pool', lambda e, T=T: e.tensor_tensor(out=T['KT'][0:64, :], in0=T['kd'][:, :], in1=T['E1'][:, :], op=ALU.mult),
                                     r=[R['kd'], R['E1']], w=[R['KT']])
                            rATs = [R['AT'], R['RT'], R['BT'], R['KT']]
                            for j in (range(nj) if d == 0 else range(nj - 1, -1, -1)):
                                U = uset[j % 2]
                                UR = U['r']
                                js = slice(j * 128, (j + 1) * 128)
                                em = T['sc'][:, j, 0:1] if d == 0 else T['sc'][:, j, 1:2]
                                e2 = T['sc'][:, j, 1:2] if d == 0 else T['sc'][:, j, 0:1]
                                e1 = T['sc'][:, j, 2:3]
                                pb, rpb = self.next_ps()
                                pv = pb[:, :].bitcast(BF16)
                                p.op('pe', lambda e, pv=pv, T=T, js=js: e.transpose(out=pv[:, 0:64], in_=T['BT'][0:64, js], identity=self.identB[0:64, 0:64]),
                                     r=[R['BT'], self.r_const], w=[rpb])
                                p.op('pe', lambda e, pv=pv, T=T, js=js: e.transpose(out=pv[:, 64:128], in_=T['KT'][0:64, js], identity=self.identB[0:64, 0:64]),
                                     r=[R['KT'], self.r_const], w=[rpb])
                                p.op('act', lambda e, pv=pv, U=U: e.activation(out=U['BK'][:, :], in_=pv[:, 0:128], func=AF.Copy), r=[rpb], w=[UR['BK']])
                                pb, rpb = self.next_ps()
                                for q, (la, ra) in enumerate((('BT', 'AT'), ('BT', 'RT'), ('KT', 'AT'), ('KT', 'RT'))):
                                    p.op('pe', lambda e, pb=pb, q=q, la=la, ra=ra, T=T, js=js: e.matmul(
                                        pb[:, q * 128:(q + 1) * 128], lhsT=T[la][:, js], rhs=T[ra][:, js], start=True, stop=True), r=rATs, w=[rpb])
                                p.op('dve', lambda e, pb=pb, U=U: e.tensor_tensor(out=U['Am'][:, :], in0=pb[:, :], in1=mask4[:, d, :], op=ALU.mult),
                                     r=[rpb, r_c], w=[UR['Am']])
                                pb, rpb = self.next_ps()
                                p.op('pe', lambda e, pb=pb, T=T, js=js: e.matmul(pb[:, 0:128], lhsT=T['AT'][:, js], rhs=T['BT'][:, js], start=True, stop=True),
                                     r=rATs, w=[rpb])
                                p.op('dve', lambda e, pb=pb, U=U: e.tensor_tensor(out=U['Nm'][:, :], in0=pb[:, 0:128], in1=maskN[:, d, :], op=ALU.mult),
                                     r=[rpb, r_c], w=[UR['Nm']])
                                p.op('pool', lambda e, U=U: e.tensor_tensor(out=U['NdT'][:, :], in0=U['Am'][:, 0:128], in1=maskN[:, 2, :], op=ALU.mult),
                                     r=[UR['Am'], r_c], w=[UR['NdT']])
                                p.op('pool', lambda e, U=U: e.tensor_tensor(out=U['NTo'][:, :], in0=U['Am'][:, 0:128], in1=maskN[:, 3, :], op=ALU.mult),
                                     r=[UR['Am'], r_c], w=[UR['NTo']])
                                p.op('pool', lambda e, U=U: e.tensor_tensor(out=U['TT'][0][:, :], in0=U['NdT'][:, :], in1=self.identB[:, :], op=ALU.add),
                                     r=[UR['NdT'], self.r_const], w=[UR['TT0']])
                                Pm, rP = U['Nm'][:, :], UR['Nm']
                                PT, rPT = U['NdT'][:, :], UR['NdT']
                                tcur = 0
                                NIT = 4
                                for it in range(NIT):
                                    pb, rpb = self.next_ps()
                                    PPt, rPP = U['PP'][it % 2], UR['PP%d' % (it % 2)]
                                    p.op('pe', lambda e, pb=pb, Pm=Pm, PT=PT: e.matmul(pb[:, 0:128], lhsT=PT, rhs=Pm, start=True, stop=True), r=[rP, rPT], w=[rpb])
                                    if it < NIT - 1:
                                        p.op('pe', lambda e, pb=pb, Pm=Pm, PT=PT: e.matmul(pb[:, 128:256], lhsT=Pm, rhs=PT, start=True, stop=True), r=[rP, rPT], w=[rpb])
                                    p.op('act', lambda e, pb=pb, PPt=PPt: e.activation(out=PPt[:, :], in_=pb[:, 0:256], func=AF.Copy), r=[rpb], w=[rPP])
                                    pb2, rpb2 = self.next_ps()
                                    TTc, rTTc = U['TT'][tcur], UR['TT%d' % tcur]
                                    TTn, rTTn = U['TT'][1 - tcur], UR['TT%d' % (1 - tcur)]
                                    p.op('pe', lambda e, pb2=pb2, PPt=PPt, TTc=TTc: e.matmul(pb2[:, 0:128], lhsT=PPt[:, 0:128], rhs=TTc[:, :], start=True, stop=True),
                                         r=[rPP, rTTc], w=[rpb2])
                                    p.op('dve', lambda e, pb2=pb2, TTc=TTc, TTn=TTn: e.tensor_tensor(out=TTn[:, :], in0=pb2[:, 0:128], in1=TTc[:, :], op=ALU.add),
                                         r=[rpb2, rTTc], w=[rTTn])
                                    tcur = 1 - tcur
                                    Pm, rP = PPt[:, 0:128], rPP
                                    PT, rPT = PPt[:, 128:256], rPP
                                TTf, rTTf = U['TT'][tcur], UR['TT%d' % tcur]
                                rS = r_S[h][d]
                                p.op('dve', lambda e, U=U, em=em: e.tensor_scalar(out=U['Sbf'][0:64, :], in0=S32[:, h, d, :], scalar1=em, scalar2=None, op0=ALU.mult),
                                     r=[rS, R['sc']], w=[UR['Sbf']])
                                pb, rpb = self.next_ps()
                                p.op('pe', lambda e, pb=pb, T=T, U=U, js=js: e.matmul(pb[:, 0:64], lhsT=T['AT'][:, js], rhs=U['Sbf'][:, :], start=True, stop=False),
                                     r=[R['AT'], UR['Sbf']], w=[rpb])
                                p.op('pe', lambda e, pb=pb, U=U, j=j, hs=hs: e.matmul(pb[:, 0:64], lhsT=U['Am'][:, 256:384], rhs=Vtok[:, j, hs], start=False, stop=True),
                                     r=[UR['Am'], r_V], w=[rpb])
                                p.op('act', lambda e, pb=pb, U=U: e.activation(out=U['Xsb'][:, :], in_=pb[:, 0:64], func=AF.Copy), r=[rpb], w=[UR['Xsb']])
                                pb, rpb = self.next_ps()
                                p.op('pe', lambda e, pb=pb, TTf=TTf, U=U: e.matmul(pb[:, 0:64], lhsT=TTf[:, :], rhs=U['Xsb'][:, :], start=True, stop=True),
                                     r=[rTTf, UR['Xsb']], w=[rpb])
                                p.op('act', lambda e, pb=pb, U=U: e.activation(out=U['Usb'][:, :], in_=pb[:, 0:64], func=AF.Copy), r=[rpb], w=[UR['Usb']])
                                for sweep in range(3):
                                    pb, rpb = self.next_ps()
                                    p.op('pe', lambda e, pb=pb, U=U: e.matmul(pb[:, 0:64], lhsT=U['NTo'][:, :], rhs=U['Usb'][:, :], start=True, stop=True),
                                         r=[UR['NTo'], UR['Usb']], w=[rpb])
                                    p.op('dve', lambda e, pb=pb, U=U: e.tensor_tensor(out=U['acc'][:, :], in0=pb[:, 0:64], in1=U['Xsb'][:, :], op=ALU.add),
                                         r=[rpb, UR['Xsb']], w=[UR['acc']])
                                    pb, rpb = self.next_ps()
                                    p.op('pe', lambda e, pb=pb, TTf=TTf, U=U: e.matmul(pb[:, 0:64], lhsT=TTf[:, :], rhs=U['acc'][:, :], start=True, stop=True),
                                         r=[rTTf, UR['acc']], w=[rpb])
                                    p.op('act', lambda e, pb=pb, U=U: e.activation(out=U['Usb'][:, :], in_=pb[:, 0:64], func=AF.Copy), r=[rpb], w=[UR['Usb']])
                                pb, rpb = self.next_ps()
                                p.op('pe', lambda e, pb=pb, T=T, U=U, js=js: e.matmul(pb[:, 0:64], lhsT=T['RT'][:, js], rhs=U['Sbf'][:, :], start=True, stop=False),
                                     r=[R['RT'], UR['Sbf']], w=[rpb])
                                p.op('pe', lambda e, pb=pb, U=U: e.matmul(pb[:, 0:64], lhsT=U['Am'][:, 128:256], rhs=U['Usb'][:, :], start=False, stop=False),
                                     r=[UR['Am'], UR['Usb']], w=[rpb])
                                p.op('pe', lambda e, pb=pb, U=U, j=j, hs=hs: e.matmul(pb[:, 0:64], lhsT=U['Am'][:, 384:512], rhs=Vtok[:, j, hs], start=False, stop=True),
                                     r=[UR['Am'], r_V], w=[rpb])
                                p.op('act', lambda e, pb=pb, j=j, hs=hs: e.activation(out=ytok[:, j, hs], in_=pb[:, 0:64], func=AF.Copy), r=[rpb], w=[r_y])
                                pb, rpb = self.next_ps()
                                p.op('pe', lambda e, pb=pb, U=U: e.matmul(pb[0:64, 0:64], lhsT=U['BK'][:, 0:64], rhs=U['Usb'][:, :], start=True, stop=False),
                                     r=[UR['BK'], UR['Usb']], w=[rpb])
                                p.op('pe', lambda e, pb=pb, U=U, j=j, hs=hs: e.matmul(pb[0:64, 0:64], lhsT=U['BK'][:, 64:128], rhs=Vtok[:, j, hs], start=False, stop=True),
                                     r=[UR['BK'], r_V], w=[rpb])
                                p.op('dve', lambda e, U=U, e1=e1: e.tensor_scalar(out=U['tmpS'][:, :], in0=S32[:, h, d, :], scalar1=e1, scalar2=None, op0=ALU.mult),
                                     r=[rS, R['sc']], w=[UR['tmpS']])
                                p.op('dve', lambda e, pb=pb, U=U, e2=e2: e.scalar_tensor_tensor(out=S32[:, h, d, :], in0=pb[0:64, 0:64], scalar=e2, in1=U['tmpS'][:, :],
                                                                                               op0=ALU.mult, op1=ALU.add), r=[rpb, UR['tmpS'], R['sc']], w=[rS])
                        p.op('act', lambda e, pbon=pbon: e.activation(out=bon[:, :, :], in_=pbon[:, 0:nj * 16], func=AF.Copy), r=[rpbon], w=[r_bon])
                        if d == 0:
                            p.dma('sp', [(yfd[t0:t0 + n, :].rearrange("(j p) f -> p j f", p=128), ytok[:, :, :]),
                                         (bfd[t0:t0 + n, :].rearrange("(j p) f -> p j f", p=128), bon[:, :, :])], r=[r_y, r_bon], w=[r_yfd, r_bfd])
                        else:
                            for j in range(nj):
                                u = units[(t0 // 128) + j]
                                js = slice(j * 128, (j + 1) * 128)
                                tt = t0 + j * 128
                                p.dma('sp', [(yf[:, :], yfd[tt:tt + 128, :]), (bf_[:, :], bfd[tt:tt + 128, :])], r=[r_yfd, r_bfd], w=[r_yf, r_bf])
                                src = u['src'] if self.first_touch else u['dst']
                                p.dma('sp', [(xt[:, :], src)], r=[u['reg']], w=[r_xt])
                                p.op('dve', lambda e, j=j: e.tensor_tensor(out=yj[:, :], in0=ytok[:, j, :], in1=yf[:, :], op=ALU.add), r=[r_y, r_yf], w=[r_yj])
                                p.op('pool', lambda e, j=j: e.tensor_tensor(out=bf_[:, :], in0=bf_[:, :], in1=bon[:, j, :], op=ALU.add), r=[r_bon], w=[r_bf])
                                yv = yj[:, :].rearrange("p (h k) -> p h k", k=64)
                                sqv = ysq[:, :].rearrange("p (h k) -> p h k", k=64)
                                p.op('act', lambda e: e.activation(out=ysq[:, :], in_=yj[:, :], func=AF.Square), r=[r_yj], w=[r_ysq])
                                p.op('dve', lambda e, yv=yv: e.tensor_reduce(out=st[:, 0, :], in_=yv, axis=AX.X, op=ALU.add), r=[r_yj], w=[r_st])
                                p.op('dve', lambda e, sqv=sqv: e.tensor_reduce(out=st[:, 1, :], in_=sqv, axis=AX.X, op=ALU.add), r=[r_ysq], w=[r_st])
                                p.op('dve', lambda e: e.tensor_scalar(out=st[:, 0, :], in0=st[:, 0, :], scalar1=1.0 / 64, scalar2=None, op0=ALU.mult), r=[r_st], w=[r_st])
                                p.op('dve', lambda e: e.tensor_tensor(out=st[:, 2, :], in0=st[:, 0, :], in1=st[:, 0, :], op=ALU.mult), r=[r_st], w=[r_st])
                                p.op('dve', lambda e: e.scalar_tensor_tensor(out=st[:, 1, :], in0=st[:, 1, :], scalar=1.0 / 64, in1=st[:, 2, :],
                                                                              op0=ALU.mult, op1=ALU.subtract), r=[r_st], w=[r_st])
                                p.op('act', lambda e: e.activation(out=st[:, 1, :], in_=st[:, 1, :], func=AF.Sqrt, bias=64e-5, scale=1.0), r=[r_st], w=[r_st])
                                p.op('dve', lambda e: e.reciprocal(out=st[:, 1, :], in_=st[:, 1, :]), r=[r_st], w=[r_st])
                                mub = st[:, 0, :].unsqueeze(2).to_broadcast([128, 16, 64])
                                rsb = st[:, 1, :].unsqueeze(2).to_broadcast([128, 16, 64])
                                bfb = bf_[:, :].unsqueeze(2).to_broadcast([128, 16, 64])
                                p.op('dve', lambda e, yv=yv, mub=mub: e.tensor_tensor(out=yv, in0=yv, in1=mub, op=ALU.subtract), r=[r_st], w=[r_yj])
                                p.op('dve', lambda e, yv=yv, rsb=rsb: e.tensor_tensor(out=yv, in0=yv, in1=rsb, op=ALU.mult), r=[r_st], w=[r_yj])
                                p.op('dve', lambda e: e.tensor_tensor(out=yj[:, :], in0=yj[:, :], in1=rows[:, 0, :], op=ALU.mult), r=[r_c], w=[r_yj])
                                p.op('dve', lambda e: e.tensor_tensor(out=yj[:, :], in0=yj[:, :], in1=rows[:, 1, :], op=ALU.add), r=[r_c], w=[r_yj])
                                vv = Vtok[:, j, :].rearrange("p (h k) -> p h k", k=64)
                                p.op('dve', lambda e, sqv=sqv, vv=vv, bfb=bfb: e.tensor_tensor(out=sqv, in0=vv, in1=bfb, op=ALU.mult), r=[r_V, r_bf], w=[r_ysq])
                                p.op('dve', lambda e: e.tensor_tensor(out=yj[:, :], in0=yj[:, :], in1=ysq[:, :], op=ALU.add), r=[r_ysq], w=[r_yj])
                                for half in range(2):
                                    pbg, rpbg = self.next_ps()
                                    for cc in range(4):
                                        c = half * 4 + cc
                                        p.op('pe', lambda e, c=c, cc=cc, pbg=pbg, js=js: e.matmul(pbg[:, cc * 128:(cc + 1) * 128], lhsT=G2[:, c * 128:(c + 1) * 128],
                                                                                                   rhs=hg[:, js], start=True, stop=True), r=[r_hg, r_wt], w=[rpbg])
                                    p.op('act', lambda e, half=half, pbg=pbg: e.activation(out=gTs[:, half * 4:(half + 1) * 4, :], in_=pbg[:, :], func=AF.Copy),
                                         r=[rpbg], w=[r_gTs])
                                    pbt, rpbt = self.next_ps()
                                    for cc in range(4):
                                        c = half * 4 + cc
                                        p.op('pe', lambda e, c=c, cc=cc, pbt=pbt: e.transpose(out=pbt[:, cc * 128:(cc + 1) * 128], in_=yj[:, c * 128:(c + 1) * 128],
                                                                                              identity=self.identF[:, :]), r=[r_yj, self.r_const], w=[rpbt])
                                    p.op('dve', lambda e, half=half, pbt=pbt: e.tensor_tensor(out=ygT[:, half * 4:(half + 1) * 4, :], in0=pbt[:, :],
                                                                                              in1=gTs[:, half * 4:(half + 1) * 4, :], op=ALU.mult),
                                         r=[rpbt, r_gTs], w=[r_ygT])
                                for hf in range(2):
                                    pbo, rpbo = self.next_ps()
                                    for c in range(8):
                                        p.op('pe', lambda e, c=c, hf=hf, pbo=pbo: e.matmul(pbo[:, :], lhsT=ygT[:, c, :], rhs=Wo[:, c, hf * 512:(hf + 1) * 512],
                                                                                           start=(c == 0), stop=(c == 7)), r=[r_ygT, r_wt], w=[rpbo])
                                    p.op('dve', lambda e, hf=hf, pbo=pbo: e.tensor_tensor(out=osb[:, hf * 512:(hf + 1) * 512], in0=pbo[:, :],
                                                                                          in1=gbc[:, hf * 512:(hf + 1) * 512], op=ALU.mult), r=[rpbo, r_gbc], w=[r_osb])
                                p.op('dve', lambda e: e.tensor_tensor(out=osb[:, :], in0=osb[:, :], in1=xt[:, :], op=ALU.add), r=[r_xt], w=[r_osb])
                                p.dma('sp', [(u['dst'], osb[:, :])], r=[r_osb], w=[u['reg']])
                if sq_['sout'] is not None:
                    for h in range(16):
                        for d in range(2):
                            pb, rpb = self.next_ps()
                            p.op('pe', lambda e, pb=pb, h=h, d=d: e.transpose(out=pb[0:64, 0:64], in_=S32[:, h, d, :], identity=self.identF[0:64, 0:64]),
                                 r=[r_S[h][d], self.r_const], w=[rpb])
                            p.op('act', lambda e, pb=pb: e.activation(out=stio[:, :], in_=pb[0:64, 0:64], func=AF.Copy), r=[rpb], w=[r_stio])
                            p.dma('sp', [(sq_['sout'][d, h], stio[:, :])], r=[r_stio])
            p.barrier()
        self.ps_lim = 8

    def att_setup(self):
        c = self.cfg
        din, dout, dint = self._din, self._dout, self._dint
        self.at_rows = din('at_rows', [128, 20, 64])
        self.at_sink = din('at_sink', [64, 16])
        self.at_cos = din('at_cos', [c.LS, 64])
        self.at_sin = din('at_sin', [c.LS, 64])
        self.at_mask = din('at_mask', [128, 2, 128])
        self.ck = din('ck', [c.PAST, 256])
        self.cv = din('cv', [c.PAST, 256])
        self.o_k = dout('o_k', [self.TP, 256])
        self.o_v = dout('o_v', [self.TP, 256])
        seqs = self.make_seqs()
        for sq in seqs:
            L = sq['L']
            sq['qTd'] = dint('at_qTd%d' % sq['idx'], [64, 16, L], BF16)
            sq['kTd'] = dint('at_kTd%d' % sq['idx'], [64, 4, L], BF16)
            sq['vd'] = dint('at_vd%d' % sq['idx'], [L, 256], BF16)
        return seqs

    def att_phase(self, l, seqs):
        p, nc, W = self.p, self.nc, self.W
        c = self.cfg
        NCB = c.PAST // 128
        self.ps_lim = 6
        with ExitStack() as s:
            def sb(name, shape, dt):
                return p.sbuf(s, 'at_' + name, shape, dt)
            r_wt = Reg()
            Wqkv = sb('Wqkv', [128, 8, 1536], BF16)
            WoH = sb('WoH', [64, 16, 1024], BF16)
            p.dma('pool', [(Wqkv[:, :, :], W['att_w_qkv'].rearrange("(c p) n -> p c n", p=128)),
                           (WoH[:, :, :], W['att_w_o'].rearrange("(h p) n -> p h n", p=64))], w=[r_wt])
            rowsN = sb('rowsN', [128, 20, 64], F32)
            sinkE = sb('sinkE', [64, 16], F32)
            bmask = sb('bmask', [128, 2, 128], F32)
            r_c = Reg()
            p.dma('sp', [(rowsN[:, :, :], self.at_rows), (sinkE[:, :], self.at_sink), (bmask[:, :, :], self.at_mask)], w=[r_c])
            p.op('act', lambda e: e.activation(out=sinkE[:, :], in_=sinkE[:, :], func=AF.Exp), r=[r_c], w=[r_c])
            ones128 = sb('ones128', [128, 64], BF16)
            p.op('dve', lambda e: e.memset(ones128[:, :], 1.0), w=[r_c])
            ckT = sb('ckT', [64, 4, c.PAST], BF16); r_ckT = Reg()
            cvt = sb('cvt', [128, NCB, 256], BF16); r_cvt = Reg()
            ckt = sb('ckt', [128, NCB, 256], F32); r_ckt = Reg()
            p.dma('sp', [(ckt[:, :, :], self.ck.rearrange("(b p) f -> p b f", p=128))], w=[r_ckt])
            p.dma('pool', [(cvt[:, :, :], self.cv.rearrange("(b p) f -> p b f", p=128))], w=[r_cvt])
            for b in range(NCB):
                pb, rpb = self.next_ps()
                for g in range(4):
                    p.op('pe', lambda e, pb=pb, b=b, g=g: e.transpose(out=pb[0:64, g * 128:(g + 1) * 128], in_=ckt[:, b, g * 64:(g + 1) * 64],
                                                                      identity=self.identF[:, :]), r=[r_ckt, self.r_const], w=[rpb])
                p.op('act', lambda e, pb=pb, b=b: e.activation(out=ckT[:, :, b * 128:(b + 1) * 128],
                                                               in_=pb[0:64, :].rearrange("p (g t) -> p g t", g=4), func=AF.Copy), r=[rpb], w=[r_ckT])
            gbc = sb('gbc', [128, 1024], F32); r_gbc = Reg()
            diag = sb('diag', [128, 128], F32); r_diag = Reg()
            xt = sb('xt', [128, 1024], F32); r_xt = Reg()
            ss = sb('ss', [128, 2], F32); xn = sb('xn', [128, 1024], F32); junk = sb('junk', [128, 1024], BF16)
            scr = (ss, xn, junk, Reg(), Reg(), Reg())
            hts = sb('hts', [128, 8, 128], BF16); r_hts = Reg()
            qkv = sb('qkv', [128, 1536], F32); r_qkv = Reg()
            sq2 = sb('sq2', [128, 1280], F32); r_sq2 = Reg()
            rs = sb('rs', [128, 20], F32); r_rs = Reg()
            rot = sb('rot', [128, 1280], F32); r_rot = Reg()
            cs = sb('cs', [128, 2, 64], F32); r_cs = Reg()
            qTs = sb('qTs', [64, 20, 128], BF16); r_qTs = Reg()
            vbf = sb('vbf', [128, 256], BF16); r_vbf = Reg()
            qTb = sb('qTb', [64, 16, 128], BF16); r_qTb = Reg()
            kTb = sb('kTb', [64, 4, 384], BF16); r_kTb = Reg()
            vb = sb('vb', [128, 3, 256], BF16); r_vb = Reg()
            Et = [sb('E%d' % i, [128, 512], BF16) for i in range(3)]; r_E = [Reg() for _ in range(3)]
            OT = sb('OT', [64, 16, 128], BF16); r_OT = Reg()
            den = sb('den', [64, 512], F32); r_den = Reg()
            osb = sb('osb', [128, 1024], F32); r_osb = Reg()

            for sq_ in seqs:
                L, g_, units = sq_['L'], sq_['g'], sq_['units']
                qTd, kTd, vd = sq_['qTd'], sq_['kTd'], sq_['vd']
                r_sd = Reg()
                latent = (g_ == 0)
                self.gate_bc(gbc, r_gbc, l, 2, g_, diag, r_diag)
                nb = L // 128
                for i, u in enumerate(units):
                    p.dma('sp', [(xt[:, :], u['src'] if self.first_touch else u['dst'])], r=[u['reg']], w=[r_xt])
                    self.norm_hT(xt[:, :], r_xt, l, 0, g_, lambda cc: hts[:, cc, :], r_hts, scr)
                    for part in range(3):
                        pb, rpb = self.next_ps()
                        for cc in range(8):
                            p.op('pe', lambda e, cc=cc, pb=pb, part=part: e.matmul(pb[:, :], lhsT=hts[:, cc, :], rhs=Wqkv[:, cc, part * 512:(part + 1) * 512],
                                                                                   start=(cc == 0), stop=(cc == 7)), r=[r_hts, r_wt], w=[rpb])
                        p.op('act', lambda e, pb=pb, part=part: e.activation(out=qkv[:, part * 512:(part + 1) * 512], in_=pb[:, :], func=AF.Copy), r=[rpb], w=[r_qkv])
                    qk3 = qkv[:, 0:1280].rearrange("p (h k) -> p h k", k=64)
                    sq3 = sq2[:, :].rearrange("p (h k) -> p h k", k=64)
                    p.op('act', lambda e: e.activation(out=sq2[:, :], in_=qkv[:, 0:1280], func=AF.Square), r=[r_qkv], w=[r_sq2])
                    p.op('dve', lambda e, sq3=sq3: e.tensor_reduce(out=rs[:, :], in_=sq3, axis=AX.X, op=ALU.add), r=[r_sq2], w=[r_rs])
                    p.op('act', lambda e: e.activation(out=rs[:, :], in_=rs[:, :], func=AF.Sqrt, scale=1.0 / 64, bias=EPS), r=[r_rs], w=[r_rs])
                    p.op('dve', lambda e: e.reciprocal(out=rs[:, :], in_=rs[:, :]), r=[r_rs], w=[r_rs])
                    rsb = rs[:, :].unsqueeze(2).to_broadcast([128, 20, 64])
                    p.op('dve', lambda e, qk3=qk3, rsb=rsb: e.tensor_tensor(out=qk3, in0=qk3, in1=rsb, op=ALU.mult), r=[r_rs], w=[r_qkv])
                    p.op('dve', lambda e, qk3=qk3: e.tensor_tensor(out=qk3, in0=qk3, in1=rowsN[:, :, :], op=ALU.mult), r=[r_c], w=[r_qkv])
                    if latent:
                        t0 = i * 128
                        p.dma('sp', [(cs[:, 0, :], self.at_cos[t0:t0 + 128, :]), (cs[:, 1, :], self.at_sin[t0:t0 + 128, :])], w=[r_cs])
                        rot3 = rot[:, :].rearrange("p (h k) -> p h k", k=64)
                        cosb = cs[:, 0, :].unsqueeze(1).to_broadcast([128, 20, 64])
                        for (lo, hi, sgn) in ((0, 32, -1.0), (32, 64, 1.0)):
                            olo, ohi = (32, 64) if lo == 0 else (0, 32)
                            sinb = cs[:, 1, lo:hi].unsqueeze(1).to_broadcast([128, 20, 32])
                            p.op('dve', lambda e, rot3=rot3, qk3=qk3, sinb=sinb, lo=lo, hi=hi, olo=olo, ohi=ohi, sgn=sgn: e.scalar_tensor_tensor(
                                out=rot3[:, :, lo:hi], in0=qk3[:, :, olo:ohi], scalar=sgn, in1=sinb, op0=ALU.mult, op1=ALU.mult), r=[r_qkv, r_cs], w=[r_rot])
                        p.op('dve', lambda e, qk3=qk3, cosb=cosb: e.tensor_tensor(out=qk3, in0=qk3, in1=cosb, op=ALU.mult), r=[r_cs, r_rot], w=[r_qkv])
                        p.op('dve', lambda e: e.tensor_tensor(out=qkv[:, 0:1280], in0=qkv[:, 0:1280], in1=rot[:, :], op=ALU.add), r=[r_rot], w=[r_qkv])
                    else:
                        pi = sq_['idx'] - 1
                        r0 = pi * L + i * 128
                        p.dma('sp', [(self.o_k[r0:r0 + 128, :], qkv[:, 1024:1280]), (self.o_v[r0:r0 + 128, :], qkv[:, 1280:1536])], r=[r_qkv])
                    for hh in range(5):
                        pb, rpb = self.next_ps()
                        for q4 in range(4):
                            hd = hh * 4 + q4
                            p.op('pe', lambda e, pb=pb, q4=q4, hd=hd: e.transpose(out=pb[0:64, q4 * 128:(q4 + 1) * 128], in_=qkv[:, hd * 64:(hd + 1) * 64],
                                                                                  identity=self.identF[:, :]), r=[r_qkv, self.r_const], w=[rpb])
                        p.op('act', lambda e, pb=pb, hh=hh: e.activation(out=qTs[:, hh * 4:(hh + 1) * 4, :], in_=pb[0:64, :].rearrange("p (g t) -> p g t", g=4),
                                                                         func=AF.Copy), r=[rpb], w=[r_qTs])
                    p.op('act', lambda e: e.activation(out=vbf[:, :], in_=qkv[:, 1280:1536], func=AF.Copy), r=[r_qkv], w=[r_vbf])
                    ts = slice(i * 128, (i + 1) * 128)
                    p.dma('sp', [(qTd[:, :, ts], qTs[:, 0:16, :]), (kTd[:, :, ts], qTs[:, 16:20, :]), (vd[ts, :], vbf[:, :])], r=[r_qTs, r_vbf], w=[r_sd])
                for i, u in enumerate(units):
                    ts = slice(i * 128, (i + 1) * 128)
                    if latent:
                        kblocks = [bb for bb in (i - 1, i, i + 1) if 0 <= bb < nb]
                    else:
                        kblocks = list(range(nb))
                    k0 = kblocks[0]
                    nkb = len(kblocks)
                    pr = [(qTb[:, :, :], qTd[:, :, ts]), (kTb[:, :, 0:nkb * 128], kTd[:, :, k0 * 128:(k0 + nkb) * 128]),
                          (vb[:, 0:nkb, :], vd[k0 * 128:(k0 + nkb) * 128, :].rearrange("(b p) f -> p b f", p=128))]
                    p.dma('sp', pr, r=[r_sd], w=[r_qTb, r_kTb, r_vb])
                    p.dma('sp', [(xt[:, :], u['src'] if self.first_touch else u['dst'])], r=[u['reg']], w=[r_xt])
                    ei = 0
                    for g in range(4):
                        po, rpo = self.ps[6], self.r_ps[6]
                        pd, rpd = self.ps[7], self.r_ps[7]
                        klist = []
                        if latent:
                            for b in range(NCB):
                                klist.append((ckT[:, g, b * 128:(b + 1) * 128], cvt[:, b, g * 64:(g + 1) * 64], None, [r_ckT, r_cvt]))
                        for bi, bb in enumerate(kblocks):
                            mk = None
                            if latent and bb == i - 1:
                                mk = 0
                            if latent and bb == i + 1:
                                mk = 1
                            klist.append((kTb[:, g, bi * 128:(bi + 1) * 128], vb[:, bi, g * 64:(g + 1) * 64], mk, [r_kTb, r_vb]))
                        qrhs = qTb[:, g * 4:(g + 1) * 4, :]
                        for ki, (kap, vap, mk, rk) in enumerate(klist):
                            psc, rpsc = self.next_ps()
                            p.op('pe', lambda e, psc=psc, kap=kap, qrhs=qrhs: e.matmul(psc[:, :], lhsT=kap, rhs=qrhs, start=True, stop=True), r=rk + [r_qTb], w=[rpsc])
                            E, rE = Et[ei % 3], r_E[ei % 3]
                            ei += 1
                            p.op('act', lambda e, psc=psc, E=E: e.activation(out=E[:, :], in_=psc[:, :], func=AF.Exp, scale=0.125), r=[rpsc], w=[rE])
                            if mk is not None:
                                E3 = E[:, :].rearrange("p (r t) -> p r t", r=4)
                                mb = bmask[:, mk, :].unsqueeze(1).to_broadcast([128, 4, 128])
                                p.op('dve', lambda e, E3=E3, mb=mb: e.tensor_tensor(out=E3, in0=E3, in1=mb, op=ALU.mult), r=[r_c], w=[rE])
                            p.op('pe', lambda e, po=po, vap=vap, E=E, ki=ki: e.matmul(po[0:64, :], lhsT=vap, rhs=E[:, :], start=(ki == 0), stop=(ki == len(klist) - 1)),
                                 r=rk + [rE], w=[rpo])
                            p.op('pe', lambda e, pd=pd, E=E, ki=ki: e.matmul(pd[0:64, :], lhsT=ones128[:, :], rhs=E[:, :], start=(ki == 0), stop=(ki == len(klist) - 1)),
                                 r=[rE, r_c], w=[rpd])
                        for rr in range(4):
                            hd = g * 4 + rr
                            p.op('dve', lambda e, pd=pd, rr=rr, hd=hd: e.tensor_scalar(out=den[:, rr * 128:(rr + 1) * 128], in0=pd[0:64, rr * 128:(rr + 1) * 128],
                                                                                        scalar1=sinkE[:, hd:hd + 1], scalar2=None, op0=ALU.add), r=[rpd, r_c], w=[r_den])
                        p.op('dve', lambda e: e.reciprocal(out=den[:, :], in_=den[:, :]), r=[r_den], w=[r_den])
                        p.op('dve', lambda e, po=po, g=g: e.tensor_tensor(out=OT[:, g * 4:(g + 1) * 4, :], in0=po[0:64, :].rearrange("p (r t) -> p r t", r=4),
                                                                          in1=den[:, :].rearrange("p (r t) -> p r t", r=4), op=ALU.mult), r=[rpo, r_den], w=[r_OT])
                    for hf in range(2):
                        pbo, rpbo = self.next_ps()
                        for hd in range(16):
                            p.op('pe', lambda e, hd=hd, hf=hf, pbo=pbo: e.matmul(pbo[:, :], lhsT=OT[:, hd, :], rhs=WoH[:, hd, hf * 512:(hf + 1) * 512],
                                                                                 start=(hd == 0), stop=(hd == 15)), r=[r_OT, r_wt], w=[rpbo])
                        p.op('dve', lambda e, hf=hf, pbo=pbo: e.tensor_tensor(out=osb[:, hf * 512:(hf + 1) * 512], in0=pbo[:, :],
                                                                              in1=gbc[:, hf * 512:(hf + 1) * 512], op=ALU.mult), r=[rpbo, r_gbc], w=[r_osb])
                    p.op('dve', lambda e: e.tensor_tensor(out=osb[:, :], in0=osb[:, :], in1=xt[:, :], op=ALU.add), r=[r_xt], w=[r_osb])
                    p.dma('sp', [(u['dst'], osb[:, :])], r=[r_osb], w=[u['reg']])
            p.barrier()
        self.ps_lim = 8

    def ret_setup(self):
        c = self.cfg
        din, dout, dint = self._din, self._dout, self._dint
        self.rt_dmask = din('rt_dmask', [128, 4, 2, 128])
        self.rt_qdec = din('rt_qdec', [128, 4, 2, 128])
        self.rt_kdec = din('rt_kdec', [128, 4, 2])
        self.rt_cos = din('rt_cos', [c.LS, 256])
        self.rt_sin = din('rt_sin', [c.LS, 256])
        self.st_ret = din('st_ret', [2, 4, 256, 512])
        self.o_ret = dout('o_ret', [c.NP, 2, 4, 256, 512])
        seqs = self.make_seqs()
        ntok = len(self.units) * 128
        self.rt_proj = dint('rt_proj', [ntok, 8192])
        self.rt_yf = dint('rt_yf', [ntok, 2048])
        for sq in seqs:
            if sq['g'] == 0:
                sq['s0'], sq['sout'] = self.st_ret, None
            else:
                sq['s0'], sq['sout'] = None, self.o_ret[sq['idx'] - 1]
        return seqs

    def ret_phase(self, l, seqs):
        p, nc, W = self.p, self.nc, self.W
        c = self.cfg
        nu = len(self.units)
        proj, yfd = self.rt_proj, self.rt_yf
        r_proj = [Reg() for _ in range(nu)]
        with ExitStack() as s:
            def sb(name, shape, dt):
                return p.sbuf(s, 'ra_' + name, shape, dt)
            hT = sb('hT', [128, 8, nu * 128], BF16)
            r_hT = [Reg() for _ in range(nu)]
            xt = [sb('xt%d' % i, [128, 1024], F32) for i in range(2)]; r_xt = [Reg(), Reg()]
            ss = sb('ss', [128, 2], F32); xn = sb('xn', [128, 1024], F32); junk = sb('junk', [128, 1024], BF16)
            scr = (ss, xn, junk, Reg(), Reg(), Reg())
            wp = [sb('wp%d' % i, [128, 8, 512], BF16) for i in range(2)]; r_wp = [Reg(), Reg()]
            stg = [sb('stg%d' % i, [128, 512], F32) for i in range(3)]; r_stg = [Reg() for _ in range(3)]
            for ui, u in enumerate(self.units):
                b = ui % 2
                p.dma('sp', [(xt[b][:, :], u['src'] if self.first_touch else u['dst'])], r=[u['reg']], w=[r_xt[b]])
                self.norm_hT(xt[b][:, :], r_xt[b], l, 0, u['g'], lambda cc, ui=ui: hT[:, cc, ui * 128:(ui + 1) * 128], r_hT[ui], scr)
            k = 0
            for cg in range(16):
                b = cg % 2
                p.dma('pool', [(wp[b][:, :, :], W['ret_w_in'][:, cg * 512:(cg + 1) * 512].rearrange("(c p) n -> p c n", p=128))], w=[r_wp[b]])
                for ui in range(nu):
                    pb, rpb = self.next_ps()
                    for cc in range(8):
                        p.op('pe', lambda e, cc=cc, pb=pb, ui=ui, b=b: e.matmul(pb[:, :], lhsT=hT[:, cc, ui * 128:(ui + 1) * 128], rhs=wp[b][:, cc, :],
                                                                                start=(cc == 0), stop=(cc == 7)), r=[r_hT[ui], r_wp[b]], w=[rpb])
                    sbi = k % 3
                    k += 1
                    p.op('act' if k % 2 else 'dve', (lambda e, pb=pb, sbi=sbi: e.activation(out=stg[sbi][:, :], in_=pb[:, :], func=AF.Copy)) if k % 2 else
                         (lambda e, pb=pb, sbi=sbi: e.tensor_copy(out=stg[sbi][:, :], in_=pb[:, :])), r=[rpb], w=[r_stg[sbi]])
                    p.dma('sp', [(proj[ui * 128:(ui + 1) * 128, cg * 512:(cg + 1) * 512], stg[sbi][:, :])], r=[r_stg[sbi]], w=[r_proj[ui]])
            p.barrier()
        lgf = [float(np.log1p(-2.0 ** (-5.0 - h))) for h in range(4)]
        lgb = [float(np.log1p(-2.0 ** (-5.5 - h))) for h in range(4)]
        cdec = [[float(np.exp(lgf[h] * 128)), float(np.exp(lgb[h] * 128))] for h in range(4)]
        self.ps_lim = 6
        with ExitStack() as s:
            def sb(name, shape, dt):
                return p.sbuf(s, 'rb_' + name, shape, dt)
            r_wt = Reg()
            Wout = sb('Wout', [128, 16, 1024], BF16)
            p.dma('pool', [(Wout[:, :, :], W['ret_w_out'].rearrange("(c p) n -> p c n", p=128))], w=[r_wt])
            dmask = sb('dmask', [128, 4, 2, 128], F32)
            qdec = sb('qdec', [128, 4, 2, 128], F32)
            kdec = sb('kdec', [128, 4, 2], F32)
            r_c = Reg()
            p.dma('sp', [(dmask[:, :, :, :], self.rt_dmask), (qdec[:, :, :, :], self.rt_qdec), (kdec[:, :, :], self.rt_kdec)], w=[r_c])
            S32 = sb('S32', [128, 4, 2, 2, 512], F32)
            Sbf = sb('Sbf', [128, 4, 2, 2, 512], BF16)
            r_S = [[Reg() for _ in range(2)] for _ in range(4)]
            r_Sb = [[Reg() for _ in range(2)] for _ in range(4)]
            gbc = sb('gbc', [128, 1024], F32); r_gbc = Reg()
            diag = sb('diag', [128, 128], F32); r_diag = Reg()
            qk = sb('qk', [128, 2048], F32); r_qk = Reg()
            rot = sb('rot', [128, 2048], F32); r_rot = Reg()
            cs = sb('cs', [128, 2, 256], F32); r_cs = Reg()
            qT = sb('qT', [128, 8, 128], BF16); kT = sb('kT', [128, 8, 128], BF16); qdT = sb('qdT', [128, 2, 128], BF16)
            r_qT, r_kT, r_qdT = Reg(), Reg(), Reg()
            Kd = sb('Kd', [128, 1024], BF16); r_Kd = Reg()
            Vb = sb('Vb', [128, 2048], BF16); r_Vb = Reg()
            gg = sb('gg', [128, 2048], F32); r_gg = Reg()
            term = sb('term', [128, 2048], F32); r_term = Reg()
            yfw = sb('yfw', [128, 2048], F32); r_yfw = Reg()
            scT = sb('scT', [128, 128], BF16); r_scT = Reg()
            st = sb('st', [128, 4], F32); r_st = Reg()
            junk2 = sb('junk2', [128, 512], BF16); r_junk2 = Reg()
            yT = sb('yT', [128, 16, 128], BF16); r_yT = Reg()
            xt2 = sb('xt2', [128, 1024], F32); r_xt2 = Reg()
            osb = sb('osb', [128, 1024], F32); r_osb = Reg()
            ubase = 0
            for sq_ in seqs:
                L, g_, units = sq_['L'], sq_['g'], sq_['units']
                latent = (g_ == 0)
                nchunk = L // 128
                self.gate_bc(gbc, r_gbc, l, 2, g_, diag, r_diag)
                allS = [r_S[h][d] for h in range(4) for d in range(2)]
                if sq_['s0'] is None:
                    p.op('dve', lambda e: e.memset(S32[:, :, :, :, :], 0.0), w=allS)
                else:
                    for rdir in range(2):
                        for h in range(4):
                            p.dma('sp', [(S32[:, h, rdir, :, :], sq_['s0'][rdir, h].rearrange("(c p) e -> p c e", p=128))], w=[r_S[h][rdir]])
                for h in range(4):
                    for d in range(2):
                        p.op('act', lambda e, h=h, d=d: e.activation(out=Sbf[:, h, d, :, :], in_=S32[:, h, d, :, :], func=AF.Copy), r=[r_S[h][d]], w=[r_Sb[h][d]])
                for d in range(2):
                    chunks = list(range(nchunk)) if d == 0 else list(range(nchunk - 1, -1, -1))
                    for ci in chunks:
                        ui = ubase + ci
                        u = units[ci]
                        rows = slice(ui * 128, (ui + 1) * 128)
                        p.dma('sp', [(qk[:, :], proj[rows, 0:2048])], r=[r_proj[ui]], w=[r_qk])
                        p.dma('pool', [(Vb[:, :], proj[rows, 2048:4096])], r=[r_proj[ui]], w=[r_Vb])
                        gc0 = 4096 + d * 2048
                        p.dma('sp', [(gg[:, :], proj[rows, gc0:gc0 + 2048])], r=[r_proj[ui]], w=[r_gg])
                        p.op('act', lambda e: e.activation(out=gg[:, :], in_=gg[:, :], func=AF.Silu), r=[r_gg], w=[r_gg])
                        p.op('dve', lambda e: e.tensor_scalar(out=qk[:, 1024:2048], in0=qk[:, 1024:2048], scalar1=1.0 / 16.0, scalar2=None, op0=ALU.mult), r=[r_qk], w=[r_qk])
                        if latent:
                            t0 = ci * 128
                            p.dma('sp', [(cs[:, 0, :], self.rt_cos[t0:t0 + 128, :]), (cs[:, 1, :], self.rt_sin[t0:t0 + 128, :])], w=[r_cs])
                            x4 = qk[:, :].rearrange("p (h i two) -> p h i two", h=8, two=2)
                            r4 = rot[:, :].rearrange("p (h i two) -> p h i two", h=8, two=2)
                            cos4 = cs[:, 0, :].rearrange("p (i two) -> p i two", two=2)
                            sin4 = cs[:, 1, :].rearrange("p (i two) -> p i two", two=2)
                            for (o_, i_, sgn) in ((0, 1, -1.0), (1, 0, 1.0)):
                                sinb = sin4[:, :, o_].unsqueeze(1).to_broadcast([128, 8, 128])
                                p.op('dve', lambda e, r4=r4, x4=x4, sinb=sinb, o_=o_, i_=i_, sgn=sgn: e.scalar_tensor_tensor(
                                    out=r4[:, :, :, o_], in0=x4[:, :, :, i_], scalar=sgn, in1=sinb, op0=ALU.mult, op1=ALU.mult), r=[r_qk, r_cs], w=[r_rot])
                            cosb = cs[:, 0, :].unsqueeze(1).to_broadcast([128, 8, 256])
                            x3 = qk[:, :].rearrange("p (h k) -> p h k", h=8)
                            p.op('dve', lambda e, x3=x3, cosb=cosb: e.tensor_tensor(out=x3, in0=x3, in1=cosb, op=ALU.mult), r=[r_cs, r_rot], w=[r_qk])
                            p.op('dve', lambda e: e.tensor_tensor(out=qk[:, :], in0=qk[:, :], in1=rot[:, :], op=ALU.add), r=[r_rot], w=[r_qk])
                        for which, dstT, rdst in ((0, qT, r_qT), (1, kT, r_kT)):
                            for half in range(2):
                                pb, rpb = self.next_ps()
                                for q4 in range(4):
                                    cc = half * 4 + q4
                                    col = which * 1024 + cc * 128
                                    p.op('pe', lambda e, pb=pb, q4=q4, col=col: e.transpose(out=pb[:, q4 * 128:(q4 + 1) * 128], in_=qk[:, col:col + 128],
                                                                                           identity=self.identF[:, :]), r=[r_qk, self.r_const], w=[rpb])
                                p.op('act', lambda e, pb=pb, half=half, dstT=dstT: e.activation(out=dstT[:, half * 4:(half + 1) * 4, :],
                                                                                              in_=pb[:, :].rearrange("p (g t) -> p g t", g=4), func=AF.Copy), r=[rpb], w=[rdst])
                        for h in range(4):
                            p.op('dve', lambda e, h=h: e.tensor_scalar(out=Kd[:, h * 256:(h + 1) * 256], in0=qk[:, 1024 + h * 256:1024 + (h + 1) * 256],
                                                                        scalar1=kdec[:, h, d:d + 1], scalar2=None, op0=ALU.mult), r=[r_qk, r_c], w=[r_Kd])
                        for h in range(4):
                            hs = slice(h * 512, (h + 1) * 512)
                            psc, rpsc = self.next_ps()
                            for dc in range(2):
                                p.op('pe', lambda e, psc=psc, h=h, dc=dc: e.matmul(psc[:, 0:128], lhsT=kT[:, h * 2 + dc, :], rhs=qT[:, h * 2 + dc, :],
                                                                                  start=(dc == 0), stop=(dc == 1)), r=[r_kT, r_qT], w=[rpsc])
                            p.op('dve', lambda e, psc=psc, h=h: e.tensor_tensor(out=scT[:, :], in0=psc[:, 0:128], in1=dmask[:, h, d, :], op=ALU.mult),
                                 r=[rpsc, r_c], w=[r_scT])
                            for dc in range(2):
                                p.op('pool', lambda e, h=h, dc=dc: e.tensor_tensor(out=qdT[:, dc, :], in0=qT[:, h * 2 + dc, :], in1=qdec[:, h, d, :], op=ALU.mult),
                                     r=[r_qT, r_c], w=[r_qdT])
                            po, rpo = self.ps[6], self.r_ps[6]
                            p.op('pe', lambda e, po=po, hs=hs: e.matmul(po[:, :], lhsT=scT[:, :], rhs=Vb[:, hs], start=True, stop=False), r=[r_scT, r_Vb], w=[rpo])
                            for dc in range(2):
                                p.op('pe', lambda e, po=po, h=h, dc=dc: e.matmul(po[:, :], lhsT=qdT[:, dc, :], rhs=Sbf[:, h, d, dc, :], start=False, stop=(dc == 1)),
                                     r=[r_qdT, r_Sb[h][d]], w=[rpo])
                            p.op('act', lambda e, po=po, h=h: e.activation(out=junk2[:, :], in_=po[:, :], func=AF.Square, accum_out=st[:, h:h + 1]),
                                 r=[rpo], w=[r_junk2, r_st])
                            p.op('act', lambda e, h=h: e.activation(out=st[:, h:h + 1], in_=st[:, h:h + 1], func=AF.Sqrt, scale=1.0 / 512, bias=EPS), r=[r_st], w=[r_st])
                            p.op('dve', lambda e, h=h: e.reciprocal(out=st[:, h:h + 1], in_=st[:, h:h + 1]), r=[r_st], w=[r_st])
                            p.op('dve', lambda e, po=po, h=h, hs=hs: e.scalar_tensor_tensor(out=term[:, hs], in0=po[:, :], scalar=st[:, h:h + 1], in1=gg[:, hs],
                                                                                           op0=ALU.mult, op1=ALU.mult), r=[rpo, r_st, r_gg], w=[r_term])
                            for dc in range(2):
                                pss, rpss = self.ps[7], self.r_ps[7]
                                p.op('pe', lambda e, pss=pss, h=h, dc=dc, hs=hs: e.matmul(pss[:, :], lhsT=Kd[:, h * 256 + dc * 128:h * 256 + (dc + 1) * 128], rhs=Vb[:, hs],
                                                                                          start=True, stop=True), r=[r_Kd, r_Vb], w=[rpss])
                                p.op('dve', lambda e, pss=pss, h=h, dc=dc: e.scalar_tensor_tensor(out=S32[:, h, d, dc, :], in0=S32[:, h, d, dc, :], scalar=cdec[h][d],
                                                                                                 in1=pss[:, :], op0=ALU.mult, op1=ALU.add), r=[rpss, r_Sb[h][d]], w=[r_S[h][d]])
                                p.op('act', lambda e, h=h, dc=dc: e.activation(out=Sbf[:, h, d, dc, :], in_=S32[:, h, d, dc, :], func=AF.Copy), r=[r_S[h][d]], w=[r_Sb[h][d]])
                        if d == 0:
                            p.dma('sp', [(yfd[rows, :], term[:, :])], r=[r_term], w=[r_proj[ui]])
                        else:
                            p.dma('sp', [(yfw[:, :], yfd[rows, :])], r=[r_proj[ui]], w=[r_yfw])
                            p.dma('sp', [(xt2[:, :], u['src'] if self.first_touch else u['dst'])], r=[u['reg']], w=[r_xt2])
                            p.op('dve', lambda e: e.tensor_tensor(out=term[:, :], in0=term[:, :], in1=yfw[:, :], op=ALU.add), r=[r_yfw], w=[r_term])
                            for qd in range(4):
                                pb, rpb = self.next_ps()
                                for q4 in range(4):
                                    cc = qd * 4 + q4
                                    p.op('pe', lambda e, pb=pb, q4=q4, cc=cc: e.transpose(out=pb[:, q4 * 128:(q4 + 1) * 128], in_=term[:, cc * 128:(cc + 1) * 128],
                                                                                          identity=self.identF[:, :]), r=[r_term, self.r_const], w=[rpb])
                                p.op('act', lambda e, pb=pb, qd=qd: e.activation(out=yT[:, qd * 4:(qd + 1) * 4, :], in_=pb[:, :].rearrange("p (g t) -> p g t", g=4),
                                                                               func=AF.Copy), r=[rpb], w=[r_yT])
                            for hf in range(2):
                                pbo, rpbo = self.next_ps()
                                for cc in range(16):
                                    p.op('pe', lambda e, cc=cc, hf=hf, pbo=pbo: e.matmul(pbo[:, :], lhsT=yT[:, cc, :], rhs=Wout[:, cc, hf * 512:(hf + 1) * 512],
                                                                                         start=(cc == 0), stop=(cc == 15)), r=[r_yT, r_wt], w=[rpbo])
                                p.op('dve', lambda e, hf=hf, pbo=pbo: e.tensor_tensor(out=osb[:, hf * 512:(hf + 1) * 512], in0=pbo[:, :],
                                                                                      in1=gbc[:, hf * 512:(hf + 1) * 512], op=ALU.mult), r=[rpbo, r_gbc], w=[r_osb])
                            p.op('dve', lambda e: e.tensor_tensor(out=osb[:, :], in0=osb[:, :], in1=xt2[:, :], op=ALU.add), r=[r_xt2], w=[r_osb])
                            p.dma('sp', [(u['dst'], osb[:, :])], r=[r_osb], w=[u['reg']])
                if sq_['sout'] is not None:
                    for rdir in range(2):
                        for h in range(4):
                            p.dma('sp', [(sq_['sout'][rdir, h].rearrange("(c p) e -> p c e", p=128), S32[:, h, rdir, :, :])], r=[r_S[h][rdir]])
                ubase += nchunk
            p.barrier()
        self.ps_lim = 8

    def hy_setup(self):
        c = self.cfg
        din, dout, dint = self._din, self._dout, self._dint
        self.hy_fm = din('hy_fm', [128, 6, 24])
        self.hy_rows = din('hy_rows', [128, 2, 1024])
        self.hy_fsm = din('hy_fsm', [64, 4])
        seqs = self.make_seqs()
        self.hy_L = {}
        for sq in seqs:
            L = sq['L']
            sq['hTd'] = dint('hy_hTd%d' % sq['idx'], [128, 8, L + 2], BF16)
            sq['zd'] = dint('hy_zd%d' % sq['idx'], [L, 1024], BF16)
            sq['x0Td'] = dint('hy_x0Td%d' % sq['idx'], [128, 8, L], BF16)
            sq['zTd'] = dint('hy_zTd%d' % sq['idx'], [128, 8, L], BF16)
            sq['gTd'] = dint('hy_gTd%d' % sq['idx'], [128, 8, L], BF16)
            if L not in self.hy_L:
                TC = L // 128
                NFc = TC + 1
                self.hy_L[L] = dict(TC=TC, NFc=NFc,
                                    zpos=din('hy_zpos%d' % L, [33, L]), tn=din('hy_tn%d' % L, [L, 1]),
                                    Fc=din('hy_Fc%d' % L, [NFc, 128, TC * 128], BF16), Fs=din('hy_Fs%d' % L, [NFc, 128, TC * 128], BF16),
                                    Gc=din('hy_Gc%d' % L, [NFc * 128, L], BF16), Gs=din('hy_Gs%d' % L, [NFc * 128, L], BF16),
                                    hsd=dint('hy_hsd%d' % L, [L, 1024], BF16), hdd=dint('hy_hdd%d' % L, [L, 1024], BF16), r=Reg())
        return seqs

    def hy_phase(self, l, seqs):
        p, nc, W = self.p, self.nc, self.W
        c = self.cfg
        with ExitStack() as s:
            def sb(name, shape, dt):
                return p.sbuf(s, 'ha_' + name, shape, dt)
            NT = 256
            r_wt = Reg()
            Win = sb('Win', [128, 8, 3072], BF16)
            p.dma('pool', [(Win[:, :, 0:1536], W['hy_w_in'][:, 0:1536].rearrange("(c p) n -> p c n", p=128)),
                           (Win[:, :, 1536:3072], W['hy_w_in'][:, 1536:3072].rearrange("(c p) n -> p c n", p=128))], w=[r_wt])
            fmv = sb('fmv', [128, 6, 24], F32); r_c = Reg()
            p.dma('sp', [(fmv[:, :, :], self.hy_fm)], w=[r_c])
            xt = sb('xt', [128, 1024], F32); r_xt = Reg()
            ss = sb('ss', [128, 2], F32); xn = sb('xn', [128, 1024], F32); junk = sb('junk', [128, 1024], BF16)
            scr = (ss, xn, junk, Reg(), Reg(), Reg())
            hts = sb('hts', [128, 8, 128], BF16); r_hts = Reg()
            zer = sb('zer', [128, 8, 1], BF16); r_zer = Reg()
            p.op('dve', lambda e: e.memset(zer[:, :, :], 0.0), w=[r_zer])
            hTt = sb('hTt', [128, 8, NT + 2], BF16); r_hTt = Reg()
            pT = [sb('pT%d' % i, [128, NT + 2], F32) for i in range(2)]; r_pT = [Reg(), Reg()]
            uu = [sb('uu%d' % i, [128, NT], F32) for i in range(2)]; r_uu = [Reg(), Reg()]
            x0T = sb('x0T', [128, 8, NT], BF16); r_x0T = Reg()
            x1T = sb('x1T', [128, 8, NT], F32); r_x1T = Reg()
            zT = sb('zT', [128, 8, NT], BF16); r_zT = Reg()
            ztok = sb('ztok', [128, NT // 128, 1024], BF16); r_ztok = Reg()
            for sq_ in seqs:
                L, g_, units = sq_['L'], sq_['g'], sq_['units']
                hTd = sq_['hTd']; r_hTd = Reg()
                sq_['r_sc'] = Reg()
                p.dma('sp', [(hTd[:, :, 0:1], zer[:, :, :]), (hTd[:, :, L + 1:L + 2], zer[:, :, :])], r=[r_zer], w=[r_hTd], allow_slow_non_contiguous=True)
                for i, u in enumerate(units):
                    p.dma('sp', [(xt[:, :], u['src'] if self.first_touch else u['dst'])], r=[u['reg']], w=[r_xt])
                    self.norm_hT(xt[:, :], r_xt, l, 0, g_, lambda cc: hts[:, cc, :], r_hts, scr)
                    p.dma('sp', [(hTd[:, :, 1 + i * 128:1 + (i + 1) * 128], hts[:, :, :])], r=[r_hts], w=[r_hTd])
                for ti in range(L // NT):
                    t0 = ti * NT
                    n = NT
                    p.dma('sp', [(hTt[:, :, :], hTd[:, :, t0:t0 + n + 2])], r=[r_hTd], w=[r_hTt])
                    for oc in range(24):
                        b = oc % 2
                        pb, rpb = self.next_ps()
                        for cc in range(8):
                            p.op('pe', lambda e, cc=cc, pb=pb, oc=oc: e.matmul(pb[:, 0:n + 2], lhsT=Win[:, cc, oc * 128:(oc + 1) * 128], rhs=hTt[:, cc, :],
                                                                               start=(cc == 0), stop=(cc == 7)), r=[r_hTt, r_wt], w=[rpb])
                        p.op('dve', lambda e, pb=pb, b=b, oc=oc: e.tensor_scalar(out=pT[b][:, :], in0=pb[:, 0:n + 2], scalar1=fmv[:, 0, oc:oc + 1], scalar2=None, op0=ALU.add),
                             r=[rpb, r_c], w=[r_pT[b]])
                        if t0 == 0:
                            p.op('dve', lambda e, b=b: e.memset(pT[b][:, 0:1], 0.0), w=[r_pT[b]])
                        if t0 + n == L:
                            p.op('dve', lambda e, b=b: e.memset(pT[b][:, n + 1:n + 2], 0.0), w=[r_pT[b]])
                        p.op('dve', lambda e, b=b, oc=oc: e.tensor_scalar(out=uu[b][:, :], in0=pT[b][:, 0:n], scalar1=fmv[:, 1, oc:oc + 1], scalar2=fmv[:, 4, oc:oc + 1],
                                                                        op0=ALU.mult, op1=ALU.add), r=[r_pT[b], r_c], w=[r_uu[b]])
                        p.op('dve', lambda e, b=b, oc=oc: e.scalar_tensor_tensor(out=uu[b][:, :], in0=pT[b][:, 1:n + 1], scalar=fmv[:, 2, oc:oc + 1], in1=uu[b][:, :],
                                                                               op0=ALU.mult, op1=ALU.add), r=[r_pT[b], r_c], w=[r_uu[b]])
                        which, cc8 = oc // 8, oc % 8
                        if which == 0:
                            p.op('dve', lambda e, b=b, oc=oc, cc8=cc8: e.scalar_tensor_tensor(out=x0T[:, cc8, :], in0=pT[b][:, 2:n + 2], scalar=fmv[:, 3, oc:oc + 1], in1=uu[b][:, :],
                                                                                              op0=ALU.mult, op1=ALU.add), r=[r_pT[b], r_uu[b], r_c], w=[r_x0T])
                        elif which == 1:
                            p.op('dve', lambda e, b=b, oc=oc, cc8=cc8: e.scalar_tensor_tensor(out=x1T[:, cc8, :], in0=pT[b][:, 2:n + 2], scalar=fmv[:, 3, oc:oc + 1], in1=uu[b][:, :],
                                                                                              op0=ALU.mult, op1=ALU.add), r=[r_pT[b], r_uu[b], r_c], w=[r_x1T])
                        else:
                            p.op('dve', lambda e, b=b, oc=oc: e.scalar_tensor_tensor(out=uu[b][:, :], in0=pT[b][:, 2:n + 2], scalar=fmv[:, 3, oc:oc + 1], in1=uu[b][:, :],
                                                                                   op0=ALU.mult, op1=ALU.add), r=[r_pT[b], r_c], w=[r_uu[b]])
                            p.op('dve', lambda e, b=b, cc8=cc8: e.tensor_tensor(out=zT[:, cc8, :], in0=uu[b][:, :], in1=x1T[:, cc8, :], op=ALU.mult),
                                 r=[r_uu[b], r_x1T], w=[r_zT])
                    for j in range(n // 128):
                        for half in range(2):
                            pb, rpb = self.next_ps()
                            pv = pb[:, :].bitcast(BF16)
                            for q4 in range(4):
                                cc = half * 4 + q4
                                p.op('pe', lambda e, pv=pv, q4=q4, cc=cc, j=j: e.transpose(out=pv[:, q4 * 128:(q4 + 1) * 128], in_=zT[:, cc, j * 128:(j + 1) * 128],
                                                                                           identity=self.identB[:, :]), r=[r_zT, self.r_const], w=[rpb])
                            p.op('act', lambda e, pv=pv, j=j, half=half: e.activation(out=ztok[:, j, half * 512:(half + 1) * 512], in_=pv[:, 0:512], func=AF.Copy),
                                 r=[rpb], w=[r_ztok])
                    p.dma('sp', [(sq_['zd'][t0:t0 + n, :].rearrange("(j p) f -> p j f", p=128), ztok[:, :, :]),
                                 (sq_['x0Td'][:, :, t0:t0 + n], x0T[:, :, :]), (sq_['zTd'][:, :, t0:t0 + n], zT[:, :, :])],
                          r=[r_ztok, r_x0T, r_zT], w=[sq_['r_sc']])
            p.barrier()
        with ExitStack() as s:
            def sb(name, shape, dt):
                return p.sbuf(s, 'hb_' + name, shape, dt)
            w1 = sb('w1', [33, 64], F32); w2 = sb('w2', [64, 64], F32); w3 = sb('w3', [64, 2048], F32)
            fsm = sb('fsm', [64, 4], F32); rows = sb('rows', [128, 2, 1024], F32)
            r_c = Reg()
            p.dma('sp', [(w1[:, :], W['hy_f_w1']), (w2[:, :], W['hy_f_w2']), (w3[:, :], W['hy_f_w3']), (fsm[:, :], self.hy_fsm), (rows[:, :, :], self.hy_rows)], w=[r_c])
            zp = sb('zp', [33, 128], F32); r_zp = Reg()
            tn = sb('tn', [128, 1], F32); r_tn = Reg()
            ar = sb('ar', [64, 128], F32); s2 = sb('s2', [64, 128], F32); s4 = sb('s4', [64, 128], F32); a1 = sb('a1', [64, 128], F32); a2 = sb('a2', [64, 128], F32)
            r_a = Reg()
            wnd = sb('wnd', [128, 1024], F32); r_wnd = Reg()
            hf = sb('hf', [128, 1024], F32); hb = sb('hb', [128, 1024], F32); r_h = Reg()
            hs = sb('hs', [128, 1024], BF16); hd = sb('hd', [128, 1024], BF16); r_hsd = Reg()

            def sin_layer(pb, rpb, bi, fi, dst):
                p.op('dve', lambda e: e.tensor_scalar(out=ar[:, :], in0=pb[0:64, 0:128], scalar1=fsm[:, bi:bi + 1], scalar2=fsm[:, fi:fi + 1], op0=ALU.add, op1=ALU.mult),
                     r=[rpb, r_c], w=[r_a])
                p.op('act', lambda e: e.activation(out=s2[:, :], in_=ar[:, :], func=AF.Sin, scale=0.5), r=[r_a], w=[r_a])
                p.op('act', lambda e: e.activation(out=s4[:, :], in_=ar[:, :], func=AF.Sin, scale=0.25), r=[r_a], w=[r_a])
                p.op('dve', lambda e: e.tensor_tensor(out=s4[:, :], in0=s4[:, :], in1=s4[:, :], op=ALU.mult), r=[r_a], w=[r_a])
                p.op('dve', lambda e: e.tensor_scalar(out=s4[:, :], in0=s4[:, :], scalar1=-2.0, scalar2=1.0, op0=ALU.mult, op1=ALU.add), r=[r_a], w=[r_a])
                p.op('dve', lambda e: e.scalar_tensor_tensor(out=dst[:, :], in0=s2[:, :], scalar=2.0, in1=s4[:, :], op0=ALU.mult, op1=ALU.mult), r=[r_a], w=[r_a])

            for L, info in self.hy_L.items():
                for ti in range(L // 128):
                    rows_t = slice(ti * 128, (ti + 1) * 128)
                    p.dma('sp', [(zp[:, :], info['zpos'][:, rows_t]), (tn[:, :], info['tn'][rows_t, :])], w=[r_zp, r_tn])
                    pb, rpb = self.next_ps()
                    p.op('pe', lambda e, pb=pb: e.matmul(pb[0:64, 0:128], lhsT=w1[:, :], rhs=zp[:, :], start=True, stop=True), r=[r_zp, r_c], w=[rpb])
                    sin_layer(pb, rpb, 0, 1, a1)
                    pb, rpb = self.next_ps()
                    p.op('pe', lambda e, pb=pb: e.matmul(pb[0:64, 0:128], lhsT=w2[:, :], rhs=a1[:, :], start=True, stop=True), r=[r_a, r_c], w=[rpb])
                    sin_layer(pb, rpb, 2, 3, a2)
                    p.op('dve', lambda e: e.tensor_scalar(out=tn[:, :], in0=tn[:, :], scalar1=-1.0, scalar2=None, op0=ALU.mult), r=[r_tn], w=[r_tn])
                    p.op('act', lambda e: e.activation(out=wnd[:, :], in_=rows[:, 1, :], func=AF.Exp, scale=tn[:, 0:1]), r=[r_tn, r_c], w=[r_wnd])
                    for q in range(4):
                        pb, rpb = self.next_ps()
                        p.op('pe', lambda e, pb=pb, q=q: e.matmul(pb[:, :], lhsT=a2[:, :], rhs=w3[:, q * 512:(q + 1) * 512], start=True, stop=True), r=[r_a, r_c], w=[rpb])
                        dst = hf if q < 2 else hb
                        qq = q % 2
                        p.op('dve', lambda e, pb=pb, dst=dst, qq=qq: e.tensor_tensor(out=dst[:, qq * 512:(qq + 1) * 512], in0=pb[:, :], in1=wnd[:, qq * 512:(qq + 1) * 512], op=ALU.mult),
                             r=[rpb, r_wnd], w=[r_h])
                    if ti == 0:
                        p.op('dve', lambda e: e.memset(hb[0:1, :], 0.0), w=[r_h])
                    p.op('dve', lambda e: e.tensor_tensor(out=hs[:, :], in0=hf[:, :], in1=hb[:, :], op=ALU.add), r=[r_h], w=[r_hsd])
                    p.op('dve', lambda e: e.tensor_tensor(out=hd[:, :], in0=hb[:, :], in1=hf[:, :], op=ALU.subtract), r=[r_h], w=[r_hsd])
                    p.dma('sp', [(info['hsd'][rows_t, :], hs[:, :]), (info['hdd'][rows_t, :], hd[:, :])], r=[r_hsd], w=[info['r']])
            p.barrier()
        self.ps_lim = 6
        with ExitStack() as s:
            def sb(name, shape, dt):
                return p.sbuf(s, 'hc_' + name, shape, dt)
            TCM = max(i_['TC'] for i_ in self.hy_L.values())
            NFM = TCM + 1
            Rc = sb('Rc', [128, TCM, 512], BF16); Rs = sb('Rs', [128, TCM, 512], BF16); r_R = Reg()
            Fcb = [sb('Fcb%d' % i, [128, TCM * 128], BF16) for i in range(2)]; Fsb = [sb('Fsb%d' % i, [128, TCM * 128], BF16) for i in range(2)]
            r_F = [Reg(), Reg()]
            Yre = sb('Yre', [128, NFM, 256], BF16); Yim = sb('Yim', [128, NFM, 256], BF16); r_Y = Reg()
            ec = sb('ec', [128, 512], F32); es_ = sb('es', [128, 512], F32); r_ec, r_es = Reg(), Reg()
            t1 = sb('t1', [128, 256], F32); t2 = sb('t2', [128, 256], F32); r_t1, r_t2 = Reg(), Reg()
            GB = 4
            Gcb = [sb('Gcb%d' % i, [128, GB, 512], BF16) for i in range(2)]; Gsb = [sb('Gsb%d' % i, [128, GB, 512], BF16) for i in range(2)]
            r_G = [Reg(), Reg()]
            x0t = sb('x0t', [128, 2, 512], BF16); zt = sb('zt', [128, 2, 512], BF16); r_xz = Reg()
            go = sb('go', [128, 2, 512], BF16); r_go = Reg()
            tmp = sb('tmp', [128, 512], F32); r_tmp = Reg()
            fmv = sb('fmv', [128, 6, 24], F32); r_c = Reg()
            p.dma('sp', [(fmv[:, :, :], self.hy_fm)], w=[r_c])
            fi = 0
            gi = 0
            for sq_ in seqs:
                L = sq_['L']
                info = self.hy_L[L]
                TC, NFc = info['TC'], info['NFc']
                TW = min(512, L)
                sq_['r_g'] = Reg()
                for gq in range(4):
                    gcols = slice(gq * 256, (gq + 1) * 256)
                    p.dma('sp', [(Rc[:, 0:TC, 0:256], sq_['zd'][:, gcols].rearrange("(c p) f -> p c f", p=128)),
                                 (Rc[:, 0:TC, 256:512], info['hsd'][:, gcols].rearrange("(c p) f -> p c f", p=128)),
                                 (Rs[:, 0:TC, 0:256], sq_['zd'][:, gcols].rearrange("(c p) f -> p c f", p=128)),
                                 (Rs[:, 0:TC, 256:512], info['hdd'][:, gcols].rearrange("(c p) f -> p c f", p=128))],
                          r=[sq_['r_sc'], info['r']], w=[r_R])
                    for fc in range(NFc):
                        b = fi % 2
                        fi += 1
                        p.dma('sp', [(Fcb[b][:, 0:TC * 128], info['Fc'][fc]), (Fsb[b][:, 0:TC * 128], info['Fs'][fc])], w=[r_F[b]])
                        pc, rpc = self.next_ps()
                        for tc in range(TC):
                            p.op('pe', lambda e, pc=pc, tc=tc, b=b: e.matmul(pc[:, :], lhsT=Fcb[b][:, tc * 128:(tc + 1) * 128], rhs=Rc[:, tc, :], start=(tc == 0), stop=(tc == TC - 1)),
                                 r=[r_F[b], r_R], w=[rpc])
                        pS, rpS = self.next_ps()
                        for tc in range(TC):
                            p.op('pe', lambda e, pS=pS, tc=tc, b=b: e.matmul(pS[:, :], lhsT=Fsb[b][:, tc * 128:(tc + 1) * 128], rhs=Rs[:, tc, :], start=(tc == 0), stop=(tc == TC - 1)),
                                 r=[r_F[b], r_R], w=[rpS])
                        p.op('act', lambda e, pc=pc: e.activation(out=ec[:, :], in_=pc[:, :], func=AF.Copy), r=[rpc], w=[r_ec])
                        p.op('act', lambda e, pS=pS: e.activation(out=es_[:, :], in_=pS[:, :], func=AF.Copy), r=[rpS], w=[r_es])
                        p.op('dve', lambda e: e.tensor_tensor(out=t1[:, :], in0=ec[:, 0:256], in1=ec[:, 256:512], op=ALU.mult), r=[r_ec], w=[r_t1])
                        p.op('pool', lambda e: e.tensor_tensor(out=t2[:, :], in0=es_[:, 0:256], in1=es_[:, 256:512], op=ALU.mult), r=[r_es], w=[r_t2])
                        p.op('dve', lambda e, fc=fc: e.tensor_tensor(out=Yre[:, fc, :], in0=t1[:, :], in1=t2[:, :], op=ALU.add), r=[r_t1, r_t2], w=[r_Y])
                        p.op('dve', lambda e: e.tensor_tensor(out=t1[:, :], in0=ec[:, 0:256], in1=es_[:, 256:512], op=ALU.mult), r=[r_ec, r_es], w=[r_t1])
                        p.op('pool', lambda e: e.tensor_tensor(out=t2[:, :], in0=es_[:, 0:256], in1=ec[:, 256:512], op=ALU.mult), r=[r_ec, r_es], w=[r_t2])
                        p.op('dve', lambda e, fc=fc: e.tensor_tensor(out=Yim[:, fc, :], in0=t1[:, :], in1=t2[:, :], op=ALU.subtract), r=[r_t1, r_t2], w=[r_Y])
                    for tt in range(L // TW):
                        tsl = slice(tt * TW, (tt + 1) * TW)
                        pa = [self.ps[6], self.ps[7]]
                        rpa = [self.r_ps[6], self.r_ps[7]]
                        p.dma('sp', [(x0t[:, :, 0:TW], sq_['x0Td'][:, gq * 2:gq * 2 + 2, tsl]), (zt[:, :, 0:TW], sq_['zTd'][:, gq * 2:gq * 2 + 2, tsl])],
                              r=[sq_['r_sc']], w=[r_xz])
                        nbat = (NFc + GB - 1) // GB
                        for bt in range(nbat):
                            f0 = bt * GB
                            nf = min(GB, NFc - f0)
                            b = gi % 2
                            gi += 1
                            p.dma('sp', [(Gcb[b][:, 0:nf, 0:TW], info['Gc'][f0 * 128:(f0 + nf) * 128, tsl].rearrange("(c p) t -> p c t", p=128)),
                                         (Gsb[b][:, 0:nf, 0:TW], info['Gs'][f0 * 128:(f0 + nf) * 128, tsl].rearrange("(c p) t -> p c t", p=128))], w=[r_G[b]])
                            for k in range(nf):
                                fc = f0 + k
                                for dq in range(2):
                                    p.op('pe', lambda e, dq=dq, fc=fc, k=k, b=b: e.matmul(pa[dq][:, 0:TW], lhsT=Yre[:, fc, dq * 128:(dq + 1) * 128], rhs=Gcb[b][:, k, 0:TW],
                                                                                        start=(fc == 0), stop=False), r=[r_Y, r_G[b]], w=[rpa[dq]])
                                    p.op('pe', lambda e, dq=dq, fc=fc, k=k, b=b: e.matmul(pa[dq][:, 0:TW], lhsT=Yim[:, fc, dq * 128:(dq + 1) * 128], rhs=Gsb[b][:, k, 0:TW],
                                                                                        start=False, stop=(fc == NFc - 1)), r=[r_Y, r_G[b]], w=[rpa[dq]])
                        for dq in range(2):
                            gch = gq * 2 + dq
                            p.op('dve', lambda e, dq=dq, gch=gch: e.scalar_tensor_tensor(out=tmp[:, 0:TW], in0=zt[:, dq, 0:TW], scalar=fmv[:, 5, gch:gch + 1], in1=pa[dq][:, 0:TW],
                                                                                       op0=ALU.mult, op1=ALU.add), r=[r_xz, rpa[dq], r_c], w=[r_tmp])
                            p.op('dve', lambda e, dq=dq: e.tensor_tensor(out=go[:, dq, 0:TW], in0=tmp[:, 0:TW], in1=x0t[:, dq, 0:TW], op=ALU.mult), r=[r_tmp, r_xz], w=[r_go])
                        p.dma('sp', [(sq_['gTd'][:, gq * 2:gq * 2 + 2, tsl], go[:, :, 0:TW])], r=[r_go], w=[sq_['r_g']])
            p.barrier()
        self.ps_lim = 8
        with ExitStack() as s:
            def sb(name, shape, dt):
                return p.sbuf(s, 'hd_' + name, shape, dt)
            r_wt = Reg()
            Wo = sb('Wo', [128, 8, 1024], BF16)
            p.dma('pool', [(Wo[:, :, :], W['hy_w_out'].rearrange("(c p) n -> p c n", p=128))], w=[r_wt])
            rows = sb('rows', [128, 2, 1024], F32); r_c = Reg()
            p.dma('sp', [(rows[:, :, :], self.hy_rows)], w=[r_c])
            gbc = sb('gbc', [128, 1024], F32); r_gbc = Reg()
            diag = sb('diag', [128, 128], F32); r_diag = Reg()
            gT = [sb('gT%d' % i, [128, 8, 128], BF16) for i in range(2)]; r_gT = [Reg(), Reg()]
            xt = [sb('xt%d' % i, [128, 1024], F32) for i in range(2)]; r_xt = [Reg(), Reg()]
            osb = [sb('osb%d' % i, [128, 1024], F32) for i in range(2)]; r_osb = [Reg(), Reg()]
            k = 0
            for sq_ in seqs:
                self.gate_bc(gbc, r_gbc, l, 2, sq_['g'], diag, r_diag)
                for i, u in enumerate(sq_['units']):
                    b = k % 2
                    k += 1
                    p.dma('sp', [(gT[b][:, :, :], sq_['gTd'][:, :, i * 128:(i + 1) * 128])], r=[sq_['r_g']], w=[r_gT[b]])
                    p.dma('sp', [(xt[b][:, :], u['src'] if self.first_touch else u['dst'])], r=[u['reg']], w=[r_xt[b]])
                    for hf_ in range(2):
                        cs_ = slice(hf_ * 512, (hf_ + 1) * 512)
                        pbo, rpbo = self.next_ps()
                        for cc in range(8):
                            p.op('pe', lambda e, cc=cc, pbo=pbo, b=b, cs_=cs_: e.matmul(pbo[:, :], lhsT=gT[b][:, cc, :], rhs=Wo[:, cc, cs_], start=(cc == 0), stop=(cc == 7)),
                                 r=[r_gT[b], r_wt], w=[rpbo])
                        p.op('dve', lambda e, pbo=pbo, b=b, cs_=cs_: e.tensor_tensor(out=osb[b][:, cs_], in0=pbo[:, :], in1=rows[:, 0, cs_], op=ALU.add), r=[rpbo, r_c], w=[r_osb[b]])
                    p.op('dve', lambda e, b=b: e.tensor_tensor(out=osb[b][:, :], in0=osb[b][:, :], in1=gbc[:, :], op=ALU.mult), r=[r_gbc], w=[r_osb[b]])
                    p.op('dve', lambda e, b=b: e.tensor_tensor(out=osb[b][:, :], in0=osb[b][:, :], in1=xt[b][:, :], op=ALU.add), r=[r_xt[b]], w=[r_osb[b]])
                    p.dma('sp', [(u['dst'], osb[b][:, :])], r=[r_osb[b]], w=[u['reg']])
            p.barrier()

    def make_seqs(self):
        c = self.cfg
        ns = c.LS // 128
        seqs = [dict(L=c.LS, g=0, units=self.units[0:ns], idx=0)]
        npu = c.LP // 128
        for i in range(c.NP):
            seqs.append(dict(L=c.LP, g=1, units=self.units[ns + i * npu: ns + (i + 1) * npu], idx=1 + i))
        return seqs

    def rwkv_setup(self):
        c = self.cfg
        din, dout, dint = self._din, self._dout, self._dint
        self.rw_mix = din('rw_mix', [128, 6, 8])
        self.rw_hm = din('rw_hm', [64, 8, 16])
        self.rw_rows = din('rw_rows', [128, 2, 1024])
        self.rw_mask4 = din('rw_mask4', [128, 2, 512])
        self.rw_maskN = din('rw_maskN', [128, 4, 128])
        self.st_rwkv = din('st_rwkv', [2, 16, 64, 64])
        self.o_rwkv = dout('o_rwkv', [c.NP, 2, 16, 64, 64])
        seqs = self.make_seqs()
        for sq in seqs:
            L = sq['L']
            sq['hTd'] = dint('rw_hTd%d' % sq['idx'], [128, 8, L + 2], BF16)
            sq['yfd'] = dint('rw_yfd%d' % sq['idx'], [L, 1024])
            sq['bfd'] = dint('rw_bfd%d' % sq['idx'], [L, 16])
            if sq['g'] == 0:
                sq['s0'], sq['sout'] = self.st_rwkv, None
            else:
                sq['s0'], sq['sout'] = None, self.o_rwkv[sq['idx'] - 1]
        return seqs

    def build(self):
        self.setup()
        for l in range(4):
            if l == 0 and 'rwkv' in self.enable:
                self.rwkv_phase(0, self.rwkv_setup())
                self.first_touch = False
            if l == 1 and 'att' in self.enable:
                self.att_phase(1, self.att_setup())
            if l == 2 and 'hy' in self.enable:
                self.hy_phase(2, self.hy_setup())
            if l == 3 and 'ret' in self.enable:
                self.ret_phase(3, self.ret_setup())
            if 'ffn' in self.enable:
                self.ffn_phase(l)
        self.p.finish()
        return self.nc


def fm(vec):
    v = np.asarray(vec, dtype=np.float32)
    return np.ascontiguousarray(v.reshape(-1, 128).T)


def hm(vec):
    v = np.asarray(vec, dtype=np.float32).reshape(-1)
    return np.ascontiguousarray(v.reshape(16, 64).T)


def rwkv_consts(inp):
    out = {}
    out['rw_mix'] = np.ascontiguousarray(np.stack([fm(inp['rwkv_mix'][i]) for i in range(6)], axis=1))
    z = np.zeros((64, 16), np.float32)
    out['rw_hm'] = np.ascontiguousarray(np.stack([hm(inp['rwkv_w0'][0]), hm(inp['rwkv_w0'][1]), hm(inp['rwkv_a0'][0]), hm(inp['rwkv_a0'][1]),
                                                  hm(inp['rwkv_k_k']), hm(inp['rwkv_k_a']), hm(inp['rwkv_r_k']), z], axis=1))
    rows = np.stack([np.asarray(inp['rwkv_ln_w'], np.float32), np.asarray(inp['rwkv_ln_b'], np.float32)], axis=0)
    out['rw_rows'] = np.ascontiguousarray(np.broadcast_to(rows[None], (128, 2, 1024)))
    s_ = np.arange(128)[:, None]
    t_ = np.arange(128)[None, :]
    m4 = np.zeros((128, 2, 512), np.float32)
    mN = np.zeros((128, 4, 128), np.float32)
    bd32 = np.kron(np.eye(4), np.ones((32, 32))).astype(np.float32)
    mN[:, 2, :] = bd32
    mN[:, 3, :] = 1.0 - bd32
    for d in range(2):
        strict = (s_ < t_) if d == 0 else (s_ > t_)
        incl = (s_ <= t_) if d == 0 else (s_ >= t_)
        m4[:, d, :] = np.concatenate([strict, incl, strict, incl], axis=1).astype(np.float32)
        mN[:, d, :] = strict.T.astype(np.float32) * bd32
    out['rw_mask4'] = m4
    out['rw_maskN'] = mN
    return out


def att_consts(inp, cfg):
    out = {}
    qn = np.asarray(inp['att_q_norm'], np.float32)
    kn = np.asarray(inp['att_k_norm'], np.float32)
    rows = np.concatenate([np.tile(qn[None], (16, 1)), np.tile(kn[None], (4, 1))], axis=0)
    out['at_rows'] = np.ascontiguousarray(np.broadcast_to(rows[None], (128, 20, 64)))
    out['at_sink'] = np.ascontiguousarray(np.broadcast_to(np.asarray(inp['att_sink'], np.float32)[None], (64, 16)))
    L = cfg.LS
    t = np.arange(L)
    row = (t // 64).astype(np.float32)
    col = (t % 64).astype(np.float32)
    nf = 16
    inv = (np.float32(10000.0) ** (-np.arange(nf, dtype=np.float32) / np.float32(nf))).astype(np.float32)
    ang = np.concatenate([row[:, None] * inv[None], col[:, None] * inv[None]], axis=-1)
    ang = np.concatenate([ang, ang], axis=-1).astype(np.float32)
    out['at_cos'] = np.cos(ang).astype(np.float32)
    out['at_sin'] = np.sin(ang).astype(np.float32)
    a = np.arange(128)[:, None]
    b = np.arange(128)[None, :]
    m = np.zeros((128, 2, 128), np.float32)
    m[:, 0, :] = (b <= a)
    m[:, 1, :] = (a <= b)
    out['at_mask'] = m
    return out


def ret_consts(cfg):
    out = {}
    lg = [np.log1p(-np.exp2(-5.0 - np.arange(4, dtype=np.float64))), np.log1p(-np.exp2(-5.5 - np.arange(4, dtype=np.float64)))]
    s_ = np.arange(128)[:, None].astype(np.float64)
    t_ = np.arange(128)[None, :].astype(np.float64)
    dm = np.zeros((128, 4, 2, 128), np.float64)
    qd = np.zeros((128, 4, 2, 128), np.float64)
    kd = np.zeros((128, 4, 2), np.float64)
    i_ = np.arange(128).astype(np.float64)
    for h in range(4):
        dm[:, h, 0, :] = np.where(t_ >= s_, np.exp(lg[0][h] * np.maximum(t_ - s_, 0)), 0.0)
        dm[:, h, 1, :] = np.where(s_ >= t_, np.exp(lg[1][h] * np.maximum(s_ - t_, 0)), 0.0)
        qd[:, h, 0, :] = np.exp(lg[0][h] * (i_ + 1.0))[None, :]
        qd[:, h, 1, :] = np.exp(lg[1][h] * (128.0 - i_))[None, :]
        kd[:, h, 0] = np.exp(lg[0][h] * (127.0 - i_))
        kd[:, h, 1] = np.exp(lg[1][h] * i_)
    out['rt_dmask'] = dm.astype(np.float32)
    out['rt_qdec'] = qd.astype(np.float32)
    out['rt_kdec'] = kd.astype(np.float32)
    L = cfg.LS
    ang = np.repeat((1.0 / (np.float32(10000.0) ** np.linspace(0.0, 1.0, 128, dtype=np.float32))).astype(np.float32), 2)
    ph = (np.arange(L, dtype=np.float32)[:, None] * ang[None, :]).astype(np.float32)
    out['rt_cos'] = np.cos(ph).astype(np.float32)
    out['rt_sin'] = np.sin(ph).astype(np.float32)
    return out


def hy_consts(inp, cfg):
    import ml_dtypes
    out = {}
    z24 = np.zeros((128, 24), np.float32)
    skip = z24.copy()
    skip[:, 0:8] = fm(inp['hy_skip'])
    cw = np.asarray(inp['hy_conv_w'], np.float32)
    out['hy_fm'] = np.ascontiguousarray(np.stack([fm(inp['hy_b_in']), fm(cw[0]), fm(cw[1]), fm(cw[2]), fm(inp['hy_conv_b']), skip], axis=1))
    deltas = np.abs(np.linspace(np.log(1e-2) / 1.5, np.log(1e-2) / 0.3, 1024, dtype=np.float32)).astype(np.float32)
    rows = np.stack([np.asarray(inp['hy_b_out'], np.float32), deltas], axis=0)
    out['hy_rows'] = np.ascontiguousarray(np.broadcast_to(rows[None], (128, 2, 1024)))
    out['hy_fsm'] = np.ascontiguousarray(np.stack([np.asarray(inp[k], np.float32) for k in ('hy_f_b1', 'hy_f_freq1', 'hy_f_b2', 'hy_f_freq2')], axis=1))
    for L in sorted(set([cfg.LS, cfg.LP])):
        t = np.arange(L, dtype=np.float32)
        tn = (t / np.float32(max(L - 1, 1))).astype(np.float32)
        bands = 16
        fr = np.linspace(1e-4, bands - 1, bands, dtype=np.float32)
        ph = (np.float32(2.0 * np.pi) * t[:, None] * fr[None, :] / np.float32(L)).astype(np.float32)
        zpos = np.concatenate([tn[:, None], np.cos(ph), -np.sin(ph)], axis=-1).astype(np.float32)
        out['hy_zpos%d' % L] = np.ascontiguousarray(zpos.T)
        out['hy_tn%d' % L] = np.ascontiguousarray(tn[:, None])
        N = 2 * L
        TC = L // 128
        NFc = TC + 1
        f = np.arange(NFc * 128, dtype=np.int64)
        tt = np.arange(L, dtype=np.int64)
        ang = (2.0 * np.pi / N) * ((f[:, None] * tt[None, :]) % N).astype(np.float64)
        valid = (f <= L).astype(np.float64)[:, None]
        C = np.cos(ang) * valid
        S = np.sin(ang) * valid
        wf = np.where((f == 0) | (f == L), 1.0, 2.0)[:, None] / N
        out['hy_Gc%d' % L] = np.ascontiguousarray((C * wf).astype(np.float32).astype(ml_dtypes.bfloat16))
        out['hy_Gs%d' % L] = np.ascontiguousarray((-S * wf).astype(np.float32).astype(ml_dtypes.bfloat16))
        Ct = C.T.reshape(TC, 128, NFc, 128).transpose(2, 1, 0, 3).reshape(NFc, 128, TC * 128)
        St = S.T.reshape(TC, 128, NFc, 128).transpose(2, 1, 0, 3).reshape(NFc, 128, TC * 128)
        out['hy_Fc%d' % L] = np.ascontiguousarray(Ct.astype(np.float32).astype(ml_dtypes.bfloat16))
        out['hy_Fs%d' % L] = np.ascontiguousarray(St.astype(np.float32).astype(ml_dtypes.bfloat16))
    return out


def make_in_maps(inp, cfg, used):
    maps = []
    adab = np.stack([fm(inp['ada_b'][l]) for l in range(4)], axis=1)
    ident = np.eye(128, dtype=np.float32)
    shared = {k: np.ascontiguousarray(np.asarray(inp[k], dtype=np.float32)) for k in used}
    for i in range(NCORES):
        m = dict(shared)
        m['xs'] = np.ascontiguousarray(inp['x_sample'][i])
        m['xp'] = np.ascontiguousarray(inp['x_prompt'][cfg.NP * i:cfg.NP * (i + 1)].reshape(cfg.NP * cfg.LP, D))
        m['cfm'] = np.ascontiguousarray(np.stack([fm(inp['c'][i]), fm(inp['c_ctx'])], axis=-1))
        m['adab'] = np.ascontiguousarray(adab)
        m['ident'] = ident
        if 'rwkv_w_rkv' in used:
            m.update(rwkv_consts(inp))
            m['st_rwkv'] = np.ascontiguousarray(inp['state_rwkv'][i])
        if 'att_w_qkv' in used:
            if i == 0:
                _att = att_consts(inp, cfg)
            m.update(_att)
            m['ck'] = np.ascontiguousarray(inp['cache_att_k'][i].reshape(cfg.PAST, 256))
            m['cv'] = np.ascontiguousarray(inp['cache_att_v'][i].reshape(cfg.PAST, 256))
        if 'hy_w_in' in used:
            if i == 0:
                _hy = hy_consts(inp, cfg)
            m.update(_hy)
        if 'ret_w_in' in used:
            if i == 0:
                _ret = ret_consts(cfg)
            m.update(_ret)
            m['st_ret'] = np.ascontiguousarray(inp['state_ret'][i])
        maps.append(m)
    return maps


_CACHE = {}


def kernel(**inp):
    cfg = Cfg()
    inp = {k: np.asarray(v) for k, v in inp.items()}
    kb = K(cfg, enable=('rwkv', 'att', 'hy', 'ret', 'ffn'))
    nc = kb.build()
    maps = make_in_maps(inp, cfg, list(kb.W.keys()))
    res = run_bass_kernel_spmd(nc, maps, core_ids=list(range(NCORES)))
    R = res.results
    NP, LP = cfg.NP, cfg.LP
    y_sample = np.stack([R[i]['ys'] for i in range(NCORES)], axis=0)
    y_prompt = np.concatenate([R[i]['yp'].reshape(NP, LP, D) for i in range(NCORES)], axis=0)
    st_rwkv = np.concatenate([R[i]['o_rwkv'] for i in range(NCORES)], axis=0)
    ck = np.concatenate([R[i]['o_k'].reshape(NP, LP, 4, 64) for i in range(NCORES)], axis=0)
    cv = np.concatenate([R[i]['o_v'].reshape(NP, LP, 4, 64) for i in range(NCORES)], axis=0)
    st_ret = np.concatenate([R[i]['o_ret'] for i in range(NCORES)], axis=0)
    return (y_prompt, y_sample, st_rwkv, ck, cv, st_ret)
```

```python
import numpy as np
from contextlib import ExitStack
import concourse.bass as bass
import concourse.mybir as mybir
from concourse.bass_utils import run_bass_kernel_spmd

F32 = mybir.dt.float32
BF16 = mybir.dt.bfloat16
AF = mybir.ActivationFunctionType
ALU = mybir.AluOpType
AX = mybir.AxisListType

D = 1024
NCORES = 8
NDSEM = 8
EPS = 1e-6


class Reg:
    __slots__ = ('lastw', 'reads')

    def __init__(self):
        self.lastw = None
        self.reads = {}


class Prog:
    def __init__(self, nc):
        self.nc = nc
        self.es = ExitStack()
        self.h = {'pe': nc.tensor, 'dve': nc.vector, 'act': nc.scalar, 'pool': nc.gpsimd, 'sp': nc.sync}
        self.cnt = {e: 0 for e in self.h}
        self.known = {e: {} for e in self.h}
        self.sem = {}
        for e in ['pe', 'dve', 'act', 'pool']:
            self.sem[e] = self.es.enter_context(nc.semaphore('s_' + e))
        self.dtot = {}
        self.drr = {}
        for q in ['sp', 'pool', 'act']:
            for i in range(NDSEM):
                k = 'd_%s_%d' % (q, i)
                self.sem[k] = self.es.enter_context(nc.semaphore(k))
                self.dtot[k] = 0
            self.drr[q] = 0
        self.ninst = 0

    def sbuf(self, es, name, shape, dtype):
        self.ninst += 0
        self.nname = getattr(self, 'nname', 0) + 1
        return es.enter_context(self.nc.sbuf_tensor('%s_%d' % (name, self.nname), list(shape), dtype))

    def psum(self, es, name, shape, dtype):
        return es.enter_context(self.nc.psum_tensor(name, list(shape), dtype))

    def _collect(self, eng, reads, writes):
        waits = {}
        kn = self.known[eng]

        def need(ev):
            if ev is None:
                return
            k, v = ev
            if k == 'pe' and eng == 'pe':
                return
            if kn.get(k, 0) >= v:
                return
            if waits.get(k, 0) < v:
                waits[k] = v

        for r in reads:
            need(r.lastw)
        for w in writes:
            need(w.lastw)
            for ev in w.reads.items():
                need(ev)
        for k, v in waits.items():
            kn[k] = v
        return waits

    def op(self, eng, fn, r=(), w=()):
        waits = self._collect(eng, r, w)
        h = self.h[eng]
        for k, v in waits.items():
            h.wait_ge(self.sem[k], v)
        self.cnt[eng] += 1
        fn(h).then_inc(self.sem[eng], 1)
        self.ninst += 1
        ev = (eng, self.cnt[eng])
        for x in r:
            x.reads[ev[0]] = ev[1]
        for x in w:
            x.lastw = ev
            x.reads = {}
        return ev

    def dma(self, q, pairs, r=(), w=(), **kw):
        waits = self._collect(q, r, w)
        i = self.drr[q]
        self.drr[q] = (i + 1) % NDSEM
        k = 'd_%s_%d' % (q, i)
        prev = self.dtot[k]
        if prev > 0 and self.known[q].get(k, 0) < prev:
            waits[k] = max(waits.get(k, 0), prev)
            self.known[q][k] = prev
        h = self.h[q]
        for k2, v2 in waits.items():
            h.wait_ge(self.sem[k2], v2)
        for (o, a) in pairs:
            h.dma_start(out=o, in_=a, **kw).then_inc(self.sem[k], 16)
            self.ninst += 1
        self.dtot[k] = prev + 16 * len(pairs)
        ev = (k, self.dtot[k])
        for x in r:
            x.reads[ev[0]] = ev[1]
        for x in w:
            x.lastw = ev
            x.reads = {}
        return ev

    def barrier(self):
        tot = {}
        for k, v in self.dtot.items():
            if v > 0:
                tot[k] = v
        for e in ['pe', 'dve', 'act', 'pool']:
            if self.cnt[e] > 0:
                tot[e] = self.cnt[e]
        for eng, h in self.h.items():
            for k, v in tot.items():
                if k == eng:
                    continue
                if self.known[eng].get(k, 0) < v:
                    h.wait_ge(self.sem[k], v)
                    self.known[eng][k] = v

    def finish(self):
        self.barrier()
        self.es.close()


class Cfg:
    def __init__(self, LS=4096, LP=256, NP=2, PAST=512):
        self.LS, self.LP, self.NP, self.PAST = LS, LP, NP, PAST


FFN_DENSE = 2816
FFN_EXPERT = 3584
N_EXPERTS = 8

WEIGHT_SHAPES = {
    'ada_w': (4, D, 6 * D),
    'rwkv_w_rkv': (3, D, D), 'rwkv_w1': (2, D, 64), 'rwkv_w2': (2, 64, D),
    'rwkv_a1': (2, D, 64), 'rwkv_a2': (2, 64, D), 'rwkv_g1': (D, 128), 'rwkv_g2': (128, D),
    'rwkv_w_o': (D, D),
    'att_w_qkv': (D, 1536), 'att_w_o': (D, D),
    'hy_w_in': (D, 3 * D), 'hy_w_out': (D, D), 'hy_f_w1': (33, 64), 'hy_f_w2': (64, 64), 'hy_f_w3': (64, 2 * D),
    'ret_w_in': (D, 8192), 'ret_w_out': (2048, D),
    'ffn_w_in': (2, D, 2 * FFN_DENSE), 'ffn_w_out': (2, FFN_DENSE, D),
    'moe_router': (2, D, 8), 'moe_w_in': (2, 8, D, 2 * FFN_EXPERT), 'moe_w_out': (2, 8, FFN_EXPERT, D),
}


class LazyW(dict):
    def __init__(self, k):
        super().__init__()
        self.k = k

    def __missing__(self, name):
        ap = self.k._din(name, WEIGHT_SHAPES[name])
        self[name] = ap
        return ap


class K:
    def __init__(self, cfg, enable=('ffn',)):
        self.cfg = cfg
        self.enable = enable
        nc = self.nc = bass.Bass("TRN2", target_bir_lowering=False)
        self.p = Prog(nc)
        c = cfg
        self.TS = c.LS
        self.TP = c.NP * c.LP

        def din(name, shape, dt=F32):
            return nc.dram_tensor(name, list(shape), dt, kind="ExternalInput").ap()

        def dout(name, shape, dt=F32):
            return nc.dram_tensor(name, list(shape), dt, kind="ExternalOutput").ap()

        self.xs = din('xs', [c.LS, D])
        self.xp = din('xp', [self.TP, D])
        self.cfm = din('cfm', [128, 8, 2])
        self.adab = din('adab', [128, 4, 48])
        self.ident_d = din('ident', [128, 128])
        self._din = din
        self.W = LazyW(self)
        self.ys = dout('ys', [c.LS, D])
        self.yp = dout('yp', [self.TP, D])
        self._dout = dout

        def dint(name, shape, dt=F32):
            return nc.dram_tensor(name, list(shape), dt, kind="Internal").ap()
        self._dint = dint
        self.units = []
        for i in range(c.LS // 128):
            self.units.append(dict(src=self.xs[i * 128:(i + 1) * 128, :], dst=self.ys[i * 128:(i + 1) * 128, :], g=0, reg=Reg()))
        for i in range(self.TP // 128):
            self.units.append(dict(src=self.xp[i * 128:(i + 1) * 128, :], dst=self.yp[i * 128:(i + 1) * 128, :], g=1, reg=Reg()))
        self.first_touch = True

    def setup(self):
        p, nc = self.p, self.nc
        es = p.es
        self.identF = p.sbuf(es, 'identF', [128, 128], F32)
        self.identB = p.sbuf(es, 'identB', [128, 128], BF16)
        self.onesF = p.sbuf(es, 'onesF', [128, 128], F32)
        self.mod = p.sbuf(es, 'mod', [128, 4, 48, 2], F32)
        self.r_const = Reg()
        self.r_mod = Reg()
        self.ps = [p.psum(es, 'ps%d' % i, [128, 512], F32) for i in range(8)]
        self.r_ps = [Reg() for _ in range(8)]
        self.ps_rr = 0
        self.ps_lim = 8
        p.dma('sp', [(self.identF[:, :], self.ident_d)], w=[self.r_const])
        p.op('dve', lambda e: e.tensor_copy(out=self.identB[:, :], in_=self.identF[:, :]), r=[self.r_const], w=[self.r_const])
        p.op('dve', lambda e: e.memset(self.onesF[:, :], 1.0), w=[self.r_const])
        with ExitStack() as s:
            sc = p.sbuf(s, 'sc', [128, 8, 2], F32)
            ab = p.sbuf(s, 'ab', [128, 4, 48], F32)
            wb = [p.sbuf(s, 'adaw%d' % i, [128, 8, 512], F32) for i in range(2)]
            r_sc, r_ab = Reg(), Reg()
            r_wb = [Reg(), Reg()]
            p.dma('sp', [(sc[:, :, :], self.cfm)], w=[r_sc])
            p.dma('sp', [(ab[:, :, :], self.adab)], w=[r_ab])
            p.op('act', lambda e: e.activation(out=sc[:, :, :], in_=sc[:, :, :], func=AF.Silu), r=[r_sc], w=[r_sc])
            it = 0
            for l in range(4):
                for blk in range(12):
                    b = it % 2
                    it += 1
                    src = self.W['ada_w'][l, :, blk * 512:(blk + 1) * 512].rearrange("(c p) n -> p c n", p=128)
                    p.dma('sp', [(wb[b][:, :, :], src)], w=[r_wb[b]])
                    pb, rpb = self.next_ps()
                    for sub in range(4):
                        for c in range(8):
                            p.op('pe', lambda e, c=c, sub=sub, b=b, pb=pb: e.matmul(
                                pb[:, sub * 2:sub * 2 + 2], lhsT=wb[b][:, c, sub * 128:(sub + 1) * 128], rhs=sc[:, c, :],
                                start=(c == 0), stop=(c == 7)), r=[r_wb[b], r_sc], w=[rpb])
                    for sub in range(4):
                        jc = blk * 4 + sub
                        p.op('dve', lambda e, sub=sub, jc=jc, l=l, pb=pb: e.tensor_scalar(
                            out=self.mod[:, l, jc, :], in0=pb[:, sub * 2:sub * 2 + 2], scalar1=ab[:, l, jc:jc + 1],
                            scalar2=None, op0=ALU.add), r=[rpb, r_ab], w=[self.r_mod])
            for j in (1, 4):
                p.op('dve', lambda e, j=j: e.tensor_scalar(
                    out=self.mod[:, :, j * 8:(j + 1) * 8, :], in0=self.mod[:, :, j * 8:(j + 1) * 8, :],
                    scalar1=1.0, scalar2=None, op0=ALU.add), r=[self.r_mod], w=[self.r_mod])
            p.barrier()

    def next_ps(self):
        i = self.ps_rr % self.ps_lim
        self.ps_rr = (i + 1) % self.ps_lim
        return self.ps[i], self.r_ps[i]

    def modap(self, l, j, c, g):
        return self.mod[:, l, j * 8 + c, g:g + 1]

    def gate_bc(self, out_tile, r_out, l, j, g, diag, r_diag):
        p = self.p
        for half in range(2):
            pb, rpb = self.next_ps()
            for cc in range(4):
                c = half * 4 + cc
                p.op('dve', lambda e, c=c: e.tensor_scalar(out=diag[:, :], in0=self.identF[:, :], scalar1=self.modap(l, j, c, g),
                                                            scalar2=None, op0=ALU.mult), r=[self.r_const, self.r_mod], w=[r_diag])
                p.op('pe', lambda e, cc=cc, pb=pb: e.matmul(pb[:, cc * 128:(cc + 1) * 128], lhsT=self.onesF[:, :], rhs=diag[:, :],
                                                            start=True, stop=True), r=[r_diag, self.r_const], w=[rpb])
            p.op('act', lambda e, half=half, pb=pb: e.activation(out=out_tile[:, half * 512:(half + 1) * 512], in_=pb[:, :], func=AF.Copy),
                 r=[rpb], w=[r_out])

    def norm_hT(self, xt, r_xt, l, jsh, g, hT_dst, r_hT, scr, hTf=None, r_hTf=None):
        p = self.p
        ss, xn, junk, r_ss, r_xn, r_junk = scr
        p.op('act', lambda e: e.activation(out=junk[:, :], in_=xt, func=AF.Square, accum_out=ss[:, 0:1]), r=[r_xt], w=[r_junk, r_ss])
        p.op('act', lambda e: e.activation(out=ss[:, 0:1], in_=ss[:, 0:1], func=AF.Sqrt, scale=1.0 / D, bias=EPS), r=[r_ss], w=[r_ss])
        p.op('dve', lambda e: e.reciprocal(out=ss[:, 0:1], in_=ss[:, 0:1]), r=[r_ss], w=[r_ss])
        p.op('act', lambda e: e.activation(out=xn[:, :], in_=xt, func=AF.Copy, scale=ss[:, 0:1]), r=[r_xt, r_ss], w=[r_xn])
        for half in range(2):
            pb, rpb = self.next_ps()
            for cc in range(4):
                c = half * 4 + cc
                p.op('pe', lambda e, c=c, cc=cc, pb=pb: e.transpose(out=pb[:, cc * 128:(cc + 1) * 128], in_=xn[:, c * 128:(c + 1) * 128],
                                                                    identity=self.identF[:, :]), r=[r_xn, self.r_const], w=[rpb])
            for cc in range(4):
                c = half * 4 + cc
                p.op('dve', lambda e, c=c, cc=cc, pb=pb: e.tensor_scalar(
                    out=hT_dst(c), in0=pb[:, cc * 128:(cc + 1) * 128], scalar1=self.modap(l, jsh + 1, c, g),
                    scalar2=self.modap(l, jsh, c, g), op0=ALU.mult, op1=ALU.add), r=[rpb, self.r_mod], w=[r_hT])
                if hTf is not None:
                    p.op('dve', lambda e, c=c, cc=cc, pb=pb: e.tensor_scalar(
                        out=hTf[:, c, :], in0=pb[:, cc * 128:(cc + 1) * 128], scalar1=self.modap(l, jsh + 1, c, g),
                        scalar2=self.modap(l, jsh, c, g), op0=ALU.mult, op1=ALU.add), r=[rpb, self.r_mod], w=[r_hTf])

    def ffn_phase(self, l):
        p, nc = self.p, self.nc
        moe = (l % 2 == 1)
        li = l // 2
        if moe:
            nexp, H = N_EXPERTS, FFN_EXPERT
        else:
            nexp, H = 1, FFN_DENSE
        HB = 512 if moe else 256
        npiece = H // HB
        nu = len(self.units)
        halves = [list(range(0, nu // 2)), list(range(nu // 2, nu))]
        for hu in halves:
            nh = len(hu)
            T = nh * 128
            with ExitStack() as s:
                hT = p.sbuf(s, 'f_hT', [128, 8, T], BF16)
                acc = p.sbuf(s, 'f_acc', [128, nh, D], F32)
                xt = [p.sbuf(s, 'f_xt%d' % i, [128, D], F32) for i in range(2)]
                r_xt = [Reg(), Reg()]
                ss = p.sbuf(s, 'f_ss', [128, 2], F32)
                xn = p.sbuf(s, 'f_xn', [128, D], F32)
                junk = p.sbuf(s, 'f_junk', [128, D], BF16)
                scr = (ss, xn, junk, Reg(), Reg(), Reg())
                gbc = [p.sbuf(s, 'f_gbc%d' % g, [128, D], F32) for g in range(2)]
                r_gbc = [Reg(), Reg()]
                diag = p.sbuf(s, 'f_diag', [128, 128], F32)
                r_diag = Reg()
                win = [p.sbuf(s, 'f_win%d' % i, [128, 8, 2 * HB], BF16) for i in range(2)]
                wout = [p.sbuf(s, 'f_wout%d' % i, [128, HB // 128, D], BF16) for i in range(2)]
                r_w = [Reg(), Reg()]
                sg = [p.sbuf(s, 'f_sg%d' % i, [128, 512], BF16) for i in range(2)]
                hid = [p.sbuf(s, 'f_hid%d' % i, [128, HB // 128, 512], BF16) for i in range(2)]
                r_sg = [Reg(), Reg()]
                r_hid = [Reg(), Reg()]
                r_hT = [Reg() for _ in range(nh)]
                r_acc = [Reg() for _ in range(nh)]
                if moe:
                    hTf = p.sbuf(s, 'f_hTf', [128, 8, 128], F32)
                    r_hTf = Reg()
                    rt = p.sbuf(s, 'f_rt', [128, 8, 8], F32)
                    r_rt = Reg()
                    gates = p.sbuf(s, 'f_gates', [128, nh, 8], F32)
                    r_gates = [Reg() for _ in range(nh)]
                    tk = p.sbuf(s, 'f_tk', [128, 48], F32)
                    r_tk = Reg()
                    p.dma('sp', [(rt[:, :, :], self.W['moe_router'][li].rearrange("(c p) e -> p c e", p=128))], w=[r_rt])
                for g in range(2):
                    self.gate_bc(gbc[g], r_gbc[g], l, 5, g, diag, r_diag)
                for ii, ui in enumerate(hu):
                    u = self.units[ui]
                    b = ii % 2
                    src = u['src'] if self.first_touch else u['dst']
                    p.dma('sp', [(xt[b][:, :], src)], r=[u['reg']], w=[r_xt[b]])
                    dbg = ''
                    norouter = 'norouter' in dbg
                    self.norm_hT(xt[b][:, :], r_xt[b], l, 3, u['g'], lambda c, ii=ii: hT[:, c, ii * 128:(ii + 1) * 128], r_hT[ii], scr,
                                 hTf if (moe and not norouter) else None, r_hTf if moe else None)
                    if moe and norouter:
                        p.op('dve', lambda e, ii=ii: e.memset(gates[:, ii, :], 0.125), w=[r_gates[ii]])
                    elif moe:
                        pb, rpb = self.next_ps()
                        for c in range(8):
                            p.op('pe', lambda e, c=c, pb=pb: e.matmul(pb[:, 0:8], lhsT=hTf[:, c, :], rhs=rt[:, c, :], start=(c == 0), stop=(c == 7)),
                                 r=[r_hTf, r_rt], w=[rpb])
                        if 'nogate' in dbg:
                            p.op('dve', lambda e, ii=ii: e.memset(gates[:, ii, :], 0.125), r=[rpb], w=[r_gates[ii]])
                            continue
                        lg, m1, eq, lg2, m2, sel, ex, sm = (tk[:, 0:8], tk[:, 8:9], tk[:, 9:17], tk[:, 17:25], tk[:, 25:26], tk[:, 26:34],
                                                            tk[:, 34:42], tk[:, 42:43])
                        rr = [r_tk]
                        p.op('dve', lambda e, pb=pb: e.tensor_copy(out=lg, in_=pb[:, 0:8]), r=[rpb], w=rr)
                        p.op('dve', lambda e: e.tensor_reduce(out=m1, in_=lg, axis=AX.X, op=ALU.max), r=rr, w=rr)
                        p.op('dve', lambda e: e.tensor_scalar(out=eq, in0=lg, scalar1=m1, scalar2=-1e30, op0=ALU.is_equal, op1=ALU.mult), r=rr, w=rr)
                        p.op('dve', lambda e: e.tensor_tensor(out=lg2, in0=eq, in1=lg, op=ALU.add), r=rr, w=rr)
                        p.op('dve', lambda e: e.tensor_reduce(out=m2, in_=lg2, axis=AX.X, op=ALU.max), r=rr, w=rr)
                        p.op('dve', lambda e: e.tensor_scalar(out=sel, in0=lg, scalar1=m2, scalar2=None, op0=ALU.is_ge), r=rr, w=rr)
                        p.op('dve', lambda e: e.tensor_scalar(out=m1, in0=m1, scalar1=-1.0, scalar2=None, op0=ALU.mult), r=rr, w=rr)
                        p.op('act', lambda e: e.activation(out=ex, in_=lg, func=AF.Exp, bias=m1, scale=1.0), r=rr, w=rr)
                        p.op('dve', lambda e: e.tensor_tensor(out=ex, in0=ex, in1=sel, op=ALU.mult), r=rr, w=rr)
                        p.op('dve', lambda e: e.tensor_reduce(out=sm, in_=ex, axis=AX.X, op=ALU.add), r=rr, w=rr)
                        p.op('dve', lambda e: e.reciprocal(out=sm, in_=sm), r=rr, w=rr)
                        p.op('dve', lambda e, ii=ii: e.tensor_scalar(out=gates[:, ii, :], in0=ex, scalar1=sm, scalar2=None, op0=ALU.mult),
                             r=rr, w=[r_gates[ii]])
                pc = 0
                chunks = [(t0, min(512, T - t0)) for t0 in range(0, T, 512)]
                for ex_i in range(nexp):
                    if moe:
                        w_in_d = self.W['moe_w_in'][li, ex_i]
                        w_out_d = self.W['moe_w_out'][li, ex_i]
                    else:
                        w_in_d = self.W['ffn_w_in'][li]
                        w_out_d = self.W['ffn_w_out'][li]
                    for pj in range(npiece):
                        b = pc % 2
                        first = (pc == 0)
                        pc += 1
                        h0 = pj * HB
                        p.dma('pool', [(win[b][:, :, 0:HB], w_in_d[:, h0:h0 + HB].rearrange("(c p) n -> p c n", p=128)),
                                       (win[b][:, :, HB:2 * HB], w_in_d[:, H + h0:H + h0 + HB].rearrange("(c p) n -> p c n", p=128)),
                                       (wout[b][:, :, :], w_out_d[h0:h0 + HB, :].rearrange("(c p) n -> p c n", p=128))], w=[r_w[b]])
                        for ci, (t0, tn) in enumerate(chunks):
                            rh = [r_hT[i] for i in range(t0 // 128, (t0 + tn) // 128)]
                            hb = (pc + ci) % 2
                            for hc in range(HB // 128):
                                pg, rpg = self.next_ps()
                                pu, rpu = self.next_ps()
                                for c in range(8):
                                    p.op('pe', lambda e, c=c, hc=hc, pg=pg: e.matmul(pg[:, 0:tn], lhsT=win[b][:, c, hc * 128:(hc + 1) * 128],
                                                                                      rhs=hT[:, c, t0:t0 + tn], start=(c == 0), stop=(c == 7)),
                                         r=rh + [r_w[b]], w=[rpg])
                                for c in range(8):
                                    p.op('pe', lambda e, c=c, hc=hc, pu=pu: e.matmul(pu[:, 0:tn], lhsT=win[b][:, c, HB + hc * 128:HB + (hc + 1) * 128],
                                                                                      rhs=hT[:, c, t0:t0 + tn], start=(c == 0), stop=(c == 7)),
                                         r=rh + [r_w[b]], w=[rpu])
                                sb = hc % 2
                                p.op('act', lambda e, pg=pg, sb=sb: e.activation(out=sg[sb][:, 0:tn], in_=pg[:, 0:tn], func=AF.Silu),
                                     r=[rpg], w=[r_sg[sb]])
                                p.op('dve', lambda e, pu=pu, sb=sb, hc=hc, hb=hb: e.tensor_tensor(out=hid[hb][:, hc, 0:tn], in0=sg[sb][:, 0:tn],
                                                                                                   in1=pu[:, 0:tn], op=ALU.mult),
                                     r=[r_sg[sb], rpu], w=[r_hid[hb]])
                            for st in range(tn // 128):
                                ui_loc = t0 // 128 + st
                                for ch in range(2):
                                    po, rpo = self.next_ps()
                                    for hc in range(HB // 128):
                                        p.op('pe', lambda e, hc=hc, st=st, ch=ch, po=po, hb=hb: e.matmul(
                                            po[:, :], lhsT=hid[hb][:, hc, st * 128:(st + 1) * 128], rhs=wout[b][:, hc, ch * 512:(ch + 1) * 512],
                                            start=(hc == 0), stop=(hc == HB // 128 - 1)), r=[r_hid[hb], r_w[b]], w=[rpo])
                                    accv = acc[:, ui_loc, ch * 512:(ch + 1) * 512]
                                    if moe:
                                        gsc = gates[:, ui_loc, ex_i:ex_i + 1]
                                        rg = [r_gates[ui_loc]]
                                    else:
                                        gsc = 1.0
                                        rg = []
                                    if first:
                                        p.op('dve', lambda e, po=po, accv=accv, gsc=gsc: e.tensor_scalar(out=accv, in0=po[:, :], scalar1=gsc, scalar2=None,
                                                                                                     op0=ALU.mult), r=[rpo] + rg, w=[r_acc[ui_loc]])
                                    else:
                                        p.op('dve', lambda e, po=po, accv=accv, gsc=gsc: e.scalar_tensor_tensor(out=accv, in0=po[:, :], scalar=gsc, in1=accv,
                                                                                                            op0=ALU.mult, op1=ALU.add),
                                             r=[rpo] + rg, w=[r_acc[ui_loc]])
                for ii, ui in enumerate(hu):
                    u = self.units[ui]
                    b = ii % 2
                    src = u['src'] if self.first_touch else u['dst']
                    p.dma('sp', [(xt[b][:, :], src)], r=[u['reg']], w=[r_xt[b]])
                    g = u['g']
                    p.op('dve', lambda e, ii=ii, g=g: e.tensor_tensor(out=acc[:, ii, :], in0=acc[:, ii, :], in1=gbc[g][:, :], op=ALU.mult),
                         r=[r_gbc[g]], w=[r_acc[ii]])
                    p.op('dve', lambda e, ii=ii, b=b: e.tensor_tensor(out=acc[:, ii, :], in0=acc[:, ii, :], in1=xt[b][:, :], op=ALU.add),
                         r=[r_xt[b]], w=[r_acc[ii]])
                    p.dma('sp', [(u['dst'], acc[:, ii, :])], r=[r_acc[ii]], w=[u['reg']])
                p.barrier()
        self.first_touch = False

    def rwkv_phase(self, l, seqs):
        p, nc, W = self.p, self.nc, self.W
        NT = 256
        self.ps_lim = 7
        with ExitStack() as s:
            def sb(name, shape, dt):
                return p.sbuf(s, 'rw_' + name, shape, dt)
            r_wt = Reg()
            Wr, Wk, Wv, Wo = (sb(n, [128, 8, 1024], BF16) for n in ('Wr', 'Wk', 'Wv', 'Wo'))
            W1 = sb('W1', [128, 8, 2, 64], BF16)
            A1 = sb('A1', [128, 8, 2, 64], BF16)
            W2 = sb('W2', [64, 2, 1024], BF16)
            A2 = sb('A2', [64, 2, 1024], BF16)
            G1 = sb('G1', [128, 8, 128], BF16)
            G2 = sb('G2', [128, 1024], BF16)
            wrkv = W['rwkv_w_rkv']
            p.dma('pool', [(Wr[:, :, :], wrkv[0].rearrange("(c p) n -> p c n", p=128)),
                           (Wk[:, :, :], wrkv[1].rearrange("(c p) n -> p c n", p=128)),
                           (Wv[:, :, :], wrkv[2].rearrange("(c p) n -> p c n", p=128)),
                           (Wo[:, :, :], W['rwkv_w_o'].rearrange("(c p) n -> p c n", p=128))], w=[r_wt])
            prs = []
            for d in range(2):
                prs += [(W1[:, :, d, :], W['rwkv_w1'][d].rearrange("(c p) r -> p c r", p=128)),
                        (A1[:, :, d, :], W['rwkv_a1'][d].rearrange("(c p) r -> p c r", p=128)),
                        (W2[:, d, :], W['rwkv_w2'][d]), (A2[:, d, :], W['rwkv_a2'][d])]
            prs += [(G1[:, :, :], W['rwkv_g1'].rearrange("(c p) r -> p c r", p=128)), (G2[:, :], W['rwkv_g2'])]
            p.dma('pool', prs, w=[r_wt])
            mixS = sb('mixS', [128, 6, 8], F32)
            hmv = sb('hmv', [64, 8, 16], F32)
            rows = sb('rows', [128, 2, 1024], F32)
            mask4 = sb('mask4', [128, 2, 512], F32)
            maskN = sb('maskN', [128, 4, 128], F32)
            r_c = Reg()
            p.dma('sp', [(mixS[:, :, :], self.rw_mix), (hmv[:, :, :], self.rw_hm), (rows[:, :, :], self.rw_rows),
                         (mask4[:, :, :], self.rw_mask4), (maskN[:, :, :], self.rw_maskN)], w=[r_c])
            omk = sb('omk', [64, 16], F32)
            p.op('dve', lambda e: e.tensor_scalar(out=omk[:, :], in0=hmv[:, 5, :], scalar1=-1.0, scalar2=1.0, op0=ALU.mult, op1=ALU.add), r=[r_c], w=[r_c])
            ones64 = sb('ones64', [64, 64], BF16)
            onesF = sb('onesF', [64, NT], F32)
            p.op('dve', lambda e: e.memset(ones64[:, :], 1.0), w=[r_c])
            p.op('dve', lambda e: e.memset(onesF[:, :], 1.0), w=[r_c])
            gbc = sb('gbc', [128, 1024], F32)
            r_gbc = Reg()
            diag = sb('diag', [128, 128], F32)
            r_diag = Reg()
            S32 = sb('S32', [64, 16, 2, 64], F32)
            r_S = [[Reg() for _ in range(2)] for _ in range(16)]
            hTt = sb('hTt', [128, 8, NT + 2], BF16); r_hTt = Reg()
            xx = sb('xx', [128, 8, NT], BF16); r_xx = Reg()
            xi = [sb('xi%d' % i, [128, 8, NT], BF16) for i in range(2)]; r_xi = [Reg(), Reg()]
            tmpx = xi[1]; r_tmpx = r_xi[1]
            rH = sb('rH', [64, 16, NT], BF16); r_rH = Reg()
            kH = sb('kH', [64, 16, NT], BF16); r_kH = Reg()
            Vtok = sb('Vtok', [128, NT // 128, 1024], BF16); r_V = Reg()
            hw = sb('hw', [64, NT], BF16); ha = sb('ha', [64, NT], BF16); hg = sb('hg', [128, NT], BF16)
            r_hw, r_ha, r_hg = Reg(), Reg(), Reg()
            ytok = sb('ytok', [128, NT // 128, 1024], F32); r_y = Reg()
            bon = sb('bon', [128, NT // 128, 16], F32); r_bon = Reg()
            TF = ['lw', 'aa', 'kkr', 'nr', 'kk', 't1', 'kd', 'bb', 'G', 'Dd', 'Dl', 'E1', 'E2']
            TB = ['sq', 'pr', 'AT', 'RT', 'BT', 'KT']
            tset = []
            for i in range(2):
                if i == 0:
                    dd = {n: sb('%s%d' % (n, i), [64, NT], F32) for n in TF}
                    dd.update({n: sb('%s%d' % (n, i), [128 if n in ('AT', 'RT', 'BT', 'KT') else 64, NT], BF16) for n in TB})
                    dd['r'] = {n: Reg() for n in TF + TB + ['sc']}
                else:
                    dd = dict(tset[0])
                    dd['r'] = dict(tset[0]['r'])
                    for n in ('AT', 'RT', 'BT', 'KT'):
                        dd[n] = sb('%s%d' % (n, i), [128, NT], BF16)
                        dd['r'][n] = Reg()
                    dd['r']['sc'] = Reg()
                for n in ('AT', 'RT', 'BT', 'KT'):
                    p.op('dve', lambda e, t=dd[n]: e.memset(t[64:128, :], 0.0), w=[r_c])
                dd['sc'] = sb('sc%d' % i, [64, NT // 128, 3], F32)
                tset.append(dd)
            uset = []
            for i in range(2):
                dd = dict(BK=sb('BK%d' % i, [128, 128], BF16), Am=sb('Am%d' % i, [128, 512], BF16), Nm=sb('Nm%d' % i, [128, 128], BF16),
                          TT=[sb('TT%d_%d' % (i, k), [128, 128], BF16) for k in range(2)],
                          PP=[sb('PP%d_%d' % (i, k), [128, 256], BF16) for k in range(2)],
                          Sbf=sb('Sbf%d' % i, [128, 64], BF16), Xsb=sb('Xsb%d' % i, [128, 64], BF16), Usb=sb('Usb%d' % i, [128, 64], BF16),
                          tmpS=sb('tmpS%d' % i, [64, 64], F32), NdT=sb('NdT%d' % i, [128, 128], BF16), NTo=sb('NTo%d' % i, [128, 128], BF16), acc=sb('acc%d' % i, [128, 64], BF16))
                dd['r'] = {n: Reg() for n in ['BK', 'Am', 'Nm', 'TT0', 'TT1', 'PP0', 'PP1', 'Sbf', 'Xsb', 'Usb', 'tmpS', 'NdT', 'NTo', 'acc']}
                p.op('dve', lambda e, t=dd['Sbf']: e.memset(t[64:128, :], 0.0), w=[r_c])
                uset.append(dd)
            xt = sb('xt', [128, 1024], F32); r_xt = Reg()
            yf = sb('yf', [128, 1024], F32); r_yf = Reg()
            bf_ = sb('bf', [128, 16], F32); r_bf = Reg()
            yj = sb('yj', [128, 1024], F32); r_yj = Reg()
            ysq = sb('ysq', [128, 1024], F32); r_ysq = Reg()
            st = sb('st', [128, 4, 16], F32); r_st = Reg()
            gTs = sb('gTs', [128, 8, 128], BF16); r_gTs = Reg()
            ygT = sb('ygT', [128, 8, 128], BF16); r_ygT = Reg()
            osb = yf; r_osb = r_yf
            ss = sb('ss', [128, 2], F32)
            scr = (ss, yj, ysq, Reg(), r_yj, r_ysq)
            hts = sb('hts', [128, 8, 128], BF16); r_hts = Reg()
            zer = sb('zer', [128, 8, 1], BF16); r_zer = Reg()
            p.op('dve', lambda e: e.memset(zer[:, :, :], 0.0), w=[r_zer])
            stio = sb('stio', [64, 64], F32); r_stio = Reg()

            for si, sq_ in enumerate(seqs):
                L, g, units = sq_['L'], sq_['g'], sq_['units']
                hTd, yfd, bfd = sq_['hTd'], sq_['yfd'], sq_['bfd']
                r_hTd, r_yfd, r_bfd = Reg(), Reg(), Reg()
                self.gate_bc(gbc, r_gbc, l, 2, g, diag, r_diag)
                p.dma('sp', [(hTd[:, :, 0:1], zer[:, :, :]), (hTd[:, :, L + 1:L + 2], zer[:, :, :])], r=[r_zer], w=[r_hTd], allow_slow_non_contiguous=True)
                for i, u in enumerate(units):
                    src = u['src'] if self.first_touch else u['dst']
                    p.dma('sp', [(xt[:, :], src)], r=[u['reg']], w=[r_xt])
                    self.norm_hT(xt[:, :], r_xt, l, 0, g, lambda c: hts[:, c, :], r_hts, scr)
                    p.dma('sp', [(hTd[:, :, 1 + i * 128:1 + (i + 1) * 128], hts[:, :, :])], r=[r_hts], w=[r_hTd])
                for h in range(16):
                    for d in range(2):
                        if sq_['s0'] is None:
                            p.op('dve', lambda e, h=h, d=d: e.memset(S32[:, h, d, :], 0.0), w=[r_S[h][d]])
                        else:
                            p.dma('sp', [(stio[:, :], sq_['s0'][d, h])], w=[r_stio])
                            pb, rpb = self.next_ps()
                            p.op('pe', lambda e, pb=pb: e.transpose(out=pb[0:64, 0:64], in_=stio[:, :], identity=self.identF[0:64, 0:64]),
                                 r=[r_stio, self.r_const], w=[rpb])
                            p.op('act', lambda e, pb=pb, h=h, d=d: e.activation(out=S32[:, h, d, :], in_=pb[0:64, 0:64], func=AF.Copy),
                                 r=[rpb], w=[r_S[h][d]])
                ntile = L // NT
                for d in range(2):
                    tiles = list(range(ntile)) if d == 0 else list(range(ntile - 1, -1, -1))
                    for ti in tiles:
                        t0 = ti * NT
                        n = NT
                        nj = n // 128
                        p.dma('sp', [(hTt[:, :, :], hTd[:, :, t0:t0 + n + 2])], r=[r_hTd], w=[r_hTt])
                        p.op('dve', lambda e: e.tensor_tensor(out=tmpx[:, :, :], in0=hTt[:, :, 0:n], in1=hTt[:, :, 2:n + 2], op=ALU.add),
                             r=[r_hTt], w=[r_tmpx])
                        p.op('dve', lambda e: e.scalar_tensor_tensor(out=xx[:, :, :], in0=tmpx[:, :, :], scalar=0.5, in1=hTt[:, :, 1:n + 1],
                                                                      op0=ALU.mult, op1=ALU.subtract), r=[r_tmpx, r_hTt], w=[r_xx])
                        vi = [0]

                        def variant(i):
                            b = vi[0] % 2
                            vi[0] += 1
                            for c in range(8):
                                eng = 'dve' if c % 2 == 0 else 'dve'
                                p.op(eng, lambda e, c=c, b=b, i=i: e.scalar_tensor_tensor(
                                    out=xi[b][:, c, :], in0=xx[:, c, :], scalar=mixS[:, i, c:c + 1], in1=hTt[:, c, 1:n + 1],
                                    op0=ALU.mult, op1=ALU.add), r=[r_xx, r_hTt, r_c], w=[r_xi[b]])
                            return xi[b], r_xi[b]

                        for (i, Wm, dst, rdst) in ((0, Wr, rH, r_rH), (2, Wk, kH, r_kH)):
                            xb, rxb = variant(i)
                            for h in range(16):
                                pb, rpb = self.next_ps()
                                for c in range(8):
                                    p.op('pe', lambda e, c=c, h=h, pb=pb, xb=xb, Wm=Wm: e.matmul(
                                        pb[0:64, 0:n], lhsT=Wm[:, c, h * 64:(h + 1) * 64], rhs=xb[:, c, :], start=(c == 0), stop=(c == 7)),
                                        r=[rxb, r_wt], w=[rpb])
                                p.op('act', lambda e, h=h, pb=pb, dst=dst: e.activation(out=dst[:, h, :], in_=pb[0:64, 0:n], func=AF.Copy),
                                     r=[rpb], w=[rdst])
                        xb, rxb = variant(3)
                        for j in range(nj):
                            for hf in range(2):
                                pb, rpb = self.next_ps()
                                for c in range(8):
                                    p.op('pe', lambda e, c=c, j=j, hf=hf, pb=pb, xb=xb: e.matmul(
                                        pb[:, :], lhsT=xb[:, c, j * 128:(j + 1) * 128], rhs=Wv[:, c, hf * 512:(hf + 1) * 512],
                                        start=(c == 0), stop=(c == 7)), r=[rxb, r_wt], w=[rpb])
                                p.op('act', lambda e, j=j, hf=hf, pb=pb: e.activation(out=Vtok[:, j, hf * 512:(hf + 1) * 512], in_=pb[:, :], func=AF.Copy),
                                     r=[rpb], w=[r_V])
                        for (i, Wm, dst, rdst, fn) in ((1, W1, hw, r_hw, AF.Tanh), (4, A1, ha, r_ha, AF.Copy)):
                            xb, rxb = variant(i)
                            pb, rpb = self.next_ps()
                            for c in range(8):
                                p.op('pe', lambda e, c=c, pb=pb, xb=xb, Wm=Wm: e.matmul(pb[0:64, 0:n], lhsT=Wm[:, c, d, :], rhs=xb[:, c, :],
                                                                                         start=(c == 0), stop=(c == 7)), r=[rxb, r_wt], w=[rpb])
                            p.op('act', lambda e, pb=pb, dst=dst, fn=fn: e.activation(out=dst[:, :], in_=pb[0:64, 0:n], func=fn), r=[rpb], w=[rdst])
                        if d == 1:
                            xb, rxb = variant(5)
                            pb, rpb = self.next_ps()
                            for c in range(8):
                                p.op('pe', lambda e, c=c, pb=pb, xb=xb: e.matmul(pb[:, 0:n], lhsT=G1[:, c, :], rhs=xb[:, c, :],
                                                                                 start=(c == 0), stop=(c == 7)), r=[rxb, r_wt], w=[rpb])
                            p.op('act', lambda e, pb=pb: e.activation(out=hg[:, :], in_=pb[:, 0:n], func=AF.Sigmoid), r=[rpb], w=[r_hg])
                        pbon, rpbon = self.ps[7], self.r_ps[7]
                        for h in range(16):
                            T = tset[h % 2]
                            R = T['r']
                            hs = slice(h * 64, (h + 1) * 64)
                            pb, rpb = self.next_ps()
                            p.op('pe', lambda e, pb=pb, hs=hs: e.matmul(pb[0:64, 0:n], lhsT=W2[:, d, hs], rhs=hw[:, :], start=True, stop=True),
                                 r=[r_hw, r_wt], w=[rpb])
                            p.op('act', lambda e, pb=pb, T=T, h=h: e.activation(out=T['lw'][:, :], in_=pb[0:64, 0:n], func=AF.Sigmoid,
                                                                                 bias=hmv[:, 0 + d, h:h + 1], scale=1.0), r=[rpb, r_c], w=[R['lw']])
                            p.op('pool', lambda e, T=T: e.tensor_scalar(out=T['lw'][:, :], in0=T['lw'][:, :], scalar1=-0.6065306597126334, scalar2=None,
                                                                         op0=ALU.mult), r=[R['lw']], w=[R['lw']])
                            pb, rpb = self.next_ps()
                            p.op('pe', lambda e, pb=pb, hs=hs: e.matmul(pb[0:64, 0:n], lhsT=A2[:, d, hs], rhs=ha[:, :], start=True, stop=True),
                                 r=[r_ha, r_wt], w=[rpb])
                            p.op('act', lambda e, pb=pb, T=T, h=h: e.activation(out=T['aa'][:, :], in_=pb[0:64, 0:n], func=AF.Sigmoid,
                                                                                 bias=hmv[:, 2 + d, h:h + 1], scale=1.0), r=[rpb, r_c], w=[R['aa']])
                            p.op('dve', lambda e, T=T, h=h: e.tensor_scalar(out=T['kkr'][:, :], in0=kH[:, h, :], scalar1=hmv[:, 4, h:h + 1], scalar2=None,
                                                                             op0=ALU.mult), r=[r_kH, r_c], w=[R['kkr']])
                            p.op('pool', lambda e, T=T: e.tensor_tensor(out=T['sq'][:, :], in0=T['kkr'][:, :], in1=T['kkr'][:, :], op=ALU.mult),
                                 r=[R['kkr']], w=[R['sq']])
                            pb, rpb = self.next_ps()
                            p.op('pe', lambda e, pb=pb, T=T: e.matmul(pb[0:64, 0:n], lhsT=ones64[:, :], rhs=T['sq'][:, :], start=True, stop=True),
                                 r=[R['sq'], r_c], w=[rpb])
                            p.op('act', lambda e, pb=pb, T=T: e.activation(out=T['nr'][:, :], in_=pb[0:64, 0:n], func=AF.Sqrt), r=[rpb], w=[R['nr']])
                            p.op('dve', lambda e, T=T: e.tensor_scalar(out=T['nr'][:, :], in0=T['nr'][:, :], scalar1=1e-12, scalar2=None, op0=ALU.max),
                                 r=[R['nr']], w=[R['nr']])
                            p.op('dve', lambda e, T=T: e.reciprocal(out=T['nr'][:, :], in_=T['nr'][:, :]), r=[R['nr']], w=[R['nr']])
                            p.op('dve', lambda e, T=T: e.tensor_tensor(out=T['kk'][:, :], in0=T['kkr'][:, :], in1=T['nr'][:, :], op=ALU.mult),
                                 r=[R['kkr'], R['nr']], w=[R['kk']])
                            p.op('dve', lambda e, T=T, h=h: e.tensor_scalar(out=T['t1'][:, :], in0=T['aa'][:, :], scalar1=hmv[:, 5, h:h + 1],
                                                                             scalar2=omk[:, h:h + 1], op0=ALU.mult, op1=ALU.add),
                                 r=[R['aa'], r_c], w=[R['t1']])
                            p.op('dve', lambda e, T=T, h=h: e.tensor_tensor(out=T['kd'][:, :], in0=T['t1'][:, :], in1=kH[:, h, :], op=ALU.mult),
                                 r=[R['t1'], r_kH], w=[R['kd']])
                            p.op('pool', lambda e, T=T: e.tensor_tensor(out=T['bb'][:, :], in0=T['kk'][:, :], in1=T['aa'][:, :], op=ALU.mult),
                                 r=[R['kk'], R['aa']], w=[R['bb']])
                            p.op('dve', lambda e, T=T, h=h: e.scalar_tensor_tensor(out=T['pr'][:, :], in0=T['kd'][:, :], scalar=hmv[:, 6, h:h + 1],
                                                                                    in1=rH[:, h, :], op0=ALU.mult, op1=ALU.mult),
                                 r=[R['kd'], r_rH, r_c], w=[R['pr']])
                            for j in range(nj):
                                p.op('pe', lambda e, T=T, j=j, h=h: e.matmul(pbon[:, j * 16 + h:j * 16 + h + 1], lhsT=T['pr'][:, j * 128:(j + 1) * 128],
                                                                             rhs=ones64[:, 0:1], start=True, stop=True), r=[R['pr'], r_c], w=[rpbon])
                            p.op('dve', lambda e, T=T: e.tensor_tensor_scan(out=T['G'][:, :], data0=onesF[:, 0:n], data1=T['lw'][:, :], initial=0.0,
                                                                             op0=ALU.mult, op1=ALU.add), r=[R['lw'], r_c], w=[R['G']])
                            for j in range(nj):
                                js = slice(j * 128, (j + 1) * 128)
                                p.op('dve', lambda e, T=T, j=j, js=js: e.tensor_scalar(out=T['Dd'][:, js], in0=T['G'][:, js],
                                                                                       scalar1=T['G'][:, j * 128 + 63:j * 128 + 64], scalar2=None,
                                                                                       op0=ALU.subtract), r=[R['G']], w=[R['Dd']])
                                if j == 0:
                                    p.op('pool', lambda e, T=T, j=j: e.tensor_copy(out=T['sc'][:, j, 0:1], in_=T['G'][:, 63:64]), r=[R['G']], w=[R['sc']])
                                else:
                                    p.op('pool', lambda e, T=T, j=j: e.tensor_tensor(out=T['sc'][:, j, 0:1], in0=T['G'][:, j * 128 + 63:j * 128 + 64],
                                                                                     in1=T['G'][:, j * 128 - 1:j * 128], op=ALU.subtract),
                                         r=[R['G']], w=[R['sc']])
                                p.op('pool', lambda e, T=T, j=j: e.tensor_tensor(out=T['sc'][:, j, 1:2], in0=T['G'][:, j * 128 + 127:j * 128 + 128],
                                                                                 in1=T['G'][:, j * 128 + 63:j * 128 + 64], op=ALU.subtract),
                                     r=[R['G']], w=[R['sc']])
                                p.op('pool', lambda e, T=T, j=j: e.tensor_tensor(out=T['sc'][:, j, 2:3], in0=T['sc'][:, j, 0:1], in1=T['sc'][:, j, 1:2],
                                                                                 op=ALU.add), r=[R['sc']], w=[R['sc']])
                            p.op('act', lambda e, T=T: e.activation(out=T['sc'][:, :, :], in_=T['sc'][:, :, :], func=AF.Exp), r=[R['sc']], w=[R['sc']])
                            p.op('pool', lambda e, T=T: e.tensor_tensor(out=T['Dl'][:, :], in0=T['Dd'][:, :], in1=T['lw'][:, :], op=ALU.subtract),
                                 r=[R['Dd'], R['lw']], w=[R['Dl']])
                            if d == 0:
                                p.op('act', lambda e, T=T: e.activation(out=T['E1'][:, :], in_=T['Dl'][:, :], func=AF.Exp), r=[R['Dl']], w=[R['E1']])
                                p.op('dve', lambda e, T=T: e.scalar_tensor_tensor(out=T['AT'][0:64, :], in0=T['kk'][:, :], scalar=-1.0, in1=T['E1'][:, :],
                                                                                   op0=ALU.mult, op1=ALU.mult), r=[R['kk'], R['E1']], w=[R['AT']])
                                p.op('act', lambda e, T=T: e.activation(out=T['E2'][:, :], in_=T['Dd'][:, :], func=AF.Exp), r=[R['Dd']], w=[R['E2']])
                                p.op('dve', lambda e, T=T, h=h: e.tensor_tensor(out=T['RT'][0:64, :], in0=rH[:, h, :], in1=T['E2'][:, :], op=ALU.mult),
                                     r=[r_rH, R['E2']], w=[R['RT']])
                                p.op('act', lambda e, T=T: e.activation(out=T['E1'][:, :], in_=T['Dd'][:, :], func=AF.Exp, scale=-1.0), r=[R['Dd'], R['AT']], w=[R['E1']])
                                p.op('dve', lambda e, T=T: e.tensor_tensor(out=T['BT'][0:64, :], in0=T['bb'][:, :], in1=T['E1'][:, :], op=ALU.mult),
                                     r=[R['bb'], R['E1']], w=[R['BT']])
                                p.op('pool', lambda e, T=T: e.tensor_tensor(out=T['KT'][0:64, :], in0=T['kd'][:, :], in1=T['E1'][:, :], op=ALU.mult),
                                     r=[R['kd'], R['E1']], w=[R['KT']])
                            else:
                                p.op('act', lambda e, T=T: e.activation(out=T['E1'][:, :], in_=T['Dd'][:, :], func=AF.Exp, scale=-1.0), r=[R['Dd']], w=[R['E1']])
                                p.op('dve', lambda e, T=T: e.scalar_tensor_tensor(out=T['AT'][0:64, :], in0=T['kk'][:, :], scalar=-1.0, in1=T['E1'][:, :],
                                                                                   op0=ALU.mult, op1=ALU.mult), r=[R['kk'], R['E1']], w=[R['AT']])
                                p.op('act', lambda e, T=T: e.activation(out=T['E2'][:, :], in_=T['Dl'][:, :], func=AF.Exp, scale=-1.0), r=[R['Dl']], w=[R['E2']])
                                p.op('dve', lambda e, T=T, h=h: e.tensor_tensor(out=T['RT'][0:64, :], in0=rH[:, h, :], in1=T['E2'][:, :], op=ALU.mult),
                                     r=[r_rH, R['E2']], w=[R['RT']])
                                p.op('act', lambda e, T=T: e.activation(out=T['E1'][:, :], in_=T['Dl'][:, :], func=AF.Exp), r=[R['Dl'], R['AT']], w=[R['E1']])
                                p.op('dve', lambda e, T=T: e.tensor_tensor(out=T['BT'][0:64, :], in0=T['bb'][:, :], in1=T['E1'][:, :], op=ALU.mult),
                                     r=[R['bb'], R['E1']], w=[R['BT']])
                                p.op('pool', lambda e, T=T: e.tensor_tensor(out=T['KT'][0:64, :], in0=T['kd'][:, :], in1=T['E1'][:, :], op=ALU.mult),
                                     r=[R['kd'], R['E1']], w=[R['KT']])
                            rATs = [R['AT'], R['RT'], R['BT'], R['KT']]
                            for j in (range(nj) if d == 0 else range(nj - 1, -1, -1)):
                                U = uset[j % 2]
                                UR = U['r']
                                js = slice(j * 128, (j + 1) * 128)
                                em = T['sc'][:, j, 0:1] if d == 0 else T['sc'][:, j, 1:2]
                                e2 = T['sc'][:, j, 1:2] if d == 0 else T['sc'][:, j, 0:1]
                                e1 = T['sc'][:, j, 2:3]
                                pb, rpb = self.next_ps()
                                pv = pb[:, :].bitcast(BF16)
                                p.op('pe', lambda e, pv=pv, T=T, js=js: e.transpose(out=pv[:, 0:64], in_=T['BT'][0:64, js], identity=self.identB[0:64, 0:64]),
                                     r=[R['BT'], self.r_const], w=[rpb])
                                p.op('pe', lambda e, pv=pv, T=T, js=js: e.transpose(out=pv[:, 64:128], in_=T['KT'][0:64, js], identity=self.identB[0:64, 0:64]),
                                     r=[R['KT'], self.r_const], w=[rpb])
                                p.op('act', lambda e, pv=pv, U=U: e.activation(out=U['BK'][:, :], in_=pv[:, 0:128], func=AF.Copy), r=[rpb], w=[UR['BK']])
                                pb, rpb = self.next_ps()
                                for q, (la, ra) in enumerate((('BT', 'AT'), ('BT', 'RT'), ('KT', 'AT'), ('KT', 'RT'))):
                                    p.op('pe', lambda e, pb=pb, q=q, la=la, ra=ra, T=T, js=js: e.matmul(
                                        pb[:, q * 128:(q + 1) * 128], lhsT=T[la][:, js], rhs=T[ra][:, js], start=True, stop=True), r=rATs, w=[rpb])
                                p.op('dve', lambda e, pb=pb, U=U: e.tensor_tensor(out=U['Am'][:, :], in0=pb[:, :], in1=mask4[:, d, :], op=ALU.mult),
                                     r=[rpb, r_c], w=[UR['Am']])
                                pb, rpb = self.next_ps()
                                p.op('pe', lambda e, pb=pb, T=T, js=js: e.matmul(pb[:, 0:128], lhsT=T['AT'][:, js], rhs=T['BT'][:, js], start=True, stop=True),
                                     r=rATs, w=[rpb])
                                p.op('dve', lambda e, pb=pb, U=U: e.tensor_tensor(out=U['Nm'][:, :], in0=pb[:, 0:128], in1=maskN[:, d, :], op=ALU.mult),
                                     r=[rpb, r_c], w=[UR['Nm']])
                                p.op('pool', lambda e, U=U: e.tensor_tensor(out=U['NdT'][:, :], in0=U['Am'][:, 0:128], in1=maskN[:, 2, :], op=ALU.mult),
                                     r=[UR['Am'], r_c], w=[UR['NdT']])
                                p.op('pool', lambda e, U=U: e.tensor_tensor(out=U['NTo'][:, :], in0=U['Am'][:, 0:128], in1=maskN[:, 3, :], op=ALU.mult),
                                     r=[UR['Am'], r_c], w=[UR['NTo']])
                                p.op('pool', lambda e, U=U: e.tensor_tensor(out=U['TT'][0][:, :], in0=U['NdT'][:, :], in1=self.identB[:, :], op=ALU.add),
                                     r=[UR['NdT'], self.r_const], w=[UR['TT0']])
                                Pm, rP = U['Nm'][:, :], UR['Nm']
                                PT, rPT = U['NdT'][:, :], UR['NdT']
                                tcur = 0
                                NIT = 4
                                for it in range(NIT):
                                    pb, rpb = self.next_ps()
                                    PPt, rPP = U['PP'][it % 2], UR['PP%d' % (it % 2)]
                                    p.op('pe', lambda e, pb=pb, Pm=Pm, PT=PT: e.matmul(pb[:, 0:128], lhsT=PT, rhs=Pm, start=True, stop=True), r=[rP, rPT], w=[rpb])
                                    if it < NIT - 1:
                                        p.op('pe', lambda e, pb=pb, Pm=Pm, PT=PT: e.matmul(pb[:, 128:256], lhsT=Pm, rhs=PT, start=True, stop=True), r=[rP, rPT], w=[rpb])
                                    p.op('act', lambda e, pb=pb, PPt=PPt: e.activation(out=PPt[:, :], in_=pb[:, 0:256], func=AF.Copy), r=[rpb], w=[rPP])
                                    pb2, rpb2 = self.next_ps()
                                    TTc, rTTc = U['TT'][tcur], UR['TT%d' % tcur]
                                    TTn, rTTn = U['TT'][1 - tcur], UR['TT%d' % (1 - tcur)]
                                    p.op('pe', lambda e, pb2=pb2, PPt=PPt, TTc=TTc: e.matmul(pb2[:, 0:128], lhsT=PPt[:, 0:128], rhs=TTc[:, :], start=True, stop=True),
                                         r=[rPP, rTTc], w=[rpb2])
                                    p.op('dve', lambda e, pb2=pb2, TTc=TTc, TTn=TTn: e.tensor_tensor(out=TTn[:, :], in0=pb2[:, 0:128], in1=TTc[:, :], op=ALU.add),
                                         r=[rpb2, rTTc], w=[rTTn])
                                    tcur = 1 - tcur
                                    Pm, rP = PPt[:, 0:128], rPP
                                    PT, rPT = PPt[:, 128:256], rPP
                                TTf, rTTf = U['TT'][tcur], UR['TT%d' % tcur]
                                rS = r_S[h][d]
                                p.op('dve', lambda e, U=U, em=em: e.tensor_scalar(out=U['Sbf'][0:64, :], in0=S32[:, h, d, :], scalar1=em, scalar2=None, op0=ALU.mult),
                                     r=[rS, R['sc']], w=[UR['Sbf']])
                                pb, rpb = self.next_ps()
                                p.op('pe', lambda e, pb=pb, T=T, U=U, js=js: e.matmul(pb[:, 0:64], lhsT=T['AT'][:, js], rhs=U['Sbf'][:, :], start=True, stop=False),
                                     r=[R['AT'], UR['Sbf']], w=[rpb])
                                p.op('pe', lambda e, pb=pb, U=U, j=j, hs=hs: e.matmul(pb[:, 0:64], lhsT=U['Am'][:, 256:384], rhs=Vtok[:, j, hs], start=False, stop=True),
                                     r=[UR['Am'], r_V], w=[rpb])
                                p.op('act', lambda e, pb=pb, U=U: e.activation(out=U['Xsb'][:, :], in_=pb[:, 0:64], func=AF.Copy), r=[rpb], w=[UR['Xsb']])
                                pb, rpb = self.next_ps()
                                p.op('pe', lambda e, pb=pb, TTf=TTf, U=U: e.matmul(pb[:, 0:64], lhsT=TTf[:, :], rhs=U['Xsb'][:, :], start=True, stop=True),
                                     r=[rTTf, UR['Xsb']], w=[rpb])
                                p.op('act', lambda e, pb=pb, U=U: e.activation(out=U['Usb'][:, :], in_=pb[:, 0:64], func=AF.Copy), r=[rpb], w=[UR['Usb']])
                                for sweep in range(3):
                                    pb, rpb = self.next_ps()
                                    p.op('pe', lambda e, pb=pb, U=U: e.matmul(pb[:, 0:64], lhsT=U['NTo'][:, :], rhs=U['Usb'][:, :], start=True, stop=True),
                                         r=[UR['NTo'], UR['Usb']], w=[rpb])
                                    p.op('dve', lambda e, pb=pb, U=U: e.tensor_tensor(out=U['acc'][:, :], in0=pb[:, 0:64], in1=U['Xsb'][:, :], op=ALU.add),
                                         r=[rpb, UR['Xsb']], w=[UR['acc']])
                                    pb, rpb = self.next_ps()
                                    p.op('pe', lambda e, pb=pb, TTf=TTf, U=U: e.matmul(pb[:, 0:64], lhsT=TTf[:, :], rhs=U['acc'][:, :], start=True, stop=True),
                                         r=[rTTf, UR['acc']], w=[rpb])
                                    p.op('act', lambda e, pb=pb, U=U: e.activation(out=U['Usb'][:, :], in_=pb[:, 0:64], func=AF.Copy), r=[rpb], w=[UR['Usb']])
                                pb, rpb = self.next_ps()
                                p.op('pe', lambda e, pb=pb, T=T, U=U, js=js: e.matmul(pb[:, 0:64], lhsT=T['RT'][:, js], rhs=U['Sbf'][:, :], start=True, stop=False),
                                     r=[R['RT'], UR['Sbf']], w=[rpb])
                                p.op('pe', lambda e, pb=pb, U=U: e.matmul(pb[:, 0:64], lhsT=U['Am'][:, 128:256], rhs=U['Usb'][:, :], start=False, stop=False),
                                     r=[UR['Am'], UR['Usb']], w=[rpb])
                                p.op('pe', lambda e, pb=pb, U=U, j=j, hs=hs: e.matmul(pb[:, 0:64], lhsT=U['Am'][:, 384:512], rhs=Vtok[:, j, hs], start=False, stop=True),
                                     r=[UR['Am'], r_V], w=[rpb])
                                p.op('act', lambda e, pb=pb, j=j, hs=hs: e.activation(out=ytok[:, j, hs], in_=pb[:, 0:64], func=AF.Copy), r=[rpb], w=[r_y])
                                pb, rpb = self.next_ps()
                                p.op('pe', lambda e, pb=pb, U=U: e.matmul(pb[0:64, 0:64], lhsT=U['BK'][:, 0:64], rhs=U['Usb'][:, :], start=True, stop=False),
                                     r=[UR['BK'], UR['Usb']], w=[rpb])
                                p.op('pe', lambda e, pb=pb, U=U, j=j, hs=hs: e.matmul(pb[0:64, 0:64], lhsT=U['BK'][:, 64:128], rhs=Vtok[:, j, hs], start=False, stop=True),
                                     r=[UR['BK'], r_V], w=[rpb])
                                p.op('dve', lambda e, U=U, e1=e1: e.tensor_scalar(out=U['tmpS'][:, :], in0=S32[:, h, d, :], scalar1=e1, scalar2=None, op0=ALU.mult),
                                     r=[rS, R['sc']], w=[UR['tmpS']])
                                p.op('dve', lambda e, pb=pb, U=U, e2=e2: e.scalar_tensor_tensor(out=S32[:, h, d, :], in0=pb[0:64, 0:64], scalar=e2, in1=U['tmpS'][:, :],
                                                                                               op0=ALU.mult, op1=ALU.add), r=[rpb, UR['tmpS'], R['sc']], w=[rS])
                        p.op('act', lambda e, pbon=pbon: e.activation(out=bon[:, :, :], in_=pbon[:, 0:nj * 16], func=AF.Copy), r=[rpbon], w=[r_bon])
                        if d == 0:
                            p.dma('sp', [(yfd[t0:t0 + n, :].rearrange("(j p) f -> p j f", p=128), ytok[:, :, :]),
                                         (bfd[t0:t0 + n, :].rearrange("(j p) f -> p j f", p=128), bon[:, :, :])], r=[r_y, r_bon], w=[r_yfd, r_bfd])
                        else:
                            for j in range(nj):
                                u = units[(t0 // 128) + j]
                                js = slice(j * 128, (j + 1) * 128)
                                tt = t0 + j * 128
                                p.dma('sp', [(yf[:, :], yfd[tt:tt + 128, :]), (bf_[:, :], bfd[tt:tt + 128, :])], r=[r_yfd, r_bfd], w=[r_yf, r_bf])
                                src = u['src'] if self.first_touch else u['dst']
                                p.dma('sp', [(xt[:, :], src)], r=[u['reg']], w=[r_xt])
                                p.op('dve', lambda e, j=j: e.tensor_tensor(out=yj[:, :], in0=ytok[:, j, :], in1=yf[:, :], op=ALU.add), r=[r_y, r_yf], w=[r_yj])
                                p.op('pool', lambda e, j=j: e.tensor_tensor(out=bf_[:, :], in0=bf_[:, :], in1=bon[:, j, :], op=ALU.add), r=[r_bon], w=[r_bf])
                                yv = yj[:, :].rearrange("p (h k) -> p h k", k=64)
                                sqv = ysq[:, :].rearrange("p (h k) -> p h k", k=64)
                                p.op('act', lambda e: e.activation(out=ysq[:, :], in_=yj[:, :], func=AF.Square), r=[r_yj], w=[r_ysq])
                                p.op('dve', lambda e, yv=yv: e.tensor_reduce(out=st[:, 0, :], in_=yv, axis=AX.X, op=ALU.add), r=[r_yj], w=[r_st])
                                p.op('dve', lambda e, sqv=sqv: e.tensor_reduce(out=st[:, 1, :], in_=sqv, axis=AX.X, op=ALU.add), r=[r_ysq], w=[r_st])
                                p.op('dve', lambda e: e.tensor_scalar(out=st[:, 0, :], in0=st[:, 0, :], scalar1=1.0 / 64, scalar2=None, op0=ALU.mult), r=[r_st], w=[r_st])
                                p.op('dve', lambda e: e.tensor_tensor(out=st[:, 2, :], in0=st[:, 0, :], in1=st[:, 0, :], op=ALU.mult), r=[r_st], w=[r_st])
                                p.op('dve', lambda e: e.scalar_tensor_tensor(out=st[:, 1, :], in0=st[:, 1, :], scalar=1.0 / 64, in1=st[:, 2, :],
                                                                              op0=ALU.mult, op1=ALU.subtract), r=[r_st], w=[r_st])
                                p.op('act', lambda e: e.activation(out=st[:, 1, :], in_=st[:, 1, :], func=AF.Sqrt, bias=64e-5, scale=1.0), r=[r_st], w=[r_st])
                                p.op('dve', lambda e: e.reciprocal(out=st[:, 1, :], in_=st[:, 1, :]), r=[r_st], w=[r_st])
                                mub = st[:, 0, :].unsqueeze(2).to_broadcast([128, 16, 64])
                                rsb = st[:, 1, :].unsqueeze(2).to_broadcast([128, 16, 64])
                                bfb = bf_[:, :].unsqueeze(2).to_broadcast([128, 16, 64])
                                p.op('dve', lambda e, yv=yv, mub=mub: e.tensor_tensor(out=yv, in0=yv, in1=mub, op=ALU.subtract), r=[r_st], w=[r_yj])
                                p.op('dve', lambda e, yv=yv, rsb=rsb: e.tensor_tensor(out=yv, in0=yv, in1=rsb, op=ALU.mult), r=[r_st], w=[r_yj])
                                p.op('dve', lambda e: e.tensor_tensor(out=yj[:, :], in0=yj[:, :], in1=rows[:, 0, :], op=ALU.mult), r=[r_c], w=[r_yj])
                                p.op('dve', lambda e: e.tensor_tensor(out=yj[:, :], in0=yj[:, :], in1=rows[:, 1, :], op=ALU.add), r=[r_c], w=[r_yj])
                                vv = Vtok[:, j, :].rearrange("p (h k) -> p h k", k=64)
                                p.op('dve', lambda e, sqv=sqv, vv=vv, bfb=bfb: e.tensor_tensor(out=sqv, in0=vv, in1=bfb, op=ALU.mult), r=[r_V, r_bf], w=[r_ysq])
                                p.op('dve', lambda e: e.tensor_tensor(out=yj[:, :], in0=yj[:, :], in1=ysq[:, :], op=ALU.add), r=[r_ysq], w=[r_yj])
                                for half in range(2):
                                    pbg, rpbg = self.next_ps()
                                    for cc in range(4):
                                        c = half * 4 + cc
                                        p.op('pe', lambda e, c=c, cc=cc, pbg=pbg, js=js: e.matmul(pbg[:, cc * 128:(cc + 1) * 128], lhsT=G2[:, c * 128:(c + 1) * 128],
                                                                                                   rhs=hg[:, js], start=True, stop=True), r=[r_hg, r_wt], w=[rpbg])
                                    p.op('act', lambda e, half=half, pbg=pbg: e.activation(out=gTs[:, half * 4:(half + 1) * 4, :], in_=pbg[:, :], func=AF.Copy),
                                         r=[rpbg], w=[r_gTs])
                                    pbt, rpbt = self.next_ps()
                                    for cc in range(4):
                                        c = half * 4 + cc
                                        p.op('pe', lambda e, c=c, cc=cc, pbt=pbt: e.transpose(out=pbt[:, cc * 128:(cc + 1) * 128], in_=yj[:, c * 128:(c + 1) * 128],
                                                                                              identity=self.identF[:, :]), r=[r_yj, self.r_const], w=[rpbt])
                                    p.op('dve', lambda e, half=half, pbt=pbt: e.tensor_tensor(out=ygT[:, half * 4:(half + 1) * 4, :], in0=pbt[:, :],
                                                                                              in1=gTs[:, half * 4:(half + 1) * 4, :], op=ALU.mult),
                                         r=[rpbt, r_gTs], w=[r_ygT])
                                for hf in range(2):
                                    pbo, rpbo = self.next_ps()
                                    for c in range(8):
                                        p.op('pe', lambda e, c=c, hf=hf, pbo=pbo: e.matmul(pbo[:, :], lhsT=ygT[:, c, :], rhs=Wo[:, c, hf * 512:(hf + 1) * 512],
                                                                                           start=(c == 0), stop=(c == 7)), r=[r_ygT, r_wt], w=[rpbo])
                                    p.op('dve', lambda e, hf=hf, pbo=pbo: e.tensor_tensor(out=osb[:, hf * 512:(hf + 1) * 512], in0=pbo[:, :],
                                                                                          in1=gbc[:, hf * 512:(hf + 1) * 512], op=ALU.mult), r=[rpbo, r_gbc], w=[r_osb])
                                p.op('dve', lambda e: e.tensor_tensor(out=osb[:, :], in0=osb[:, :], in1=xt[:, :], op=ALU.add), r=[r_xt], w=[r_osb])
                                p.dma('sp', [(u['dst'], osb[:, :])], r=[r_osb], w=[u['reg']])
                if sq_['sout'] is not None:
                    for h in range(16):
                        for d in range(2):
                            pb, rpb = self.next_ps()
                            p.op('pe', lambda e, pb=pb, h=h, d=d: e.transpose(out=pb[0:64, 0:64], in_=S32[:, h, d, :], identity=self.identF[0:64, 0:64]),
                                 r=[r_S[h][d], self.r_const], w=[rpb])
                            p.op('act', lambda e, pb=pb: e.activation(out=stio[:, :], in_=pb[0:64, 0:64], func=AF.Copy), r=[rpb], w=[r_stio])
                            p.dma('sp', [(sq_['sout'][d, h], stio[:, :])], r=[r_stio])
            p.barrier()
        self.ps_lim = 8

    def att_setup(self):
        c = self.cfg
        din, dout, dint = self._din, self._dout, self._dint
        self.at_rows = din('at_rows', [128, 20, 64])
        self.at_sink = din('at_sink', [64, 16])
        self.at_cos = din('at_cos', [c.LS, 64])
        self.at_sin = din('at_sin', [c.LS, 64])
        self.at_mask = din('at_mask', [128, 2, 128])
        self.ck = din('ck', [c.PAST, 256])
        self.cv = din('cv', [c.PAST, 256])
        self.o_k = dout('o_k', [self.TP, 256])
        self.o_v = dout('o_v', [self.TP, 256])
        seqs = self.make_seqs()
        for sq in seqs:
            L = sq['L']
            sq['qTd'] = dint('at_qTd%d' % sq['idx'], [64, 16, L], BF16)
            sq['kTd'] = dint('at_kTd%d' % sq['idx'], [64, 4, L], BF16)
            sq['vd'] = dint('at_vd%d' % sq['idx'], [L, 256], BF16)
        return seqs

    def att_phase(self, l, seqs):
        p, nc, W = self.p, self.nc, self.W
        c = self.cfg
        NCB = c.PAST // 128
        self.ps_lim = 6
        with ExitStack() as s:
            def sb(name, shape, dt):
                return p.sbuf(s, 'at_' + name, shape, dt)
            r_wt = Reg()
            Wqkv = sb('Wqkv', [128, 8, 1536], BF16)
            WoH = sb('WoH', [64, 16, 1024], BF16)
            p.dma('pool', [(Wqkv[:, :, :], W['att_w_qkv'].rearrange("(c p) n -> p c n", p=128)),
                           (WoH[:, :, :], W['att_w_o'].rearrange("(h p) n -> p h n", p=64))], w=[r_wt])
            rowsN = sb('rowsN', [128, 20, 64], F32)
            sinkE = sb('sinkE', [64, 16], F32)
            bmask = sb('bmask', [128, 2, 128], F32)
            r_c = Reg()
            p.dma('sp', [(rowsN[:, :, :], self.at_rows), (sinkE[:, :], self.at_sink), (bmask[:, :, :], self.at_mask)], w=[r_c])
            p.op('act', lambda e: e.activation(out=sinkE[:, :], in_=sinkE[:, :], func=AF.Exp), r=[r_c], w=[r_c])
            ones128 = sb('ones128', [128, 64], BF16)
            p.op('dve', lambda e: e.memset(ones128[:, :], 1.0), w=[r_c])
            ckT = sb('ckT', [64, 4, c.PAST], BF16); r_ckT = Reg()
            cvt = sb('cvt', [128, NCB, 256], BF16); r_cvt = Reg()
            ckt = sb('ckt', [128, NCB, 256], F32); r_ckt = Reg()
            p.dma('sp', [(ckt[:, :, :], self.ck.rearrange("(b p) f -> p b f", p=128))], w=[r_ckt])
            p.dma('pool', [(cvt[:, :, :], self.cv.rearrange("(b p) f -> p b f", p=128))], w=[r_cvt])
            for b in range(NCB):
                pb, rpb = self.next_ps()
                for g in range(4):
                    p.op('pe', lambda e, pb=pb, b=b, g=g: e.transpose(out=pb[0:64, g * 128:(g + 1) * 128], in_=ckt[:, b, g * 64:(g + 1) * 64],
                                                                      identity=self.identF[:, :]), r=[r_ckt, self.r_const], w=[rpb])
                p.op('act', lambda e, pb=pb, b=b: e.activation(out=ckT[:, :, b * 128:(b + 1) * 128],
                                                               in_=pb[0:64, :].rearrange("p (g t) -> p g t", g=4), func=AF.Copy), r=[rpb], w=[r_ckT])
            gbc = sb('gbc', [128, 1024], F32); r_gbc = Reg()
            diag = sb('diag', [128, 128], F32); r_diag = Reg()
            xt = sb('xt', [128, 1024], F32); r_xt = Reg()
            ss = sb('ss', [128, 2], F32); xn = sb('xn', [128, 1024], F32); junk = sb('junk', [128, 1024], BF16)
            scr = (ss, xn, junk, Reg(), Reg(), Reg())
            hts = sb('hts', [128, 8, 128], BF16); r_hts = Reg()
            qkv = sb('qkv', [128, 1536], F32); r_qkv = Reg()
            sq2 = sb('sq2', [128, 1280], F32); r_sq2 = Reg()
            rs = sb('rs', [128, 20], F32); r_rs = Reg()
            rot = sb('rot', [128, 1280], F32); r_rot = Reg()
            cs = sb('cs', [128, 2, 64], F32); r_cs = Reg()
            qTs = sb('qTs', [64, 20, 128], BF16); r_qTs = Reg()
            vbf = sb('vbf', [128, 256], BF16); r_vbf = Reg()
            qTb = sb('qTb', [64, 16, 128], BF16); r_qTb = Reg()
            kTb = sb('kTb', [64, 4, 384], BF16); r_kTb = Reg()
            vb = sb('vb', [128, 3, 256], BF16); r_vb = Reg()
            Et = [sb('E%d' % i, [128, 512], BF16) for i in range(3)]; r_E = [Reg() for _ in range(3)]
            OT = sb('OT', [64, 16, 128], BF16); r_OT = Reg()
            den = sb('den', [64, 512], F32); r_den = Reg()
            osb = sb('osb', [128, 1024], F32); r_osb = Reg()

            for sq_ in seqs:
                L, g_, units = sq_['L'], sq_['g'], sq_['units']
                qTd, kTd, vd = sq_['qTd'], sq_['kTd'], sq_['vd']
                r_sd = Reg()
                latent = (g_ == 0)
                self.gate_bc(gbc, r_gbc, l, 2, g_, diag, r_diag)
                nb = L // 128
                for i, u in enumerate(units):
                    p.dma('sp', [(xt[:, :], u['src'] if self.first_touch else u['dst'])], r=[u['reg']], w=[r_xt])
                    self.norm_hT(xt[:, :], r_xt, l, 0, g_, lambda cc: hts[:, cc, :], r_hts, scr)
                    for part in range(3):
                        pb, rpb = self.next_ps()
                        for cc in range(8):
                            p.op('pe', lambda e, cc=cc, pb=pb, part=part: e.matmul(pb[:, :], lhsT=hts[:, cc, :], rhs=Wqkv[:, cc, part * 512:(part + 1) * 512],
                                                                                   start=(cc == 0), stop=(cc == 7)), r=[r_hts, r_wt], w=[rpb])
                        p.op('act', lambda e, pb=pb, part=part: e.activation(out=qkv[:, part * 512:(part + 1) * 512], in_=pb[:, :], func=AF.Copy), r=[rpb], w=[r_qkv])
                    qk3 = qkv[:, 0:1280].rearrange("p (h k) -> p h k", k=64)
                    sq3 = sq2[:, :].rearrange("p (h k) -> p h k", k=64)
                    p.op('act', lambda e: e.activation(out=sq2[:, :], in_=qkv[:, 0:1280], func=AF.Square), r=[r_qkv], w=[r_sq2])
                    p.op('dve', lambda e, sq3=sq3: e.tensor_reduce(out=rs[:, :], in_=sq3, axis=AX.X, op=ALU.add), r=[r_sq2], w=[r_rs])
                    p.op('act', lambda e: e.activation(out=rs[:, :], in_=rs[:, :], func=AF.Sqrt, scale=1.0 / 64, bias=EPS), r=[r_rs], w=[r_rs])
                    p.op('dve', lambda e: e.reciprocal(out=rs[:, :], in_=rs[:, :]), r=[r_rs], w=[r_rs])
                    rsb = rs[:, :].unsqueeze(2).to_broadcast([128, 20, 64])
                    p.op('dve', lambda e, qk3=qk3, rsb=rsb: e.tensor_tensor(out=qk3, in0=qk3, in1=rsb, op=ALU.mult), r=[r_rs], w=[r_qkv])
                    p.op('dve', lambda e, qk3=qk3: e.tensor_tensor(out=qk3, in0=qk3, in1=rowsN[:, :, :], op=ALU.mult), r=[r_c], w=[r_qkv])
                    if latent:
                        t0 = i * 128
                        p.dma('sp', [(cs[:, 0, :], self.at_cos[t0:t0 + 128, :]), (cs[:, 1, :], self.at_sin[t0:t0 + 128, :])], w=[r_cs])
                        rot3 = rot[:, :].rearrange("p (h k) -> p h k", k=64)
                        cosb = cs[:, 0, :].unsqueeze(1).to_broadcast([128, 20, 64])
                        for (lo, hi, sgn) in ((0, 32, -1.0), (32, 64, 1.0)):
                            olo, ohi = (32, 64) if lo == 0 else (0, 32)
                            sinb = cs[:, 1, lo:hi].unsqueeze(1).to_broadcast([128, 20, 32])
                            p.op('dve', lambda e, rot3=rot3, qk3=qk3, sinb=sinb, lo=lo, hi=hi, olo=olo, ohi=ohi, sgn=sgn: e.scalar_tensor_tensor(
                                out=rot3[:, :, lo:hi], in0=qk3[:, :, olo:ohi], scalar=sgn, in1=sinb, op0=ALU.mult, op1=ALU.mult), r=[r_qkv, r_cs], w=[r_rot])
                        p.op('dve', lambda e, qk3=qk3, cosb=cosb: e.tensor_tensor(out=qk3, in0=qk3, in1=cosb, op=ALU.mult), r=[r_cs, r_rot], w=[r_qkv])
                        p.op('dve', lambda e: e.tensor_tensor(out=qkv[:, 0:1280], in0=qkv[:, 0:1280], in1=rot[:, :], op=ALU.add), r=[r_rot], w=[r_qkv])
                    else:
                        pi = sq_['idx'] - 1
                        r0 = pi * L + i * 128
                        p.dma('sp', [(self.o_k[r0:r0 + 128, :], qkv[:, 1024:1280]), (self.o_v[r0:r0 + 128, :], qkv[:, 1280:1536])], r=[r_qkv])
                    for hh in range(5):
                        pb, rpb = self.next_ps()
                        for q4 in range(4):
                            hd = hh * 4 + q4
                            p.op('pe', lambda e, pb=pb, q4=q4, hd=hd: e.transpose(out=pb[0:64, q4 * 128:(q4 + 1) * 128], in_=qkv[:, hd * 64:(hd + 1) * 64],
                                                                                  identity=self.identF[:, :]), r=[r_qkv, self.r_const], w=[rpb])
                        p.op('act', lambda e, pb=pb, hh=hh: e.activation(out=qTs[:, hh * 4:(hh + 1) * 4, :], in_=pb[0:64, :].rearrange("p (g t) -> p g t", g=4),
                                                                         func=AF.Copy), r=[rpb], w=[r_qTs])
                    p.op('act', lambda e: e.activation(out=vbf[:, :], in_=qkv[:, 1280:1536], func=AF.Copy), r=[r_qkv], w=[r_vbf])
                    ts = slice(i * 128, (i + 1) * 128)
                    p.dma('sp', [(qTd[:, :, ts], qTs[:, 0:16, :]), (kTd[:, :, ts], qTs[:, 16:20, :]), (vd[ts, :], vbf[:, :])], r=[r_qTs, r_vbf], w=[r_sd])
                for i, u in enumerate(units):
                    ts = slice(i * 128, (i + 1) * 128)
                    if latent:
                        kblocks = [bb for bb in (i - 1, i, i + 1) if 0 <= bb < nb]
                    else:
                        kblocks = list(range(nb))
                    k0 = kblocks[0]
                    nkb = len(kblocks)
                    pr = [(qTb[:, :, :], qTd[:, :, ts]), (kTb[:, :, 0:nkb * 128], kTd[:, :, k0 * 128:(k0 + nkb) * 128]),
                          (vb[:, 0:nkb, :], vd[k0 * 128:(k0 + nkb) * 128, :].rearrange("(b p) f -> p b f", p=128))]
                    p.dma('sp', pr, r=[r_sd], w=[r_qTb, r_kTb, r_vb])
                    p.dma('sp', [(xt[:, :], u['src'] if self.first_touch else u['dst'])], r=[u['reg']], w=[r_xt])
                    ei = 0
                    for g in range(4):
                        po, rpo = self.ps[6], self.r_ps[6]
                        pd, rpd = self.ps[7], self.r_ps[7]
                        klist = []
                        if latent:
                            for b in range(NCB):
                                klist.append((ckT[:, g, b * 128:(b + 1) * 128], cvt[:, b, g * 64:(g + 1) * 64], None, [r_ckT, r_cvt]))
                        for bi, bb in enumerate(kblocks):
                            mk = None
                            if latent and bb == i - 1:
                                mk = 0
                            if latent and bb == i + 1:
                                mk = 1
                            klist.append((kTb[:, g, bi * 128:(bi + 1) * 128], vb[:, bi, g * 64:(g + 1) * 64], mk, [r_kTb, r_vb]))
                        qrhs = qTb[:, g * 4:(g + 1) * 4, :]
                        for ki, (kap, vap, mk, rk) in enumerate(klist):
                            psc, rpsc = self.next_ps()
                            p.op('pe', lambda e, psc=psc, kap=kap, qrhs=qrhs: e.matmul(psc[:, :], lhsT=kap, rhs=qrhs, start=True, stop=True), r=rk + [r_qTb], w=[rpsc])
                            E, rE = Et[ei % 3], r_E[ei % 3]
                            ei += 1
                            p.op('act', lambda e, psc=psc, E=E: e.activation(out=E[:, :], in_=psc[:, :], func=AF.Exp, scale=0.125), r=[rpsc], w=[rE])
                            if mk is not None:
                                E3 = E[:, :].rearrange("p (r t) -> p r t", r=4)
                                mb = bmask[:, mk, :].unsqueeze(1).to_broadcast([128, 4, 128])
                                p.op('dve', lambda e, E3=E3, mb=mb: e.tensor_tensor(out=E3, in0=E3, in1=mb, op=ALU.mult), r=[r_c], w=[rE])
                            p.op('pe', lambda e, po=po, vap=vap, E=E, ki=ki: e.matmul(po[0:64, :], lhsT=vap, rhs=E[:, :], start=(ki == 0), stop=(ki == len(klist) - 1)),
                                 r=rk + [rE], w=[rpo])
                            p.op('pe', lambda e, pd=pd, E=E, ki=ki: e.matmul(pd[0:64, :], lhsT=ones128[:, :], rhs=E[:, :], start=(ki == 0), stop=(ki == len(klist) - 1)),
                                 r=[rE, r_c], w=[rpd])
                        for rr in range(4):
                            hd = g * 4 + rr
                            p.op('dve', lambda e, pd=pd, rr=rr, hd=hd: e.tensor_scalar(out=den[:, rr * 128:(rr + 1) * 128], in0=pd[0:64, rr * 128:(rr + 1) * 128],
                                                                                        scalar1=sinkE[:, hd:hd + 1], scalar2=None, op0=ALU.add), r=[rpd, r_c], w=[r_den])
                        p.op('dve', lambda e: e.reciprocal(out=den[:, :], in_=den[:, :]), r=[r_den], w=[r_den])
                        p.op('dve', lambda e, po=po, g=g: e.tensor_tensor(out=OT[:, g * 4:(g + 1) * 4, :], in0=po[0:64, :].rearrange("p (r t) -> p r t", r=4),
                                                                          in1=den[:, :].rearrange("p (r t) -> p r t", r=4), op=ALU.mult), r=[rpo, r_den], w=[r_OT])
                    for hf in range(2):
                        pbo, rpbo = self.next_ps()
                        for hd in range(16):
                            p.op('pe', lambda e, hd=hd, hf=hf, pbo=pbo: e.matmul(pbo[:, :], lhsT=OT[:, hd, :], rhs=WoH[:, hd, hf * 512:(hf + 1) * 512],
                                                                                 start=(hd == 0), stop=(hd == 15)), r=[r_OT, r_wt], w=[rpbo])
                        p.op('dve', lambda e, hf=hf, pbo=pbo: e.tensor_tensor(out=osb[:, hf * 512:(hf + 1) * 512], in0=pbo[:, :],
                                                                              in1=gbc[:, hf * 512:(hf + 1) * 512], op=ALU.mult), r=[rpbo, r_gbc], w=[r_osb])
                    p.op('dve', lambda e: e.tensor_tensor(out=osb[:, :], in0=osb[:, :], in1=xt[:, :], op=ALU.add), r=[r_xt], w=[r_osb])
                    p.dma('sp', [(u['dst'], osb[:, :])], r=[r_osb], w=[u['reg']])
            p.barrier()
        self.ps_lim = 8

    def ret_setup(self):
        c = self.cfg
        din, dout, dint = self._din, self._dout, self._dint
        self.rt_dmask = din('rt_dmask', [128, 4, 2, 128])
        self.rt_qdec = din('rt_qdec', [128, 4, 2, 128])
        self.rt_kdec = din('rt_kdec', [128, 4, 2])
        self.rt_cos = din('rt_cos', [c.LS, 256])
        self.rt_sin = din('rt_sin', [c.LS, 256])
        self.st_ret = din('st_ret', [2, 4, 256, 512])
        self.o_ret = dout('o_ret', [c.NP, 2, 4, 256, 512])
        seqs = self.make_seqs()
        ntok = len(self.units) * 128
        self.rt_proj = dint('rt_proj', [ntok, 8192])
        self.rt_yf = dint('rt_yf', [ntok, 2048])
        for sq in seqs:
            if sq['g'] == 0:
                sq['s0'], sq['sout'] = self.st_ret, None
            else:
                sq['s0'], sq['sout'] = None, self.o_ret[sq['idx'] - 1]
        return seqs

    def ret_phase(self, l, seqs):
        p, nc, W = self.p, self.nc, self.W
        c = self.cfg
        nu = len(self.units)
        proj, yfd = self.rt_proj, self.rt_yf
        r_proj = [Reg() for _ in range(nu)]
        with ExitStack() as s:
            def sb(name, shape, dt):
                return p.sbuf(s, 'ra_' + name, shape, dt)
            hT = sb('hT', [128, 8, nu * 128], BF16)
            r_hT = [Reg() for _ in range(nu)]
            xt = [sb('xt%d' % i, [128, 1024], F32) for i in range(2)]; r_xt = [Reg(), Reg()]
            ss = sb('ss', [128, 2], F32); xn = sb('xn', [128, 1024], F32); junk = sb('junk', [128, 1024], BF16)
            scr = (ss, xn, junk, Reg(), Reg(), Reg())
            wp = [sb('wp%d' % i, [128, 8, 512], BF16) for i in range(2)]; r_wp = [Reg(), Reg()]
            stg = [sb('stg%d' % i, [128, 512], F32) for i in range(3)]; r_stg = [Reg() for _ in range(3)]
            for ui, u in enumerate(self.units):
                b = ui % 2
                p.dma('sp', [(xt[b][:, :], u['src'] if self.first_touch else u['dst'])], r=[u['reg']], w=[r_xt[b]])
                self.norm_hT(xt[b][:, :], r_xt[b], l, 0, u['g'], lambda cc, ui=ui: hT[:, cc, ui * 128:(ui + 1) * 128], r_hT[ui], scr)
            k = 0
            for cg in range(16):
                b = cg % 2
                p.dma('pool', [(wp[b][:, :, :], W['ret_w_in'][:, cg * 512:(cg + 1) * 512].rearrange("(c p) n -> p c n", p=128))], w=[r_wp[b]])
                for ui in range(nu):
                    pb, rpb = self.next_ps()
                    for cc in range(8):
                        p.op('pe', lambda e, cc=cc, pb=pb, ui=ui, b=b: e.matmul(pb[:, :], lhsT=hT[:, cc, ui * 128:(ui + 1) * 128], rhs=wp[b][:, cc, :],
                                                                                start=(cc == 0), stop=(cc == 7)), r=[r_hT[ui], r_wp[b]], w=[rpb])
                    sbi = k % 3
                    k += 1
                    p.op('act' if k % 2 else 'dve', (lambda e, pb=pb, sbi=sbi: e.activation(out=stg[sbi][:, :], in_=pb[:, :], func=AF.Copy)) if k % 2 else
                         (lambda e, pb=pb, sbi=sbi: e.tensor_copy(out=stg[sbi][:, :], in_=pb[:, :])), r=[rpb], w=[r_stg[sbi]])
                    p.dma('sp', [(proj[ui * 128:(ui + 1) * 128, cg * 512:(cg + 1) * 512], stg[sbi][:, :])], r=[r_stg[sbi]], w=[r_proj[ui]])
            p.barrier()
        lgf = [float(np.log1p(-2.0 ** (-5.0 - h))) for h in range(4)]
        lgb = [float(np.log1p(-2.0 ** (-5.5 - h))) for h in range(4)]
        cdec = [[float(np.exp(lgf[h] * 128)), float(np.exp(lgb[h] * 128))] for h in range(4)]
        self.ps_lim = 6
        with ExitStack() as s:
            def sb(name, shape, dt):
                return p.sbuf(s, 'rb_' + name, shape, dt)
            r_wt = Reg()
            Wout = sb('Wout', [128, 16, 1024], BF16)
            p.dma('pool', [(Wout[:, :, :], W['ret_w_out'].rearrange("(c p) n -> p c n", p=128))], w=[r_wt])
            dmask = sb('dmask', [128, 4, 2, 128], F32)
            qdec = sb('qdec', [128, 4, 2, 128], F32)
            kdec = sb('kdec', [128, 4, 2], F32)
            r_c = Reg()
            p.dma('sp', [(dmask[:, :, :, :], self.rt_dmask), (qdec[:, :, :, :], self.rt_qdec), (kdec[:, :, :], self.rt_kdec)], w=[r_c])
            S32 = sb('S32', [128, 4, 2, 2, 512], F32)
            Sbf = sb('Sbf', [128, 4, 2, 2, 512], BF16)
            r_S = [[Reg() for _ in range(2)] for _ in range(4)]
            r_Sb = [[Reg() for _ in range(2)] for _ in range(4)]
            gbc = sb('gbc', [128, 1024], F32); r_gbc = Reg()
            diag = sb('diag', [128, 128], F32); r_diag = Reg()
            qk = sb('qk', [128, 2048], F32); r_qk = Reg()
            rot = sb('rot', [128, 2048], F32); r_rot = Reg()
            cs = sb('cs', [128, 2, 256], F32); r_cs = Reg()
            qT = sb('qT', [128, 8, 128], BF16); kT = sb('kT', [128, 8, 128], BF16); qdT = sb('qdT', [128, 2, 128], BF16)
            r_qT, r_kT, r_qdT = Reg(), Reg(), Reg()
            Kd = sb('Kd', [128, 1024], BF16); r_Kd = Reg()
            Vb = sb('Vb', [128, 2048], BF16); r_Vb = Reg()
            gg = sb('gg', [128, 2048], F32); r_gg = Reg()
            term = sb('term', [128, 2048], F32); r_term = Reg()
            yfw = sb('yfw', [128, 2048], F32); r_yfw = Reg()
            scT = sb('scT', [128, 128], BF16); r_scT = Reg()
            st = sb('st', [128, 4], F32); r_st = Reg()
            junk2 = sb('junk2', [128, 512], BF16); r_junk2 = Reg()
            yT = sb('yT', [128, 16, 128], BF16); r_yT = Reg()
            xt2 = sb('xt2', [128, 1024], F32); r_xt2 = Reg()
            osb = sb('osb', [128, 1024], F32); r_osb = Reg()
            ubase = 0
            for sq_ in seqs:
                L, g_, units = sq_['L'], sq_['g'], sq_['units']
                latent = (g_ == 0)
                nchunk = L // 128
                self.gate_bc(gbc, r_gbc, l, 2, g_, diag, r_diag)
                allS = [r_S[h][d] for h in range(4) for d in range(2)]
                if sq_['s0'] is None:
                    p.op('dve', lambda e: e.memset(S32[:, :, :, :, :], 0.0), w=allS)
                else:
                    for rdir in range(2):
                        for h in range(4):
                            p.dma('sp', [(S32[:, h, rdir, :, :], sq_['s0'][rdir, h].rearrange("(c p) e -> p c e", p=128))], w=[r_S[h][rdir]])
                for h in range(4):
                    for d in range(2):
                        p.op('act', lambda e, h=h, d=d: e.activation(out=Sbf[:, h, d, :, :], in_=S32[:, h, d, :, :], func=AF.Copy), r=[r_S[h][d]], w=[r_Sb[h][d]])
                for d in range(2):
                    chunks = list(range(nchunk)) if d == 0 else list(range(nchunk - 1, -1, -1))
                    for ci in chunks:
                        ui = ubase + ci
                        u = units[ci]
                        rows = slice(ui * 128, (ui + 1) * 128)
                        p.dma('sp', [(qk[:, :], proj[rows, 0:2048])], r=[r_proj[ui]], w=[r_qk])
                        p.dma('pool', [(Vb[:, :], proj[rows, 2048:4096])], r=[r_proj[ui]], w=[r_Vb])
                        gc0 = 4096 + d * 2048
                        p.dma('sp', [(gg[:, :], proj[rows, gc0:gc0 + 2048])], r=[r_proj[ui]], w=[r_gg])
                        p.op('act', lambda e: e.activation(out=gg[:, :], in_=gg[:, :], func=AF.Silu), r=[r_gg], w=[r_gg])
                        p.op('dve', lambda e: e.tensor_scalar(out=qk[:, 1024:2048], in0=qk[:, 1024:2048], scalar1=1.0 / 16.0, scalar2=None, op0=ALU.mult), r=[r_qk], w=[r_qk])
                        if latent:
                            t0 = ci * 128
                            p.dma('sp', [(cs[:, 0, :], self.rt_cos[t0:t0 + 128, :]), (cs[:, 1, :], self.rt_sin[t0:t0 + 128, :])], w=[r_cs])
                            x4 = qk[:, :].rearrange("p (h i two) -> p h i two", h=8, two=2)
                            r4 = rot[:, :].rearrange("p (h i two) -> p h i two", h=8, two=2)
                            cos4 = cs[:, 0, :].rearrange("p (i two) -> p i two", two=2)
                            sin4 = cs[:, 1, :].rearrange("p (i two) -> p i two", two=2)
                            for (o_, i_, sgn) in ((0, 1, -1.0), (1, 0, 1.0)):
                                sinb = sin4[:, :, o_].unsqueeze(1).to_broadcast([128, 8, 128])
                                p.op('dve', lambda e, r4=r4, x4=x4, sinb=sinb, o_=o_, i_=i_, sgn=sgn: e.scalar_tensor_tensor(
                                    out=r4[:, :, :, o_], in0=x4[:, :, :, i_], scalar=sgn, in1=sinb, op0=ALU.mult, op1=ALU.mult), r=[r_qk, r_cs], w=[r_rot])
                            cosb = cs[:, 0, :].unsqueeze(1).to_broadcast([128, 8, 256])
                            x3 = qk[:, :].rearrange("p (h k) -> p h k", h=8)
                            p.op('dve', lambda e, x3=x3, cosb=cosb: e.tensor_tensor(out=x3, in0=x3, in1=cosb, op=ALU.mult), r=[r_cs, r_rot], w=[r_qk])
                            p.op('dve', lambda e: e.tensor_tensor(out=qk[:, :], in0=qk[:, :], in1=rot[:, :], op=ALU.add), r=[r_rot], w=[r_qk])
                        for which, dstT, rdst in ((0, qT, r_qT), (1, kT, r_kT)):
                            for half in range(2):
                                pb, rpb = self.next_ps()
                                for q4 in range(4):
                                    cc = half * 4 + q4
                                    col = which * 1024 + cc * 128
                                    p.op('pe', lambda e, pb=pb, q4=q4, col=col: e.transpose(out=pb[:, q4 * 128:(q4 + 1) * 128], in_=qk[:, col:col + 128],
                                                                                           identity=self.identF[:, :]), r=[r_qk, self.r_const], w=[rpb])
                                p.op('act', lambda e, pb=pb, half=half, dstT=dstT: e.activation(out=dstT[:, half * 4:(half + 1) * 4, :],
                                                                                              in_=pb[:, :].rearrange("p (g t) -> p g t", g=4), func=AF.Copy), r=[rpb], w=[rdst])
                        for h in range(4):
                            p.op('dve', lambda e, h=h: e.tensor_scalar(out=Kd[:, h * 256:(h + 1) * 256], in0=qk[:, 1024 + h * 256:1024 + (h + 1) * 256],
                                                                        scalar1=kdec[:, h, d:d + 1], scalar2=None, op0=ALU.mult), r=[r_qk, r_c], w=[r_Kd])
                        for h in range(4):
                            hs = slice(h * 512, (h + 1) * 512)
                            psc, rpsc = self.next_ps()
                            for dc in range(2):
                                p.op('pe', lambda e, psc=psc, h=h, dc=dc: e.matmul(psc[:, 0:128], lhsT=kT[:, h * 2 + dc, :], rhs=qT[:, h * 2 + dc, :],
                                                                                  start=(dc == 0), stop=(dc == 1)), r=[r_kT, r_qT], w=[rpsc])
                            p.op('dve', lambda e, psc=psc, h=h: e.tensor_tensor(out=scT[:, :], in0=psc[:, 0:128], in1=dmask[:, h, d, :], op=ALU.mult),
                                 r=[rpsc, r_c], w=[r_scT])
                            for dc in range(2):
                                p.op('pool', lambda e, h=h, dc=dc: e.tensor_tensor(out=qdT[:, dc, :], in0=qT[:, h * 2 + dc, :], in1=qdec[:, h, d, :], op=ALU.mult),
                                     r=[r_qT, r_c], w=[r_qdT])
                            po, rpo = self.ps[6], self.r_ps[6]
                            p.op('pe', lambda e, po=po, hs=hs: e.matmul(po[:, :], lhsT=scT[:, :], rhs=Vb[:, hs], start=True, stop=False), r=[r_scT, r_Vb], w=[rpo])
                            for dc in range(2):
                                p.op('pe', lambda e, po=po, h=h, dc=dc: e.matmul(po[:, :], lhsT=qdT[:, dc, :], rhs=Sbf[:, h, d, dc, :], start=False, stop=(dc == 1)),
                                     r=[r_qdT, r_Sb[h][d]], w=[rpo])
                            p.op('act', lambda e, po=po, h=h: e.activation(out=junk2[:, :], in_=po[:, :], func=AF.Square, accum_out=st[:, h:h + 1]),
                                 r=[rpo], w=[r_junk2, r_st])
                            p.op('act', lambda e, h=h: e.activation(out=st[:, h:h + 1], in_=st[:, h:h + 1], func=AF.Sqrt, scale=1.0 / 512, bias=EPS), r=[r_st], w=[r_st])
                            p.op('dve', lambda e, h=h: e.reciprocal(out=st[:, h:h + 1], in_=st[:, h:h + 1]), r=[r_st], w=[r_st])
                            p.op('dve', lambda e, po=po, h=h, hs=hs: e.scalar_tensor_tensor(out=term[:, hs], in0=po[:, :], scalar=st[:, h:h + 1], in1=gg[:, hs],
                                                                                           op0=ALU.mult, op1=ALU.mult), r=[rpo, r_st, r_gg], w=[r_term])
                            for dc in range(2):
                                pss, rpss = self.ps[7], self.r_ps[7]
                                p.op('pe', lambda e, pss=pss, h=h, dc=dc, hs=hs: e.matmul(pss[:, :], lhsT=Kd[:, h * 256 + dc * 128:h * 256 + (dc + 1) * 128], rhs=Vb[:, hs],
                                                                                          start=True, stop=True), r=[r_Kd, r_Vb], w=[rpss])
                                p.op('dve', lambda e, pss=pss, h=h, dc=dc: e.scalar_tensor_tensor(out=S32[:, h, d, dc, :], in0=S32[:, h, d, dc, :], scalar=cdec[h][d],
                                                                                                 in1=pss[:, :], op0=ALU.mult, op1=ALU.add), r=[rpss, r_Sb[h][d]], w=[r_S[h][d]])
                                p.op('act', lambda e, h=h, dc=dc: e.activation(out=Sbf[:, h, d, dc, :], in_=S32[:, h, d, dc, :], func=AF.Copy), r=[r_S[h][d]], w=[r_Sb[h][d]])
                        if d == 0:
                            p.dma('sp', [(yfd[rows, :], term[:, :])], r=[r_term], w=[r_proj[ui]])
                        else:
                            p.dma('sp', [(yfw[:, :], yfd[rows, :])], r=[r_proj[ui]], w=[r_yfw])
                            p.dma('sp', [(xt2[:, :], u['src'] if self.first_touch else u['dst'])], r=[u['reg']], w=[r_xt2])
                            p.op('dve', lambda e: e.tensor_tensor(out=term[:, :], in0=term[:, :], in1=yfw[:, :], op=ALU.add), r=[r_yfw], w=[r_term])
                            for qd in range(4):
                                pb, rpb = self.next_ps()
                                for q4 in range(4):
                                    cc = qd * 4 + q4
                                    p.op('pe', lambda e, pb=pb, q4=q4, cc=cc: e.transpose(out=pb[:, q4 * 128:(q4 + 1) * 128], in_=term[:, cc * 128:(cc + 1) * 128],
                                                                                          identity=self.identF[:, :]), r=[r_term, self.r_const], w=[rpb])
                                p.op('act', lambda e, pb=pb, qd=qd: e.activation(out=yT[:, qd * 4:(qd + 1) * 4, :], in_=pb[:, :].rearrange("p (g t) -> p g t", g=4),
                                                                               func=AF.Copy), r=[rpb], w=[r_yT])
                            for hf in range(2):
                                pbo, rpbo = self.next_ps()
                                for cc in range(16):
                                    p.op('pe', lambda e, cc=cc, hf=hf, pbo=pbo: e.matmul(pbo[:, :], lhsT=yT[:, cc, :], rhs=Wout[:, cc, hf * 512:(hf + 1) * 512],
                                                                                         start=(cc == 0), stop=(cc == 15)), r=[r_yT, r_wt], w=[rpbo])
                                p.op('dve', lambda e, hf=hf, pbo=pbo: e.tensor_tensor(out=osb[:, hf * 512:(hf + 1) * 512], in0=pbo[:, :],
                                                                                      in1=gbc[:, hf * 512:(hf + 1) * 512], op=ALU.mult), r=[rpbo, r_gbc], w=[r_osb])
                            p.op('dve', lambda e: e.tensor_tensor(out=osb[:, :], in0=osb[:, :], in1=xt2[:, :], op=ALU.add), r=[r_xt2], w=[r_osb])
                            p.dma('sp', [(u['dst'], osb[:, :])], r=[r_osb], w=[u['reg']])
                if sq_['sout'] is not None:
                    for rdir in range(2):
                        for h in range(4):
                            p.dma('sp', [(sq_['sout'][rdir, h].rearrange("(c p) e -> p c e", p=128), S32[:, h, rdir, :, :])], r=[r_S[h][rdir]])
                ubase += nchunk
            p.barrier()
        self.ps_lim = 8

    def hy_setup(self):
        c = self.cfg
        din, dout, dint = self._din, self._dout, self._dint
        self.hy_fm = din('hy_fm', [128, 6, 24])
        self.hy_rows = din('hy_rows', [128, 2, 1024])
        self.hy_fsm = din('hy_fsm', [64, 4])
        seqs = self.make_seqs()
        self.hy_L = {}
        for sq in seqs:
            L = sq['L']
            sq['hTd'] = dint('hy_hTd%d' % sq['idx'], [128, 8, L + 2], BF16)
            sq['zd'] = dint('hy_zd%d' % sq['idx'], [L, 1024], BF16)
            sq['x0Td'] = dint('hy_x0Td%d' % sq['idx'], [128, 8, L], BF16)
            sq['zTd'] = dint('hy_zTd%d' % sq['idx'], [128, 8, L], BF16)
            sq['gTd'] = dint('hy_gTd%d' % sq['idx'], [128, 8, L], BF16)
            if L not in self.hy_L:
                TC = L // 128
                NFc = TC + 1
                self.hy_L[L] = dict(TC=TC, NFc=NFc,
                                    zpos=din('hy_zpos%d' % L, [33, L]), tn=din('hy_tn%d' % L, [L, 1]),
                                    Fc=din('hy_Fc%d' % L, [NFc, 128, TC * 128], BF16), Fs=din('hy_Fs%d' % L, [NFc, 128, TC * 128], BF16),
                                    Gc=din('hy_Gc%d' % L, [NFc * 128, L], BF16), Gs=din('hy_Gs%d' % L, [NFc * 128, L], BF16),
                                    hsd=dint('hy_hsd%d' % L, [L, 1024], BF16), hdd=dint('hy_hdd%d' % L, [L, 1024], BF16), r=Reg())
        return seqs

    def hy_phase(self, l, seqs):
        p, nc, W = self.p, self.nc, self.W
        c = self.cfg
        with ExitStack() as s:
            def sb(name, shape, dt):
                return p.sbuf(s, 'ha_' + name, shape, dt)
            NT = 256
            r_wt = Reg()
            Win = sb('Win', [128, 8, 3072], BF16)
            p.dma('pool', [(Win[:, :, 0:1536], W['hy_w_in'][:, 0:1536].rearrange("(c p) n -> p c n", p=128)),
                           (Win[:, :, 1536:3072], W['hy_w_in'][:, 1536:3072].rearrange("(c p) n -> p c n", p=128))], w=[r_wt])
            fmv = sb('fmv', [128, 6, 24], F32); r_c = Reg()
            p.dma('sp', [(fmv[:, :, :], self.hy_fm)], w=[r_c])
            xt = sb('xt', [128, 1024], F32); r_xt = Reg()
            ss = sb('ss', [128, 2], F32); xn = sb('xn', [128, 1024], F32); junk = sb('junk', [128, 1024], BF16)
            scr = (ss, xn, junk, Reg(), Reg(), Reg())
            hts = sb('hts', [128, 8, 128], BF16); r_hts = Reg()
            zer = sb('zer', [128, 8, 1], BF16); r_zer = Reg()
            p.op('dve', lambda e: e.memset(zer[:, :, :], 0.0), w=[r_zer])
            hTt = sb('hTt', [128, 8, NT + 2], BF16); r_hTt = Reg()
            pT = [sb('pT%d' % i, [128, NT + 2], F32) for i in range(2)]; r_pT = [Reg(), Reg()]
            uu = [sb('uu%d' % i, [128, NT], F32) for i in range(2)]; r_uu = [Reg(), Reg()]
            x0T = sb('x0T', [128, 8, NT], BF16); r_x0T = Reg()
            x1T = sb('x1T', [128, 8, NT], F32); r_x1T = Reg()
            zT = sb('zT', [128, 8, NT], BF16); r_zT = Reg()
            ztok = sb('ztok', [128, NT // 128, 1024], BF16); r_ztok = Reg()
            for sq_ in seqs:
                L, g_, units = sq_['L'], sq_['g'], sq_['units']
                hTd = sq_['hTd']; r_hTd = Reg()
                sq_['r_sc'] = Reg()
                p.dma('sp', [(hTd[:, :, 0:1], zer[:, :, :]), (hTd[:, :, L + 1:L + 2], zer[:, :, :])], r=[r_zer], w=[r_hTd], allow_slow_non_contiguous=True)
                for i, u in enumerate(units):
                    p.dma('sp', [(xt[:, :], u['src'] if self.first_touch else u['dst'])], r=[u['reg']], w=[r_xt])
                    self.norm_hT(xt[:, :], r_xt, l, 0, g_, lambda cc: hts[:, cc, :], r_hts, scr)
                    p.dma('sp', [(hTd[:, :, 1 + i * 128:1 + (i + 1) * 128], hts[:, :, :])], r=[r_hts], w=[r_hTd])
                for ti in range(L // NT):
                    t0 = ti * NT
                    n = NT
                    p.dma('sp', [(hTt[:, :, :], hTd[:, :, t0:t0 + n + 2])], r=[r_hTd], w=[r_hTt])
                    for oc in range(24):
                        b = oc % 2
                        pb, rpb = self.next_ps()
                        for cc in range(8):
                            p.op('pe', lambda e, cc=cc, pb=pb, oc=oc: e.matmul(pb[:, 0:n + 2], lhsT=Win[:, cc, oc * 128:(oc + 1) * 128], rhs=hTt[:, cc, :],
                                                                               start=(cc == 0), stop=(cc == 7)), r=[r_hTt, r_wt], w=[rpb])
                        p.op('dve', lambda e, pb=pb, b=b, oc=oc: e.tensor_scalar(out=pT[b][:, :], in0=pb[:, 0:n + 2], scalar1=fmv[:, 0, oc:oc + 1], scalar2=None, op0=ALU.add),
                             r=[rpb, r_c], w=[r_pT[b]])
                        if t0 == 0:
                            p.op('dve', lambda e, b=b: e.memset(pT[b][:, 0:1], 0.0), w=[r_pT[b]])
                        if t0 + n == L:
                            p.op('dve', lambda e, b=b: e.memset(pT[b][:, n + 1:n + 2], 0.0), w=[r_pT[b]])
                        p.op('dve', lambda e, b=b, oc=oc: e.tensor_scalar(out=uu[b][:, :], in0=pT[b][:, 0:n], scalar1=fmv[:, 1, oc:oc + 1], scalar2=fmv[:, 4, oc:oc + 1],
                                                                        op0=ALU.mult, op1=ALU.add), r=[r_pT[b], r_c], w=[r_uu[b]])
                        p.op('dve', lambda e, b=b, oc=oc: e.scalar_tensor_tensor(out=uu[b][:, :], in0=pT[b][:, 1:n + 1], scalar=fmv[:, 2, oc:oc + 1], in1=uu[b][:, :],
                                                                               op0=ALU.mult, op1=ALU.add), r=[r_pT[b], r_c], w=[r_uu[b]])
                        which, cc8 = oc // 8, oc % 8
                        if which == 0:
                            p.op('dve', lambda e, b=b, oc=oc, cc8=cc8: e.scalar_tensor_tensor(out=x0T[:, cc8, :], in0=pT[b][:, 2:n + 2], scalar=fmv[:, 3, oc:oc + 1], in1=uu[b][:, :],
                                                                                              op0=ALU.mult, op1=ALU.add), r=[r_pT[b], r_uu[b], r_c], w=[r_x0T])
                        elif which == 1:
                            p.op('dve', lambda e, b=b, oc=oc, cc8=cc8: e.scalar_tensor_tensor(out=x1T[:, cc8, :], in0=pT[b][:, 2:n + 2], scalar=fmv[:, 3, oc:oc + 1], in1=uu[b][:, :],
                                                                                              op0=ALU.mult, op1=ALU.add), r=[r_pT[b], r_uu[b], r_c], w=[r_x1T])
                        else:
                            p.op('dve', lambda e, b=b, oc=oc: e.scalar_tensor_tensor(out=uu[b][:, :], in0=pT[b][:, 2:n + 2], scalar=fmv[:, 3, oc:oc + 1], in1=uu[b][:, :],
                                                                                   op0=ALU.mult, op1=ALU.add), r=[r_pT[b], r_c], w=[r_uu[b]])
                            p.op('dve', lambda e, b=b, cc8=cc8: e.tensor_tensor(out=zT[:, cc8, :], in0=uu[b][:, :], in1=x1T[:, cc8, :], op=ALU.mult),
                                 r=[r_uu[b], r_x1T], w=[r_zT])
                    for j in range(n // 128):
                        for half in range(2):
                            pb, rpb = self.next_ps()
                            pv = pb[:, :].bitcast(BF16)
                            for q4 in range(4):
                                cc = half * 4 + q4
                                p.op('pe', lambda e, pv=pv, q4=q4, cc=cc, j=j: e.transpose(out=pv[:, q4 * 128:(q4 + 1) * 128], in_=zT[:, cc, j * 128:(j + 1) * 128],
                                                                                           identity=self.identB[:, :]), r=[r_zT, self.r_const], w=[rpb])
                            p.op('act', lambda e, pv=pv, j=j, half=half: e.activation(out=ztok[:, j, half * 512:(half + 1) * 512], in_=pv[:, 0:512], func=AF.Copy),
                                 r=[rpb], w=[r_ztok])
                    p.dma('sp', [(sq_['zd'][t0:t0 + n, :].rearrange("(j p) f -> p j f", p=128), ztok[:, :, :]),
                                 (sq_['x0Td'][:, :, t0:t0 + n], x0T[:, :, :]), (sq_['zTd'][:, :, t0:t0 + n], zT[:, :, :])],
                          r=[r_ztok, r_x0T, r_zT], w=[sq_['r_sc']])
            p.barrier()
        with ExitStack() as s:
            def sb(name, shape, dt):
                return p.sbuf(s, 'hb_' + name, shape, dt)
            w1 = sb('w1', [33, 64], F32); w2 = sb('w2', [64, 64], F32); w3 = sb('w3', [64, 2048], F32)
            fsm = sb('fsm', [64, 4], F32); rows = sb('rows', [128, 2, 1024], F32)
            r_c = Reg()
            p.dma('sp', [(w1[:, :], W['hy_f_w1']), (w2[:, :], W['hy_f_w2']), (w3[:, :], W['hy_f_w3']), (fsm[:, :], self.hy_fsm), (rows[:, :, :], self.hy_rows)], w=[r_c])
            zp = sb('zp', [33, 128], F32); r_zp = Reg()
            tn = sb('tn', [128, 1], F32); r_tn = Reg()
            ar = sb('ar', [64, 128], F32); s2 = sb('s2', [64, 128], F32); s4 = sb('s4', [64, 128], F32); a1 = sb('a1', [64, 128], F32); a2 = sb('a2', [64, 128], F32)
            r_a = Reg()
            wnd = sb('wnd', [128, 1024], F32); r_wnd = Reg()
            hf = sb('hf', [128, 1024], F32); hb = sb('hb', [128, 1024], F32); r_h = Reg()
            hs = sb('hs', [128, 1024], BF16); hd = sb('hd', [128, 1024], BF16); r_hsd = Reg()

            def sin_layer(pb, rpb, bi, fi, dst):
                p.op('dve', lambda e: e.tensor_scalar(out=ar[:, :], in0=pb[0:64, 0:128], scalar1=fsm[:, bi:bi + 1], scalar2=fsm[:, fi:fi + 1], op0=ALU.add, op1=ALU.mult),
                     r=[rpb, r_c], w=[r_a])
                p.op('act', lambda e: e.activation(out=s2[:, :], in_=ar[:, :], func=AF.Sin, scale=0.5), r=[r_a], w=[r_a])
                p.op('act', lambda e: e.activation(out=s4[:, :], in_=ar[:, :], func=AF.Sin, scale=0.25), r=[r_a], w=[r_a])
                p.op('dve', lambda e: e.tensor_tensor(out=s4[:, :], in0=s4[:, :], in1=s4[:, :], op=ALU.mult), r=[r_a], w=[r_a])
                p.op('dve', lambda e: e.tensor_scalar(out=s4[:, :], in0=s4[:, :], scalar1=-2.0, scalar2=1.0, op0=ALU.mult, op1=ALU.add), r=[r_a], w=[r_a])
                p.op('dve', lambda e: e.scalar_tensor_tensor(out=dst[:, :], in0=s2[:, :], scalar=2.0, in1=s4[:, :], op0=ALU.mult, op1=ALU.mult), r=[r_a], w=[r_a])

            for L, info in self.hy_L.items():
                for ti in range(L // 128):
                    rows_t = slice(ti * 128, (ti + 1) * 128)
                    p.dma('sp', [(zp[:, :], info['zpos'][:, rows_t]), (tn[:, :], info['tn'][rows_t, :])], w=[r_zp, r_tn])
                    pb, rpb = self.next_ps()
                    p.op('pe', lambda e, pb=pb: e.matmul(pb[0:64, 0:128], lhsT=w1[:, :], rhs=zp[:, :], start=True, stop=True), r=[r_zp, r_c], w=[rpb])
                    sin_layer(pb, rpb, 0, 1, a1)
                    pb, rpb = self.next_ps()
                    p.op('pe', lambda e, pb=pb: e.matmul(pb[0:64, 0:128], lhsT=w2[:, :], rhs=a1[:, :], start=True, stop=True), r=[r_a, r_c], w=[rpb])
                    sin_layer(pb, rpb, 2, 3, a2)
                    p.op('dve', lambda e: e.tensor_scalar(out=tn[:, :], in0=tn[:, :], scalar1=-1.0, scalar2=None, op0=ALU.mult), r=[r_tn], w=[r_tn])
                    p.op('act', lambda e: e.activation(out=wnd[:, :], in_=rows[:, 1, :], func=AF.Exp, scale=tn[:, 0:1]), r=[r_tn, r_c], w=[r_wnd])
                    for q in range(4):
                        pb, rpb = self.next_ps()
                        p.op('pe', lambda e, pb=pb, q=q: e.matmul(pb[:, :], lhsT=a2[:, :], rhs=w3[:, q * 512:(q + 1) * 512], start=True, stop=True), r=[r_a, r_c], w=[rpb])
                        dst = hf if q < 2 else hb
                        qq = q % 2
                        p.op('dve', lambda e, pb=pb, dst=dst, qq=qq: e.tensor_tensor(out=dst[:, qq * 512:(qq + 1) * 512], in0=pb[:, :], in1=wnd[:, qq * 512:(qq + 1) * 512], op=ALU.mult),
                             r=[rpb, r_wnd], w=[r_h])
                    if ti == 0:
                        p.op('dve', lambda e: e.memset(hb[0:1, :], 0.0), w=[r_h])
                    p.op('dve', lambda e: e.tensor_tensor(out=hs[:, :], in0=hf[:, :], in1=hb[:, :], op=ALU.add), r=[r_h], w=[r_hsd])
                    p.op('dve', lambda e: e.tensor_tensor(out=hd[:, :], in0=hb[:, :], in1=hf[:, :], op=ALU.subtract), r=[r_h], w=[r_hsd])
                    p.dma('sp', [(info['hsd'][rows_t, :], hs[:, :]), (info['hdd'][rows_t, :], hd[:, :])], r=[r_hsd], w=[info['r']])
            p.barrier()
        self.ps_lim = 6
        with ExitStack() as s:
            def sb(name, shape, dt):
                return p.sbuf(s, 'hc_' + name, shape, dt)
            TCM = max(i_['TC'] for i_ in self.hy_L.values())
            NFM = TCM + 1
            Rc = sb('Rc', [128, TCM, 512], BF16); Rs = sb('Rs', [128, TCM, 512], BF16); r_R = Reg()
            Fcb = [sb('Fcb%d' % i, [128, TCM * 128], BF16) for i in range(2)]; Fsb = [sb('Fsb%d' % i, [128, TCM * 128], BF16) for i in range(2)]
            r_F = [Reg(), Reg()]
            Yre = sb('Yre', [128, NFM, 256], BF16); Yim = sb('Yim', [128, NFM, 256], BF16); r_Y = Reg()
            ec = sb('ec', [128, 512], F32); es_ = sb('es', [128, 512], F32); r_ec, r_es = Reg(), Reg()
            t1 = sb('t1', [128, 256], F32); t2 = sb('t2', [128, 256], F32); r_t1, r_t2 = Reg(), Reg()
            GB = 4
            Gcb = [sb('Gcb%d' % i, [128, GB, 512], BF16) for i in range(2)]; Gsb = [sb('Gsb%d' % i, [128, GB, 512], BF16) for i in range(2)]
            r_G = [Reg(), Reg()]
            x0t = sb('x0t', [128, 2, 512], BF16); zt = sb('zt', [128, 2, 512], BF16); r_xz = Reg()
            go = sb('go', [128, 2, 512], BF16); r_go = Reg()
            tmp = sb('tmp', [128, 512], F32); r_tmp = Reg()
            fmv = sb('fmv', [128, 6, 24], F32); r_c = Reg()
            p.dma('sp', [(fmv[:, :, :], self.hy_fm)], w=[r_c])
            fi = 0
            gi = 0
            for sq_ in seqs:
                L = sq_['L']
                info = self.hy_L[L]
                TC, NFc = info['TC'], info['NFc']
                TW = min(512, L)
                sq_['r_g'] = Reg()
                for gq in range(4):
                    gcols = slice(gq * 256, (gq + 1) * 256)
                    p.dma('sp', [(Rc[:, 0:TC, 0:256], sq_['zd'][:, gcols].rearrange("(c p) f -> p c f", p=128)),
                                 (Rc[:, 0:TC, 256:512], info['hsd'][:, gcols].rearrange("(c p) f -> p c f", p=128)),
                                 (Rs[:, 0:TC, 0:256], sq_['zd'][:, gcols].rearrange("(c p) f -> p c f", p=128)),
                                 (Rs[:, 0:TC, 256:512], info['hdd'][:, gcols].rearrange("(c p) f -> p c f", p=128))],
                          r=[sq_['r_sc'], info['r']], w=[r_R])
                    for fc in range(NFc):
                        b = fi % 2
                        fi += 1
                        p.dma('sp', [(Fcb[b][:, 0:TC * 128], info['Fc'][fc]), (Fsb[b][:, 0:TC * 128], info['Fs'][fc])], w=[r_F[b]])
                        pc, rpc = self.next_ps()
                        for tc in range(TC):
                            p.op('pe', lambda e, pc=pc, tc=tc, b=b: e.matmul(pc[:, :], lhsT=Fcb[b][:, tc * 128:(tc + 1) * 128], rhs=Rc[:, tc, :], start=(tc == 0), stop=(tc == TC - 1)),
                                 r=[r_F[b], r_R], w=[rpc])
                        pS, rpS = self.next_ps()
                        for tc in range(TC):
                            p.op('pe', lambda e, pS=pS, tc=tc, b=b: e.matmul(pS[:, :], lhsT=Fsb[b][:, tc * 128:(tc + 1) * 128], rhs=Rs[:, tc, :], start=(tc == 0), stop=(tc == TC - 1)),
                                 r=[r_F[b], r_R], w=[rpS])
                        p.op('act', lambda e, pc=pc: e.activation(out=ec[:, :], in_=pc[:, :], func=AF.Copy), r=[rpc], w=[r_ec])
                        p.op('act', lambda e, pS=pS: e.activation(out=es_[:, :], in_=pS[:, :], func=AF.Copy), r=[rpS], w=[r_es])
                        p.op('dve', lambda e: e.tensor_tensor(out=t1[:, :], in0=ec[:, 0:256], in1=ec[:, 256:512], op=ALU.mult), r=[r_ec], w=[r_t1])
                        p.op('pool', lambda e: e.tensor_tensor(out=t2[:, :], in0=es_[:, 0:256], in1=es_[:, 256:512], op=ALU.mult), r=[r_es], w=[r_t2])
                        p.op('dve', lambda e, fc=fc: e.tensor_tensor(out=Yre[:, fc, :], in0=t1[:, :], in1=t2[:, :], op=ALU.add), r=[r_t1, r_t2], w=[r_Y])
                        p.op('dve', lambda e: e.tensor_tensor(out=t1[:, :], in0=ec[:, 0:256], in1=es_[:, 256:512], op=ALU.mult), r=[r_ec, r_es], w=[r_t1])
                        p.op('pool', lambda e: e.tensor_tensor(out=t2[:, :], in0=es_[:, 0:256], in1=ec[:, 256:512], op=ALU.mult), r=[r_ec, r_es], w=[r_t2])
                        p.op('dve', lambda e, fc=fc: e.tensor_tensor(out=Yim[:, fc, :], in0=t1[:, :], in1=t2[:, :], op=ALU.subtract), r=[r_t1, r_t2], w=[r_Y])
                    for tt in range(L // TW):
                        tsl = slice(tt * TW, (tt + 1) * TW)
                        pa = [self.ps[6], self.ps[7]]
                        rpa = [self.r_ps[6], self.r_ps[7]]
                        p.dma('sp', [(x0t[:, :, 0:TW], sq_['x0Td'][:, gq * 2:gq * 2 + 2, tsl]), (zt[:, :, 0:TW], sq_['zTd'][:, gq * 2:gq * 2 + 2, tsl])],
                              r=[sq_['r_sc']], w=[r_xz])
                        nbat = (NFc + GB - 1) // GB
                        for bt in range(nbat):
                            f0 = bt * GB
                            nf = min(GB, NFc - f0)
                            b = gi % 2
                            gi += 1
                            p.dma('sp', [(Gcb[b][:, 0:nf, 0:TW], info['Gc'][f0 * 128:(f0 + nf) * 128, tsl].rearrange("(c p) t -> p c t", p=128)),
                                         (Gsb[b][:, 0:nf, 0:TW], info['Gs'][f0 * 128:(f0 + nf) * 128, tsl].rearrange("(c p) t -> p c t", p=128))], w=[r_G[b]])
                            for k in range(nf):
                                fc = f0 + k
                                for dq in range(2):
                                    p.op('pe', lambda e, dq=dq, fc=fc, k=k, b=b: e.matmul(pa[dq][:, 0:TW], lhsT=Yre[:, fc, dq * 128:(dq + 1) * 128], rhs=Gcb[b][:, k, 0:TW],
                                                                                        start=(fc == 0), stop=False), r=[r_Y, r_G[b]], w=[rpa[dq]])
                                    p.op('pe', lambda e, dq=dq, fc=fc, k=k, b=b: e.matmul(pa[dq][:, 0:TW], lhsT=Yim[:, fc, dq * 128:(dq + 1) * 128], rhs=Gsb[b][:, k, 0:TW],
                                                                                        start=False, stop=(fc == NFc - 1)), r=[r_Y, r_G[b]], w=[rpa[dq]])
                        for dq in range(2):
                            gch = gq * 2 + dq
                            p.op('dve', lambda e, dq=dq, gch=gch: e.scalar_tensor_tensor(out=tmp[:, 0:TW], in0=zt[:, dq, 0:TW], scalar=fmv[:, 5, gch:gch + 1], in1=pa[dq][:, 0:TW],
                                                                                       op0=ALU.mult, op1=ALU.add), r=[r_xz, rpa[dq], r_c], w=[r_tmp])
                            p.op('dve', lambda e, dq=dq: e.tensor_tensor(out=go[:, dq, 0:TW], in0=tmp[:, 0:TW], in1=x0t[:, dq, 0:TW], op=ALU.mult), r=[r_tmp, r_xz], w=[r_go])
                        p.dma('sp', [(sq_['gTd'][:, gq * 2:gq * 2 + 2, tsl], go[:, :, 0:TW])], r=[r_go], w=[sq_['r_g']])
            p.barrier()
        self.ps_lim = 8
        with ExitStack() as s:
            def sb(name, shape, dt):
                return p.sbuf(s, 'hd_' + name, shape, dt)
            r_wt = Reg()
            Wo = sb('Wo', [128, 8, 1024], BF16)
            p.dma('pool', [(Wo[:, :, :], W['hy_w_out'].rearrange("(c p) n -> p c n", p=128))], w=[r_wt])
            rows = sb('rows', [128, 2, 1024], F32); r_c = Reg()
            p.dma('sp', [(rows[:, :, :], self.hy_rows)], w=[r_c])
            gbc = sb('gbc', [128, 1024], F32); r_gbc = Reg()
            diag = sb('diag', [128, 128], F32); r_diag = Reg()
            gT = [sb('gT%d' % i, [128, 8, 128], BF16) for i in range(2)]; r_gT = [Reg(), Reg()]
            xt = [sb('xt%d' % i, [128, 1024], F32) for i in range(2)]; r_xt = [Reg(), Reg()]
            osb = [sb('osb%d' % i, [128, 1024], F32) for i in range(2)]; r_osb = [Reg(), Reg()]
            k = 0
            for sq_ in seqs:
                self.gate_bc(gbc, r_gbc, l, 2, sq_['g'], diag, r_diag)
                for i, u in enumerate(sq_['units']):
                    b = k % 2
                    k += 1
                    p.dma('sp', [(gT[b][:, :, :], sq_['gTd'][:, :, i * 128:(i + 1) * 128])], r=[sq_['r_g']], w=[r_gT[b]])
                    p.dma('sp', [(xt[b][:, :], u['src'] if self.first_touch else u['dst'])], r=[u['reg']], w=[r_xt[b]])
                    for hf_ in range(2):
                        cs_ = slice(hf_ * 512, (hf_ + 1) * 512)
                        pbo, rpbo = self.next_ps()
                        for cc in range(8):
                            p.op('pe', lambda e, cc=cc, pbo=pbo, b=b, cs_=cs_: e.matmul(pbo[:, :], lhsT=gT[b][:, cc, :], rhs=Wo[:, cc, cs_], start=(cc == 0), stop=(cc == 7)),
                                 r=[r_gT[b], r_wt], w=[rpbo])
                        p.op('dve', lambda e, pbo=pbo, b=b, cs_=cs_: e.tensor_tensor(out=osb[b][:, cs_], in0=pbo[:, :], in1=rows[:, 0, cs_], op=ALU.add), r=[rpbo, r_c], w=[r_osb[b]])
                    p.op('dve', lambda e, b=b: e.tensor_tensor(out=osb[b][:, :], in0=osb[b][:, :], in1=gbc[:, :], op=ALU.mult), r=[r_gbc], w=[r_osb[b]])
                    p.op('dve', lambda e, b=b: e.tensor_tensor(out=osb[b][:, :], in0=osb[b][:, :], in1=xt[b][:, :], op=ALU.add), r=[r_xt[b]], w=[r_osb[b]])
                    p.dma('sp', [(u['dst'], osb[b][:, :])], r=[r_osb[b]], w=[u['reg']])
            p.barrier()

    def make_seqs(self):
        c = self.cfg
        ns = c.LS // 128
        seqs = [dict(L=c.LS, g=0, units=self.units[0:ns], idx=0)]
        npu = c.LP // 128
        for i in range(c.NP):
            seqs.append(dict(L=c.LP, g=1, units=self.units[ns + i * npu: ns + (i + 1) * npu], idx=1 + i))
        return seqs

    def rwkv_setup(self):
        c = self.cfg
        din, dout, dint = self._din, self._dout, self._dint
        self.rw_mix = din('rw_mix', [128, 6, 8])
        self.rw_hm = din('rw_hm', [64, 8, 16])
        self.rw_rows = din('rw_rows', [128, 2, 1024])
        self.rw_mask4 = din('rw_mask4', [128, 2, 512])
        self.rw_maskN = din('rw_maskN', [128, 4, 128])
        self.st_rwkv = din('st_rwkv', [2, 16, 64, 64])
        self.o_rwkv = dout('o_rwkv', [c.NP, 2, 16, 64, 64])
        seqs = self.make_seqs()
        for sq in seqs:
            L = sq['L']
            sq['hTd'] = dint('rw_hTd%d' % sq['idx'], [128, 8, L + 2], BF16)
            sq['yfd'] = dint('rw_yfd%d' % sq['idx'], [L, 1024])
            sq['bfd'] = dint('rw_bfd%d' % sq['idx'], [L, 16])
            if sq['g'] == 0:
                sq['s0'], sq['sout'] = self.st_rwkv, None
            else:
                sq['s0'], sq['sout'] = None, self.o_rwkv[sq['idx'] - 1]
        return seqs

    def build(self):
        self.setup()
        for l in range(4):
            if l == 0 and 'rwkv' in self.enable:
                self.rwkv_phase(0, self.rwkv_setup())
                self.first_touch = False
            if l == 1 and 'att' in self.enable:
                self.att_phase(1, self.att_setup())
            if l == 2 and 'hy' in self.enable:
                self.hy_phase(2, self.hy_setup())
            if l == 3 and 'ret' in self.enable:
                self.ret_phase(3, self.ret_setup())
            if 'ffn' in self.enable:
                self.ffn_phase(l)
        self.p.finish()
        return self.nc


def fm(vec):
    v = np.asarray(vec, dtype=np.float32)
    return np.ascontiguousarray(v.reshape(-1, 128).T)


def hm(vec):
    v = np.asarray(vec, dtype=np.float32).reshape(-1)
    return np.ascontiguousarray(v.reshape(16, 64).T)


def rwkv_consts(inp):
    out = {}
    out['rw_mix'] = np.ascontiguousarray(np.stack([fm(inp['rwkv_mix'][i]) for i in range(6)], axis=1))
    z = np.zeros((64, 16), np.float32)
    out['rw_hm'] = np.ascontiguousarray(np.stack([hm(inp['rwkv_w0'][0]), hm(inp['rwkv_w0'][1]), hm(inp['rwkv_a0'][0]), hm(inp['rwkv_a0'][1]),
                                                  hm(inp['rwkv_k_k']), hm(inp['rwkv_k_a']), hm(inp['rwkv_r_k']), z], axis=1))
    rows = np.stack([np.asarray(inp['rwkv_ln_w'], np.float32), np.asarray(inp['rwkv_ln_b'], np.float32)], axis=0)
    out['rw_rows'] = np.ascontiguousarray(np.broadcast_to(rows[None], (128, 2, 1024)))
    s_ = np.arange(128)[:, None]
    t_ = np.arange(128)[None, :]
    m4 = np.zeros((128, 2, 512), np.float32)
    mN = np.zeros((128, 4, 128), np.float32)
    bd32 = np.kron(np.eye(4), np.ones((32, 32))).astype(np.float32)
    mN[:, 2, :] = bd32
    mN[:, 3, :] = 1.0 - bd32
    for d in range(2):
        strict = (s_ < t_) if d == 0 else (s_ > t_)
        incl = (s_ <= t_) if d == 0 else (s_ >= t_)
        m4[:, d, :] = np.concatenate([strict, incl, strict, incl], axis=1).astype(np.float32)
        mN[:, d, :] = strict.T.astype(np.float32) * bd32
    out['rw_mask4'] = m4
    out['rw_maskN'] = mN
    return out


def att_consts(inp, cfg):
    out = {}
    qn = np.asarray(inp['att_q_norm'], np.float32)
    kn = np.asarray(inp['att_k_norm'], np.float32)
    rows = np.concatenate([np.tile(qn[None], (16, 1)), np.tile(kn[None], (4, 1))], axis=0)
    out['at_rows'] = np.ascontiguousarray(np.broadcast_to(rows[None], (128, 20, 64)))
    out['at_sink'] = np.ascontiguousarray(np.broadcast_to(np.asarray(inp['att_sink'], np.float32)[None], (64, 16)))
    L = cfg.LS
    t = np.arange(L)
    row = (t // 64).astype(np.float32)
    col = (t % 64).astype(np.float32)
    nf = 16
    inv = (np.float32(10000.0) ** (-np.arange(nf, dtype=np.float32) / np.float32(nf))).astype(np.float32)
    ang = np.concatenate([row[:, None] * inv[None], col[:, None] * inv[None]], axis=-1)
    ang = np.concatenate([ang, ang], axis=-1).astype(np.float32)
    out['at_cos'] = np.cos(ang).astype(np.float32)
    out['at_sin'] = np.sin(ang).astype(np.float32)
    a = np.arange(128)[:, None]
    b = np.arange(128)[None, :]
    m = np.zeros((128, 2, 128), np.float32)
    m[:, 0, :] = (b <= a)
    m[:, 1, :] = (a <= b)
    out['at_mask'] = m
    return out


def ret_consts(cfg):
    out = {}
    lg = [np.log1p(-np.exp2(-5.0 - np.arange(4, dtype=np.float64))), np.log1p(-np.exp2(-5.5 - np.arange(4, dtype=np.float64)))]
    s_ = np.arange(128)[:, None].astype(np.float64)
    t_ = np.arange(128)[None, :].astype(np.float64)
    dm = np.zeros((128, 4, 2, 128), np.float64)
    qd = np.zeros((128, 4, 2, 128), np.float64)
    kd = np.zeros((128, 4, 2), np.float64)
    i_ = np.arange(128).astype(np.float64)
    for h in range(4):
        dm[:, h, 0, :] = np.where(t_ >= s_, np.exp(lg[0][h] * np.maximum(t_ - s_, 0)), 0.0)
        dm[:, h, 1, :] = np.where(s_ >= t_, np.exp(lg[1][h] * np.maximum(s_ - t_, 0)), 0.0)
        qd[:, h, 0, :] = np.exp(lg[0][h] * (i_ + 1.0))[None, :]
        qd[:, h, 1, :] = np.exp(lg[1][h] * (128.0 - i_))[None, :]
        kd[:, h, 0] = np.exp(lg[0][h] * (127.0 - i_))
        kd[:, h, 1] = np.exp(lg[1][h] * i_)
    out['rt_dmask'] = dm.astype(np.float32)
    out['rt_qdec'] = qd.astype(np.float32)
    out['rt_kdec'] = kd.astype(np.float32)
    L = cfg.LS
    ang = np.repeat((1.0 / (np.float32(10000.0) ** np.linspace(0.0, 1.0, 128, dtype=np.float32))).astype(np.float32), 2)
    ph = (np.arange(L, dtype=np.float32)[:, None] * ang[None, :]).astype(np.float32)
    out['rt_cos'] = np.cos(ph).astype(np.float32)
    out['rt_sin'] = np.sin(ph).astype(np.float32)
    return out


def hy_consts(inp, cfg):
    import ml_dtypes
    out = {}
    z24 = np.zeros((128, 24), np.float32)
    skip = z24.copy()
    skip[:, 0:8] = fm(inp['hy_skip'])
    cw = np.asarray(inp['hy_conv_w'], np.float32)
    out['hy_fm'] = np.ascontiguousarray(np.stack([fm(inp['hy_b_in']), fm(cw[0]), fm(cw[1]), fm(cw[2]), fm(inp['hy_conv_b']), skip], axis=1))
    deltas = np.abs(np.linspace(np.log(1e-2) / 1.5, np.log(1e-2) / 0.3, 1024, dtype=np.float32)).astype(np.float32)
    rows = np.stack([np.asarray(inp['hy_b_out'], np.float32), deltas], axis=0)
    out['hy_rows'] = np.ascontiguousarray(np.broadcast_to(rows[None], (128, 2, 1024)))
    out['hy_fsm'] = np.ascontiguousarray(np.stack([np.asarray(inp[k], np.float32) for k in ('hy_f_b1', 'hy_f_freq1', 'hy_f_b2', 'hy_f_freq2')], axis=1))
    for L in sorted(set([cfg.LS, cfg.LP])):
        t = np.arange(L, dtype=np.float32)
        tn = (t / np.float32(max(L - 1, 1))).astype(np.float32)
        bands = 16
        fr = np.linspace(1e-4, bands - 1, bands, dtype=np.float32)
        ph = (np.float32(2.0 * np.pi) * t[:, None] * fr[None, :] / np.float32(L)).astype(np.float32)
        zpos = np.concatenate([tn[:, None], np.cos(ph), -np.sin(ph)], axis=-1).astype(np.float32)
        out['hy_zpos%d' % L] = np.ascontiguousarray(zpos.T)
        out['hy_tn%d' % L] = np.ascontiguousarray(tn[:, None])
        N = 2 * L
        TC = L // 128
        NFc = TC + 1
        f = np.arange(NFc * 128, dtype=np.int64)
        tt = np.arange(L, dtype=np.int64)
        ang = (2.0 * np.pi / N) * ((f[:, None] * tt[None, :]) % N).astype(np.float64)
        valid = (f <= L).astype(np.float64)[:, None]
        C = np.cos(ang) * valid
        S = np.sin(ang) * valid
        wf = np.where((f == 0) | (f == L), 1.0, 2.0)[:, None] / N
        out['hy_Gc%d' % L] = np.ascontiguousarray((C * wf).astype(np.float32).astype(ml_dtypes.bfloat16))
        out['hy_Gs%d' % L] = np.ascontiguousarray((-S * wf).astype(np.float32).astype(ml_dtypes.bfloat16))
        Ct = C.T.reshape(TC, 128, NFc, 128).transpose(2, 1, 0, 3).reshape(NFc, 128, TC * 128)
        St = S.T.reshape(TC, 128, NFc, 128).transpose(2, 1, 0, 3).reshape(NFc, 128, TC * 128)
        out['hy_Fc%d' % L] = np.ascontiguousarray(Ct.astype(np.float32).astype(ml_dtypes.bfloat16))
        out['hy_Fs%d' % L] = np.ascontiguousarray(St.astype(np.float32).astype(ml_dtypes.bfloat16))
    return out


def make_in_maps(inp, cfg, used):
    maps = []
    adab = np.stack([fm(inp['ada_b'][l]) for l in range(4)], axis=1)
    ident = np.eye(128, dtype=np.float32)
    shared = {k: np.ascontiguousarray(np.asarray(inp[k], dtype=np.float32)) for k in used}
    for i in range(NCORES):
        m = dict(shared)
        m['xs'] = np.ascontiguousarray(inp['x_sample'][i])
        m['xp'] = np.ascontiguousarray(inp['x_prompt'][cfg.NP * i:cfg.NP * (i + 1)].reshape(cfg.NP * cfg.LP, D))
        m['cfm'] = np.ascontiguousarray(np.stack([fm(inp['c'][i]), fm(inp['c_ctx'])], axis=-1))
        m['adab'] = np.ascontiguousarray(adab)
        m['ident'] = ident
        if 'rwkv_w_rkv' in used:
            m.update(rwkv_consts(inp))
            m['st_rwkv'] = np.ascontiguousarray(inp['state_rwkv'][i])
        if 'att_w_qkv' in used:
            if i == 0:
                _att = att_consts(inp, cfg)
            m.update(_att)
            m['ck'] = np.ascontiguousarray(inp['cache_att_k'][i].reshape(cfg.PAST, 256))
            m['cv'] = np.ascontiguousarray(inp['cache_att_v'][i].reshape(cfg.PAST, 256))
        if 'hy_w_in' in used:
            if i == 0:
                _hy = hy_consts(inp, cfg)
            m.update(_hy)
        if 'ret_w_in' in used:
            if i == 0:
                _ret = ret_consts(cfg)
            m.update(_ret)
            m['st_ret'] = np.ascontiguousarray(inp['state_ret'][i])
        maps.append(m)
    return maps


_CACHE = {}


def kernel(**inp):
    cfg = Cfg()
    inp = {k: np.asarray(v) for k, v in inp.items()}
    kb = K(cfg, enable=('rwkv', 'att', 'hy', 'ret', 'ffn'))
    nc = kb.build()
    maps = make_in_maps(inp, cfg, list(kb.W.keys()))
    res = run_bass_kernel_spmd(nc, maps, core_ids=list(range(NCORES)))
    R = res.results
    NP, LP = cfg.NP, cfg.LP
    y_sample = np.stack([R[i]['ys'] for i in range(NCORES)], axis=0)
    y_prompt = np.concatenate([R[i]['yp'].reshape(NP, LP, D) for i in range(NCORES)], axis=0)
    st_rwkv = np.concatenate([R[i]['o_rwkv'] for i in range(NCORES)], axis=0)
    ck = np.concatenate([R[i]['o_k'].reshape(NP, LP, 4, 64) for i in range(NCORES)], axis=0)
    cv = np.concatenate([R[i]['o_v'].reshape(NP, LP, 4, 64) for i in range(NCORES)], axis=0)
    st_ret = np.concatenate([R[i]['o_ret'] for i in range(NCORES)], axis=0)
    return (y_prompt, y_sample, st_rwkv, ck, cv, st_ret)
```
